# Optimizing a Trainium2 kernel written in Bass

```python
import math
import jax, jax.numpy as jnp
from jax import lax
import numpy as np

D_MODEL = 1024
BATCH = 4
SEQ = 8192
DEPTH = 4

N_MIXERS = 4
EPS = 1e-6
Q_BLOCK = 128
CHUNK = 128

MLA_HEADS = 8
MLA_NOPE = 128
MLA_ROPE = 64
MLA_V = 128
MLA_Q_LORA = 384
MLA_KV_LORA = 256
ROPE_THETA = 10000.0

DIFF_HEADS = 8
DIFF_HD = D_MODEL // (2 * DIFF_HEADS)

MLSTM_HEADS = 8
MLSTM_QK = D_MODEL // 2 // MLSTM_HEADS
MLSTM_V = D_MODEL // MLSTM_HEADS

RET_HEADS = D_MODEL // 256
RET_QK = 256
RET_V = 2 * D_MODEL // RET_HEADS

FFN_HIDDEN = 2816
CONV_W = 3

kernel_name = 'hybrid_interleaved_encoder'


def n_of(kind):
    return len(range(kind, DEPTH, N_MIXERS))


def rms_norm(x, g):
    xf = x.astype(jnp.float32)
    y = xf * lax.rsqrt(jnp.mean(xf * xf, -1, keepdims=True) + EPS)
    return (y * g.astype(jnp.float32)).astype(x.dtype)


def head_group_norm(x, g):
    xf = x.astype(jnp.float32)
    mu = jnp.mean(xf, -1, keepdims=True)
    xc = xf - mu
    y = xc * lax.rsqrt(jnp.mean(xc * xc, -1, keepdims=True) + EPS)
    return (y * g.astype(jnp.float32)).astype(x.dtype)


def query_blocks(t):
    b, s = t.shape[:2]
    return jnp.moveaxis(t.reshape(b, s // Q_BLOCK, Q_BLOCK, *t.shape[2:]), 1, 0)


def merge_blocks(t):
    t = jnp.moveaxis(t, 0, 1)
    return t.reshape(t.shape[0], -1, *t.shape[3:])


def rope_cos_sin(s, dim):
    inv = ROPE_THETA ** (-jnp.arange(0, dim, 2, dtype=jnp.float32) / dim)
    ang = jnp.arange(s, dtype=jnp.float32)[:, None] * inv[None, :]
    return jnp.cos(ang), jnp.sin(ang)


def apply_rope(x, cos, sin):
    half = x.shape[-1] // 2
    x1, x2 = x[..., :half], x[..., half:]
    cos = cos.astype(x.dtype)
    sin = sin.astype(x.dtype)
    return jnp.concatenate([x1 * cos - x2 * sin, x1 * sin + x2 * cos], -1)


def to_chunks(t):
    b, s = t.shape[:2]
    t = t.reshape(b, s // CHUNK, CHUNK, *t.shape[2:])
    return jnp.moveaxis(jnp.moveaxis(t, 1, 0), 3, 2)


def from_chunks(t):
    t = jnp.moveaxis(jnp.moveaxis(t, 2, 3), 0, 1)
    return t.reshape(t.shape[0], -1, *t.shape[3:])


def mla_mixer(x, w_dq, q_norm_g, w_uq, w_dkv, kv_norm_g, w_ukv, w_o):
    b, s, _ = x.shape
    H = MLA_HEADS
    cq = rms_norm(x @ w_dq, q_norm_g)
    q = (cq @ w_uq).reshape(b, s, H, MLA_NOPE + MLA_ROPE)
    ckv_kr = x @ w_dkv
    ckv = rms_norm(ckv_kr[..., :MLA_KV_LORA], kv_norm_g)
    kv = (ckv @ w_ukv).reshape(b, s, H, MLA_NOPE + MLA_V)
    k_nope, v = kv[..., :MLA_NOPE], kv[..., MLA_NOPE:]
    cos, sin = rope_cos_sin(s, MLA_ROPE)
    q_nope = q[..., :MLA_NOPE]
    q_rope = apply_rope(q[..., MLA_NOPE:], cos[:, None, :], sin[:, None, :])
    k_rope = apply_rope(ckv_kr[..., MLA_KV_LORA:], cos, sin)
    scale = (MLA_NOPE + MLA_ROPE) ** -0.5

    def block(args):
        qn, qr = args
        sc = (jnp.einsum('bqhd,bkhd->bhqk', qn, k_nope).astype(jnp.float32)
              + jnp.einsum('bqhr,bkr->bhqk', qr, k_rope).astype(jnp.float32)) * scale
        p = jax.nn.softmax(sc, -1).astype(v.dtype)
        return jnp.einsum('bhqk,bkhd->bqhd', p, v)

    o = merge_blocks(lax.map(block, (query_blocks(q_nope), query_blocks(q_rope))))
    return o.reshape(b, s, H * MLA_V) @ w_o


def diff_mixer(x, w_qkv, lam_p, subln_g, w_o, layer_idx):
    b, s, _ = x.shape
    H, d = DIFF_HEADS, DIFF_HD
    qkv = x @ w_qkv
    q = qkv[..., :2 * H * d].reshape(b, s, H, 2, d)
    k = qkv[..., 2 * H * d:4 * H * d].reshape(b, s, H, 2, d)
    v = qkv[..., 4 * H * d:].reshape(b, s, H, 2 * d)
    lam_init = 0.8 - 0.6 * math.exp(-0.3 * layer_idx)
    lp = lam_p.astype(jnp.float32)
    lam = jnp.exp(jnp.sum(lp[0] * lp[1])) - jnp.exp(jnp.sum(lp[2] * lp[3])) + lam_init
    slopes = 2.0 ** (-8.0 * jnp.arange(1, H + 1, dtype=jnp.float32) / H)
    pos = jnp.arange(s, dtype=jnp.int32)
    scale = d ** -0.5

    def block(args):
        qb, qpos = args
        sc = jnp.einsum('bqhjd,bkhjd->bhjqk', qb, k).astype(jnp.float32) * scale
        dist = jnp.abs(qpos[:, None] - pos[None, :]).astype(jnp.float32)
        sc = sc - slopes[None, :, None, None, None] * dist[None, None, None]
        p = jax.nn.softmax(sc, -1)
        a = (p[:, :, 0] - lam * p[:, :, 1]).astype(v.dtype)
        return jnp.einsum('bhqk,bkhd->bqhd', a, v)

    o = merge_blocks(lax.map(block, (query_blocks(q), pos.reshape(-1, Q_BLOCK))))
    o = rms_norm(o, subln_g) * (1.0 - lam_init)
    return o.reshape(b, s, H * 2 * d) @ w_o


def mlstm_chunkwise(q, k, v, i_pre, log_f):
    b, s, H, dk = q.shape
    dv = v.shape[-1]
    f32 = jnp.float32
    xs = (to_chunks(q.astype(f32)), to_chunks(k.astype(f32)), to_chunks(v.astype(f32)),
          to_chunks(i_pre), to_chunks(log_f))
    tri = jnp.tril(jnp.ones((CHUNK, CHUNK), bool))

    def step(carry, inp):
        C, n, m = carry
        qc, kc, vc, ic, fc = inp
        bcum = jnp.cumsum(fc, -1)
        dmat = jnp.where(tri, bcum[..., :, None] - bcum[..., None, :] + ic[..., None, :], -jnp.inf)
        m_inter = bcum + m[..., None]
        m_t = jnp.maximum(jnp.max(dmat, -1), m_inter)
        w_intra = jnp.exp(dmat - m_t[..., None])
        w_inter = jnp.exp(m_inter - m_t)
        sqk = jnp.einsum('bhtd,bhsd->bhts', qc, kc) * w_intra
        num = (w_inter[..., None] * jnp.einsum('bhtd,bhde->bhte', qc, C)
               + jnp.einsum('bhts,bhse->bhte', sqk, vc))
        den = w_inter * jnp.einsum('bhtd,bhd->bht', qc, n) + jnp.sum(sqk, -1)
        h = num / jnp.maximum(jnp.abs(den), jnp.exp(-m_t))[..., None]
        b_last = bcum[..., -1]
        g_s = b_last[..., None] - bcum + ic
        m_new = jnp.maximum(b_last + m, jnp.max(g_s, -1))
        w_s = jnp.exp(g_s - m_new[..., None])
        decay = jnp.exp(b_last + m - m_new)
        C_new = decay[..., None, None] * C + jnp.einsum('bhs,bhsd,bhse->bhde', w_s, kc, vc)
        n_new = decay[..., None] * n + jnp.einsum('bhs,bhsd->bhd', w_s, kc)
        return (C_new, n_new, m_new), h

    init = (jnp.zeros((b, H, dk, dv), f32), jnp.zeros((b, H, dk), f32), jnp.zeros((b, H), f32))
    _, hs = lax.scan(step, init, xs)
    return from_chunks(hs).astype(v.dtype)


def mlstm_mixer(x, w_in, b_gates, norm_g, w_out):
    b, s, _ = x.shape
    H, dk, dv = MLSTM_HEADS, MLSTM_QK, MLSTM_V
    proj = x @ w_in
    o1, o2, o3, o4 = H * dk, 2 * H * dk, 2 * H * dk + H * dv, 2 * H * dk + 2 * H * dv
    q = proj[..., :o1].reshape(b, s, H, dk)
    k = proj[..., o1:o2].reshape(b, s, H, dk) * (dk ** -0.5)
    v = proj[..., o2:o3].reshape(b, s, H, dv)
    o_gate = jax.nn.sigmoid(proj[..., o3:o4])
    g = (proj[..., o4:] + b_gates).astype(jnp.float32).reshape(b, s, 4, H)
    h_f = mlstm_chunkwise(q, k, v, g[:, :, 0], jax.nn.log_sigmoid(g[:, :, 1]))
    fl = lambda t: jnp.flip(t, 1)
    h_b = fl(mlstm_chunkwise(fl(q), fl(k), fl(v), fl(g[:, :, 2]), jax.nn.log_sigmoid(fl(g[:, :, 3]))))
    h = head_group_norm(h_f + h_b, norm_g.reshape(H, dv))
    return (h.reshape(b, s, H * dv) * o_gate) @ w_out


def retention_chunkwise(q, k, v, log_gamma):
    b, s, H, dk = q.shape
    dv = v.shape[-1]
    f32 = jnp.float32
    idx = jnp.arange(CHUNK, dtype=f32)
    rel = idx[:, None] - idx[None, :]
    dmask = jnp.exp(jnp.maximum(rel, 0.0)[None] * log_gamma[:, None, None]) * (rel >= 0)[None]
    xi = jnp.exp((idx[None] + 1.0) * log_gamma[:, None])
    zeta = jnp.exp((CHUNK - 1.0 - idx[None]) * log_gamma[:, None])
    g_chunk = jnp.exp(CHUNK * log_gamma)
    xs = (to_chunks(q.astype(f32)), to_chunks(k.astype(f32)), to_chunks(v.astype(f32)))

    def step(R, inp):
        qc, kc, vc = inp
        inner = jnp.einsum('bhts,bhse->bhte', jnp.einsum('bhtd,bhsd->bhts', qc, kc) * dmask, vc)
        cross = jnp.einsum('bhtd,bhde->bhte', qc, R) * xi[None, :, :, None]
        R_new = g_chunk[None, :, None, None] * R + jnp.einsum('bhsd,bhse->bhde', kc * zeta[None, :, :, None], vc)
        return R_new, inner + cross

    _, ys = lax.scan(step, jnp.zeros((b, H, dk, dv), f32), xs)
    return from_chunks(ys).astype(v.dtype)


def retention_mixer(x, w_in, decay_logit, norm_g, w_o):
    b, s, _ = x.shape
    H, dk, dv = RET_HEADS, RET_QK, RET_V
    proj = x @ w_in
    o1, o2, o3 = H * dk, 2 * H * dk, 2 * H * dk + H * dv
    q = proj[..., :o1].reshape(b, s, H, dk)
    k = proj[..., o1:o2].reshape(b, s, H, dk) * (dk ** -0.5)
    v = proj[..., o2:o3].reshape(b, s, H, dv)
    gate = proj[..., o3:]
    lg = jax.nn.log_sigmoid(decay_logit.astype(jnp.float32))
    fl = lambda t: jnp.flip(t, 1)
    y = retention_chunkwise(q, k, v, lg[0]) + fl(retention_chunkwise(fl(q), fl(k), fl(v), lg[1]))
    y = head_group_norm(y, norm_g.reshape(H, dv)).reshape(b, s, H * dv)
    return (jax.nn.silu(gate) * y) @ w_o


def conv_ffn(x, w_up, conv_w, conv_b, w_down):
    u = x @ w_up
    s = u.shape[1]
    half = CONV_W // 2
    up = jnp.pad(u, ((0, 0), (half, half), (0, 0)))
    c = conv_b
    for j in range(CONV_W):
        c = c + conv_w[j] * up[:, j:j + s]
    g, val = c[..., :FFN_HIDDEN], c[..., FFN_HIDDEN:]
    return (jax.nn.gelu(g) * val) @ w_down


def setup_inputs(seed: int = 0) -> dict:
    key = jax.random.key(seed)
    ks = iter(jax.random.split(key, 40))
    D = D_MODEL
    nA, nB, nC, nD = n_of(0), n_of(1), n_of(2), n_of(3)

    def w(shape, fan_in):
        return jax.random.normal(next(ks), shape, jnp.float32) * (fan_in ** -0.5)

    def gain(shape):
        return 1.0 + 0.02 * jax.random.normal(next(ks), shape, jnp.float32)

    inp = {}
    inp['x'] = jax.random.normal(next(ks), (BATCH, SEQ, D), jnp.float32)
    inp['norm_g'] = gain((DEPTH, 4, D))
    inp['ffn_w_up'] = w((DEPTH, D, 2 * FFN_HIDDEN), D)
    inp['ffn_conv_w'] = w((DEPTH, CONV_W, 2 * FFN_HIDDEN), CONV_W)
    inp['ffn_conv_b'] = 0.01 * jax.random.normal(next(ks), (DEPTH, 2 * FFN_HIDDEN), jnp.float32)
    inp['ffn_w_down'] = w((DEPTH, FFN_HIDDEN, D), FFN_HIDDEN)
    inp['mla_w_dq'] = w((nA, D, MLA_Q_LORA), D)
    inp['mla_q_norm_g'] = gain((nA, MLA_Q_LORA))
    inp['mla_w_uq'] = w((nA, MLA_Q_LORA, MLA_HEADS * (MLA_NOPE + MLA_ROPE)), MLA_Q_LORA)
    inp['mla_w_dkv'] = w((nA, D, MLA_KV_LORA + MLA_ROPE), D)
    inp['mla_kv_norm_g'] = gain((nA, MLA_KV_LORA))
    inp['mla_w_ukv'] = w((nA, MLA_KV_LORA, MLA_HEADS * (MLA_NOPE + MLA_V)), MLA_KV_LORA)
    inp['mla_w_o'] = w((nA, MLA_HEADS * MLA_V, D), MLA_HEADS * MLA_V)
    inp['diff_w_qkv'] = w((nB, D, 3 * D), D)
    inp['diff_lambda'] = 0.1 * jax.random.normal(next(ks), (nB, 4, DIFF_HD), jnp.float32)
    inp['diff_subln_g'] = gain((nB, 2 * DIFF_HD))
    inp['diff_w_o'] = w((nB, D, D), D)
    H = MLSTM_HEADS
    inp['mlstm_w_in'] = w((nC, D, 2 * H * MLSTM_QK + 2 * H * MLSTM_V + 4 * H), D)
    i_bias = 0.1 * jax.random.normal(next(ks), (nC, 2, H), jnp.float32)
    f_bias = jnp.linspace(3.0, 6.0, H, dtype=jnp.float32)[None, None] + 0.1 * jax.random.normal(next(ks), (nC, 2, H), jnp.float32)
    inp['mlstm_b_gates'] = jnp.stack([i_bias[:, 0], f_bias[:, 0], i_bias[:, 1], f_bias[:, 1]], 1).reshape(nC, 4 * H)
    inp['mlstm_norm_g'] = gain((nC, H * MLSTM_V))
    inp['mlstm_w_out'] = w((nC, H * MLSTM_V, D), H * MLSTM_V)
    Hr = RET_HEADS
    inp['ret_w_in'] = w((nD, D, 2 * Hr * RET_QK + 2 * Hr * RET_V), D)
    eps_h = 2.0 ** (-5.0 - np.arange(Hr, dtype=np.float32))
    base_logit = jnp.asarray(np.log((1.0 - eps_h) / eps_h), jnp.float32)
    inp['ret_decay_logit'] = base_logit[None, None] + 0.1 * jax.random.normal(next(ks), (nD, 2, Hr), jnp.float32)
    inp['ret_norm_g'] = gain((nD, Hr * RET_V))
    inp['ret_w_o'] = w((nD, Hr * RET_V, D), Hr * RET_V)
    return inp


def reference(x, norm_g, ffn_w_up, ffn_conv_w, ffn_conv_b, ffn_w_down,
              mla_w_dq, mla_q_norm_g, mla_w_uq, mla_w_dkv, mla_kv_norm_g, mla_w_ukv, mla_w_o,
              diff_w_qkv, diff_lambda, diff_subln_g, diff_w_o,
              mlstm_w_in, mlstm_b_gates, mlstm_norm_g, mlstm_w_out,
              ret_w_in, ret_decay_logit, ret_norm_g, ret_w_o):
    h = x
    for i in range(DEPTH):
        kind, j = i % N_MIXERS, i // N_MIXERS
        a = rms_norm(h, norm_g[i, 0])
        if kind == 0:
            a = mla_mixer(a, mla_w_dq[j], mla_q_norm_g[j], mla_w_uq[j], mla_w_dkv[j],
                          mla_kv_norm_g[j], mla_w_ukv[j], mla_w_o[j])
        elif kind == 1:
            a = diff_mixer(a, diff_w_qkv[j], diff_lambda[j], diff_subln_g[j], diff_w_o[j], i)
        elif kind == 2:
            a = mlstm_mixer(a, mlstm_w_in[j], mlstm_b_gates[j], mlstm_norm_g[j], mlstm_w_out[j])
        else:
            a = retention_mixer(a, ret_w_in[j], ret_decay_logit[j], ret_norm_g[j], ret_w_o[j])
        h = h + rms_norm(a, norm_g[i, 1])
        f = conv_ffn(rms_norm(h, norm_g[i, 2]), ffn_w_up[i], ffn_conv_w[i], ffn_conv_b[i], ffn_w_down[i])
        h = h + rms_norm(f, norm_g[i, 3])
    return h
```

```python
import contextlib
import numpy as np
import concourse.bass as bass
import concourse.mybir as mybir

F32 = mybir.dt.float32
BF16 = mybir.dt.bfloat16
AF = mybir.ActivationFunctionType
ALU = mybir.AluOpType
AX = mybir.AxisListType

ENGS = ("pe", "dve", "act", "pool", "sp")


class Res:
    def __init__(self, name, t=None):
        self.name = name
        self.t = t
        self.lw = None
        self.rd = []
        self.sem = None
        self.psum = False

    def __getitem__(self, idx):
        return self.t[idx]


class Sem:
    def __init__(self, h):
        self.h = h
        self.n = 0


class Prog:
    def __init__(self, nc, es):
        self.nc = nc
        self.es = es
        self.ops = []
        self.eng_ops = {e: [] for e in ENGS}
        self.engsem = {e: es.enter_context(nc.semaphore("S_" + e)) for e in ENGS}
        self.res = []
        self.free_sems = []
        self.sems = []

    def sb(self, name, shape, dt, es=None):
        self.uid = getattr(self, "uid", 0) + 1
        name = "%s_u%d" % (name, self.uid)
        t = (es or self.es).enter_context(self.nc.sbuf_tensor(name, list(shape), dt))
        r = Res(name, t)
        self.res.append(r)
        return r

    def ps(self, name, shape, dt=F32, es=None):
        t = (es or self.es).enter_context(self.nc.psum_tensor(name, list(shape), dt))
        r = Res(name, t)
        r.psum = True
        self.res.append(r)
        return r

    def _dsem(self, r):
        if r.sem is None:
            if self.free_sems:
                r.sem = self.free_sems.pop()
            else:
                h = self.es.enter_context(self.nc.semaphore("D%d" % len(self.sems)))
                r.sem = Sem(h)
                self.sems.append(r.sem)
        return r.sem

    def release(self, rs):
        for r in rs:
            if r.sem is not None:
                self.free_sems.append(r.sem)
                r.sem = None
            if r in self.res:
                self.res.remove(r)

    def op(self, eng, fn, reads=(), writes=(), dma=None, waw=True):
        idx = len(self.ops)
        tok = ("op", idx)
        if dma is not None:
            sm = self._dsem(dma)
            sm.n += 1
            tok = ("dma", sm, sm.n)
        deps = set()
        for r in reads:
            if r.lw is not None:
                deps.add(r.lw)
            if r.psum:
                for d in r.rd:
                    if d[0] == "op" and self.ops[d[1]]["eng"] != eng:
                        deps.add(d)
        for w in writes:
            if w.lw is not None:
                d = w.lw
                if d[0] == "op" and self.ops[d[1]]["eng"] == eng:
                    pass
                elif d[0] == "dma" and dma is not None and not waw and d[1] is dma.sem:
                    pass
                else:
                    deps.add(d)
            for d in w.rd:
                if d[0] == "op" and self.ops[d[1]]["eng"] == eng:
                    continue
                deps.add(d)
        if eng == "pe":
            deps = {d for d in deps if not (d[0] == "op" and self.ops[d[1]]["eng"] == "pe")}
        deps.discard(tok)
        o = dict(eng=eng, fn=fn, deps=deps, dma=dma, tok=tok, marked=False,
                 dmasem=(dma.sem if dma is not None else None))
        self.ops.append(o)
        self.eng_ops[eng].append(idx)
        for d in deps:
            if d[0] == "op":
                self.ops[d[1]]["marked"] = True
        for r in reads:
            r.rd.append(tok)
        for w in writes:
            w.lw = tok
            w.rd = []
        return idx

    def barrier(self):
        last = {}
        for e in ENGS:
            for i in reversed(self.eng_ops[e]):
                if self.ops[i]["fn"] is not None and self.ops[i]["dma"] is None:
                    last[e] = i
                    break
        dmas = [("dma", sm, sm.n) for sm in self.sems if sm.n > 0]
        for e in ENGS:
            deps = set(dmas)
            for e2, i in last.items():
                if e2 != e:
                    deps.add(("op", i))
                    self.ops[i]["marked"] = True
            idx = len(self.ops)
            self.ops.append(dict(eng=e, fn=None, deps=deps, dma=None, tok=("op", idx), marked=False))
            self.eng_ops[e].append(idx)
        for r in self.res:
            r.lw = None
            r.rd = []

    def emit(self):
        nc = self.nc
        semval = {}
        cnt = {e: 0 for e in ENGS}
        for i, o in enumerate(self.ops):
            if o["marked"] and o["dma"] is None and o["fn"] is not None:
                cnt[o["eng"]] += 1
                semval[i] = cnt[o["eng"]]
        self.stats = dict(cnt)

        def run(ename, eng):
            waited = {}
            for i in self.eng_ops[ename]:
                o = self.ops[i]
                for d in sorted(o["deps"], key=lambda d: (d[0], d[1] if d[0] == "op" else id(d[1]))):
                    if d[0] == "op":
                        p = self.ops[d[1]]
                        if p["fn"] is None:
                            continue
                        sem, val = self.engsem[p["eng"]], semval[d[1]]
                    else:
                        sem, val = d[1].h, 16 * d[2]
                    k = id(sem)
                    if waited.get(k, 0) < val:
                        eng.wait_ge(sem, val)
                        waited[k] = val
                if o["fn"] is None:
                    continue
                ins = o["fn"](eng)
                if o["dma"] is not None:
                    ins.then_inc(o["dmasem"].h, 16)
                elif o["marked"]:
                    ins.then_inc(self.engsem[ename], 1)

        with nc.Block() as block:
            @block.tensor
            def _(e):
                run("pe", e)

            @block.vector
            def _(e):
                run("dve", e)

            @block.scalar
            def _(e):
                run("act", e)

            @block.gpsimd
            def _(e):
                run("pool", e)

            @block.sync
            def _(e):
                run("sp", e)

    def mm(self, out_r, out_ap, lhsT, rhs, reads, start=True, stop=True):
        return self.op("pe", lambda e: e.matmul(out_ap, lhsT, rhs, start=start, stop=stop),
                       reads=reads, writes=[out_r])

    def tr(self, out_r, out_ap, in_ap, ident_ap, reads):
        return self.op("pe", lambda e: e.transpose(out_ap, in_ap, ident_ap), reads=reads, writes=[out_r])

    def load(self, r, out_ap, in_ap, q="sp", waw=False):
        return self.op(q, lambda e: e.dma_start(out=out_ap, in_=in_ap), writes=[r], dma=r, waw=waw)

    def store(self, r, out_ap, in_ap, q="sp"):
        return self.op(q, lambda e: e.dma_start(out=out_ap, in_=in_ap), reads=[r], dma=r)

from contextlib import ExitStack
from concourse.bass_utils import run_bass_kernel_spmd

D = 1024
FH = 2816
NFB = FH // 128
PAD = 8
EPS = 1e-6


class Ctx:
    pass


def make_ctx(nc, es, T, NT=384):
    C = Ctx()
    C.nc = nc
    C.T = T
    C.NT = NT
    C.P = Prog(nc, es)
    P = C.P
    C.ps = [P.ps("ps%d" % i, [128, 512], F32) for i in range(8)]
    C.ones_bf = P.sb("ones_bf", [128, 128], BF16)
    C.ident = P.sb("ident", [128, 128], F32)
    C.zeros = P.sb("zeros", [128, 8, PAD], F32)
    P.op("dve", lambda e: e.memset(C.ones_bf[:], 1.0), writes=[C.ones_bf])
    P.op("dve", lambda e: e.memset(C.zeros[:], 0.0), writes=[C.zeros])
    ident_d = nc.dram_tensor("ident_in", [128, 128], F32, kind="ExternalInput").ap()
    P.load(C.ident, C.ident[:], ident_d[:, :])
    C.ones_f = P.sb("ones_fc", [128, 128], F32)
    P.op("dve", lambda e: e.memset(C.ones_f[:], 1.0), writes=[C.ones_f])
    C.eps_t = P.sb("eps_t", [128, 1], F32)
    P.op("dve", lambda e: e.memset(C.eps_t[:], EPS), writes=[C.eps_t])
    return C


def hview(h):
    return h.rearrange("(c p) w -> p c w", p=128)


def zero_pads(C, h):
    P = C.P
    hv = hview(h)
    T = C.T
    P.store(C.zeros, hv[:, :, 0:PAD], C.zeros[:])
    P.store(C.zeros, hv[:, :, PAD + T:PAD + T + PAD], C.zeros[:])


def phase_in(C, x, h):
    P, T = C.P, C.T
    hv = hview(h)
    with ExitStack() as es:
        xin = [P.sb("xin%d" % i, [128, D], F32, es) for i in range(8)]
        stage = [P.sb("stg%d" % i, [128, 8, 512], F32, es) for i in range(2)]
        loc = xin + stage
        k = 0
        for g in range(T // 512):
            xs = []
            for j in range(4):
                xt = xin[(g % 2) * 4 + j]
                r0 = g * 512 + j * 128
                P.load(xt, xt[:], x[r0:r0 + 128, :])
                xs.append(xt)
            st = stage[g % 2]
            for c in range(8):
                pb = C.ps[k % 4]
                for j in range(4):
                    P.tr(pb, pb[:, j * 128:(j + 1) * 128], xs[j][:, c * 128:(c + 1) * 128], C.ident[:],
                         reads=[xs[j], C.ident])
                if k % 2 == 0:
                    P.op("dve", lambda e, st=st, c=c, pb=pb: e.tensor_copy(st[:, c, :], pb[:]),
                         reads=[pb], writes=[st])
                else:
                    P.op("act", lambda e, st=st, c=c, pb=pb: e.copy(st[:, c, :], pb[:]),
                         reads=[pb], writes=[st])
                k += 1
            P.store(st, hv[:, :, PAD + g * 512:PAD + (g + 1) * 512], st[:])
        P.barrier()
        P.release(loc)


def phase_out(C, h, out):
    P, T = C.P, C.T
    hv = hview(h)
    with ExitStack() as es:
        hin = [P.sb("hin%d" % i, [128, 8, 512], F32, es) for i in range(2)]
        ot = [P.sb("ot%d" % i, [128, D], F32, es) for i in range(4)]
        loc = hin + ot
        k = 0
        n = 0
        for g in range(T // 512):
            hi = hin[g % 2]
            P.load(hi, hi[:], hv[:, :, PAD + g * 512:PAD + (g + 1) * 512])
            for j in range(4):
                o = ot[n % 4]
                n += 1
                for half in range(2):
                    pb = C.ps[k % 4]
                    for q in range(4):
                        c = half * 4 + q
                        P.tr(pb, pb[:, q * 128:(q + 1) * 128], hi[:, c, j * 128:(j + 1) * 128], C.ident[:],
                             reads=[hi, C.ident])
                    if k % 2 == 0:
                        P.op("dve", lambda e, o=o, half=half, pb=pb: e.tensor_copy(o[:, half * 512:(half + 1) * 512], pb[:]),
                             reads=[pb], writes=[o])
                    else:
                        P.op("act", lambda e, o=o, half=half, pb=pb: e.copy(o[:, half * 512:(half + 1) * 512], pb[:]),
                             reads=[pb], writes=[o])
                    k += 1
                r0 = g * 512 + j * 128
                P.store(o, out[r0:r0 + 128, :], o[:])
        P.barrier()
        P.release(loc)


def rstd_from_sumsq(C, ps_sum, n, width, tmp, rstd):
    P = C.P
    P.op("act", lambda e: e.activation(tmp[:, :width], ps_sum[:, :width], AF.Sqrt, bias=C.eps_t[:, 0:1], scale=1.0 / n),
         reads=[ps_sum, C.eps_t], writes=[tmp])
    P.op("dve", lambda e: e.reciprocal(rstd[:, :width], tmp[:, :width]), reads=[tmp], writes=[rstd])


def phase_ffn(C, li, hin, hout, W):
    P, T, NT = C.P, C.T, C.NT
    NO = NT - 2
    hiv, hov = hview(hin), hview(hout)
    with ExitStack() as es:
        Wup = P.sb("Wup", [128, 8, 2 * FH], BF16, es)
        Wdn = P.sb("Wdn", [128, NFB, D], BF16, es)
        cw = P.sb("cw", [128, 44 * 3], F32, es)
        cb = P.sb("cb", [128, 44], F32, es)
        g2 = P.sb("g2", [128, 8], F32, es)
        g3 = P.sb("g3", [128, 8], F32, es)
        H = P.sb("H", [128, 8, NT], F32, es)
        xn = P.sb("xn", [128, 8, NT], BF16, es)
        hm = P.sb("hm", [128, NFB, NT], BF16, es)
        fT = P.sb("fT", [128, 8, NT], F32, es)
        sq = [P.sb("sq%d" % i, [128, NT], BF16, es) for i in range(2)]
        tmp = P.sb("tmp", [128, NT], F32, es)
        rstd = P.sb("rstd", [128, NT], F32, es)
        rstd2 = P.sb("rstd2", [128, NT], F32, es)
        tg = [[P.sb("tg%d%d" % (i, j), [128, NT], F32, es) for j in range(2)] for i in range(3)]
        tv = [[P.sb("tv%d%d" % (i, j), [128, NT], F32, es) for j in range(2)] for i in range(3)]
        loc = [Wup, Wdn, cw, cb, g2, g3, H, xn, hm, fT, tmp, rstd, rstd2] + sq + sum(tg, []) + sum(tv, [])

        wu = W["ffn_w_up"][li].rearrange("(c p) f -> p c f", p=128)
        for c in range(8):
            P.load(Wup, Wup[:, c, :], wu[:, c, :], q="pool")
        wd = W["ffn_w_down"][li].rearrange("(c p) d -> p c d", p=128)
        for c0 in range(0, NFB, 6):
            c1 = min(NFB, c0 + 6)
            P.load(Wdn, Wdn[:, c0:c1, :], wd[:, c0:c1, :], q="pool")
        P.load(cw, cw[:], W["ffn_cw"][li])
        P.load(cb, cb[:], W["ffn_cb"][li])
        P.load(g2, g2[:], W["norm_g"][li * 4 + 2])
        P.load(g3, g3[:], W["norm_g"][li * 4 + 3])

        import os
        STOP = int(os.environ.get("FFN_STOP", "99"))
        starts = []
        s = 0
        while True:
            if s + NO >= T:
                starts.append(T - NO)
                break
            starts.append(s)
            s += NO
        psS = C.ps[6]
        for ti, s in enumerate(starts):
            c0 = PAD + s - 1
            P.load(H, H[:], hiv[:, :, c0:c0 + NT])
            for c in range(8):
                sqt = sq[c % 2]
                P.op("act", lambda e, sqt=sqt, c=c: e.activation(sqt[:], H[:, c, :], AF.Square),
                     reads=[H], writes=[sqt])
                P.mm(psS, psS[:, :NT], C.ones_bf[:], sqt[:], reads=[C.ones_bf, sqt], start=(c == 0), stop=(c == 7))
            if STOP <= 1:
                continue
            rstd_from_sumsq(C, psS, D, NT, tmp, rstd)
            if STOP <= 2:
                continue
            for c in range(8):
                P.op("dve", lambda e, c=c: e.scalar_tensor_tensor(out=xn[:, c, :], in0=H[:, c, :], scalar=g2[:, c:c + 1],
                                                                  in1=rstd[:], op0=ALU.mult, op1=ALU.mult),
                     reads=[H, g2, rstd], writes=[xn])
            if STOP <= 3:
                continue
            for fb in range(NFB):
                pg = C.ps[(fb % 3) * 2]
                pv = C.ps[(fb % 3) * 2 + 1]
                for c in range(8):
                    P.mm(pg, pg[:, :NT], Wup[:, c, fb * 128:(fb + 1) * 128], xn[:, c, :], reads=[Wup, xn],
                         start=(c == 0), stop=(c == 7))
                for c in range(8):
                    P.mm(pv, pv[:, :NT], Wup[:, c, FH + fb * 128:FH + (fb + 1) * 128], xn[:, c, :], reads=[Wup, xn],
                         start=(c == 0), stop=(c == 7))
                outs = []
                for (pp, tt, fi) in ((pg, tg[fb % 3], fb), (pv, tv[fb % 3], fb + NFB)):
                    t1, t2 = tt
                    w0 = cw[:, fi * 3 + 0:fi * 3 + 1]
                    w1 = cw[:, fi * 3 + 1:fi * 3 + 2]
                    w2 = cw[:, fi * 3 + 2:fi * 3 + 3]
                    bb = cb[:, fi:fi + 1]
                    P.op("act", lambda e, t1=t1, pp=pp, w1=w1, bb=bb: e.activation(t1[:, :NO], pp[:, 1:1 + NO], AF.Identity,
                                                                                  bias=bb, scale=w1),
                         reads=[pp, cw, cb], writes=[t1])
                    P.op("dve", lambda e, t1=t1, t2=t2, pp=pp, w0=w0: e.scalar_tensor_tensor(
                        out=t2[:, :NO], in0=pp[:, 0:NO], scalar=w0, in1=t1[:, :NO], op0=ALU.mult, op1=ALU.add),
                        reads=[pp, cw, t1], writes=[t2])
                    P.op("dve", lambda e, t1=t1, t2=t2, pp=pp, w2=w2: e.scalar_tensor_tensor(
                        out=t1[:, :NO], in0=pp[:, 2:2 + NO], scalar=w2, in1=t2[:, :NO], op0=ALU.mult, op1=ALU.add),
                        reads=[pp, cw, t2], writes=[t1])
                    outs.append((t1, t2))
                (cg, gbuf), (cv, _) = outs
                P.op("act", lambda e, gbuf=gbuf, cg=cg: e.activation(gbuf[:, :NO], cg[:, :NO], AF.Gelu_apprx_tanh),
                     reads=[cg], writes=[gbuf])
                P.op("pool", lambda e, fb=fb, gbuf=gbuf, cv=cv: e.tensor_tensor(out=hm[:, fb, :NO], in0=gbuf[:, :NO], in1=cv[:, :NO],
                                                                                op=ALU.mult),
                     reads=[gbuf, cv], writes=[hm])
            if STOP <= 4:
                continue
            for db in range(8):
                pd = C.ps[db % 6]
                for fc in range(NFB):
                    P.mm(pd, pd[:, :NO], Wdn[:, fc, db * 128:(db + 1) * 128], hm[:, fc, :NO], reads=[Wdn, hm],
                         start=(fc == 0), stop=(fc == NFB - 1))
                sqt = sq[db % 2]
                P.op("dve", lambda e, db=db, pd=pd: e.tensor_copy(fT[:, db, :NO], pd[:, :NO]), reads=[pd], writes=[fT])
                P.op("act", lambda e, sqt=sqt, db=db: e.activation(sqt[:, :NO], fT[:, db, :NO], AF.Square),
                     reads=[fT], writes=[sqt])
                if STOP >= 6:
                    P.mm(psS, psS[:, :NO], C.ones_bf[:], sqt[:, :NO], reads=[C.ones_bf, sqt], start=(db == 0), stop=(db == 7))
            if STOP <= 6:
                continue
            rstd_from_sumsq(C, psS, D, NO, tmp, rstd2)
            if STOP <= 7:
                continue
            for c in range(8):
                P.op("dve", lambda e, c=c: e.scalar_tensor_tensor(out=fT[:, c, :NO], in0=fT[:, c, :NO], scalar=g3[:, c:c + 1],
                                                                  in1=rstd2[:, :NO], op0=ALU.mult, op1=ALU.mult),
                     reads=[fT, g3, rstd2], writes=[fT])
                P.op("pool", lambda e, c=c: e.tensor_tensor(out=fT[:, c, :NO], in0=fT[:, c, :NO], in1=H[:, c, 1:1 + NO], op=ALU.add),
                     reads=[fT, H], writes=[fT])
            if STOP <= 8:
                continue
            P.store(fT, hov[:, :, PAD + s:PAD + s + NO], fT[:, :, :NO])
        P.barrier()
        P.release(loc)


def gelu_tanh(C, out, x, n):
    P = C.P
    P.op("act", lambda e: e.activation(out[:, :n], x[:, :n], AF.Square), reads=[x], writes=[out])
    P.op("pool", lambda e: e.tensor_scalar(out[:, :n], out[:, :n], 0.044715, 1.0, ALU.mult, ALU.add), reads=[out], writes=[out])
    P.op("pool", lambda e: e.tensor_tensor(out=out[:, :n], in0=out[:, :n], in1=x[:, :n], op=ALU.mult), reads=[out, x], writes=[out])
    P.op("act", lambda e: e.activation(out[:, :n], out[:, :n], AF.Sigmoid, scale=1.5957691216057308), reads=[out], writes=[out])
    P.op("pool", lambda e: e.tensor_tensor(out=out[:, :n], in0=out[:, :n], in1=x[:, :n], op=ALU.mult), reads=[out, x], writes=[out])


def declare_inputs(nc, shapes):
    W = {}
    for name, shp in shapes.items():
        W[name] = nc.dram_tensor(name, list(shp), F32, kind="ExternalInput").ap()
    return W


def host_ffn_params(inp):
    L = inp["ffn_conv_w"].shape[0]
    cw = np.ascontiguousarray(inp["ffn_conv_w"].transpose(0, 2, 1).reshape(L, 44, 128, 3).transpose(0, 2, 1, 3).reshape(L, 128, 132))
    cb = np.ascontiguousarray(inp["ffn_conv_b"].reshape(L, 44, 128).transpose(0, 2, 1))
    ng = np.ascontiguousarray(inp["norm_g"].reshape(L * 4, 8, 128).transpose(0, 2, 1))
    return cw, cb, ng


def load_w(C, es, name, ap, q="pool"):
    P = C.P
    K, N = ap.shape
    kc = K // 128
    t = P.sb(name, [128, kc, N], BF16, es)
    v = ap.rearrange("(c p) n -> p c n", p=128)
    step = max(1, 4096 // N)
    for c0 in range(0, kc, step):
        c1 = min(kc, c0 + step)
        P.load(t, t[:, c0:c1, :], v[:, c0:c1, :], q=q)
    return t


def load_small(C, es, name, ap):
    P = C.P
    t = P.sb(name, list(ap.shape), F32, es)
    P.load(t, t[:], ap)
    return t


class NormBufs:
    def __init__(self, C, es, n, pref):
        P = C.P
        self.H = P.sb(pref + "H", [128, 8, n], F32, es)
        self.xns = [P.sb(pref + "xn%d" % i, [128, 8, n], BF16, es) for i in range(2)]
        self.xn = self.xns[0]
        self.ncall = 0
        self.sq = [P.sb(pref + "sq%d" % i, [128, n], BF16, es) for i in range(2)]
        self.tmp = P.sb(pref + "tmp", [128, n], F32, es)
        self.rstd = P.sb(pref + "rstd", [128, n], F32, es)
        self.all = [self.H, self.tmp, self.rstd] + self.xns + self.sq


def norm_in(C, hv, col0, n, g, B, psS):
    P = C.P
    B.xn = B.xns[B.ncall % 2]
    B.ncall += 1
    P.load(B.H, B.H[:, :, :n], hv[:, :, col0:col0 + n])
    for c in range(8):
        sqt = B.sq[c % 2]
        P.op("act", lambda e, sqt=sqt, c=c: e.activation(sqt[:, :n], B.H[:, c, :n], AF.Square), reads=[B.H], writes=[sqt])
        P.mm(psS, psS[:, :n], C.ones_bf[:], sqt[:, :n], reads=[C.ones_bf, sqt], start=(c == 0), stop=(c == 7))
    rstd_from_sumsq(C, psS, D, n, B.tmp, B.rstd)
    for c in range(8):
        P.op("dve", lambda e, c=c, xn=B.xn: e.scalar_tensor_tensor(out=xn[:, c, :n], in0=B.H[:, c, :n], scalar=g[:, c:c + 1],
                                                          in1=B.rstd[:, :n], op0=ALU.mult, op1=ALU.mult),
             reads=[B.H, g, B.rstd], writes=[B.xn])


def sub_norm(C, src, nch, n, nfeat, g, dst, sq, tmp, rstd, psS, extra_scale=None):
    P = C.P
    for c in range(nch):
        sqt = sq[c % 2]
        P.op("act", lambda e, sqt=sqt, c=c: e.activation(sqt[:, :n], src[:, c, :n], AF.Square), reads=[src], writes=[sqt])
        P.mm(psS, psS[:, :n], C.ones_bf[:], sqt[:, :n], reads=[C.ones_bf, sqt], start=(c == 0), stop=(c == nch - 1))
    rstd_from_sumsq(C, psS, nfeat, n, tmp, rstd)
    for c in range(nch):
        P.op("dve", lambda e, c=c: e.scalar_tensor_tensor(out=dst[:, c, :n], in0=src[:, c, :n], scalar=g[:, c:c + 1],
                                                          in1=rstd[:, :n], op0=ALU.mult, op1=ALU.mult),
             reads=[src, g, rstd], writes=[dst])


def phase_tail(C, ao, Wo_ap, g1_ap, hin, hout, NTK=512):
    P, T = C.P, C.T
    KF = ao.shape[0]
    kc = KF // 128
    hiv, hov = hview(hin), hview(hout)
    aov = ao.rearrange("(c p) t -> p c t", p=128)
    with ExitStack() as es:
        Wo = load_w(C, es, "Wo", Wo_ap)
        g1 = load_small(C, es, "g1", g1_ap)
        A = [P.sb("tA%d" % i, [128, kc, NTK], BF16, es) for i in range(2)]
        H = [P.sb("tH%d" % i, [128, 8, NTK], F32, es) for i in range(2)]
        fTs = [P.sb("tfT%d" % i, [128, 8, NTK], F32, es) for i in range(2)]
        sq = [P.sb("tsq%d" % i, [128, NTK], BF16, es) for i in range(4)]
        tmps = [P.sb("ttmp%d" % i, [128, NTK], F32, es) for i in range(2)]
        rstds = [P.sb("trstd%d" % i, [128, NTK], F32, es) for i in range(2)]
        loc = [Wo, g1] + fTs + tmps + rstds + A + H + sq
        psS = C.ps[6]
        for ti in range(T // NTK):
            t0 = ti * NTK
            a, h = A[ti % 2], H[ti % 2]
            fT, tmp, rstd = fTs[ti % 2], tmps[ti % 2], rstds[ti % 2]
            P.load(a, a[:], aov[:, :, t0:t0 + NTK])
            P.load(h, h[:], hiv[:, :, PAD + t0:PAD + t0 + NTK])
            for db in range(8):
                pd = C.ps[db % 6]
                for c in range(kc):
                    P.mm(pd, pd[:, :NTK], Wo[:, c, db * 128:(db + 1) * 128], a[:, c, :], reads=[Wo, a],
                         start=(c == 0), stop=(c == kc - 1))
                sqt = sq[db % 4]
                P.op("dve", lambda e, db=db, pd=pd, fT=fT: e.tensor_copy(fT[:, db, :], pd[:, :NTK]), reads=[pd], writes=[fT])
                P.op("act", lambda e, sqt=sqt, db=db, fT=fT: e.activation(sqt[:], fT[:, db, :], AF.Square), reads=[fT], writes=[sqt])
                P.mm(psS, psS[:, :NTK], C.ones_bf[:], sqt[:], reads=[C.ones_bf, sqt], start=(db == 0), stop=(db == 7))
            rstd_from_sumsq(C, psS, D, NTK, tmp, rstd)
            for c in range(8):
                P.op("dve", lambda e, c=c, fT=fT, rstd=rstd: e.scalar_tensor_tensor(out=fT[:, c, :], in0=fT[:, c, :], scalar=g1[:, c:c + 1],
                                                                  in1=rstd[:], op0=ALU.mult, op1=ALU.mult),
                     reads=[fT, g1, rstd], writes=[fT])
                P.op("pool", lambda e, c=c, h=h, fT=fT: e.tensor_tensor(out=fT[:, c, :], in0=fT[:, c, :], in1=h[:, c, :], op=ALU.add),
                     reads=[fT, h], writes=[fT])
            P.store(fT, hov[:, :, PAD + t0:PAD + t0 + NTK], fT[:])
        P.barrier()
        P.release(loc)


def phase_mla_proj(C, li, hin, W, S, NTK=512):
    P, T = C.P, C.T
    hiv = hview(hin)
    with ExitStack() as es:
        Wdq = load_w(C, es, "Wdq", W["mla_w_dq"])
        Wuq = load_w(C, es, "Wuq", W["mla_w_uq"])
        Wdkv = load_w(C, es, "Wdkv", W["mla_w_dkv"])
        Wukv = load_w(C, es, "Wukv", W["mla_w_ukv"])
        g0 = load_small(C, es, "g0", W["norm_g"][li * 4 + 0])
        qg = load_small(C, es, "qg", W["mla_qg"])
        kvg = load_small(C, es, "kvg", W["mla_kvg"])
        B = NormBufs(C, es, NTK, "m")
        cqf = P.sb("cqf", [128, 3, NTK], F32, es)
        cqn = P.sb("cqn", [128, 3, NTK], BF16, es)
        ckf = P.sb("ckf", [128, 2, NTK], F32, es)
        ckn = P.sb("ckn", [128, 2, NTK], BF16, es)
        cs = P.sb("cs", [64, 2, NTK], F32, es)
        qn_st = P.sb("qn_st", [128, 8, NTK], BF16, es)
        kn_st = P.sb("kn_st", [128, 8, NTK], BF16, es)
        qr_st = P.sb("qr_st", [64, 8, NTK], BF16, es)
        kr_st = P.sb("kr_st", [64, NTK], BF16, es)
        v_st = P.sb("v_st", [128, NTK // 128, 1024], BF16, es)
        r1 = [P.sb("r1_%d" % i, [64, NTK], F32, es) for i in range(2)]
        r2 = [P.sb("r2_%d" % i, [64, NTK], F32, es) for i in range(2)]
        tmp2 = P.sb("mtmp2", [128, NTK], F32, es)
        rstd2 = P.sb("mrstd2", [128, NTK], F32, es)
        loc = [Wdq, Wuq, Wdkv, Wukv, g0, qg, kvg, cqf, cqn, ckf, ckn, cs, qn_st, kn_st, qr_st, kr_st, v_st, tmp2, rstd2] + B.all + r1 + r2
        psS = C.ps[6]
        qnv = S["qn"].rearrange("(h p) t -> p h t", p=128)
        knv = S["kn"].rearrange("(h p) t -> p h t", p=128)
        qrv = S["qr"].rearrange("(h p) t -> p h t", p=64)
        vv = S["v"].rearrange("(tb p) f -> p tb f", p=128)
        k = 0

        def rope(psA, psB, out_ap, out_r, i):
            a, b = r1[i % 2], r2[i % 2]
            P.op("dve", lambda e: e.tensor_tensor(out=a[:], in0=psA[0:64, :NTK], in1=cs[:, 0, :], op=ALU.mult), reads=[psA, cs], writes=[a])
            P.op("dve", lambda e: e.tensor_tensor(out=b[:], in0=psB[0:64, :NTK], in1=cs[:, 1, :], op=ALU.mult), reads=[psB, cs], writes=[b])
            P.op("pool", lambda e: e.tensor_tensor(out=out_ap, in0=a[:], in1=b[:], op=ALU.add), reads=[a, b], writes=[out_r])

        for ti in range(T // NTK):
            t0 = ti * NTK
            norm_in(C, hiv, PAD + t0, NTK, g0, B, psS)
            P.load(cs, cs[:], W["rope_cs"][:, :, t0:t0 + NTK])
            for fo in range(3):
                pb = C.ps[k % 4]; k += 1
                for c in range(8):
                    P.mm(pb, pb[:, :NTK], Wdq[:, c, fo * 128:(fo + 1) * 128], B.xn[:, c, :], reads=[Wdq, B.xn], start=(c == 0), stop=(c == 7))
                P.op("dve", lambda e, fo=fo, pb=pb: e.tensor_copy(cqf[:, fo, :], pb[:, :NTK]), reads=[pb], writes=[cqf])
            sub_norm(C, cqf, 3, NTK, 384, qg, cqn, B.sq, tmp2, rstd2, psS)
            for h in range(8):
                pb = C.ps[k % 4]; k += 1
                for c in range(3):
                    P.mm(pb, pb[:, :NTK], Wuq[:, c, h * 128:(h + 1) * 128], cqn[:, c, :], reads=[Wuq, cqn], start=(c == 0), stop=(c == 2))
                if h % 2 == 0:
                    P.op("act", lambda e, h=h, pb=pb: e.copy(qn_st[:, h, :], pb[:, :NTK]), reads=[pb], writes=[qn_st])
                else:
                    P.op("dve", lambda e, h=h, pb=pb: e.tensor_copy(qn_st[:, h, :], pb[:, :NTK]), reads=[pb], writes=[qn_st])
            P.store(qn_st, qnv[:, :, t0:t0 + NTK], qn_st[:])
            for h in range(8):
                pa = C.ps[k % 4]; k += 1
                pb = C.ps[k % 4]; k += 1
                for c in range(3):
                    P.mm(pa, pa[0:64, :NTK], Wuq[:, c, 1024 + h * 64:1024 + (h + 1) * 64], cqn[:, c, :], reads=[Wuq, cqn], start=(c == 0), stop=(c == 2))
                for c in range(3):
                    P.mm(pb, pb[0:64, :NTK], Wuq[:, c, 1536 + h * 64:1536 + (h + 1) * 64], cqn[:, c, :], reads=[Wuq, cqn], start=(c == 0), stop=(c == 2))
                rope(pa, pb, qr_st[:, h, :], qr_st, h)
            P.store(qr_st, qrv[:, :, t0:t0 + NTK], qr_st[:])
            for fo in range(2):
                pb = C.ps[k % 4]; k += 1
                for c in range(8):
                    P.mm(pb, pb[:, :NTK], Wdkv[:, c, fo * 128:(fo + 1) * 128], B.xn[:, c, :], reads=[Wdkv, B.xn], start=(c == 0), stop=(c == 7))
                P.op("dve", lambda e, fo=fo, pb=pb: e.tensor_copy(ckf[:, fo, :], pb[:, :NTK]), reads=[pb], writes=[ckf])
            pa = C.ps[k % 4]; k += 1
            pb = C.ps[k % 4]; k += 1
            for c in range(8):
                P.mm(pa, pa[0:64, :NTK], Wdkv[:, c, 256:320], B.xn[:, c, :], reads=[Wdkv, B.xn], start=(c == 0), stop=(c == 7))
            for c in range(8):
                P.mm(pb, pb[0:64, :NTK], Wdkv[:, c, 320:384], B.xn[:, c, :], reads=[Wdkv, B.xn], start=(c == 0), stop=(c == 7))
            rope(pa, pb, kr_st[:], kr_st, 0)
            P.store(kr_st, S["kr"][:, t0:t0 + NTK], kr_st[:])
            sub_norm(C, ckf, 2, NTK, 256, kvg, ckn, B.sq, tmp2, rstd2, psS)
            for h in range(8):
                pb = C.ps[k % 4]; k += 1
                for c in range(2):
                    P.mm(pb, pb[:, :NTK], Wukv[:, c, h * 128:(h + 1) * 128], ckn[:, c, :], reads=[Wukv, ckn], start=(c == 0), stop=(c == 1))
                if h % 2 == 0:
                    P.op("act", lambda e, h=h, pb=pb: e.copy(kn_st[:, h, :], pb[:, :NTK]), reads=[pb], writes=[kn_st])
                else:
                    P.op("dve", lambda e, h=h, pb=pb: e.tensor_copy(kn_st[:, h, :], pb[:, :NTK]), reads=[pb], writes=[kn_st])
            P.store(kn_st, knv[:, :, t0:t0 + NTK], kn_st[:])
            for tb in range(NTK // 128):
                for half in range(2):
                    pb = C.ps[k % 4]; k += 1
                    for c in range(2):
                        P.mm(pb, pb[:, :512], ckn[:, c, tb * 128:(tb + 1) * 128], Wukv[:, c, 1024 + half * 512:1024 + (half + 1) * 512],
                             reads=[Wukv, ckn], start=(c == 0), stop=(c == 1))
                    if half == 0:
                        P.op("act", lambda e, tb=tb, half=half, pb=pb: e.copy(v_st[:, tb, half * 512:(half + 1) * 512], pb[:, :512]), reads=[pb], writes=[v_st])
                    else:
                        P.op("dve", lambda e, tb=tb, half=half, pb=pb: e.tensor_copy(v_st[:, tb, half * 512:(half + 1) * 512], pb[:, :512]), reads=[pb], writes=[v_st])
            P.store(v_st, vv[:, t0 // 128:(t0 + NTK) // 128, :], v_st[:])
        P.barrier()
        P.release(loc)


def phase_mla_core(C, S, TQ0=0, TQ=None, NQ=512):
    P, T = C.P, C.T
    TQ = TQ or T
    NKB = T // 128
    scale = float(192 ** -0.5)
    with ExitStack() as es:
        Kn = [P.sb("Kn%d" % i, [128, T], BF16, es) for i in range(2)]
        Vh = [P.sb("Vh%d" % i, [128, NKB, 128], BF16, es) for i in range(2)]
        Kr = P.sb("Kr", [64, T], BF16, es)
        Qn = [P.sb("Qn%d" % i, [128, NQ], BF16, es) for i in range(2)]
        Qr = [P.sb("Qr%d" % i, [64, NQ], BF16, es) for i in range(2)]
        Pt = [P.sb("Pt%d" % i, [128, NQ], BF16, es) for i in range(4)]
        rl = P.sb("rl", [128, NQ], F32, es)
        ob = [P.sb("ob%d" % i, [128, NQ], BF16, es) for i in range(2)]
        loc = Kn + Vh + [Kr, rl] + Qn + Qr + Pt + ob
        P.load(Kr, Kr[:], S["kr"][:, :])
        vv = S["v"].rearrange("(kb p) f -> p kb f", p=128)
        qrv = S["qr"].rearrange("(h p) t -> p h t", p=64)
        it = 0
        for h in range(8):
            kn, vh = Kn[h % 2], Vh[h % 2]
            P.load(kn, kn[:], S["kn"][h * 128:(h + 1) * 128, :])
            P.load(vh, vh[:], vv[:, :, h * 128:(h + 1) * 128])
            for qi in range(TQ // NQ):
                q0 = TQ0 + qi * NQ
                qn, qr = Qn[it % 2], Qr[it % 2]
                o_sb = ob[it % 2]
                pO, pL = C.ps[3 + it % 2], C.ps[5 + it % 2]
                it += 1
                P.load(qn, qn[:], S["qn"][h * 128:(h + 1) * 128, q0:q0 + NQ])
                P.load(qr, qr[:], qrv[:, h, q0:q0 + NQ])

                def stA(kb, kn=kn, qn=qn, qr=qr):
                    pS = C.ps[kb % 3]
                    pt = Pt[kb % 4]
                    ks = slice(kb * 128, (kb + 1) * 128)
                    P.mm(pS, pS[:, :NQ], kn[:, ks], qn[:], reads=[kn, qn], start=True, stop=False)
                    P.mm(pS, pS[:, :NQ], Kr[:, ks], qr[:], reads=[Kr, qr], start=False, stop=True)
                    P.op("act", lambda e, pt=pt, pS=pS: e.activation(pt[:], pS[:, :NQ], AF.Exp, scale=scale), reads=[pS], writes=[pt])

                def stB(kb, vh=vh, pO=pO, pL=pL):
                    pt = Pt[kb % 4]
                    P.mm(pO, pO[:, :NQ], vh[:, kb, :], pt[:], reads=[vh, pt], start=(kb == 0), stop=(kb == NKB - 1))
                    P.mm(pL, pL[:, :NQ], C.ones_bf[:], pt[:], reads=[C.ones_bf, pt], start=(kb == 0), stop=(kb == NKB - 1))

                stA(0)
                if NKB > 1:
                    stA(1)
                for kb in range(NKB):
                    if kb + 2 < NKB:
                        stA(kb + 2)
                    stB(kb)
                P.op("dve", lambda e, pL=pL: e.reciprocal(rl[:], pL[:, :NQ]), reads=[pL], writes=[rl])
                P.op("dve", lambda e, pO=pO, o_sb=o_sb: e.tensor_tensor(out=o_sb[:], in0=pO[:, :NQ], in1=rl[:], op=ALU.mult), reads=[pO, rl], writes=[o_sb])
                P.store(o_sb, S["ao"][h * 128:(h + 1) * 128, q0:q0 + NQ], o_sb[:])
        P.barrier()
        P.release(loc)


def pm(v):
    v = np.asarray(v)
    return np.ascontiguousarray(v.reshape(-1, 128).T)


def host_mla_params(inp, pos):
    wuq = inp["mla_w_uq"][0].reshape(384, 8, 192)
    nope = wuq[:, :, :128].reshape(384, 1024)
    ropew = wuq[:, :, 128:]
    rope_sw = np.concatenate([ropew[:, :, 32:], ropew[:, :, :32]], -1)
    w_uq = np.ascontiguousarray(np.concatenate([nope, ropew.reshape(384, 512), rope_sw.reshape(384, 512)], 1))
    wdkv = inp["mla_w_dkv"][0]
    kr = wdkv[:, 256:]
    w_dkv = np.ascontiguousarray(np.concatenate([wdkv[:, :256], kr, kr[:, 32:], kr[:, :32]], 1))
    wukv = inp["mla_w_ukv"][0].reshape(256, 8, 256)
    w_ukv = np.ascontiguousarray(np.concatenate([wukv[:, :, :128].reshape(256, 1024), wukv[:, :, 128:].reshape(256, 1024)], 1))
    inv = (10000.0 ** (-np.arange(0, 64, 2, dtype=np.float32) / 64)).astype(np.float32)
    ang = pos.astype(np.float32)[None, :] * inv[:, None]
    cos, sin = np.cos(ang).astype(np.float32), np.sin(ang).astype(np.float32)
    cs = np.stack([np.concatenate([cos, cos], 0), np.concatenate([-sin, sin], 0)], 1)
    return dict(mla_w_dq=np.ascontiguousarray(inp["mla_w_dq"][0]), mla_w_uq=w_uq, mla_w_dkv=w_dkv, mla_w_ukv=w_ukv,
                mla_w_o=np.ascontiguousarray(inp["mla_w_o"][0]),
                mla_qg=pm(inp["mla_q_norm_g"][0]), mla_kvg=pm(inp["mla_kv_norm_g"][0]), rope_cs=np.ascontiguousarray(cs.astype(np.float32)))


def phase_diff_proj(C, li, hin, W, S, NTK=512):
    P, T = C.P, C.T
    hiv = hview(hin)
    with ExitStack() as es:
        Wqkv = load_w(C, es, "Wqkv", W["diff_w_qkv"])
        g0 = load_small(C, es, "g0", W["norm_g"][li * 4 + 0])
        B = NormBufs(C, es, NTK, "d")
        q_st = P.sb("dq_st", [128, 8, NTK], BF16, es)
        k_st = P.sb("dk_st", [128, 8, NTK], BF16, es)
        v_st = P.sb("dv_st", [128, NTK // 128, 1024], BF16, es)
        loc = [Wqkv, g0, q_st, k_st, v_st] + B.all
        psS = C.ps[6]
        qv = S["qn"].rearrange("(h p) t -> p h t", p=128)
        kv = S["kn"].rearrange("(h p) t -> p h t", p=128)
        vv = S["v"].rearrange("(tb p) f -> p tb f", p=128)
        k = 0
        for ti in range(T // NTK):
            t0 = ti * NTK
            norm_in(C, hiv, PAD + t0, NTK, g0, B, psS)
            for (st, off, dst) in ((q_st, 0, qv), (k_st, 1024, kv)):
                for h in range(8):
                    pb = C.ps[k % 4]; k += 1
                    for c in range(8):
                        P.mm(pb, pb[:, :NTK], Wqkv[:, c, off + h * 128:off + (h + 1) * 128], B.xn[:, c, :], reads=[Wqkv, B.xn],
                             start=(c == 0), stop=(c == 7))
                    if h % 2 == 0:
                        P.op("act", lambda e, h=h, pb=pb, st=st: e.copy(st[:, h, :], pb[:, :NTK]), reads=[pb], writes=[st])
                    else:
                        P.op("dve", lambda e, h=h, pb=pb, st=st: e.tensor_copy(st[:, h, :], pb[:, :NTK]), reads=[pb], writes=[st])
                P.store(st, dst[:, :, t0:t0 + NTK], st[:])
            for tb in range(NTK // 128):
                for half in range(2):
                    pb = C.ps[k % 4]; k += 1
                    for c in range(8):
                        P.mm(pb, pb[:, :512], B.xn[:, c, tb * 128:(tb + 1) * 128], Wqkv[:, c, 2048 + half * 512:2048 + (half + 1) * 512],
                             reads=[Wqkv, B.xn], start=(c == 0), stop=(c == 7))
                    if half == 0:
                        P.op("act", lambda e, tb=tb, half=half, pb=pb: e.copy(v_st[:, tb, half * 512:(half + 1) * 512], pb[:, :512]), reads=[pb], writes=[v_st])
                    else:
                        P.op("dve", lambda e, tb=tb, half=half, pb=pb: e.tensor_copy(v_st[:, tb, half * 512:(half + 1) * 512], pb[:, :512]), reads=[pb], writes=[v_st])
            P.store(v_st, vv[:, t0 // 128:(t0 + NTK) // 128, :], v_st[:])
        P.barrier()
        P.release(loc)


def phase_diff_core(C, li, W, S, NQ=512):
    P, T = C.P, C.T
    NKB = T // 128
    scale = float(64 ** -0.5)
    lam_init = 0.8 - 0.6 * float(np.exp(-0.3 * li))
    SKIP = 160.0
    with ExitStack() as es:
        Kh = [P.sb("dK%d" % i, [128, T], BF16, es) for i in range(2)]
        Vh = [P.sb("dV%d" % i, [128, NKB, 128], BF16, es) for i in range(2)]
        Q = [P.sb("dQ%d" % i, [128, NQ], BF16, es) for i in range(2)]
        RAW = P.sb("dRAW", [128, 6, NQ], F32, es)
        BRAW = P.sb("dBRAW", [128, 128], F32, es)
        MT = P.sb("dMT", [128, 6, NQ], BF16, es)
        BT = P.sb("dBT", [128, 128], F32, es)
        E = [P.sb("dE%d" % i, [128, NQ], BF16, es) for i in range(4)]
        Pt = [P.sb("dPt%d" % i, [128, NQ], BF16, es) for i in range(6)]
        lp = P.sb("dlp", [1, 256], F32, es)
        lw = P.sb("dlw", [1, 8], F32, es)
        ones1 = P.sb("dones1", [1, 128], F32, es)
        nlam = P.sb("dnlam", [128, 1], F32, es)
        sg = P.sb("dsg", [128, 1], F32, es)
        r1 = P.sb("dr1", [128, NQ], F32, es)
        r2 = P.sb("dr2", [128, NQ], F32, es)
        a1 = P.sb("da1", [128, NQ], F32, es)
        a2 = P.sb("da2", [128, NQ], F32, es)
        sqd = P.sb("dsq", [128, NQ], BF16, es)
        ob = [P.sb("dob%d" % i, [128, NQ], BF16, es) for i in range(2)]
        loc = Kh + Vh + Q + [RAW, BRAW, MT, BT, lp, lw, ones1, nlam, sg, r1, r2, a1, a2, sqd] + E + Pt + ob
        P.op("pool", lambda e: e.iota(RAW[:, 0, :], [[1, NQ]], base=0, channel_multiplier=0, allow_small_or_imprecise_dtypes=True), writes=[RAW])
        P.op("pool", lambda e: e.iota(RAW[:, 1, :], [[-1, NQ]], base=NQ - 1, channel_multiplier=0, allow_small_or_imprecise_dtypes=True), writes=[RAW])
        for kk in range(4):
            P.op("pool", lambda e, kk=kk: e.iota(RAW[:, 2 + kk, :], [[1, NQ]], base=-128 * kk, channel_multiplier=-1,
                                                allow_small_or_imprecise_dtypes=True), writes=[RAW])
        P.op("dve", lambda e: e.scalar_tensor_tensor(out=RAW[:, 2:6, :], in0=RAW[:, 2:6, :], scalar=-1.0, in1=RAW[:, 2:6, :], op0=ALU.mult, op1=ALU.max),
             reads=[RAW], writes=[RAW])
        P.op("pool", lambda e: e.iota(BRAW[:, 0:64], [[-128, 64]], base=0, channel_multiplier=1, allow_small_or_imprecise_dtypes=True), writes=[BRAW])
        P.op("pool", lambda e: e.iota(BRAW[:, 64:128], [[-128, 64]], base=NQ - 1, channel_multiplier=-1, allow_small_or_imprecise_dtypes=True), writes=[BRAW])
        P.load(lp, lp[:], W["diff_lambda"])
        P.load(sg, sg[:], W["diff_subln_g"])
        P.op("dve", lambda e: e.memset(ones1[:], 1.0), writes=[ones1])
        P.op("dve", lambda e: e.tensor_tensor(out=lp[:, 0:64], in0=lp[:, 0:64], in1=lp[:, 64:128], op=ALU.mult), reads=[lp], writes=[lp])
        P.op("dve", lambda e: e.tensor_tensor(out=lp[:, 128:192], in0=lp[:, 128:192], in1=lp[:, 192:256], op=ALU.mult), reads=[lp], writes=[lp])
        P.op("dve", lambda e: e.reduce_sum(lw[:, 0:1], lp[:, 0:64], axis=AX.X), reads=[lp], writes=[lw])
        P.op("dve", lambda e: e.reduce_sum(lw[:, 1:2], lp[:, 128:192], axis=AX.X), reads=[lp], writes=[lw])
        P.op("act", lambda e: e.activation(lw[:, 2:4], lw[:, 0:2], AF.Exp), reads=[lw], writes=[lw])
        P.op("dve", lambda e: e.scalar_tensor_tensor(out=lw[:, 4:5], in0=lw[:, 3:4], scalar=-lam_init, in1=lw[:, 2:3], op0=ALU.add, op1=ALU.subtract),
             reads=[lw], writes=[lw])
        pc = C.ps[7]
        P.mm(pc, pc[:, 0:1], ones1[:], lw[:, 4:5], reads=[ones1, lw])
        P.op("dve", lambda e: e.tensor_copy(nlam[:], pc[:, 0:1]), reads=[pc], writes=[nlam])
        P.op("dve", lambda e: e.tensor_single_scalar(sg[:], sg[:], 1.0 - lam_init, ALU.mult), reads=[sg], writes=[sg])

        it = 0
        for h in range(8):
            slope = float(2.0 ** (-(h + 1)))
            kh, vh = Kh[h % 2], Vh[h % 2]
            P.load(kh, kh[:], S["kn"][h * 128:(h + 1) * 128, :])
            P.load(vh, vh[:], S["v"].rearrange("(kb p) f -> p kb f", p=128)[:, :, h * 128:(h + 1) * 128])
            P.op("act", lambda e, slope=slope: e.activation(MT[:], RAW[:], AF.Exp, scale=-slope), reads=[RAW], writes=[MT])
            P.op("dve", lambda e, slope=slope: e.tensor_single_scalar(BT[:], BRAW[:], slope, ALU.mult), reads=[BRAW], writes=[BT])
            for qi in range(T // NQ):
                q0 = qi * NQ
                q = Q[it % 2]
                o_sb = ob[it % 2]
                it += 1
                P.load(q, q[:], S["qn"][h * 128:(h + 1) * 128, q0:q0 + NQ])
                pO = [C.ps[4], C.ps[5]]
                pL = [C.ps[6], C.ps[7]]
                kbs = []
                for kb in range(NKB):
                    j0 = kb * 128
                    dmin = max(0, q0 - (j0 + 127), j0 - (q0 + NQ - 1))
                    if slope * dmin >= SKIP:
                        continue
                    kbs.append(kb)

                def stA(i, kh=kh, q=q, q0=q0):
                    kb = kbs[i]
                    ks = slice(kb * 128, (kb + 1) * 128)
                    j0 = kb * 128
                    if j0 + 128 <= q0:
                        m = (q0 - j0) // 128
                        bias, mt = BT[:, m:m + 1], MT[:, 0, :]
                    elif j0 >= q0 + NQ:
                        m = (j0 - q0) // 128
                        bias, mt = BT[:, 64 + m:64 + m + 1], MT[:, 1, :]
                    else:
                        kk = (j0 - q0) // 128
                        bias, mt = None, MT[:, 2 + kk, :]
                    for j in range(2):
                        pS = C.ps[(i % 2) * 2 + j]
                        e_t = E[(i % 2) * 2 + j]
                        pt = Pt[(i % 3) * 2 + j]
                        js = slice(j * 64, (j + 1) * 64)
                        P.mm(pS, pS[:, :NQ], kh[js, ks], q[js, :], reads=[kh, q])
                        if bias is None:
                            P.op("act", lambda e, e_t=e_t, pS=pS: e.activation(e_t[:], pS[:, :NQ], AF.Exp, scale=scale), reads=[pS], writes=[e_t])
                        else:
                            P.op("act", lambda e, e_t=e_t, pS=pS, bias=bias: e.activation(e_t[:], pS[:, :NQ], AF.Exp, scale=scale, bias=bias),
                                 reads=[pS, BT], writes=[e_t])
                        eng = "dve" if j == 0 else "pool"
                        P.op(eng, lambda e, pt=pt, e_t=e_t, mt=mt: e.tensor_tensor(out=pt[:], in0=e_t[:], in1=mt, op=ALU.mult),
                             reads=[e_t, MT], writes=[pt])

                def stB(i, vh=vh, pO=pO, pL=pL):
                    kb = kbs[i]
                    for j in range(2):
                        pt = Pt[(i % 3) * 2 + j]
                        P.mm(pO[j], pO[j][:, :NQ], vh[:, kb, :], pt[:], reads=[vh, pt], start=(i == 0), stop=(i == len(kbs) - 1))
                        P.mm(pL[j], pL[j][:, :NQ], C.ones_bf[:], pt[:], reads=[C.ones_bf, pt], start=(i == 0), stop=(i == len(kbs) - 1))

                stA(0)
                if len(kbs) > 1:
                    stA(1)
                for i in range(len(kbs)):
                    if i + 2 < len(kbs):
                        stA(i + 2)
                    stB(i)
                P.op("dve", lambda e, pL=pL: e.reciprocal(r1[:], pL[0][:, :NQ]), reads=[pL[0]], writes=[r1])
                P.op("dve", lambda e, pL=pL: e.reciprocal(r2[:], pL[1][:, :NQ]), reads=[pL[1]], writes=[r2])
                P.op("dve", lambda e, pO=pO: e.tensor_tensor(out=a1[:], in0=pO[0][:, :NQ], in1=r1[:], op=ALU.mult), reads=[pO[0], r1], writes=[a1])
                P.op("dve", lambda e, pO=pO: e.tensor_tensor(out=a2[:], in0=pO[1][:, :NQ], in1=r2[:], op=ALU.mult), reads=[pO[1], r2], writes=[a2])
                P.op("dve", lambda e: e.scalar_tensor_tensor(out=a1[:], in0=a2[:], scalar=nlam[:, 0:1], in1=a1[:], op0=ALU.mult, op1=ALU.add),
                     reads=[a1, a2, nlam], writes=[a1])
                psS = C.ps[0]
                P.op("act", lambda e: e.activation(sqd[:], a1[:], AF.Square), reads=[a1], writes=[sqd])
                P.mm(psS, psS[:, :NQ], C.ones_bf[:], sqd[:], reads=[C.ones_bf, sqd])
                rstd_from_sumsq(C, psS, 128, NQ, r1, r2)
                P.op("dve", lambda e, o_sb=o_sb: e.scalar_tensor_tensor(out=o_sb[:], in0=a1[:], scalar=sg[:, 0:1], in1=r2[:], op0=ALU.mult, op1=ALU.mult),
                     reads=[a1, sg, r2], writes=[o_sb])
                P.store(o_sb, S["ao"][h * 128:(h + 1) * 128, q0:q0 + NQ], o_sb[:])
        P.barrier()
        P.release(loc)


def host_diff_params(inp):
    return dict(diff_w_qkv=np.ascontiguousarray(inp["diff_w_qkv"][0]), diff_w_o=np.ascontiguousarray(inp["diff_w_o"][0]),
                diff_lambda=np.ascontiguousarray(inp["diff_lambda"][0].reshape(1, 256)),
                diff_subln_g=np.ascontiguousarray(inp["diff_subln_g"][0].reshape(128, 1)))


def make_tri(C, es):
    P = C.P
    R = Ctx()
    R.M01F = P.sb("M01F", [128, 128], F32, es)
    R.M01B = P.sb("M01B", [128, 128], F32, es)
    R.ones_f = P.sb("ones_f", [128, 128], F32, es)
    P.op("dve", lambda e: e.memset(R.ones_f[:], 1.0), writes=[R.ones_f])
    P.op("pool", lambda e: e.iota(R.M01F[:], [[1, 128]], base=0, channel_multiplier=-1, allow_small_or_imprecise_dtypes=True), writes=[R.M01F])
    P.op("pool", lambda e: e.iota(R.M01B[:], [[-1, 128]], base=0, channel_multiplier=1, allow_small_or_imprecise_dtypes=True), writes=[R.M01B])
    for m in (R.M01F, R.M01B):
        P.op("dve", lambda e, m=m: e.tensor_scalar(m[:], m[:], 1.0, 0.0, ALU.add, ALU.max), reads=[m], writes=[m])
        P.op("dve", lambda e, m=m: e.tensor_single_scalar(m[:], m[:], 1.0, ALU.min), reads=[m], writes=[m])
    R.all = [R.M01F, R.M01B, R.ones_f]
    return R


def phase_mlstm_proj(C, li, hin, W, S, Gtok, NTK=512):
    P, T = C.P, C.T
    hiv = hview(hin)
    sc = float(64 ** -0.5)
    with ExitStack() as es:
        Win = load_w(C, es, "mWin", W["mlstm_w_in"])
        g0 = load_small(C, es, "g0", W["norm_g"][li * 4 + 0])
        bg = load_small(C, es, "mbg", W["mlstm_bg"])
        B = NormBufs(C, es, NTK, "l")
        q_st = P.sb("lq_st", [64, 8, NTK], BF16, es)
        k_st = P.sb("lk_st", [64, 8, NTK], BF16, es)
        o_st = P.sb("lo_st", [128, 8, NTK], BF16, es)
        kt_st = P.sb("lkt_st", [128, NTK // 128, 512], BF16, es)
        v_st = P.sb("lv_st", [128, NTK // 128, 8, 129], BF16, es)
        loc = [Win, g0, bg, q_st, k_st, o_st, kt_st, v_st] + B.all
        P.op("dve", lambda e: e.memset(v_st[:], 1.0), writes=[v_st])
        psS = C.ps[6]
        qv = S["mq"].rearrange("(h p) t -> p h t", p=64)
        kv = S["mk"].rearrange("(h p) t -> p h t", p=64)
        ov = S["og"].rearrange("(h p) t -> p h t", p=128)
        ktv = S["mkt"].rearrange("(tb p) f -> p tb f", p=128)
        vv = S["mv"].rearrange("(tb p) h e -> p tb h e", p=128)
        k = 0
        for ti in range(T // NTK):
            t0 = ti * NTK
            norm_in(C, hiv, PAD + t0, NTK, g0, B, psS)
            for h in range(8):
                pb = C.ps[k % 4]; k += 1
                for c in range(8):
                    P.mm(pb, pb[0:64, :NTK], Win[:, c, h * 64:(h + 1) * 64], B.xn[:, c, :], reads=[Win, B.xn], start=(c == 0), stop=(c == 7))
                P.op("act", lambda e, h=h, pb=pb: e.copy(q_st[:, h, :], pb[0:64, :NTK]), reads=[pb], writes=[q_st])
                pb = C.ps[k % 4]; k += 1
                for c in range(8):
                    P.mm(pb, pb[0:64, :NTK], Win[:, c, 512 + h * 64:512 + (h + 1) * 64], B.xn[:, c, :], reads=[Win, B.xn], start=(c == 0), stop=(c == 7))
                P.op("dve", lambda e, h=h, pb=pb: e.tensor_single_scalar(k_st[:, h, :], pb[0:64, :NTK], sc, ALU.mult), reads=[pb], writes=[k_st])
            P.store(q_st, qv[:, :, t0:t0 + NTK], q_st[:])
            P.store(k_st, kv[:, :, t0:t0 + NTK], k_st[:])
            for h in range(8):
                pb = C.ps[k % 4]; k += 1
                for c in range(8):
                    P.mm(pb, pb[:, :NTK], Win[:, c, 2048 + h * 128:2048 + (h + 1) * 128], B.xn[:, c, :], reads=[Win, B.xn], start=(c == 0), stop=(c == 7))
                P.op("act", lambda e, h=h, pb=pb: e.activation(o_st[:, h, :], pb[:, :NTK], AF.Sigmoid), reads=[pb], writes=[o_st])
            P.store(o_st, ov[:, :, t0:t0 + NTK], o_st[:])
            for tb in range(NTK // 128):
                ts_ = slice(tb * 128, (tb + 1) * 128)
                ch = t0 // 128 + tb
                pb = C.ps[k % 4]; k += 1
                for c in range(8):
                    P.mm(pb, pb[:, :512], B.xn[:, c, ts_], Win[:, c, 512:1024], reads=[Win, B.xn], start=(c == 0), stop=(c == 7))
                P.op("dve", lambda e, tb=tb, pb=pb: e.tensor_single_scalar(kt_st[:, tb, :], pb[:, :512], sc, ALU.mult), reads=[pb], writes=[kt_st])
                for half in range(2):
                    pb = C.ps[k % 4]; k += 1
                    for c in range(8):
                        P.mm(pb, pb[:, :512], B.xn[:, c, ts_], Win[:, c, 1024 + half * 512:1024 + (half + 1) * 512], reads=[Win, B.xn], start=(c == 0), stop=(c == 7))
                    P.op("act", lambda e, tb=tb, half=half, pb=pb: e.copy(v_st[:, tb, half * 4:(half + 1) * 4, 0:128],
                                                                         pb[:, :512].rearrange("p (h e) -> p h e", e=128)), reads=[pb], writes=[v_st])
                pb = C.ps[k % 4]; k += 1
                for c in range(8):
                    P.mm(pb, pb[:, :32], B.xn[:, c, ts_], Win[:, c, 3072:3104], reads=[Win, B.xn], start=(c == 0), stop=(c == 7))
                P.op("dve", lambda e, ch=ch, pb=pb: e.tensor_tensor(out=Gtok[:, ch, :], in0=pb[:, :32], in1=bg[:], op=ALU.add), reads=[pb, bg], writes=[Gtok])
            P.store(kt_st, ktv[:, t0 // 128:(t0 + NTK) // 128, :], kt_st[:])
            P.store(v_st, vv[:, t0 // 128:(t0 + NTK) // 128, :, :], v_st[:])
        P.barrier()
        P.release(loc)


def phase_mlstm_gates(C, R, Gtok, GS, es):
    P, T = C.P, C.T
    NCH = T // 128
    t8 = [P.sb("g8_%d" % i, [128, 8], F32, es) for i in range(4)]
    bcc = P.sb("gbcc", [128, 16], F32, es)
    rhsD = [P.sb("grhsD%d" % i, [128, 4, 128], F32, es) for i in range(2)]
    tmpD = [P.sb("gtmpD%d" % i, [128, 4, 128], F32, es) for i in range(2)]
    Mneg = [P.sb("gMneg%d" % i, [128, 4, 128], F32, es) for i in range(2)]
    Sel = [P.sb("gSel%d" % i, [128, 128], F32, es) for i in range(2)]
    one_t = P.sb("gone", [128, 1], F32, es)
    mcur = P.sb("gmcur", [128, 8], F32, es)
    loc = t8 + [bcc, one_t, mcur] + rhsD + tmpD + Mneg + Sel
    P.op("dve", lambda e: e.memset(one_t[:], 1.0), writes=[one_t])
    for d, msrc in ((0, R.M01B), (1, R.M01F)):
        for j in range(4):
            P.op("dve", lambda e, d=d, j=j, msrc=msrc: e.tensor_scalar(Mneg[d][:, j, :], msrc[:], -1.0, 1e30, ALU.add, ALU.mult),
                 reads=[msrc], writes=[Mneg[d]])
    P.op("dve", lambda e: e.tensor_scalar(Sel[0][:], R.ones_f[:], R.M01B[:, 127:128], None, ALU.mult), reads=[R.ones_f, R.M01B], writes=[Sel[0]])
    P.op("dve", lambda e: e.tensor_scalar(Sel[1][:], R.ones_f[:], R.M01F[:, 0:1], None, ALU.mult), reads=[R.ones_f, R.M01F], writes=[Sel[1]])
    tri = [R.M01F, R.M01B]
    k = 0
    for c in range(NCH):
        for d in range(2):
            G = GS[d]
            fs = slice(8 + 16 * d, 16 + 16 * d)
            is_ = slice(16 * d, 16 * d + 8)
            nl = t8[0]
            P.op("act", lambda e, c=c, fs=fs: e.activation(nl[:], Gtok[:, c, fs], AF.Exp, scale=-1.0), reads=[Gtok], writes=[nl])
            P.op("act", lambda e: e.activation(nl[:], nl[:], AF.Ln, bias=one_t[:, 0:1]), reads=[nl, one_t], writes=[nl])
            pb = C.ps[k % 4]; k += 1
            P.mm(pb, pb[:, 0:8], tri[d][:], nl[:], reads=[tri[d], nl])
            P.op("dve", lambda e, c=c, pb=pb, G=G: e.tensor_single_scalar(G["BC"][:, c, :], pb[:, 0:8], -1.0, ALU.mult), reads=[pb], writes=[G["BC"]])
            P.op("dve", lambda e, c=c, pb=pb, G=G, is_=is_: e.tensor_tensor(out=G["A"][:, c, :], in0=pb[:, 0:8], in1=Gtok[:, c, is_], op=ALU.add),
                 reads=[pb, Gtok], writes=[G["A"]])
            for g in range(2):
                rd, td = rhsD[g], tmpD[g]
                for j in range(4):
                    if j == 3:
                        P.op("dve", lambda e, c=c, g=g, j=j, rd=rd, G=G: e.tensor_scalar(rd[:, j, :], C.ident[:], G["A"][:, c, 4 * g + j:4 * g + j + 1], None, ALU.mult),
                             reads=[C.ident, G["A"]], writes=[rd])
                    elif j % 2 == 0:
                        P.op("act", lambda e, c=c, g=g, j=j, rd=rd, G=G: e.activation(rd[:, j, :], C.ident[:], AF.Identity, scale=G["A"][:, c, 4 * g + j:4 * g + j + 1]),
                             reads=[C.ident, G["A"]], writes=[rd])
                    else:
                        P.op("pool", lambda e, c=c, g=g, j=j, rd=rd, G=G: e.tensor_scalar(rd[:, j, :], C.ident[:], G["A"][:, c, 4 * g + j:4 * g + j + 1], None, ALU.mult),
                             reads=[C.ident, G["A"]], writes=[rd])
                pa = C.ps[4 + k % 2]; k += 1
                P.mm(pa, pa[:, :512], R.ones_f[:], rd[:].rearrange("p j s -> p (j s)"), reads=[R.ones_f, rd])
                P.op("dve", lambda e, td=td, pa=pa, d=d: e.tensor_tensor(out=td[:].rearrange("p j s -> p (j s)"), in0=pa[:, :512],
                                                                         in1=Mneg[d][:].rearrange("p j s -> p (j s)"), op=ALU.add),
                     reads=[pa, Mneg[d]], writes=[td])
                P.op("dve", lambda e, td=td, c=c, g=g, G=G: e.reduce_max(G["CM"][:, c, 4 * g:4 * g + 4], td[:], axis=AX.X), reads=[td], writes=[G["CM"]])
            P.op("dve", lambda e, c=c, G=G: e.tensor_copy(bcc[:, 0:8], G["BC"][:, c, :]), reads=[G["BC"]], writes=[bcc])
            P.op("dve", lambda e, c=c, G=G: e.tensor_copy(bcc[:, 8:16], G["CM"][:, c, :]), reads=[G["CM"]], writes=[bcc])
            pb = C.ps[k % 4]; k += 1
            P.mm(pb, pb[:, 0:16], Sel[d][:], bcc[:], reads=[Sel[d], bcc])
            P.op("act", lambda e, c=c, pb=pb, G=G: e.copy(G["BL"][:, c, :], pb[:, 0:8]), reads=[pb], writes=[G["BL"]])
            P.op("act", lambda e, c=c, pb=pb, G=G: e.copy(G["CML"][:, c, :], pb[:, 8:16]), reads=[pb], writes=[G["CML"]])
    for d in range(2):
        G = GS[d]
        P.op("dve", lambda e: e.memset(mcur[:], 0.0), writes=[mcur])
        order = range(NCH) if d == 0 else range(NCH - 1, -1, -1)
        for c in order:
            P.op("dve", lambda e, c=c, G=G: e.tensor_copy(G["M"][:, c, :], mcur[:]), reads=[mcur], writes=[G["M"]])
            P.op("dve", lambda e, c=c, G=G: e.tensor_tensor(out=mcur[:], in0=mcur[:], in1=G["CML"][:, c, :], op=ALU.max), reads=[mcur, G["CML"]], writes=[mcur])
            P.op("dve", lambda e, c=c, G=G: e.tensor_tensor(out=mcur[:], in0=mcur[:], in1=G["BL"][:, c, :], op=ALU.add), reads=[mcur, G["BL"]], writes=[mcur])
            P.op("dve", lambda e, c=c, G=G: e.tensor_copy(G["MN"][:, c, :], mcur[:]), reads=[mcur], writes=[G["MN"]])
        def fl(nm):
            return G[nm][:].rearrange("p c h -> p (c h)")
        for nm in ("MX", "EU", "WI", "NM", "WS", "DEC", "EA"):
            pass
        P.op("dve", lambda e, G=G: e.tensor_tensor(out=G["MX"][:], in0=G["CM"][:], in1=G["M"][:], op=ALU.max), reads=[G["CM"], G["M"]], writes=[G["MX"]])
        P.op("act", lambda e, G=G: e.activation(G["EU"][:], G["MX"][:], AF.Exp, scale=-1.0), reads=[G["MX"]], writes=[G["EU"]])
        P.op("dve", lambda e, G=G: e.tensor_tensor(out=G["WI"][:], in0=G["M"][:], in1=G["MX"][:], op=ALU.subtract), reads=[G["M"], G["MX"]], writes=[G["WI"]])
        P.op("act", lambda e, G=G: e.activation(G["WI"][:], G["WI"][:], AF.Exp), reads=[G["WI"]], writes=[G["WI"]])
        P.op("dve", lambda e, G=G: e.tensor_tensor(out=G["NM"][:], in0=G["BC"][:], in1=G["MX"][:], op=ALU.add), reads=[G["BC"], G["MX"]], writes=[G["NM"]])
        P.op("act", lambda e, G=G: e.activation(G["NM"][:], G["NM"][:], AF.Exp, scale=-1.0), reads=[G["NM"]], writes=[G["NM"]])
        P.op("dve", lambda e, G=G: e.tensor_tensor(out=G["WS"][:], in0=G["BL"][:], in1=G["A"][:], op=ALU.add), reads=[G["BL"], G["A"]], writes=[G["WS"]])
        P.op("dve", lambda e, G=G: e.tensor_tensor(out=G["WS"][:], in0=G["WS"][:], in1=G["MN"][:], op=ALU.subtract), reads=[G["WS"], G["MN"]], writes=[G["WS"]])
        P.op("act", lambda e, G=G: e.activation(G["WS"][:], G["WS"][:], AF.Exp), reads=[G["WS"]], writes=[G["WS"]])
        P.op("dve", lambda e, G=G: e.tensor_tensor(out=G["DEC"][:], in0=G["BL"][:], in1=G["M"][:], op=ALU.add), reads=[G["BL"], G["M"]], writes=[G["DEC"]])
        P.op("dve", lambda e, G=G: e.tensor_tensor(out=G["DEC"][:], in0=G["DEC"][:], in1=G["MN"][:], op=ALU.subtract), reads=[G["DEC"], G["MN"]], writes=[G["DEC"]])
        P.op("act", lambda e, G=G: e.activation(G["DEC"][:], G["DEC"][:], AF.Exp), reads=[G["DEC"]], writes=[G["DEC"]])
        P.op("act", lambda e, G=G: e.activation(G["EA"][:], G["A"][:], AF.Exp), reads=[G["A"]], writes=[G["EA"]])
    return loc


def phase_mlstm_core(C, li, hin, W, S):
    P, T = C.P, C.T
    NCH = T // 128
    with ExitStack() as es:
        R = make_tri(C, es)
        Gtok = P.sb("Gtok", [128, NCH, 32], F32, es)
        phase_mlstm_proj(C, li, hin, W, S, Gtok)
        names = ("BC", "A", "CM", "BL", "CML", "M", "MN", "MX", "EU", "WI", "NM", "WS", "DEC", "EA")
        GS = [{nm: P.sb("G%s%d" % (nm, d), [128, NCH, 8], F32, es) for nm in names} for d in range(2)]
        loc = R.all + [Gtok] + [GS[d][nm] for d in range(2) for nm in names]
        with ExitStack() as es2:
            loc2 = phase_mlstm_gates(C, R, Gtok, GS, es2)
            P.barrier()
            P.release(loc2)
        M01b = []
        mask = [R.M01F, R.M01B]
        Qc = [[P.sb("cQ%d%d" % (d, i), [64, 8, 128], BF16, es) for i in range(2)] for d in range(2)]
        Kc = [[P.sb("cK%d%d" % (d, i), [64, 8, 128], BF16, es) for i in range(2)] for d in range(2)]
        Ktc = [[P.sb("cKt%d%d" % (d, i), [128, 512], BF16, es) for i in range(2)] for d in range(2)]
        Vc = [[P.sb("cV%d%d" % (d, i), [128, 8, 129], BF16, es) for i in range(2)] for d in range(2)]
        Cf = [P.sb("cCf%d" % d, [64, 8, 129], F32, es) for d in range(2)]
        Cb = [P.sb("cCb%d" % d, [64, 8, 129], BF16, es) for d in range(2)]
        Hacc = [[P.sb("cH%d%d" % (d, i), [128, 1024], F32, es) for i in range(2)] for d in range(2)]
        sqk = [P.sb("csqk%d" % i, [128, 128], BF16, es) for i in range(3)]
        t1 = [P.sb("ct1%d" % i, [128, 129], F32, es) for i in range(3)]
        tot = [P.sb("ctot%d" % i, [128, 129], F32, es) for i in range(3)]
        kw = [P.sb("ckw%d" % i, [128, 64], BF16, es) for i in range(3)]
        dd = [P.sb("cdd%d" % i, [128, 2], F32, es) for i in range(3)]
        loc += M01b + sum(Qc, []) + sum(Kc, []) + sum(Ktc, []) + sum(Vc, []) + Cf + Cb + sum(Hacc, []) + sqk + t1 + tot + kw + dd
        for d in range(2):
            P.op("dve", lambda e, d=d: e.memset(Cf[d][:], 0.0), writes=[Cf[d]])
            P.op("dve", lambda e, d=d: e.memset(Cb[d][:], 0.0), writes=[Cb[d]])
        qv = S["mq"].rearrange("(h p) t -> p h t", p=64)
        kv = S["mk"].rearrange("(h p) t -> p h t", p=64)
        hdst = [S["hf"], S["hb"]]
        n = 0
        for step in range(NCH):
            for d in range(2):
                c = step if d == 0 else NCH - 1 - step
                cs = slice(c * 128, (c + 1) * 128)
                G = GS[d]
                q, kk_, kt, v = Qc[d][step % 2], Kc[d][step % 2], Ktc[d][step % 2], Vc[d][step % 2]
                hacc = Hacc[d][step % 2]
                P.load(q, q[:], qv[:, :, cs])
                P.load(kk_, kk_[:], kv[:, :, cs])
                P.load(kt, kt[:], S["mkt"][cs, :])
                P.load(v, v[:], S["mv"][cs, :, :])
                for h in range(8):
                    i3 = n % 3
                    n += 1
                    pS = C.ps[n % 2]
                    pI = C.ps[2 + n % 2]
                    pX = C.ps[4 + n % 2]
                    pC = C.ps[6 + n % 2]
                    col = lambda nm, G=G, c=c, h=h: G[nm][:, c, h:h + 1]
                    P.mm(pS, pS[:, :128], kk_[:, h, :], q[:, h, :], reads=[kk_, q])
                    P.op("dve", lambda e, i3=i3, pS=pS, ea=col("EA"), d=d: e.scalar_tensor_tensor(out=sqk[i3][:], in0=pS[:, :128], scalar=ea, in1=mask[d][:],
                                                                                             op0=ALU.mult, op1=ALU.mult),
                         reads=[pS, G["EA"], mask[d]], writes=[sqk[i3]])
                    P.mm(pI, pI[:, :129], sqk[i3][:], v[:, h, :], reads=[sqk[i3], v])
                    P.mm(pX, pX[:, :129], q[:, h, :], Cb[d][:, h, :], reads=[q, Cb[d]])
                    P.op("act", lambda e, i3=i3, pI=pI, eu=col("EU"): e.activation(t1[i3][:], pI[:, :129], AF.Identity, scale=eu), reads=[pI, G["EU"]], writes=[t1[i3]])
                    P.op("dve", lambda e, i3=i3, pX=pX, wi=col("WI"): e.scalar_tensor_tensor(out=tot[i3][:], in0=pX[:, :129], scalar=wi, in1=t1[i3][:],
                                                                                             op0=ALU.mult, op1=ALU.add),
                         reads=[pX, G["WI"], t1[i3]], writes=[tot[i3]])
                    P.op("dve", lambda e, i3=i3: e.scalar_tensor_tensor(out=dd[i3][:, 0:1], in0=tot[i3][:, 128:129], scalar=-1.0, in1=tot[i3][:, 128:129],
                                                                        op0=ALU.mult, op1=ALU.max), reads=[tot[i3]], writes=[dd[i3]])
                    P.op("dve", lambda e, i3=i3, nmc=col("NM"): e.tensor_tensor(out=dd[i3][:, 0:1], in0=dd[i3][:, 0:1], in1=nmc, op=ALU.max),
                         reads=[dd[i3], G["NM"]], writes=[dd[i3]])
                    P.op("dve", lambda e, i3=i3: e.reciprocal(dd[i3][:, 1:2], dd[i3][:, 0:1]), reads=[dd[i3]], writes=[dd[i3]])
                    P.op("act", lambda e, i3=i3, h=h, hacc=hacc: e.activation(hacc[:, h * 128:(h + 1) * 128], tot[i3][:, 0:128], AF.Identity, scale=dd[i3][:, 1:2]),
                         reads=[tot[i3], dd[i3]], writes=[hacc])
                    P.op("pool", lambda e, i3=i3, h=h, kt=kt, ws=col("WS"): e.tensor_scalar(kw[i3][:], kt[:, h * 64:(h + 1) * 64], ws, None, ALU.mult),
                         reads=[kt, G["WS"]], writes=[kw[i3]])
                    P.mm(pC, pC[0:64, :129], kw[i3][:], v[:, h, :], reads=[kw[i3], v])
                    P.op("dve", lambda e, h=h, d=d, pC=pC, dec=G["DEC"][0:64, c, h:h + 1]: e.scalar_tensor_tensor(
                        out=Cf[d][:, h, :], in0=Cf[d][:, h, :], scalar=dec, in1=pC[0:64, :129], op0=ALU.mult, op1=ALU.add),
                        reads=[Cf[d], G["DEC"], pC], writes=[Cf[d]])
                    P.op("act", lambda e, h=h, d=d: e.copy(Cb[d][:, h, :], Cf[d][:, h, :]), reads=[Cf[d]], writes=[Cb[d]])
                P.store(hacc, hdst[d][cs, :], hacc[:])
        P.barrier()
        P.release(loc)


def phase_gn_post(C, S, F, dv, g_ap):
    P, T = C.P, C.T
    NH = F // dv
    NFB_ = F // 128
    with ExitStack() as es:
        gt = P.sb("pg", [128, F], F32, es)
        P.load(gt, gt[:], g_ap)
        A = [P.sb("pA%d" % i, [128, F], F32, es) for i in range(2)]
        Bt = [P.sb("pB%d" % i, [128, F], F32, es) for i in range(2)]
        junks = [P.sb("pjunk%d" % i, [128, dv], F32, es) for i in range(2)]
        sts = [P.sb("pst%d" % i, [128, 4, NH], F32, es) for i in range(2)]
        gate = [P.sb("pgate%d" % i, [128, NFB_, 128], BF16, es) for i in range(2)]
        ao = [P.sb("pao%d" % i, [128, NFB_, 128], BF16, es) for i in range(2)]
        eps_g = P.sb("peps", [128, 1], F32, es)
        loc = [gt, eps_g] + junks + sts + A + Bt + gate + ao
        P.op("dve", lambda e: e.memset(eps_g[:], EPS), writes=[eps_g])
        gv = S["og"].rearrange("(c p) t -> p c t", p=128)
        aov = S["ao2"].rearrange("(c p) t -> p c t", p=128)
        k = 0
        for c in range(T // 128):
            cs = slice(c * 128, (c + 1) * 128)
            a, b, gtile, aot = A[c % 2], Bt[c % 2], gate[c % 2], ao[c % 2]
            st, junk = sts[c % 2], junks[c % 2]
            P.load(a, a[:], S["hf"][cs, :])
            P.load(b, b[:], S["hb"][cs, :])
            P.load(gtile, gtile[:], gv[:, :, cs])
            P.op("pool", lambda e, a=a, b=b: e.tensor_tensor(out=a[:], in0=a[:], in1=b[:], op=ALU.add), reads=[a, b], writes=[a])
            for h in range(NH):
                hs = slice(h * dv, (h + 1) * dv)
                P.op("act", lambda e, a=a, hs=hs, h=h, st=st, junk=junk: e.activation(junk[:], a[:, hs], AF.Identity, accum_out=st[:, 0, h:h + 1]), reads=[a], writes=[junk, st])
                P.op("act", lambda e, a=a, hs=hs, h=h, st=st, junk=junk: e.activation(junk[:], a[:, hs], AF.Square, accum_out=st[:, 1, h:h + 1]), reads=[a], writes=[junk, st])
            P.op("dve", lambda e, st=st: e.tensor_single_scalar(st[:, 0, :], st[:, 0, :], 1.0 / dv, ALU.mult), reads=[st], writes=[st])
            P.op("dve", lambda e, st=st: e.tensor_tensor(out=st[:, 2, :], in0=st[:, 0, :], in1=st[:, 0, :], op=ALU.mult), reads=[st], writes=[st])
            P.op("dve", lambda e, st=st: e.scalar_tensor_tensor(out=st[:, 1, :], in0=st[:, 1, :], scalar=1.0 / dv, in1=st[:, 2, :], op0=ALU.mult, op1=ALU.subtract),
                 reads=[st], writes=[st])
            P.op("act", lambda e, st=st: e.activation(st[:, 1, :], st[:, 1, :], AF.Sqrt, bias=eps_g[:, 0:1]), reads=[st, eps_g], writes=[st])
            P.op("dve", lambda e, st=st: e.reciprocal(st[:, 1, :], st[:, 1, :]), reads=[st], writes=[st])
            P.op("dve", lambda e, st=st: e.scalar_tensor_tensor(out=st[:, 3, :], in0=st[:, 0, :], scalar=-1.0, in1=st[:, 1, :], op0=ALU.mult, op1=ALU.mult),
                 reads=[st], writes=[st])
            for h in range(NH):
                hs = slice(h * dv, (h + 1) * dv)
                P.op("act", lambda e, a=a, hs=hs, h=h, st=st, junk=junk: e.activation(a[:, hs], a[:, hs], AF.Identity, scale=st[:, 1, h:h + 1], bias=st[:, 3, h:h + 1]),
                     reads=[a, st], writes=[a])
            P.op("dve", lambda e, a=a: e.tensor_tensor(out=a[:], in0=a[:], in1=gt[:], op=ALU.mult), reads=[a, gt], writes=[a])
            for fb in range(NFB_):
                pb = C.ps[k % 4]; k += 1
                P.tr(pb, pb[:, 0:128], a[:, fb * 128:(fb + 1) * 128], C.ident[:], reads=[a, C.ident])
                P.op("dve", lambda e, fb=fb, pb=pb, aot=aot, gtile=gtile: e.tensor_tensor(out=aot[:, fb, :], in0=pb[:, 0:128], in1=gtile[:, fb, :], op=ALU.mult),
                     reads=[pb, gtile], writes=[aot])
            P.store(aot, aov[:, :, cs], aot[:])
        P.barrier()
        P.release(loc)


def host_mlstm_params(inp):
    return dict(mlstm_w_in=np.ascontiguousarray(inp["mlstm_w_in"][0]), mlstm_w_out=np.ascontiguousarray(inp["mlstm_w_out"][0]),
                mlstm_bg=np.ascontiguousarray(np.broadcast_to(inp["mlstm_b_gates"][0][None, :], (128, 32))),
                mlstm_ng=np.ascontiguousarray(np.broadcast_to(inp["mlstm_norm_g"][0][None, :], (128, 1024))))


def phase_ret_proj(C, li, hin, W, S, NTK=512):
    P, T = C.P, C.T
    hiv = hview(hin)
    sc = float(256 ** -0.5)
    with ExitStack() as es:
        Win = load_w(C, es, "rWin", W["ret_w_in"])
        g0 = load_small(C, es, "g0", W["norm_g"][li * 4 + 0])
        B = NormBufs(C, es, NTK, "r")
        q_st = P.sb("rq_st", [128, 8, NTK], BF16, es)
        k_st = P.sb("rk_st", [128, 8, NTK], BF16, es)
        g_st = P.sb("rg_st", [128, 16, NTK], BF16, es)
        kt_st = P.sb("rkt_st", [128, NTK // 128, 1024], BF16, es)
        v_st = P.sb("rv_st", [128, NTK // 128, 2048], BF16, es)
        loc = [Win, g0, q_st, k_st, g_st, kt_st, v_st] + B.all
        psS = C.ps[6]
        qv = S["rq"].rearrange("(c p) t -> p c t", p=128)
        kv = S["rk"].rearrange("(c p) t -> p c t", p=128)
        gv = S["og"].rearrange("(c p) t -> p c t", p=128)
        ktv = S["rkt"].rearrange("(tb p) f -> p tb f", p=128)
        vv = S["rv"].rearrange("(tb p) f -> p tb f", p=128)
        k = 0
        for ti in range(T // NTK):
            t0 = ti * NTK
            norm_in(C, hiv, PAD + t0, NTK, g0, B, psS)
            for fb in range(8):
                pb = C.ps[k % 4]; k += 1
                for c in range(8):
                    P.mm(pb, pb[:, :NTK], Win[:, c, fb * 128:(fb + 1) * 128], B.xn[:, c, :], reads=[Win, B.xn], start=(c == 0), stop=(c == 7))
                P.op("act", lambda e, fb=fb, pb=pb: e.copy(q_st[:, fb, :], pb[:, :NTK]), reads=[pb], writes=[q_st])
                pb = C.ps[k % 4]; k += 1
                for c in range(8):
                    P.mm(pb, pb[:, :NTK], Win[:, c, 1024 + fb * 128:1024 + (fb + 1) * 128], B.xn[:, c, :], reads=[Win, B.xn], start=(c == 0), stop=(c == 7))
                P.op("dve", lambda e, fb=fb, pb=pb: e.tensor_single_scalar(k_st[:, fb, :], pb[:, :NTK], sc, ALU.mult), reads=[pb], writes=[k_st])
            P.store(q_st, qv[:, :, t0:t0 + NTK], q_st[:])
            P.store(k_st, kv[:, :, t0:t0 + NTK], k_st[:])
            for fb in range(16):
                pb = C.ps[k % 4]; k += 1
                for c in range(8):
                    P.mm(pb, pb[:, :NTK], Win[:, c, 4096 + fb * 128:4096 + (fb + 1) * 128], B.xn[:, c, :], reads=[Win, B.xn], start=(c == 0), stop=(c == 7))
                P.op("act", lambda e, fb=fb, pb=pb: e.activation(g_st[:, fb, :], pb[:, :NTK], AF.Silu), reads=[pb], writes=[g_st])
            P.store(g_st, gv[:, :, t0:t0 + NTK], g_st[:])
            for tb in range(NTK // 128):
                ts_ = slice(tb * 128, (tb + 1) * 128)
                for half in range(2):
                    pb = C.ps[k % 4]; k += 1
                    for c in range(8):
                        P.mm(pb, pb[:, :512], B.xn[:, c, ts_], Win[:, c, 1024 + half * 512:1024 + (half + 1) * 512], reads=[Win, B.xn], start=(c == 0), stop=(c == 7))
                    P.op("dve", lambda e, tb=tb, half=half, pb=pb: e.tensor_single_scalar(kt_st[:, tb, half * 512:(half + 1) * 512], pb[:, :512], sc, ALU.mult),
                         reads=[pb], writes=[kt_st])
                for q4 in range(4):
                    pb = C.ps[k % 4]; k += 1
                    for c in range(8):
                        P.mm(pb, pb[:, :512], B.xn[:, c, ts_], Win[:, c, 2048 + q4 * 512:2048 + (q4 + 1) * 512], reads=[Win, B.xn], start=(c == 0), stop=(c == 7))
                    if q4 % 2 == 0:
                        P.op("act", lambda e, tb=tb, q4=q4, pb=pb: e.copy(v_st[:, tb, q4 * 512:(q4 + 1) * 512], pb[:, :512]), reads=[pb], writes=[v_st])
                    else:
                        P.op("dve", lambda e, tb=tb, q4=q4, pb=pb: e.tensor_copy(v_st[:, tb, q4 * 512:(q4 + 1) * 512], pb[:, :512]), reads=[pb], writes=[v_st])
            P.store(kt_st, ktv[:, t0 // 128:(t0 + NTK) // 128, :], kt_st[:])
            P.store(v_st, vv[:, t0 // 128:(t0 + NTK) // 128, :], v_st[:])
        P.barrier()
        P.release(loc)


def phase_ret_core(C, W, S):
    P, T = C.P, C.T
    NCH = T // 128
    with ExitStack() as es:
        R = make_tri(C, es)
        dl = P.sb("rdl", [1, 8], F32, es)
        ones1 = P.sb("rones1", [1, 128], F32, es)
        one_t = P.sb("rone", [128, 1], F32, es)
        LG = P.sb("rLG", [128, 8], F32, es)
        RAWd = P.sb("rRAWd", [128, 128], F32, es)
        rawc = P.sb("rrawc", [128, 4], F32, es)
        DM = P.sb("rDM", [128, 8, 128], F32, es)
        XI = P.sb("rXI", [128, 8], F32, es)
        ZE = P.sb("rZE", [128, 8], F32, es)
        GL = P.sb("rGL", [128, 8], F32, es)
        Qc = [[P.sb("rQ%d%d" % (d, i), [128, 8, 128], BF16, es) for i in range(2)] for d in range(2)]
        Kc = [[P.sb("rK%d%d" % (d, i), [128, 8, 128], BF16, es) for i in range(2)] for d in range(2)]
        Ktc = [[P.sb("rKt%d%d" % (d, i), [128, 1024], BF16, es) for i in range(2)] for d in range(2)]
        Vc = [[P.sb("rV%d%d" % (d, i), [128, 2048], BF16, es) for i in range(2)] for d in range(2)]
        Rf = [P.sb("rRf%d" % d, [128, 8, 512], F32, es) for d in range(2)]
        Rb = [P.sb("rRb%d" % d, [128, 8, 512], BF16, es) for d in range(2)]
        Hacc = [[P.sb("rH%d%d" % (d, i), [128, 2048], F32, es) for i in range(2)] for d in range(2)]
        sqk = [P.sb("rsqk%d" % i, [128, 128], BF16, es) for i in range(3)]
        t1 = [P.sb("rt1%d" % i, [128, 512], F32, es) for i in range(3)]
        kz = [P.sb("rkz%d" % i, [128, 256], BF16, es) for i in range(3)]
        loc = R.all + [dl, ones1, one_t, LG, RAWd, rawc, DM, XI, ZE, GL] + sum(Qc, []) + sum(Kc, []) + sum(Ktc, []) + sum(Vc, []) + Rf + Rb + sum(Hacc, []) + sqk + t1 + kz
        P.load(dl, dl[:], W["ret_decay"])
        P.op("dve", lambda e: e.memset(ones1[:], 1.0), writes=[ones1])
        P.op("dve", lambda e: e.memset(one_t[:], 1.0), writes=[one_t])
        P.op("act", lambda e: e.activation(dl[:], dl[:], AF.Exp, scale=-1.0), reads=[dl], writes=[dl])
        P.op("act", lambda e: e.activation(dl[:], dl[:], AF.Ln, bias=one_t[0:1, 0:1]), reads=[dl, one_t], writes=[dl])
        pc = C.ps[0]
        P.mm(pc, pc[:, 0:8], ones1[:], dl[:], reads=[ones1, dl])
        P.op("dve", lambda e: e.tensor_single_scalar(LG[:], pc[:, 0:8], -1.0, ALU.mult), reads=[pc], writes=[LG])
        P.op("pool", lambda e: e.iota(RAWd[:], [[1, 128]], base=0, channel_multiplier=-1, allow_small_or_imprecise_dtypes=True), writes=[RAWd])
        P.op("dve", lambda e: e.scalar_tensor_tensor(out=RAWd[:], in0=RAWd[:], scalar=-1.0, in1=RAWd[:], op0=ALU.mult, op1=ALU.max), reads=[RAWd], writes=[RAWd])
        for j, (b0, cm) in enumerate(((1, 1), (128, -1), (127, -1), (0, 1))):
            P.op("pool", lambda e, j=j, b0=b0, cm=cm: e.iota(rawc[:, j:j + 1], [[0, 1]], base=b0, channel_multiplier=cm, allow_small_or_imprecise_dtypes=True), writes=[rawc])
        mask = [R.M01F, R.M01B]
        for d in range(2):
            for h in range(4):
                dh = d * 4 + h
                P.op("act", lambda e, dh=dh: e.activation(DM[:, dh, :], RAWd[:], AF.Exp, scale=LG[:, dh:dh + 1]), reads=[RAWd, LG], writes=[DM])
                P.op("dve", lambda e, dh=dh, d=d: e.tensor_tensor(out=DM[:, dh, :], in0=DM[:, dh, :], in1=mask[d][:], op=ALU.mult), reads=[DM, mask[d]], writes=[DM])
                P.op("act", lambda e, dh=dh, d=d: e.activation(XI[:, dh:dh + 1], rawc[:, d:d + 1], AF.Exp, scale=LG[:, dh:dh + 1]), reads=[rawc, LG], writes=[XI])
                P.op("act", lambda e, dh=dh, d=d: e.activation(ZE[:, dh:dh + 1], rawc[:, 2 + d:3 + d], AF.Exp, scale=LG[:, dh:dh + 1]), reads=[rawc, LG], writes=[ZE])
        P.op("act", lambda e: e.activation(GL[:], LG[:], AF.Exp, scale=128.0), reads=[LG], writes=[GL])
        for d in range(2):
            P.op("dve", lambda e, d=d: e.memset(Rf[d][:], 0.0), writes=[Rf[d]])
            P.op("pool", lambda e, d=d: e.memset(Rb[d][:], 0.0), writes=[Rb[d]])
        qv = S["rq"].rearrange("(c p) t -> p c t", p=128)
        kv = S["rk"].rearrange("(c p) t -> p c t", p=128)
        hdst = [S["hf"], S["hb"]]
        n = 0
        for step in range(NCH):
            for d in range(2):
                c = step if d == 0 else NCH - 1 - step
                cs = slice(c * 128, (c + 1) * 128)
                q, kk_, kt, v = Qc[d][step % 2], Kc[d][step % 2], Ktc[d][step % 2], Vc[d][step % 2]
                hacc = Hacc[d][step % 2]
                P.load(q, q[:], qv[:, :, cs])
                P.load(kk_, kk_[:], kv[:, :, cs])
                P.load(kt, kt[:], S["rkt"][cs, :])
                P.load(v, v[:], S["rv"][cs, :])
                for h in range(4):
                    dh = d * 4 + h
                    i3 = n % 3
                    n += 1
                    pS = C.ps[n % 2]
                    pI = C.ps[2 + n % 2]
                    pX = C.ps[4 + n % 2]
                    vs = v[:, h * 512:(h + 1) * 512]
                    for kc in range(2):
                        P.mm(pS, pS[:, :128], kk_[:, h * 2 + kc, :], q[:, h * 2 + kc, :], reads=[kk_, q], start=(kc == 0), stop=(kc == 1))
                    P.op("dve", lambda e, i3=i3, pS=pS, dh=dh: e.tensor_tensor(out=sqk[i3][:], in0=pS[:, :128], in1=DM[:, dh, :], op=ALU.mult),
                         reads=[pS, DM], writes=[sqk[i3]])
                    P.mm(pI, pI[:, :512], sqk[i3][:], vs, reads=[sqk[i3], v])
                    for kc in range(2):
                        P.mm(pX, pX[:, :512], q[:, h * 2 + kc, :], Rb[d][:, h * 2 + kc, :], reads=[q, Rb[d]], start=(kc == 0), stop=(kc == 1))
                    P.op("act", lambda e, i3=i3, pI=pI: e.copy(t1[i3][:], pI[:, :512]), reads=[pI], writes=[t1[i3]])
                    P.op("dve", lambda e, i3=i3, pX=pX, dh=dh, h=h, hacc=hacc: e.scalar_tensor_tensor(
                        out=hacc[:, h * 512:(h + 1) * 512], in0=pX[:, :512], scalar=XI[:, dh:dh + 1], in1=t1[i3][:], op0=ALU.mult, op1=ALU.add),
                        reads=[pX, XI, t1[i3]], writes=[hacc])
                    P.op("pool", lambda e, i3=i3, h=h, kt=kt, dh=dh: e.tensor_scalar(kz[i3][:], kt[:, h * 256:(h + 1) * 256], ZE[:, dh:dh + 1], None, ALU.mult),
                         reads=[kt, ZE], writes=[kz[i3]])
                    for kc in range(2):
                        pC = C.ps[6 + kc]
                        P.mm(pC, pC[:, :512], kz[i3][:, kc * 128:(kc + 1) * 128], vs, reads=[kz[i3], v])
                        P.op("dve", lambda e, d=d, h=h, kc=kc, pC=pC, dh=dh: e.scalar_tensor_tensor(
                            out=Rf[d][:, h * 2 + kc, :], in0=Rf[d][:, h * 2 + kc, :], scalar=GL[:, dh:dh + 1], in1=pC[:, :512], op0=ALU.mult, op1=ALU.add),
                            reads=[Rf[d], GL, pC], writes=[Rf[d]])
                        P.op("act", lambda e, d=d, h=h, kc=kc: e.copy(Rb[d][:, h * 2 + kc, :], Rf[d][:, h * 2 + kc, :]), reads=[Rf[d]], writes=[Rb[d]])
                P.store(hacc, hdst[d][cs, :], hacc[:])
        P.barrier()
        P.release(loc)


def host_ret_params(inp):
    return dict(ret_w_in=np.ascontiguousarray(inp["ret_w_in"][0]), ret_w_o=np.ascontiguousarray(inp["ret_w_o"][0]),
                ret_decay=np.ascontiguousarray(inp["ret_decay_logit"][0].reshape(1, 8)),
                ret_ng=np.ascontiguousarray(np.broadcast_to(inp["ret_norm_g"][0][None, :], (128, 2048))))


SEQ = 8192
NCORES = 4


def build_full(T, hp_shapes, layers=(0, 1, 2, 3)):
    nc = bass.Bass("TRN2", target_bir_lowering=False)
    with ExitStack() as es:
        C = make_ctx(nc, es, T)
        xd = nc.dram_tensor("x", [T, 1024], F32, kind="ExternalInput").ap()
        od = nc.dram_tensor("out", [T, 1024], F32, kind="ExternalOutput").ap()
        W = declare_inputs(nc, hp_shapes)
        ha = nc.dram_tensor("ha", [1024, T + 2 * PAD], F32, kind="Internal").ap()
        hb = nc.dram_tensor("hb", [1024, T + 2 * PAD], F32, kind="Internal").ap()

        def scratch(specs):
            return {nm: nc.dram_tensor("s_" + nm, list(shp), dt, kind="Internal").ap() for nm, shp, dt in specs}

        zero_pads(C, ha)
        zero_pads(C, hb)
        phase_in(C, xd, ha)
        if 0 in layers:
            S = scratch((("qn", (1024, T), BF16), ("kn", (1024, T), BF16), ("qr", (512, T), BF16), ("kr", (64, T), BF16),
                         ("v", (T, 1024), BF16), ("ao", (1024, T), BF16)))
            phase_mla_proj(C, 0, ha, W, S)
            phase_mla_core(C, S)
            phase_tail(C, S["ao"], W["mla_w_o"], W["norm_g"][1], ha, hb)
            phase_ffn(C, 0, hb, ha, W)
        if 1 in layers:
            S = scratch((("dqn", (1024, T), BF16), ("dkn", (1024, T), BF16), ("dv", (T, 1024), BF16), ("dao", (1024, T), BF16)))
            S = dict(qn=S["dqn"], kn=S["dkn"], v=S["dv"], ao=S["dao"])
            phase_diff_proj(C, 1, ha, W, S)
            phase_diff_core(C, 1, W, S)
            phase_tail(C, S["ao"], W["diff_w_o"], W["norm_g"][5], ha, hb)
            phase_ffn(C, 1, hb, ha, W)
        if 2 in layers:
            S = scratch((("mq", (512, T), BF16), ("mk", (512, T), BF16), ("mkt", (T, 512), BF16), ("mv", (T, 8, 129), BF16),
                         ("mog", (1024, T), BF16), ("mhf", (T, 1024), F32), ("mhb", (T, 1024), F32), ("mao2", (1024, T), BF16)))
            S.update(og=S["mog"], hf=S["mhf"], hb=S["mhb"], ao2=S["mao2"])
            phase_mlstm_core(C, 2, ha, W, S)
            phase_gn_post(C, S, 1024, 128, W["mlstm_ng"])
            phase_tail(C, S["ao2"], W["mlstm_w_out"], W["norm_g"][9], ha, hb)
            phase_ffn(C, 2, hb, ha, W)
        if 3 in layers:
            S = scratch((("rq", (1024, T), BF16), ("rk", (1024, T), BF16), ("rkt", (T, 1024), BF16), ("rv", (T, 2048), BF16),
                         ("rog", (2048, T), BF16), ("rhf", (T, 2048), F32), ("rhb", (T, 2048), F32), ("rao2", (2048, T), BF16)))
            S.update(og=S["rog"], hf=S["rhf"], hb=S["rhb"], ao2=S["rao2"])
            phase_ret_proj(C, 3, ha, W, S)
            phase_ret_core(C, W, S)
            phase_gn_post(C, S, 2048, 512, W["ret_ng"])
            phase_tail(C, S["ao2"], W["ret_w_o"], W["norm_g"][13], ha, hb)
            phase_ffn(C, 3, hb, ha, W)
        phase_out(C, ha, od)
        C.P.emit()
    return nc, C


def host_params(inp, T):
    inp = {k: np.asarray(v, dtype=np.float32) for k, v in inp.items() if k != "x"}
    cw, cb, ng = host_ffn_params(inp)
    hp = dict(ffn_w_up=np.ascontiguousarray(inp["ffn_w_up"]), ffn_w_down=np.ascontiguousarray(inp["ffn_w_down"]),
              ffn_cw=cw, ffn_cb=cb, norm_g=ng)
    hp.update(host_mla_params(inp, np.arange(T)))
    hp.update(host_diff_params(inp))
    hp.update(host_mlstm_params(inp))
    hp.update(host_ret_params(inp))
    return hp


REAL_CORES = (0, 1, 4, 5)


def kernel(**inputs):
    x = np.asarray(inputs["x"], dtype=np.float32)
    Bn, T, _ = x.shape
    hp = host_params(inputs, T)
    nc, C = build_full(T, {k: v.shape for k, v in hp.items()})
    ident = np.eye(128, dtype=np.float32)
    zx = np.zeros((T, 1024), np.float32)
    in_maps = []
    real = REAL_CORES[:Bn]
    for c in range(8):
        xb = np.ascontiguousarray(x[real.index(c)]) if c in real else zx
        in_maps.append(dict(x=xb, ident_in=ident, **hp))
    res = run_bass_kernel_spmd(nc, in_maps, core_ids=list(range(8)))
    return np.stack([np.asarray(res.results[c]["out"]) for c in real]).astype(np.float32)
```

```python
import contextlib
import numpy as np
import concourse.bass as bass
import concourse.mybir as mybir

F32 = mybir.dt.float32
BF16 = mybir.dt.bfloat16
AF = mybir.ActivationFunctionType
ALU = mybir.AluOpType
AX = mybir.AxisListType

ENGS = ("pe", "dve", "act", "pool", "sp")


class Res:
    def __init__(self, name, t=None):
        self.name = name
        self.t = t
        self.lw = None
        self.rd = []
        self.sem = None
        self.psum = False

    def __getitem__(self, idx):
        return self.t[idx]


class Sem:
    def __init__(self, h):
        self.h = h
        self.n = 0


class Prog:
    def __init__(self, nc, es):
        self.nc = nc
        self.es = es
        self.ops = []
        self.eng_ops = {e: [] for e in ENGS}
        self.engsem = {e: es.enter_context(nc.semaphore("S_" + e)) for e in ENGS}
        self.res = []
        self.free_sems = []
        self.sems = []

    def sb(self, name, shape, dt, es=None):
        self.uid = getattr(self, "uid", 0) + 1
        name = "%s_u%d" % (name, self.uid)
        t = (es or self.es).enter_context(self.nc.sbuf_tensor(name, list(shape), dt))
        r = Res(name, t)
        self.res.append(r)
        return r

    def ps(self, name, shape, dt=F32, es=None):
        t = (es or self.es).enter_context(self.nc.psum_tensor(name, list(shape), dt))
        r = Res(name, t)
        r.psum = True
        self.res.append(r)
        return r

    def _dsem(self, r):
        if r.sem is None:
            if self.free_sems:
                r.sem = self.free_sems.pop()
            else:
                h = self.es.enter_context(self.nc.semaphore("D%d" % len(self.sems)))
                r.sem = Sem(h)
                self.sems.append(r.sem)
        return r.sem

    def release(self, rs):
        for r in rs:
            if r.sem is not None:
                self.free_sems.append(r.sem)
                r.sem = None
            if r in self.res:
                self.res.remove(r)

    def op(self, eng, fn, reads=(), writes=(), dma=None, waw=True):
        idx = len(self.ops)
        tok = ("op", idx)
        if dma is not None:
            sm = self._dsem(dma)
            sm.n += 1
            tok = ("dma", sm, sm.n)
        deps = set()
        for r in reads:
            if r.lw is not None:
                deps.add(r.lw)
            if r.psum:
                for d in r.rd:
                    if d[0] == "op" and self.ops[d[1]]["eng"] != eng:
                        deps.add(d)
        for w in writes:
            if w.lw is not None:
                d = w.lw
                if d[0] == "op" and self.ops[d[1]]["eng"] == eng:
                    pass
                elif d[0] == "dma" and dma is not None and not waw and d[1] is dma.sem:
                    pass
                else:
                    deps.add(d)
            for d in w.rd:
                if d[0] == "op" and self.ops[d[1]]["eng"] == eng:
                    continue
                deps.add(d)
        if eng == "pe":
            deps = {d for d in deps if not (d[0] == "op" and self.ops[d[1]]["eng"] == "pe")}
        deps.discard(tok)
        o = dict(eng=eng, fn=fn, deps=deps, dma=dma, tok=tok, marked=False,
                 dmasem=(dma.sem if dma is not None else None))
        self.ops.append(o)
        self.eng_ops[eng].append(idx)
        for d in deps:
            if d[0] == "op":
                self.ops[d[1]]["marked"] = True
        for r in reads:
            r.rd.append(tok)
        for w in writes:
            w.lw = tok
            w.rd = []
        return idx

    def barrier(self):
        last = {}
        for e in ENGS:
            for i in reversed(self.eng_ops[e]):
                if self.ops[i]["fn"] is not None and self.ops[i]["dma"] is None:
                    last[e] = i
                    break
        dmas = [("dma", sm, sm.n) for sm in self.sems if sm.n > 0]
        for e in ENGS:
            deps = set(dmas)
            for e2, i in last.items():
                if e2 != e:
                    deps.add(("op", i))
                    self.ops[i]["marked"] = True
            idx = len(self.ops)
            self.ops.append(dict(eng=e, fn=None, deps=deps, dma=None, tok=("op", idx), marked=False))
            self.eng_ops[e].append(idx)
        for r in self.res:
            r.lw = None
            r.rd = []

    def emit(self):
        nc = self.nc
        semval = {}
        cnt = {e: 0 for e in ENGS}
        for i, o in enumerate(self.ops):
            if o["marked"] and o["dma"] is None and o["fn"] is not None:
                cnt[o["eng"]] += 1
                semval[i] = cnt[o["eng"]]
        self.stats = dict(cnt)

        def run(ename, eng):
            waited = {}
            for i in self.eng_ops[ename]:
                o = self.ops[i]
                for d in sorted(o["deps"], key=lambda d: (d[0], d[1] if d[0] == "op" else id(d[1]))):
                    if d[0] == "op":
                        p = self.ops[d[1]]
                        if p["fn"] is None:
                            continue
                        sem, val = self.engsem[p["eng"]], semval[d[1]]
                    else:
                        sem, val = d[1].h, 16 * d[2]
                    k = id(sem)
                    if waited.get(k, 0) < val:
                        eng.wait_ge(sem, val)
                        waited[k] = val
                if o["fn"] is None:
                    continue
                ins = o["fn"](eng)
                if o["dma"] is not None:
                    ins.then_inc(o["dmasem"].h, 16)
                elif o["marked"]:
                    ins.then_inc(self.engsem[ename], 1)

        with nc.Block() as block:
            @block.tensor
            def _(e):
                run("pe", e)

            @block.vector
            def _(e):
                run("dve", e)

            @block.scalar
            def _(e):
                run("act", e)

            @block.gpsimd
            def _(e):
                run("pool", e)

            @block.sync
            def _(e):
                run("sp", e)

    def mm(self, out_r, out_ap, lhsT, rhs, reads, start=True, stop=True):
        return self.op("pe", lambda e: e.matmul(out_ap, lhsT, rhs, start=start, stop=stop),
                       reads=reads, writes=[out_r])

    def tr(self, out_r, out_ap, in_ap, ident_ap, reads):
        return self.op("pe", lambda e: e.transpose(out_ap, in_ap, ident_ap), reads=reads, writes=[out_r])

    def load(self, r, out_ap, in_ap, q="sp", waw=False):
        return self.op(q, lambda e: e.dma_start(out=out_ap, in_=in_ap), writes=[r], dma=r, waw=waw)

    def store(self, r, out_ap, in_ap, q="sp"):
        return self.op(q, lambda e: e.dma_start(out=out_ap, in_=in_ap), reads=[r], dma=r)

from contextlib import ExitStack
from concourse.bass_utils import run_bass_kernel_spmd

D = 1024
FH = 2816
NFB = FH // 128
PAD = 8
EPS = 1e-6


class Ctx:
    pass


def make_ctx(nc, es, T, NT=384):
    C = Ctx()
    C.nc = nc
    C.T = T
    C.NT = NT
    C.P = Prog(nc, es)
    P = C.P
    C.ps = [P.ps("ps%d" % i, [128, 512], F32) for i in range(8)]
    C.ones_bf = P.sb("ones_bf", [128, 128], BF16)
    C.ident = P.sb("ident", [128, 128], F32)
    C.zeros = P.sb("zeros", [128, 8, PAD], F32)
    P.op("dve", lambda e: e.memset(C.ones_bf[:], 1.0), writes=[C.ones_bf])
    P.op("dve", lambda e: e.memset(C.zeros[:], 0.0), writes=[C.zeros])
    ident_d = nc.dram_tensor("ident_in", [128, 128], F32, kind="ExternalInput").ap()
    P.load(C.ident, C.ident[:], ident_d[:, :])
    C.ones_f = P.sb("ones_fc", [128, 128], F32)
    P.op("dve", lambda e: e.memset(C.ones_f[:], 1.0), writes=[C.ones_f])
    C.eps_t = P.sb("eps_t", [128, 1], F32)
    P.op("dve", lambda e: e.memset(C.eps_t[:], EPS), writes=[C.eps_t])
    return C


def hview(h):
    return h.rearrange("(c p) w -> p c w", p=128)


def zero_pads(C, h):
    P = C.P
    hv = hview(h)
    T = C.T
    P.store(C.zeros, hv[:, :, 0:PAD], C.zeros[:])
    P.store(C.zeros, hv[:, :, PAD + T:PAD + T + PAD], C.zeros[:])


def phase_in(C, x, h):
    P, T = C.P, C.T
    hv = hview(h)
    with ExitStack() as es:
        xin = [P.sb("xin%d" % i, [128, D], F32, es) for i in range(8)]
        stage = [P.sb("stg%d" % i, [128, 8, 512], F32, es) for i in range(2)]
        loc = xin + stage
        k = 0
        for g in range(T // 512):
            xs = []
            for j in range(4):
                xt = xin[(g % 2) * 4 + j]
                r0 = g * 512 + j * 128
                P.load(xt, xt[:], x[r0:r0 + 128, :])
                xs.append(xt)
            st = stage[g % 2]
            for c in range(8):
                pb = C.ps[k % 4]
                for j in range(4):
                    P.tr(pb, pb[:, j * 128:(j + 1) * 128], xs[j][:, c * 128:(c + 1) * 128], C.ident[:],
                         reads=[xs[j], C.ident])
                if k % 2 == 0:
                    P.op("dve", lambda e, st=st, c=c, pb=pb: e.tensor_copy(st[:, c, :], pb[:]),
                         reads=[pb], writes=[st])
                else:
                    P.op("act", lambda e, st=st, c=c, pb=pb: e.copy(st[:, c, :], pb[:]),
                         reads=[pb], writes=[st])
                k += 1
            P.store(st, hv[:, :, PAD + g * 512:PAD + (g + 1) * 512], st[:])
        P.barrier()
        P.release(loc)


def phase_out(C, h, out):
    P, T = C.P, C.T
    hv = hview(h)
    with ExitStack() as es:
        hin = [P.sb("hin%d" % i, [128, 8, 512], F32, es) for i in range(2)]
        ot = [P.sb("ot%d" % i, [128, D], F32, es) for i in range(4)]
        loc = hin + ot
        k = 0
        n = 0
        for g in range(T // 512):
            hi = hin[g % 2]
            P.load(hi, hi[:], hv[:, :, PAD + g * 512:PAD + (g + 1) * 512])
            for j in range(4):
                o = ot[n % 4]
                n += 1
                for half in range(2):
                    pb = C.ps[k % 4]
                    for q in range(4):
                        c = half * 4 + q
                        P.tr(pb, pb[:, q * 128:(q + 1) * 128], hi[:, c, j * 128:(j + 1) * 128], C.ident[:],
                             reads=[hi, C.ident])
                    if k % 2 == 0:
                        P.op("dve", lambda e, o=o, half=half, pb=pb: e.tensor_copy(o[:, half * 512:(half + 1) * 512], pb[:]),
                             reads=[pb], writes=[o])
                    else:
                        P.op("act", lambda e, o=o, half=half, pb=pb: e.copy(o[:, half * 512:(half + 1) * 512], pb[:]),
                             reads=[pb], writes=[o])
                    k += 1
                r0 = g * 512 + j * 128
                P.store(o, out[r0:r0 + 128, :], o[:])
        P.barrier()
        P.release(loc)


def rstd_from_sumsq(C, ps_sum, n, width, tmp, rstd):
    P = C.P
    P.op("act", lambda e: e.activation(tmp[:, :width], ps_sum[:, :width], AF.Sqrt, bias=C.eps_t[:, 0:1], scale=1.0 / n),
         reads=[ps_sum, C.eps_t], writes=[tmp])
    P.op("dve", lambda e: e.reciprocal(rstd[:, :width], tmp[:, :width]), reads=[tmp], writes=[rstd])


def phase_ffn(C, li, hin, hout, W):
    P, T, NT = C.P, C.T, C.NT
    NO = NT - 2
    hiv, hov = hview(hin), hview(hout)
    with ExitStack() as es:
        Wup = P.sb("Wup", [128, 8, 2 * FH], BF16, es)
        Wdn = P.sb("Wdn", [128, NFB, D], BF16, es)
        cw = P.sb("cw", [128, 44 * 3], F32, es)
        cb = P.sb("cb", [128, 44], F32, es)
        g2 = P.sb("g2", [128, 8], F32, es)
        g3 = P.sb("g3", [128, 8], F32, es)
        H = P.sb("H", [128, 8, NT], F32, es)
        xn = P.sb("xn", [128, 8, NT], BF16, es)
        hm = P.sb("hm", [128, NFB, NT], BF16, es)
        fT = P.sb("fT", [128, 8, NT], F32, es)
        sq = [P.sb("sq%d" % i, [128, NT], BF16, es) for i in range(2)]
        tmp = P.sb("tmp", [128, NT], F32, es)
        rstd = P.sb("rstd", [128, NT], F32, es)
        rstd2 = P.sb("rstd2", [128, NT], F32, es)
        tg = [[P.sb("tg%d%d" % (i, j), [128, NT], F32, es) for j in range(2)] for i in range(3)]
        tv = [[P.sb("tv%d%d" % (i, j), [128, NT], F32, es) for j in range(2)] for i in range(3)]
        loc = [Wup, Wdn, cw, cb, g2, g3, H, xn, hm, fT, tmp, rstd, rstd2] + sq + sum(tg, []) + sum(tv, [])

        wu = W["ffn_w_up"][li].rearrange("(c p) f -> p c f", p=128)
        for c in range(8):
            P.load(Wup, Wup[:, c, :], wu[:, c, :], q="pool")
        wd = W["ffn_w_down"][li].rearrange("(c p) d -> p c d", p=128)
        for c0 in range(0, NFB, 6):
            c1 = min(NFB, c0 + 6)
            P.load(Wdn, Wdn[:, c0:c1, :], wd[:, c0:c1, :], q="pool")
        P.load(cw, cw[:], W["ffn_cw"][li])
        P.load(cb, cb[:], W["ffn_cb"][li])
        P.load(g2, g2[:], W["norm_g"][li * 4 + 2])
        P.load(g3, g3[:], W["norm_g"][li * 4 + 3])

        import os
        STOP = int(os.environ.get("FFN_STOP", "99"))
        starts = []
        s = 0
        while True:
            if s + NO >= T:
                starts.append(T - NO)
                break
            starts.append(s)
            s += NO
        psS = C.ps[6]
        for ti, s in enumerate(starts):
            c0 = PAD + s - 1
            P.load(H, H[:], hiv[:, :, c0:c0 + NT])
            for c in range(8):
                sqt = sq[c % 2]
                P.op("act", lambda e, sqt=sqt, c=c: e.activation(sqt[:], H[:, c, :], AF.Square),
                     reads=[H], writes=[sqt])
                P.mm(psS, psS[:, :NT], C.ones_bf[:], sqt[:], reads=[C.ones_bf, sqt], start=(c == 0), stop=(c == 7))
            if STOP <= 1:
                continue
            rstd_from_sumsq(C, psS, D, NT, tmp, rstd)
            if STOP <= 2:
                continue
            for c in range(8):
                P.op("dve", lambda e, c=c: e.scalar_tensor_tensor(out=xn[:, c, :], in0=H[:, c, :], scalar=g2[:, c:c + 1],
                                                                  in1=rstd[:], op0=ALU.mult, op1=ALU.mult),
                     reads=[H, g2, rstd], writes=[xn])
            if STOP <= 3:
                continue
            for fb in range(NFB):
                pg = C.ps[(fb % 3) * 2]
                pv = C.ps[(fb % 3) * 2 + 1]
                for c in range(8):
                    P.mm(pg, pg[:, :NT], Wup[:, c, fb * 128:(fb + 1) * 128], xn[:, c, :], reads=[Wup, xn],
                         start=(c == 0), stop=(c == 7))
                for c in range(8):
                    P.mm(pv, pv[:, :NT], Wup[:, c, FH + fb * 128:FH + (fb + 1) * 128], xn[:, c, :], reads=[Wup, xn],
                         start=(c == 0), stop=(c == 7))
                outs = []
                for (pp, tt, fi) in ((pg, tg[fb % 3], fb), (pv, tv[fb % 3], fb + NFB)):
                    t1, t2 = tt
                    w0 = cw[:, fi * 3 + 0:fi * 3 + 1]
                    w1 = cw[:, fi * 3 + 1:fi * 3 + 2]
                    w2 = cw[:, fi * 3 + 2:fi * 3 + 3]
                    bb = cb[:, fi:fi + 1]
                    P.op("act", lambda e, t1=t1, pp=pp, w1=w1, bb=bb: e.activation(t1[:, :NO], pp[:, 1:1 + NO], AF.Identity,
                                                                                  bias=bb, scale=w1),
                         reads=[pp, cw, cb], writes=[t1])
                    P.op("dve", lambda e, t1=t1, t2=t2, pp=pp, w0=w0: e.scalar_tensor_tensor(
                        out=t2[:, :NO], in0=pp[:, 0:NO], scalar=w0, in1=t1[:, :NO], op0=ALU.mult, op1=ALU.add),
                        reads=[pp, cw, t1], writes=[t2])
                    P.op("dve", lambda e, t1=t1, t2=t2, pp=pp, w2=w2: e.scalar_tensor_tensor(
                        out=t1[:, :NO], in0=pp[:, 2:2 + NO], scalar=w2, in1=t2[:, :NO], op0=ALU.mult, op1=ALU.add),
                        reads=[pp, cw, t2], writes=[t1])
                    outs.append((t1, t2))
                (cg, gbuf), (cv, _) = outs
                P.op("act", lambda e, gbuf=gbuf, cg=cg: e.activation(gbuf[:, :NO], cg[:, :NO], AF.Gelu_apprx_tanh),
                     reads=[cg], writes=[gbuf])
                P.op("pool", lambda e, fb=fb, gbuf=gbuf, cv=cv: e.tensor_tensor(out=hm[:, fb, :NO], in0=gbuf[:, :NO], in1=cv[:, :NO],
                                                                                op=ALU.mult),
                     reads=[gbuf, cv], writes=[hm])
            if STOP <= 4:
                continue
            for db in range(8):
                pd = C.ps[db % 6]
                for fc in range(NFB):
                    P.mm(pd, pd[:, :NO], Wdn[:, fc, db * 128:(db + 1) * 128], hm[:, fc, :NO], reads=[Wdn, hm],
                         start=(fc == 0), stop=(fc == NFB - 1))
                sqt = sq[db % 2]
                P.op("dve", lambda e, db=db, pd=pd: e.tensor_copy(fT[:, db, :NO], pd[:, :NO]), reads=[pd], writes=[fT])
                P.op("act", lambda e, sqt=sqt, db=db: e.activation(sqt[:, :NO], fT[:, db, :NO], AF.Square),
                     reads=[fT], writes=[sqt])
                if STOP >= 6:
                    P.mm(psS, psS[:, :NO], C.ones_bf[:], sqt[:, :NO], reads=[C.ones_bf, sqt], start=(db == 0), stop=(db == 7))
            if STOP <= 6:
                continue
            rstd_from_sumsq(C, psS, D, NO, tmp, rstd2)
            if STOP <= 7:
                continue
            for c in range(8):
                P.op("dve", lambda e, c=c: e.scalar_tensor_tensor(out=fT[:, c, :NO], in0=fT[:, c, :NO], scalar=g3[:, c:c + 1],
                                                                  in1=rstd2[:, :NO], op0=ALU.mult, op1=ALU.mult),
                     reads=[fT, g3, rstd2], writes=[fT])
                P.op("pool", lambda e, c=c: e.tensor_tensor(out=fT[:, c, :NO], in0=fT[:, c, :NO], in1=H[:, c, 1:1 + NO], op=ALU.add),
                     reads=[fT, H], writes=[fT])
            if STOP <= 8:
                continue
            P.store(fT, hov[:, :, PAD + s:PAD + s + NO], fT[:, :, :NO])
        P.barrier()
        P.release(loc)


def gelu_tanh(C, out, x, n):
    P = C.P
    P.op("act", lambda e: e.activation(out[:, :n], x[:, :n], AF.Square), reads=[x], writes=[out])
    P.op("pool", lambda e: e.tensor_scalar(out[:, :n], out[:, :n], 0.044715, 1.0, ALU.mult, ALU.add), reads=[out], writes=[out])
    P.op("pool", lambda e: e.tensor_tensor(out=out[:, :n], in0=out[:, :n], in1=x[:, :n], op=ALU.mult), reads=[out, x], writes=[out])
    P.op("act", lambda e: e.activation(out[:, :n], out[:, :n], AF.Sigmoid, scale=1.5957691216057308), reads=[out], writes=[out])
    P.op("pool", lambda e: e.tensor_tensor(out=out[:, :n], in0=out[:, :n], in1=x[:, :n], op=ALU.mult), reads=[out, x], writes=[out])


def declare_inputs(nc, shapes):
    W = {}
    for name, shp in shapes.items():
        W[name] = nc.dram_tensor(name, list(shp), F32, kind="ExternalInput").ap()
    return W


def host_ffn_params(inp):
    L = inp["ffn_conv_w"].shape[0]
    cw = np.ascontiguousarray(inp["ffn_conv_w"].transpose(0, 2, 1).reshape(L, 44, 128, 3).transpose(0, 2, 1, 3).reshape(L, 128, 132))
    cb = np.ascontiguousarray(inp["ffn_conv_b"].reshape(L, 44, 128).transpose(0, 2, 1))
    ng = np.ascontiguousarray(inp["norm_g"].reshape(L * 4, 8, 128).transpose(0, 2, 1))
    return cw, cb, ng


def load_w(C, es, name, ap, q="pool"):
    P = C.P
    K, N = ap.shape
    kc = K // 128
    t = P.sb(name, [128, kc, N], BF16, es)
    v = ap.rearrange("(c p) n -> p c n", p=128)
    step = max(1, 4096 // N)
    for c0 in range(0, kc, step):
        c1 = min(kc, c0 + step)
        P.load(t, t[:, c0:c1, :], v[:, c0:c1, :], q=q)
    return t


def load_small(C, es, name, ap):
    P = C.P
    t = P.sb(name, list(ap.shape), F32, es)
    P.load(t, t[:], ap)
    return t


class NormBufs:
    def __init__(self, C, es, n, pref):
        P = C.P
        self.H = P.sb(pref + "H", [128, 8, n], F32, es)
        self.xns = [P.sb(pref + "xn%d" % i, [128, 8, n], BF16, es) for i in range(2)]
        self.xn = self.xns[0]
        self.ncall = 0
        self.sq = [P.sb(pref + "sq%d" % i, [128, n], BF16, es) for i in range(2)]
        self.tmp = P.sb(pref + "tmp", [128, n], F32, es)
        self.rstd = P.sb(pref + "rstd", [128, n], F32, es)
        self.all = [self.H, self.tmp, self.rstd] + self.xns + self.sq


def norm_in(C, hv, col0, n, g, B, psS):
    P = C.P
    B.xn = B.xns[B.ncall % 2]
    B.ncall += 1
    P.load(B.H, B.H[:, :, :n], hv[:, :, col0:col0 + n])
    for c in range(8):
        sqt = B.sq[c % 2]
        P.op("act", lambda e, sqt=sqt, c=c: e.activation(sqt[:, :n], B.H[:, c, :n], AF.Square), reads=[B.H], writes=[sqt])
        P.mm(psS, psS[:, :n], C.ones_bf[:], sqt[:, :n], reads=[C.ones_bf, sqt], start=(c == 0), stop=(c == 7))
    rstd_from_sumsq(C, psS, D, n, B.tmp, B.rstd)
    for c in range(8):
        P.op("dve", lambda e, c=c, xn=B.xn: e.scalar_tensor_tensor(out=xn[:, c, :n], in0=B.H[:, c, :n], scalar=g[:, c:c + 1],
                                                          in1=B.rstd[:, :n], op0=ALU.mult, op1=ALU.mult),
             reads=[B.H, g, B.rstd], writes=[B.xn])


def sub_norm(C, src, nch, n, nfeat, g, dst, sq, tmp, rstd, psS, extra_scale=None):
    P = C.P
    for c in range(nch):
        sqt = sq[c % 2]
        P.op("act", lambda e, sqt=sqt, c=c: e.activation(sqt[:, :n], src[:, c, :n], AF.Square), reads=[src], writes=[sqt])
        P.mm(psS, psS[:, :n], C.ones_bf[:], sqt[:, :n], reads=[C.ones_bf, sqt], start=(c == 0), stop=(c == nch - 1))
    rstd_from_sumsq(C, psS, nfeat, n, tmp, rstd)
    for c in range(nch):
        P.op("dve", lambda e, c=c: e.scalar_tensor_tensor(out=dst[:, c, :n], in0=src[:, c, :n], scalar=g[:, c:c + 1],
                                                          in1=rstd[:, :n], op0=ALU.mult, op1=ALU.mult),
             reads=[src, g, rstd], writes=[dst])


def phase_tail(C, ao, Wo_ap, g1_ap, hin, hout, NTK=512):
    P, T = C.P, C.T
    KF = ao.shape[0]
    kc = KF // 128
    hiv, hov = hview(hin), hview(hout)
    aov = ao.rearrange("(c p) t -> p c t", p=128)
    with ExitStack() as es:
        Wo = load_w(C, es, "Wo", Wo_ap)
        g1 = load_small(C, es, "g1", g1_ap)
        A = [P.sb("tA%d" % i, [128, kc, NTK], BF16, es) for i in range(2)]
        H = [P.sb("tH%d" % i, [128, 8, NTK], F32, es) for i in range(2)]
        fTs = [P.sb("tfT%d" % i, [128, 8, NTK], F32, es) for i in range(2)]
        sq = [P.sb("tsq%d" % i, [128, NTK], BF16, es) for i in range(4)]
        tmps = [P.sb("ttmp%d" % i, [128, NTK], F32, es) for i in range(2)]
        rstds = [P.sb("trstd%d" % i, [128, NTK], F32, es) for i in range(2)]
        loc = [Wo, g1] + fTs + tmps + rstds + A + H + sq
        psS = C.ps[6]
        for ti in range(T // NTK):
            t0 = ti * NTK
            a, h = A[ti % 2], H[ti % 2]
            fT, tmp, rstd = fTs[ti % 2], tmps[ti % 2], rstds[ti % 2]
            P.load(a, a[:], aov[:, :, t0:t0 + NTK])
            P.load(h, h[:], hiv[:, :, PAD + t0:PAD + t0 + NTK])
            for db in range(8):
                pd = C.ps[db % 6]
                for c in range(kc):
                    P.mm(pd, pd[:, :NTK], Wo[:, c, db * 128:(db + 1) * 128], a[:, c, :], reads=[Wo, a],
                         start=(c == 0), stop=(c == kc - 1))
                sqt = sq[db % 4]
                P.op("dve", lambda e, db=db, pd=pd, fT=fT: e.tensor_copy(fT[:, db, :], pd[:, :NTK]), reads=[pd], writes=[fT])
                P.op("act", lambda e, sqt=sqt, db=db, fT=fT: e.activation(sqt[:], fT[:, db, :], AF.Square), reads=[fT], writes=[sqt])
                P.mm(psS, psS[:, :NTK], C.ones_bf[:], sqt[:], reads=[C.ones_bf, sqt], start=(db == 0), stop=(db == 7))
            rstd_from_sumsq(C, psS, D, NTK, tmp, rstd)
            for c in range(8):
                P.op("dve", lambda e, c=c, fT=fT, rstd=rstd: e.scalar_tensor_tensor(out=fT[:, c, :], in0=fT[:, c, :], scalar=g1[:, c:c + 1],
                                                                  in1=rstd[:], op0=ALU.mult, op1=ALU.mult),
                     reads=[fT, g1, rstd], writes=[fT])
                P.op("pool", lambda e, c=c, h=h, fT=fT: e.tensor_tensor(out=fT[:, c, :], in0=fT[:, c, :], in1=h[:, c, :], op=ALU.add),
                     reads=[fT, h], writes=[fT])
            P.store(fT, hov[:, :, PAD + t0:PAD + t0 + NTK], fT[:])
        P.barrier()
        P.release(loc)


def phase_mla_proj(C, li, hin, W, S, NTK=512):
    P, T = C.P, C.T
    hiv = hview(hin)
    with ExitStack() as es:
        Wdq = load_w(C, es, "Wdq", W["mla_w_dq"])
        Wuq = load_w(C, es, "Wuq", W["mla_w_uq"])
        Wdkv = load_w(C, es, "Wdkv", W["mla_w_dkv"])
        Wukv = load_w(C, es, "Wukv", W["mla_w_ukv"])
        g0 = load_small(C, es, "g0", W["norm_g"][li * 4 + 0])
        qg = load_small(C, es, "qg", W["mla_qg"])
        kvg = load_small(C, es, "kvg", W["mla_kvg"])
        B = NormBufs(C, es, NTK, "m")
        cqf = P.sb("cqf", [128, 3, NTK], F32, es)
        cqn = P.sb("cqn", [128, 3, NTK], BF16, es)
        ckf = P.sb("ckf", [128, 2, NTK], F32, es)
        ckn = P.sb("ckn", [128, 2, NTK], BF16, es)
        cs = P.sb("cs", [64, 2, NTK], F32, es)
        qn_st = P.sb("qn_st", [128, 8, NTK], BF16, es)
        kn_st = P.sb("kn_st", [128, 8, NTK], BF16, es)
        qr_st = P.sb("qr_st", [64, 8, NTK], BF16, es)
        kr_st = P.sb("kr_st", [64, NTK], BF16, es)
        v_st = P.sb("v_st", [128, NTK // 128, 1024], BF16, es)
        r1 = [P.sb("r1_%d" % i, [64, NTK], F32, es) for i in range(2)]
        r2 = [P.sb("r2_%d" % i, [64, NTK], F32, es) for i in range(2)]
        tmp2 = P.sb("mtmp2", [128, NTK], F32, es)
        rstd2 = P.sb("mrstd2", [128, NTK], F32, es)
        loc = [Wdq, Wuq, Wdkv, Wukv, g0, qg, kvg, cqf, cqn, ckf, ckn, cs, qn_st, kn_st, qr_st, kr_st, v_st, tmp2, rstd2] + B.all + r1 + r2
        psS = C.ps[6]
        qnv = S["qn"].rearrange("(h p) t -> p h t", p=128)
        knv = S["kn"].rearrange("(h p) t -> p h t", p=128)
        qrv = S["qr"].rearrange("(h p) t -> p h t", p=64)
        vv = S["v"].rearrange("(tb p) f -> p tb f", p=128)
        k = 0

        def rope(psA, psB, out_ap, out_r, i):
            a, b = r1[i % 2], r2[i % 2]
            P.op("dve", lambda e: e.tensor_tensor(out=a[:], in0=psA[0:64, :NTK], in1=cs[:, 0, :], op=ALU.mult), reads=[psA, cs], writes=[a])
            P.op("dve", lambda e: e.tensor_tensor(out=b[:], in0=psB[0:64, :NTK], in1=cs[:, 1, :], op=ALU.mult), reads=[psB, cs], writes=[b])
            P.op("pool", lambda e: e.tensor_tensor(out=out_ap, in0=a[:], in1=b[:], op=ALU.add), reads=[a, b], writes=[out_r])

        for ti in range(T // NTK):
            t0 = ti * NTK
            norm_in(C, hiv, PAD + t0, NTK, g0, B, psS)
            P.load(cs, cs[:], W["rope_cs"][:, :, t0:t0 + NTK])
            for fo in range(3):
                pb = C.ps[k % 4]; k += 1
                for c in range(8):
                    P.mm(pb, pb[:, :NTK], Wdq[:, c, fo * 128:(fo + 1) * 128], B.xn[:, c, :], reads=[Wdq, B.xn], start=(c == 0), stop=(c == 7))
                P.op("dve", lambda e, fo=fo, pb=pb: e.tensor_copy(cqf[:, fo, :], pb[:, :NTK]), reads=[pb], writes=[cqf])
            sub_norm(C, cqf, 3, NTK, 384, qg, cqn, B.sq, tmp2, rstd2, psS)
            for h in range(8):
                pb = C.ps[k % 4]; k += 1
                for c in range(3):
                    P.mm(pb, pb[:, :NTK], Wuq[:, c, h * 128:(h + 1) * 128], cqn[:, c, :], reads=[Wuq, cqn], start=(c == 0), stop=(c == 2))
                if h % 2 == 0:
                    P.op("act", lambda e, h=h, pb=pb: e.copy(qn_st[:, h, :], pb[:, :NTK]), reads=[pb], writes=[qn_st])
                else:
                    P.op("dve", lambda e, h=h, pb=pb: e.tensor_copy(qn_st[:, h, :], pb[:, :NTK]), reads=[pb], writes=[qn_st])
            P.store(qn_st, qnv[:, :, t0:t0 + NTK], qn_st[:])
            for h in range(8):
                pa = C.ps[k % 4]; k += 1
                pb = C.ps[k % 4]; k += 1
                for c in range(3):
                    P.mm(pa, pa[0:64, :NTK], Wuq[:, c, 1024 + h * 64:1024 + (h + 1) * 64], cqn[:, c, :], reads=[Wuq, cqn], start=(c == 0), stop=(c == 2))
                for c in range(3):
                    P.mm(pb, pb[0:64, :NTK], Wuq[:, c, 1536 + h * 64:1536 + (h + 1) * 64], cqn[:, c, :], reads=[Wuq, cqn], start=(c == 0), stop=(c == 2))
                rope(pa, pb, qr_st[:, h, :], qr_st, h)
            P.store(qr_st, qrv[:, :, t0:t0 + NTK], qr_st[:])
            for fo in range(2):
                pb = C.ps[k % 4]; k += 1
                for c in range(8):
                    P.mm(pb, pb[:, :NTK], Wdkv[:, c, fo * 128:(fo + 1) * 128], B.xn[:, c, :], reads=[Wdkv, B.xn], start=(c == 0), stop=(c == 7))
                P.op("dve", lambda e, fo=fo, pb=pb: e.tensor_copy(ckf[:, fo, :], pb[:, :NTK]), reads=[pb], writes=[ckf])
            pa = C.ps[k % 4]; k += 1
            pb = C.ps[k % 4]; k += 1
            for c in range(8):
                P.mm(pa, pa[0:64, :NTK], Wdkv[:, c, 256:320], B.xn[:, c, :], reads=[Wdkv, B.xn], start=(c == 0), stop=(c == 7))
            for c in range(8):
                P.mm(pb, pb[0:64, :NTK], Wdkv[:, c, 320:384], B.xn[:, c, :], reads=[Wdkv, B.xn], start=(c == 0), stop=(c == 7))
            rope(pa, pb, kr_st[:], kr_st, 0)
            P.store(kr_st, S["kr"][:, t0:t0 + NTK], kr_st[:])
            sub_norm(C, ckf, 2, NTK, 256, kvg, ckn, B.sq, tmp2, rstd2, psS)
            for h in range(8):
                pb = C.ps[k % 4]; k += 1
                for c in range(2):
                    P.mm(pb, pb[:, :NTK], Wukv[:, c, h * 128:(h + 1) * 128], ckn[:, c, :], reads=[Wukv, ckn], start=(c == 0), stop=(c == 1))
                if h % 2 == 0:
                    P.op("act", lambda e, h=h, pb=pb: e.copy(kn_st[:, h, :], pb[:, :NTK]), reads=[pb], writes=[kn_st])
                else:
                    P.op("dve", lambda e, h=h, pb=pb: e.tensor_copy(kn_st[:, h, :], pb[:, :NTK]), reads=[pb], writes=[kn_st])
            P.store(kn_st, knv[:, :, t0:t0 + NTK], kn_st[:])
            for tb in range(NTK // 128):
                for half in range(2):
                    pb = C.ps[k % 4]; k += 1
                    for c in range(2):
                        P.mm(pb, pb[:, :512], ckn[:, c, tb * 128:(tb + 1) * 128], Wukv[:, c, 1024 + half * 512:1024 + (half + 1) * 512],
                             reads=[Wukv, ckn], start=(c == 0), stop=(c == 1))
                    if half == 0:
                        P.op("act", lambda e, tb=tb, half=half, pb=pb: e.copy(v_st[:, tb, half * 512:(half + 1) * 512], pb[:, :512]), reads=[pb], writes=[v_st])
                    else:
                        P.op("dve", lambda e, tb=tb, half=half, pb=pb: e.tensor_copy(v_st[:, tb, half * 512:(half + 1) * 512], pb[:, :512]), reads=[pb], writes=[v_st])
            P.store(v_st, vv[:, t0 // 128:(t0 + NTK) // 128, :], v_st[:])
        P.barrier()
        P.release(loc)


def phase_mla_core(C, S, TQ0=0, TQ=None, NQ=512):
    P, T = C.P, C.T
    TQ = TQ or T
    NKB = T // 128
    scale = float(192 ** -0.5)
    with ExitStack() as es:
        Kn = [P.sb("Kn%d" % i, [128, T], BF16, es) for i in range(2)]
        Vh = [P.sb("Vh%d" % i, [128, NKB, 128], BF16, es) for i in range(2)]
        Kr = P.sb("Kr", [64, T], BF16, es)
        Qn = [P.sb("Qn%d" % i, [128, NQ], BF16, es) for i in range(2)]
        Qr = [P.sb("Qr%d" % i, [64, NQ], BF16, es) for i in range(2)]
        Pt = [P.sb("Pt%d" % i, [128, NQ], BF16, es) for i in range(4)]
        rl = P.sb("rl", [128, NQ], F32, es)
        ob = [P.sb("ob%d" % i, [128, NQ], BF16, es) for i in range(2)]
        loc = Kn + Vh + [Kr, rl] + Qn + Qr + Pt + ob
        P.load(Kr, Kr[:], S["kr"][:, :])
        vv = S["v"].rearrange("(kb p) f -> p kb f", p=128)
        qrv = S["qr"].rearrange("(h p) t -> p h t", p=64)
        it = 0
        for h in range(8):
            kn, vh = Kn[h % 2], Vh[h % 2]
            P.load(kn, kn[:], S["kn"][h * 128:(h + 1) * 128, :])
            P.load(vh, vh[:], vv[:, :, h * 128:(h + 1) * 128])
            for qi in range(TQ // NQ):
                q0 = TQ0 + qi * NQ
                qn, qr = Qn[it % 2], Qr[it % 2]
                o_sb = ob[it % 2]
                pO, pL = C.ps[3 + it % 2], C.ps[5 + it % 2]
                it += 1
                P.load(qn, qn[:], S["qn"][h * 128:(h + 1) * 128, q0:q0 + NQ])
                P.load(qr, qr[:], qrv[:, h, q0:q0 + NQ])

                def stA(kb, kn=kn, qn=qn, qr=qr):
                    pS = C.ps[kb % 3]
                    pt = Pt[kb % 4]
                    ks = slice(kb * 128, (kb + 1) * 128)
                    P.mm(pS, pS[:, :NQ], kn[:, ks], qn[:], reads=[kn, qn], start=True, stop=False)
                    P.mm(pS, pS[:, :NQ], Kr[:, ks], qr[:], reads=[Kr, qr], start=False, stop=True)
                    P.op("act", lambda e, pt=pt, pS=pS: e.activation(pt[:], pS[:, :NQ], AF.Exp, scale=scale), reads=[pS], writes=[pt])

                def stB(kb, vh=vh, pO=pO, pL=pL):
                    pt = Pt[kb % 4]
                    P.mm(pO, pO[:, :NQ], vh[:, kb, :], pt[:], reads=[vh, pt], start=(kb == 0), stop=(kb == NKB - 1))
                    P.mm(pL, pL[:, :NQ], C.ones_bf[:], pt[:], reads=[C.ones_bf, pt], start=(kb == 0), stop=(kb == NKB - 1))

                stA(0)
                if NKB > 1:
                    stA(1)
                for kb in range(NKB):
                    if kb + 2 < NKB:
                        stA(kb + 2)
                    stB(kb)
                P.op("dve", lambda e, pL=pL: e.reciprocal(rl[:], pL[:, :NQ]), reads=[pL], writes=[rl])
                P.op("dve", lambda e, pO=pO, o_sb=o_sb: e.tensor_tensor(out=o_sb[:], in0=pO[:, :NQ], in1=rl[:], op=ALU.mult), reads=[pO, rl], writes=[o_sb])
                P.store(o_sb, S["ao"][h * 128:(h + 1) * 128, q0:q0 + NQ], o_sb[:])
        P.barrier()
        P.release(loc)


def pm(v):
    v = np.asarray(v)
    return np.ascontiguousarray(v.reshape(-1, 128).T)


def host_mla_params(inp, pos):
    wuq = inp["mla_w_uq"][0].reshape(384, 8, 192)
    nope = wuq[:, :, :128].reshape(384, 1024)
    ropew = wuq[:, :, 128:]
    rope_sw = np.concatenate([ropew[:, :, 32:], ropew[:, :, :32]], -1)
    w_uq = np.ascontiguousarray(np.concatenate([nope, ropew.reshape(384, 512), rope_sw.reshape(384, 512)], 1))
    wdkv = inp["mla_w_dkv"][0]
    kr = wdkv[:, 256:]
    w_dkv = np.ascontiguousarray(np.concatenate([wdkv[:, :256], kr, kr[:, 32:], kr[:, :32]], 1))
    wukv = inp["mla_w_ukv"][0].reshape(256, 8, 256)
    w_ukv = np.ascontiguousarray(np.concatenate([wukv[:, :, :128].reshape(256, 1024), wukv[:, :, 128:].reshape(256, 1024)], 1))
    inv = (10000.0 ** (-np.arange(0, 64, 2, dtype=np.float32) / 64)).astype(np.float32)
    ang = pos.astype(np.float32)[None, :] * inv[:, None]
    cos, sin = np.cos(ang).astype(np.float32), np.sin(ang).astype(np.float32)
    cs = np.stack([np.concatenate([cos, cos], 0), np.concatenate([-sin, sin], 0)], 1)
    return dict(mla_w_dq=np.ascontiguousarray(inp["mla_w_dq"][0]), mla_w_uq=w_uq, mla_w_dkv=w_dkv, mla_w_ukv=w_ukv,
                mla_w_o=np.ascontiguousarray(inp["mla_w_o"][0]),
                mla_qg=pm(inp["mla_q_norm_g"][0]), mla_kvg=pm(inp["mla_kv_norm_g"][0]), rope_cs=np.ascontiguousarray(cs.astype(np.float32)))


def phase_diff_proj(C, li, hin, W, S, NTK=512):
    P, T = C.P, C.T
    hiv = hview(hin)
    with ExitStack() as es:
        Wqkv = load_w(C, es, "Wqkv", W["diff_w_qkv"])
        g0 = load_small(C, es, "g0", W["norm_g"][li * 4 + 0])
        B = NormBufs(C, es, NTK, "d")
        q_st = P.sb("dq_st", [128, 8, NTK], BF16, es)
        k_st = P.sb("dk_st", [128, 8, NTK], BF16, es)
        v_st = P.sb("dv_st", [128, NTK // 128, 1024], BF16, es)
        loc = [Wqkv, g0, q_st, k_st, v_st] + B.all
        psS = C.ps[6]
        qv = S["qn"].rearrange("(h p) t -> p h t", p=128)
        kv = S["kn"].rearrange("(h p) t -> p h t", p=128)
        vv = S["v"].rearrange("(tb p) f -> p tb f", p=128)
        k = 0
        for ti in range(T // NTK):
            t0 = ti * NTK
            norm_in(C, hiv, PAD + t0, NTK, g0, B, psS)
            for (st, off, dst) in ((q_st, 0, qv), (k_st, 1024, kv)):
                for h in range(8):
                    pb = C.ps[k % 4]; k += 1
                    for c in range(8):
                        P.mm(pb, pb[:, :NTK], Wqkv[:, c, off + h * 128:off + (h + 1) * 128], B.xn[:, c, :], reads=[Wqkv, B.xn],
                             start=(c == 0), stop=(c == 7))
                    if h % 2 == 0:
                        P.op("act", lambda e, h=h, pb=pb, st=st: e.copy(st[:, h, :], pb[:, :NTK]), reads=[pb], writes=[st])
                    else:
                        P.op("dve", lambda e, h=h, pb=pb, st=st: e.tensor_copy(st[:, h, :], pb[:, :NTK]), reads=[pb], writes=[st])
                P.store(st, dst[:, :, t0:t0 + NTK], st[:])
            for tb in range(NTK // 128):
                for half in range(2):
                    pb = C.ps[k % 4]; k += 1
                    for c in range(8):
                        P.mm(pb, pb[:, :512], B.xn[:, c, tb * 128:(tb + 1) * 128], Wqkv[:, c, 2048 + half * 512:2048 + (half + 1) * 512],
                             reads=[Wqkv, B.xn], start=(c == 0), stop=(c == 7))
                    if half == 0:
                        P.op("act", lambda e, tb=tb, half=half, pb=pb: e.copy(v_st[:, tb, half * 512:(half + 1) * 512], pb[:, :512]), reads=[pb], writes=[v_st])
                    else:
                        P.op("dve", lambda e, tb=tb, half=half, pb=pb: e.tensor_copy(v_st[:, tb, half * 512:(half + 1) * 512], pb[:, :512]), reads=[pb], writes=[v_st])
            P.store(v_st, vv[:, t0 // 128:(t0 + NTK) // 128, :], v_st[:])
        P.barrier()
        P.release(loc)


def phase_diff_core(C, li, W, S, NQ=512):
    P, T = C.P, C.T
    NKB = T // 128
    scale = float(64 ** -0.5)
    lam_init = 0.8 - 0.6 * float(np.exp(-0.3 * li))
    SKIP = 160.0
    with ExitStack() as es:
        Kh = [P.sb("dK%d" % i, [128, T], BF16, es) for i in range(2)]
        Vh = [P.sb("dV%d" % i, [128, NKB, 128], BF16, es) for i in range(2)]
        Q = [P.sb("dQ%d" % i, [128, NQ], BF16, es) for i in range(2)]
        RAW = P.sb("dRAW", [128, 6, NQ], F32, es)
        BRAW = P.sb("dBRAW", [128, 128], F32, es)
        MT = P.sb("dMT", [128, 6, NQ], BF16, es)
        BT = P.sb("dBT", [128, 128], F32, es)
        E = [P.sb("dE%d" % i, [128, NQ], BF16, es) for i in range(4)]
        Pt = [P.sb("dPt%d" % i, [128, NQ], BF16, es) for i in range(6)]
        lp = P.sb("dlp", [1, 256], F32, es)
        lw = P.sb("dlw", [1, 8], F32, es)
        ones1 = P.sb("dones1", [1, 128], F32, es)
        nlam = P.sb("dnlam", [128, 1], F32, es)
        sg = P.sb("dsg", [128, 1], F32, es)
        r1 = P.sb("dr1", [128, NQ], F32, es)
        r2 = P.sb("dr2", [128, NQ], F32, es)
        a1 = P.sb("da1", [128, NQ], F32, es)
        a2 = P.sb("da2", [128, NQ], F32, es)
        sqd = P.sb("dsq", [128, NQ], BF16, es)
        ob = [P.sb("dob%d" % i, [128, NQ], BF16, es) for i in range(2)]
        loc = Kh + Vh + Q + [RAW, BRAW, MT, BT, lp, lw, ones1, nlam, sg, r1, r2, a1, a2, sqd] + E + Pt + ob
        P.op("pool", lambda e: e.iota(RAW[:, 0, :], [[1, NQ]], base=0, channel_multiplier=0, allow_small_or_imprecise_dtypes=True), writes=[RAW])
        P.op("pool", lambda e: e.iota(RAW[:, 1, :], [[-1, NQ]], base=NQ - 1, channel_multiplier=0, allow_small_or_imprecise_dtypes=True), writes=[RAW])
        for kk in range(4):
            P.op("pool", lambda e, kk=kk: e.iota(RAW[:, 2 + kk, :], [[1, NQ]], base=-128 * kk, channel_multiplier=-1,
                                                allow_small_or_imprecise_dtypes=True), writes=[RAW])
        P.op("dve", lambda e: e.scalar_tensor_tensor(out=RAW[:, 2:6, :], in0=RAW[:, 2:6, :], scalar=-1.0, in1=RAW[:, 2:6, :], op0=ALU.mult, op1=ALU.max),
             reads=[RAW], writes=[RAW])
        P.op("pool", lambda e: e.iota(BRAW[:, 0:64], [[-128, 64]], base=0, channel_multiplier=1, allow_small_or_imprecise_dtypes=True), writes=[BRAW])
        P.op("pool", lambda e: e.iota(BRAW[:, 64:128], [[-128, 64]], base=NQ - 1, channel_multiplier=-1, allow_small_or_imprecise_dtypes=True), writes=[BRAW])
        P.load(lp, lp[:], W["diff_lambda"])
        P.load(sg, sg[:], W["diff_subln_g"])
        P.op("dve", lambda e: e.memset(ones1[:], 1.0), writes=[ones1])
        P.op("dve", lambda e: e.tensor_tensor(out=lp[:, 0:64], in0=lp[:, 0:64], in1=lp[:, 64:128], op=ALU.mult), reads=[lp], writes=[lp])
        P.op("dve", lambda e: e.tensor_tensor(out=lp[:, 128:192], in0=lp[:, 128:192], in1=lp[:, 192:256], op=ALU.mult), reads=[lp], writes=[lp])
        P.op("dve", lambda e: e.reduce_sum(lw[:, 0:1], lp[:, 0:64], axis=AX.X), reads=[lp], writes=[lw])
        P.op("dve", lambda e: e.reduce_sum(lw[:, 1:2], lp[:, 128:192], axis=AX.X), reads=[lp], writes=[lw])
        P.op("act", lambda e: e.activation(lw[:, 2:4], lw[:, 0:2], AF.Exp), reads=[lw], writes=[lw])
        P.op("dve", lambda e: e.scalar_tensor_tensor(out=lw[:, 4:5], in0=lw[:, 3:4], scalar=-lam_init, in1=lw[:, 2:3], op0=ALU.add, op1=ALU.subtract),
             reads=[lw], writes=[lw])
        pc = C.ps[7]
        P.mm(pc, pc[:, 0:1], ones1[:], lw[:, 4:5], reads=[ones1, lw])
        P.op("dve", lambda e: e.tensor_copy(nlam[:], pc[:, 0:1]), reads=[pc], writes=[nlam])
        P.op("dve", lambda e: e.tensor_single_scalar(sg[:], sg[:], 1.0 - lam_init, ALU.mult), reads=[sg], writes=[sg])

        it = 0
        for h in range(8):
            slope = float(2.0 ** (-(h + 1)))
            kh, vh = Kh[h % 2], Vh[h % 2]
            P.load(kh, kh[:], S["kn"][h * 128:(h + 1) * 128, :])
            P.load(vh, vh[:], S["v"].rearrange("(kb p) f -> p kb f", p=128)[:, :, h * 128:(h + 1) * 128])
            P.op("act", lambda e, slope=slope: e.activation(MT[:], RAW[:], AF.Exp, scale=-slope), reads=[RAW], writes=[MT])
            P.op("dve", lambda e, slope=slope: e.tensor_single_scalar(BT[:], BRAW[:], slope, ALU.mult), reads=[BRAW], writes=[BT])
            for qi in range(T // NQ):
                q0 = qi * NQ
                q = Q[it % 2]
                o_sb = ob[it % 2]
                it += 1
                P.load(q, q[:], S["qn"][h * 128:(h + 1) * 128, q0:q0 + NQ])
                pO = [C.ps[4], C.ps[5]]
                pL = [C.ps[6], C.ps[7]]
                kbs = []
                for kb in range(NKB):
                    j0 = kb * 128
                    dmin = max(0, q0 - (j0 + 127), j0 - (q0 + NQ - 1))
                    if slope * dmin >= SKIP:
                        continue
                    kbs.append(kb)

                def stA(i, kh=kh, q=q, q0=q0):
                    kb = kbs[i]
                    ks = slice(kb * 128, (kb + 1) * 128)
                    j0 = kb * 128
                    if j0 + 128 <= q0:
                        m = (q0 - j0) // 128
                        bias, mt = BT[:, m:m + 1], MT[:, 0, :]
                    elif j0 >= q0 + NQ:
                        m = (j0 - q0) // 128
                        bias, mt = BT[:, 64 + m:64 + m + 1], MT[:, 1, :]
                    else:
                        kk = (j0 - q0) // 128
                        bias, mt = None, MT[:, 2 + kk, :]
                    for j in range(2):
                        pS = C.ps[(i % 2) * 2 + j]
                        e_t = E[(i % 2) * 2 + j]
                        pt = Pt[(i % 3) * 2 + j]
                        js = slice(j * 64, (j + 1) * 64)
                        P.mm(pS, pS[:, :NQ], kh[js, ks], q[js, :], reads=[kh, q])
                        if bias is None:
                            P.op("act", lambda e, e_t=e_t, pS=pS: e.activation(e_t[:], pS[:, :NQ], AF.Exp, scale=scale), reads=[pS], writes=[e_t])
                        else:
                            P.op("act", lambda e, e_t=e_t, pS=pS, bias=bias: e.activation(e_t[:], pS[:, :NQ], AF.Exp, scale=scale, bias=bias),
                                 reads=[pS, BT], writes=[e_t])
                        eng = "dve" if j == 0 else "pool"
                        P.op(eng, lambda e, pt=pt, e_t=e_t, mt=mt: e.tensor_tensor(out=pt[:], in0=e_t[:], in1=mt, op=ALU.mult),
                             reads=[e_t, MT], writes=[pt])

                def stB(i, vh=vh, pO=pO, pL=pL):
                    kb = kbs[i]
                    for j in range(2):
                        pt = Pt[(i % 3) * 2 + j]
                        P.mm(pO[j], pO[j][:, :NQ], vh[:, kb, :], pt[:], reads=[vh, pt], start=(i == 0), stop=(i == len(kbs) - 1))
                        P.mm(pL[j], pL[j][:, :NQ], C.ones_bf[:], pt[:], reads=[C.ones_bf, pt], start=(i == 0), stop=(i == len(kbs) - 1))

                stA(0)
                if len(kbs) > 1:
                    stA(1)
                for i in range(len(kbs)):
                    if i + 2 < len(kbs):
                        stA(i + 2)
                    stB(i)
                P.op("dve", lambda e, pL=pL: e.reciprocal(r1[:], pL[0][:, :NQ]), reads=[pL[0]], writes=[r1])
                P.op("dve", lambda e, pL=pL: e.reciprocal(r2[:], pL[1][:, :NQ]), reads=[pL[1]], writes=[r2])
                P.op("dve", lambda e, pO=pO: e.tensor_tensor(out=a1[:], in0=pO[0][:, :NQ], in1=r1[:], op=ALU.mult), reads=[pO[0], r1], writes=[a1])
                P.op("dve", lambda e, pO=pO: e.tensor_tensor(out=a2[:], in0=pO[1][:, :NQ], in1=r2[:], op=ALU.mult), reads=[pO[1], r2], writes=[a2])
                P.op("dve", lambda e: e.scalar_tensor_tensor(out=a1[:], in0=a2[:], scalar=nlam[:, 0:1], in1=a1[:], op0=ALU.mult, op1=ALU.add),
                     reads=[a1, a2, nlam], writes=[a1])
                psS = C.ps[0]
                P.op("act", lambda e: e.activation(sqd[:], a1[:], AF.Square), reads=[a1], writes=[sqd])
                P.mm(psS, psS[:, :NQ], C.ones_bf[:], sqd[:], reads=[C.ones_bf, sqd])
                rstd_from_sumsq(C, psS, 128, NQ, r1, r2)
                P.op("dve", lambda e, o_sb=o_sb: e.scalar_tensor_tensor(out=o_sb[:], in0=a1[:], scalar=sg[:, 0:1], in1=r2[:], op0=ALU.mult, op1=ALU.mult),
                     reads=[a1, sg, r2], writes=[o_sb])
                P.store(o_sb, S["ao"][h * 128:(h + 1) * 128, q0:q0 + NQ], o_sb[:])
        P.barrier()
        P.release(loc)


def host_diff_params(inp):
    return dict(diff_w_qkv=np.ascontiguousarray(inp["diff_w_qkv"][0]), diff_w_o=np.ascontiguousarray(inp["diff_w_o"][0]),
                diff_lambda=np.ascontiguousarray(inp["diff_lambda"][0].reshape(1, 256)),
                diff_subln_g=np.ascontiguousarray(inp["diff_subln_g"][0].reshape(128, 1)))


def make_tri(C, es):
    P = C.P
    R = Ctx()
    R.M01F = P.sb("M01F", [128, 128], F32, es)
    R.M01B = P.sb("M01B", [128, 128], F32, es)
    R.ones_f = P.sb("ones_f", [128, 128], F32, es)
    P.op("dve", lambda e: e.memset(R.ones_f[:], 1.0), writes=[R.ones_f])
    P.op("pool", lambda e: e.iota(R.M01F[:], [[1, 128]], base=0, channel_multiplier=-1, allow_small_or_imprecise_dtypes=True), writes=[R.M01F])
    P.op("pool", lambda e: e.iota(R.M01B[:], [[-1, 128]], base=0, channel_multiplier=1, allow_small_or_imprecise_dtypes=True), writes=[R.M01B])
    for m in (R.M01F, R.M01B):
        P.op("dve", lambda e, m=m: e.tensor_scalar(m[:], m[:], 1.0, 0.0, ALU.add, ALU.max), reads=[m], writes=[m])
        P.op("dve", lambda e, m=m: e.tensor_single_scalar(m[:], m[:], 1.0, ALU.min), reads=[m], writes=[m])
    R.all = [R.M01F, R.M01B, R.ones_f]
    return R


def phase_mlstm_proj(C, li, hin, W, S, Gtok, NTK=512):
    P, T = C.P, C.T
    hiv = hview(hin)
    sc = float(64 ** -0.5)
    with ExitStack() as es:
        Win = load_w(C, es, "mWin", W["mlstm_w_in"])
        g0 = load_small(C, es, "g0", W["norm_g"][li * 4 + 0])
        bg = load_small(C, es, "mbg", W["mlstm_bg"])
        B = NormBufs(C, es, NTK, "l")
        q_st = P.sb("lq_st", [64, 8, NTK], BF16, es)
        k_st = P.sb("lk_st", [64, 8, NTK], BF16, es)
        o_st = P.sb("lo_st", [128, 8, NTK], BF16, es)
        kt_st = P.sb("lkt_st", [128, NTK // 128, 512], BF16, es)
        v_st = P.sb("lv_st", [128, NTK // 128, 8, 129], BF16, es)
        loc = [Win, g0, bg, q_st, k_st, o_st, kt_st, v_st] + B.all
        P.op("dve", lambda e: e.memset(v_st[:], 1.0), writes=[v_st])
        psS = C.ps[6]
        qv = S["mq"].rearrange("(h p) t -> p h t", p=64)
        kv = S["mk"].rearrange("(h p) t -> p h t", p=64)
        ov = S["og"].rearrange("(h p) t -> p h t", p=128)
        ktv = S["mkt"].rearrange("(tb p) f -> p tb f", p=128)
        vv = S["mv"].rearrange("(tb p) h e -> p tb h e", p=128)
        k = 0
        for ti in range(T // NTK):
            t0 = ti * NTK
            norm_in(C, hiv, PAD + t0, NTK, g0, B, psS)
            for h in range(8):
                pb = C.ps[k % 4]; k += 1
                for c in range(8):
                    P.mm(pb, pb[0:64, :NTK], Win[:, c, h * 64:(h + 1) * 64], B.xn[:, c, :], reads=[Win, B.xn], start=(c == 0), stop=(c == 7))
                P.op("act", lambda e, h=h, pb=pb: e.copy(q_st[:, h, :], pb[0:64, :NTK]), reads=[pb], writes=[q_st])
                pb = C.ps[k % 4]; k += 1
                for c in range(8):
                    P.mm(pb, pb[0:64, :NTK], Win[:, c, 512 + h * 64:512 + (h + 1) * 64], B.xn[:, c, :], reads=[Win, B.xn], start=(c == 0), stop=(c == 7))
                P.op("dve", lambda e, h=h, pb=pb: e.tensor_single_scalar(k_st[:, h, :], pb[0:64, :NTK], sc, ALU.mult), reads=[pb], writes=[k_st])
            P.store(q_st, qv[:, :, t0:t0 + NTK], q_st[:])
            P.store(k_st, kv[:, :, t0:t0 + NTK], k_st[:])
            for h in range(8):
                pb = C.ps[k % 4]; k += 1
                for c in range(8):
                    P.mm(pb, pb[:, :NTK], Win[:, c, 2048 + h * 128:2048 + (h + 1) * 128], B.xn[:, c, :], reads=[Win, B.xn], start=(c == 0), stop=(c == 7))
                P.op("act", lambda e, h=h, pb=pb: e.activation(o_st[:, h, :], pb[:, :NTK], AF.Sigmoid), reads=[pb], writes=[o_st])
            P.store(o_st, ov[:, :, t0:t0 + NTK], o_st[:])
            for tb in range(NTK // 128):
                ts_ = slice(tb * 128, (tb + 1) * 128)
                ch = t0 // 128 + tb
                pb = C.ps[k % 4]; k += 1
                for c in range(8):
                    P.mm(pb, pb[:, :512], B.xn[:, c, ts_], Win[:, c, 512:1024], reads=[Win, B.xn], start=(c == 0), stop=(c == 7))
                P.op("dve", lambda e, tb=tb, pb=pb: e.tensor_single_scalar(kt_st[:, tb, :], pb[:, :512], sc, ALU.mult), reads=[pb], writes=[kt_st])
                for half in range(2):
                    pb = C.ps[k % 4]; k += 1
                    for c in range(8):
                        P.mm(pb, pb[:, :512], B.xn[:, c, ts_], Win[:, c, 1024 + half * 512:1024 + (half + 1) * 512], reads=[Win, B.xn], start=(c == 0), stop=(c == 7))
                    P.op("act", lambda e, tb=tb, half=half, pb=pb: e.copy(v_st[:, tb, half * 4:(half + 1) * 4, 0:128],
                                                                         pb[:, :512].rearrange("p (h e) -> p h e", e=128)), reads=[pb], writes=[v_st])
                pb = C.ps[k % 4]; k += 1
                for c in range(8):
                    P.mm(pb, pb[:, :32], B.xn[:, c, ts_], Win[:, c, 3072:3104], reads=[Win, B.xn], start=(c == 0), stop=(c == 7))
                P.op("dve", lambda e, ch=ch, pb=pb: e.tensor_tensor(out=Gtok[:, ch, :], in0=pb[:, :32], in1=bg[:], op=ALU.add), reads=[pb, bg], writes=[Gtok])
            P.store(kt_st, ktv[:, t0 // 128:(t0 + NTK) // 128, :], kt_st[:])
            P.store(v_st, vv[:, t0 // 128:(t0 + NTK) // 128, :, :], v_st[:])
        P.barrier()
        P.release(loc)


def phase_mlstm_gates(C, R, Gtok, GS, es):
    P, T = C.P, C.T
    NCH = T // 128
    t8 = [P.sb("g8_%d" % i, [128, 8], F32, es) for i in range(4)]
    bcc = P.sb("gbcc", [128, 16], F32, es)
    rhsD = [P.sb("grhsD%d" % i, [128, 4, 128], F32, es) for i in range(2)]
    tmpD = [P.sb("gtmpD%d" % i, [128, 4, 128], F32, es) for i in range(2)]
    Mneg = [P.sb("gMneg%d" % i, [128, 4, 128], F32, es) for i in range(2)]
    Sel = [P.sb("gSel%d" % i, [128, 128], F32, es) for i in range(2)]
    one_t = P.sb("gone", [128, 1], F32, es)
    mcur = P.sb("gmcur", [128, 8], F32, es)
    loc = t8 + [bcc, one_t, mcur] + rhsD + tmpD + Mneg + Sel
    P.op("dve", lambda e: e.memset(one_t[:], 1.0), writes=[one_t])
    for d, msrc in ((0, R.M01B), (1, R.M01F)):
        for j in range(4):
            P.op("dve", lambda e, d=d, j=j, msrc=msrc: e.tensor_scalar(Mneg[d][:, j, :], msrc[:], -1.0, 1e30, ALU.add, ALU.mult),
                 reads=[msrc], writes=[Mneg[d]])
    P.op("dve", lambda e: e.tensor_scalar(Sel[0][:], R.ones_f[:], R.M01B[:, 127:128], None, ALU.mult), reads=[R.ones_f, R.M01B], writes=[Sel[0]])
    P.op("dve", lambda e: e.tensor_scalar(Sel[1][:], R.ones_f[:], R.M01F[:, 0:1], None, ALU.mult), reads=[R.ones_f, R.M01F], writes=[Sel[1]])
    tri = [R.M01F, R.M01B]
    k = 0
    for c in range(NCH):
        for d in range(2):
            G = GS[d]
            fs = slice(8 + 16 * d, 16 + 16 * d)
            is_ = slice(16 * d, 16 * d + 8)
            nl = t8[0]
            P.op("act", lambda e, c=c, fs=fs: e.activation(nl[:], Gtok[:, c, fs], AF.Exp, scale=-1.0), reads=[Gtok], writes=[nl])
            P.op("act", lambda e: e.activation(nl[:], nl[:], AF.Ln, bias=one_t[:, 0:1]), reads=[nl, one_t], writes=[nl])
            pb = C.ps[k % 4]; k += 1
            P.mm(pb, pb[:, 0:8], tri[d][:], nl[:], reads=[tri[d], nl])
            P.op("dve", lambda e, c=c, pb=pb, G=G: e.tensor_single_scalar(G["BC"][:, c, :], pb[:, 0:8], -1.0, ALU.mult), reads=[pb], writes=[G["BC"]])
            P.op("dve", lambda e, c=c, pb=pb, G=G, is_=is_: e.tensor_tensor(out=G["A"][:, c, :], in0=pb[:, 0:8], in1=Gtok[:, c, is_], op=ALU.add),
                 reads=[pb, Gtok], writes=[G["A"]])
            for g in range(2):
                rd, td = rhsD[g], tmpD[g]
                for j in range(4):
                    if j == 3:
                        P.op("dve", lambda e, c=c, g=g, j=j, rd=rd, G=G: e.tensor_scalar(rd[:, j, :], C.ident[:], G["A"][:, c, 4 * g + j:4 * g + j + 1], None, ALU.mult),
                             reads=[C.ident, G["A"]], writes=[rd])
                    elif j % 2 == 0:
                        P.op("act", lambda e, c=c, g=g, j=j, rd=rd, G=G: e.activation(rd[:, j, :], C.ident[:], AF.Identity, scale=G["A"][:, c, 4 * g + j:4 * g + j + 1]),
                             reads=[C.ident, G["A"]], writes=[rd])
                    else:
                        P.op("pool", lambda e, c=c, g=g, j=j, rd=rd, G=G: e.tensor_scalar(rd[:, j, :], C.ident[:], G["A"][:, c, 4 * g + j:4 * g + j + 1], None, ALU.mult),
                             reads=[C.ident, G["A"]], writes=[rd])
                pa = C.ps[4 + k % 2]; k += 1
                P.mm(pa, pa[:, :512], R.ones_f[:], rd[:].rearrange("p j s -> p (j s)"), reads=[R.ones_f, rd])
                P.op("dve", lambda e, td=td, pa=pa, d=d: e.tensor_tensor(out=td[:].rearrange("p j s -> p (j s)"), in0=pa[:, :512],
                                                                         in1=Mneg[d][:].rearrange("p j s -> p (j s)"), op=ALU.add),
                     reads=[pa, Mneg[d]], writes=[td])
                P.op("dve", lambda e, td=td, c=c, g=g, G=G: e.reduce_max(G["CM"][:, c, 4 * g:4 * g + 4], td[:], axis=AX.X), reads=[td], writes=[G["CM"]])
            P.op("dve", lambda e, c=c, G=G: e.tensor_copy(bcc[:, 0:8], G["BC"][:, c, :]), reads=[G["BC"]], writes=[bcc])
            P.op("dve", lambda e, c=c, G=G: e.tensor_copy(bcc[:, 8:16], G["CM"][:, c, :]), reads=[G["CM"]], writes=[bcc])
            pb = C.ps[k % 4]; k += 1
            P.mm(pb, pb[:, 0:16], Sel[d][:], bcc[:], reads=[Sel[d], bcc])
            P.op("act", lambda e, c=c, pb=pb, G=G: e.copy(G["BL"][:, c, :], pb[:, 0:8]), reads=[pb], writes=[G["BL"]])
            P.op("act", lambda e, c=c, pb=pb, G=G: e.copy(G["CML"][:, c, :], pb[:, 8:16]), reads=[pb], writes=[G["CML"]])
    for d in range(2):
        G = GS[d]
        P.op("dve", lambda e: e.memset(mcur[:], 0.0), writes=[mcur])
        order = range(NCH) if d == 0 else range(NCH - 1, -1, -1)
        for c in order:
            P.op("dve", lambda e, c=c, G=G: e.tensor_copy(G["M"][:, c, :], mcur[:]), reads=[mcur], writes=[G["M"]])
            P.op("dve", lambda e, c=c, G=G: e.tensor_tensor(out=mcur[:], in0=mcur[:], in1=G["CML"][:, c, :], op=ALU.max), reads=[mcur, G["CML"]], writes=[mcur])
            P.op("dve", lambda e, c=c, G=G: e.tensor_tensor(out=mcur[:], in0=mcur[:], in1=G["BL"][:, c, :], op=ALU.add), reads=[mcur, G["BL"]], writes=[mcur])
            P.op("dve", lambda e, c=c, G=G: e.tensor_copy(G["MN"][:, c, :], mcur[:]), reads=[mcur], writes=[G["MN"]])
        def fl(nm):
            return G[nm][:].rearrange("p c h -> p (c h)")
        for nm in ("MX", "EU", "WI", "NM", "WS", "DEC", "EA"):
            pass
        P.op("dve", lambda e, G=G: e.tensor_tensor(out=G["MX"][:], in0=G["CM"][:], in1=G["M"][:], op=ALU.max), reads=[G["CM"], G["M"]], writes=[G["MX"]])
        P.op("act", lambda e, G=G: e.activation(G["EU"][:], G["MX"][:], AF.Exp, scale=-1.0), reads=[G["MX"]], writes=[G["EU"]])
        P.op("dve", lambda e, G=G: e.tensor_tensor(out=G["WI"][:], in0=G["M"][:], in1=G["MX"][:], op=ALU.subtract), reads=[G["M"], G["MX"]], writes=[G["WI"]])
        P.op("act", lambda e, G=G: e.activation(G["WI"][:], G["WI"][:], AF.Exp), reads=[G["WI"]], writes=[G["WI"]])
        P.op("dve", lambda e, G=G: e.tensor_tensor(out=G["NM"][:], in0=G["BC"][:], in1=G["MX"][:], op=ALU.add), reads=[G["BC"], G["MX"]], writes=[G["NM"]])
        P.op("act", lambda e, G=G: e.activation(G["NM"][:], G["NM"][:], AF.Exp, scale=-1.0), reads=[G["NM"]], writes=[G["NM"]])
        P.op("dve", lambda e, G=G: e.tensor_tensor(out=G["WS"][:], in0=G["BL"][:], in1=G["A"][:], op=ALU.add), reads=[G["BL"], G["A"]], writes=[G["WS"]])
        P.op("dve", lambda e, G=G: e.tensor_tensor(out=G["WS"][:], in0=G["WS"][:], in1=G["MN"][:], op=ALU.subtract), reads=[G["WS"], G["MN"]], writes=[G["WS"]])
        P.op("act", lambda e, G=G: e.activation(G["WS"][:], G["WS"][:], AF.Exp), reads=[G["WS"]], writes=[G["WS"]])
        P.op("dve", lambda e, G=G: e.tensor_tensor(out=G["DEC"][:], in0=G["BL"][:], in1=G["M"][:], op=ALU.add), reads=[G["BL"], G["M"]], writes=[G["DEC"]])
        P.op("dve", lambda e, G=G: e.tensor_tensor(out=G["DEC"][:], in0=G["DEC"][:], in1=G["MN"][:], op=ALU.subtract), reads=[G["DEC"], G["MN"]], writes=[G["DEC"]])
        P.op("act", lambda e, G=G: e.activation(G["DEC"][:], G["DEC"][:], AF.Exp), reads=[G["DEC"]], writes=[G["DEC"]])
        P.op("act", lambda e, G=G: e.activation(G["EA"][:], G["A"][:], AF.Exp), reads=[G["A"]], writes=[G["EA"]])
    return loc


def phase_mlstm_core(C, li, hin, W, S):
    P, T = C.P, C.T
    NCH = T // 128
    with ExitStack() as es:
        R = make_tri(C, es)
        Gtok = P.sb("Gtok", [128, NCH, 32], F32, es)
        phase_mlstm_proj(C, li, hin, W, S, Gtok)
        names = ("BC", "A", "CM", "BL", "CML", "M", "MN", "MX", "EU", "WI", "NM", "WS", "DEC", "EA")
        GS = [{nm: P.sb("G%s%d" % (nm, d), [128, NCH, 8], F32, es) for nm in names} for d in range(2)]
        loc = R.all + [Gtok] + [GS[d][nm] for d in range(2) for nm in names]
        with ExitStack() as es2:
            loc2 = phase_mlstm_gates(C, R, Gtok, GS, es2)
            P.barrier()
            P.release(loc2)
        M01b = []
        mask = [R.M01F, R.M01B]
        Qc = [[P.sb("cQ%d%d" % (d, i), [64, 8, 128], BF16, es) for i in range(2)] for d in range(2)]
        Kc = [[P.sb("cK%d%d" % (d, i), [64, 8, 128], BF16, es) for i in range(2)] for d in range(2)]
        Ktc = [[P.sb("cKt%d%d" % (d, i), [128, 512], BF16, es) for i in range(2)] for d in range(2)]
        Vc = [[P.sb("cV%d%d" % (d, i), [128, 8, 129], BF16, es) for i in range(2)] for d in range(2)]
        Cf = [P.sb("cCf%d" % d, [64, 8, 129], F32, es) for d in range(2)]
        Cb = [P.sb("cCb%d" % d, [64, 8, 129], BF16, es) for d in range(2)]
        Hacc = [[P.sb("cH%d%d" % (d, i), [128, 1024], F32, es) for i in range(2)] for d in range(2)]
        sqk = [P.sb("csqk%d" % i, [128, 128], BF16, es) for i in range(3)]
        t1 = [P.sb("ct1%d" % i, [128, 129], F32, es) for i in range(3)]
        tot = [P.sb("ctot%d" % i, [128, 129], F32, es) for i in range(3)]
        kw = [P.sb("ckw%d" % i, [128, 64], BF16, es) for i in range(3)]
        dd = [P.sb("cdd%d" % i, [128, 2], F32, es) for i in range(3)]
        loc += M01b + sum(Qc, []) + sum(Kc, []) + sum(Ktc, []) + sum(Vc, []) + Cf + Cb + sum(Hacc, []) + sqk + t1 + tot + kw + dd
        for d in range(2):
            P.op("dve", lambda e, d=d: e.memset(Cf[d][:], 0.0), writes=[Cf[d]])
            P.op("dve", lambda e, d=d: e.memset(Cb[d][:], 0.0), writes=[Cb[d]])
        qv = S["mq"].rearrange("(h p) t -> p h t", p=64)
        kv = S["mk"].rearrange("(h p) t -> p h t", p=64)
        hdst = [S["hf"], S["hb"]]
        iters = [(step, d, h) for step in range(NCH) for d in range(2) for h in range(8)]

        def ctx_of(n):
            step, d, h = iters[n]
            c = step if d == 0 else NCH - 1 - step
            return step, d, h, c, slice(c * 128, (c + 1) * 128), GS[d]

        def stA(n):
            step, d, h, c, cs, G = ctx_of(n)
            q, kk_, kt, v = Qc[d][step % 2], Kc[d][step % 2], Ktc[d][step % 2], Vc[d][step % 2]
            if h == 0:
                P.load(q, q[:], qv[:, :, cs])
                P.load(kk_, kk_[:], kv[:, :, cs])
                P.load(kt, kt[:], S["mkt"][cs, :])
                P.load(v, v[:], S["mv"][cs, :, :])
            i3 = n % 3
            pS, pI, pX, pC = C.ps[n % 2], C.ps[2 + n % 2], C.ps[4 + n % 2], C.ps[6 + n % 2]
            col = lambda nm: G[nm][:, c, h:h + 1]
            P.mm(pS, pS[:, :128], kk_[:, h, :], q[:, h, :], reads=[kk_, q])
            P.op("dve", lambda e, ea=col("EA"): e.scalar_tensor_tensor(out=sqk[i3][:], in0=pS[:, :128], scalar=ea, in1=mask[d][:],
                                                                     op0=ALU.mult, op1=ALU.mult),
                 reads=[pS, G["EA"], mask[d]], writes=[sqk[i3]])
            P.mm(pI, pI[:, :129], sqk[i3][:], v[:, h, :], reads=[sqk[i3], v])
            P.mm(pX, pX[:, :129], q[:, h, :], Cb[d][:, h, :], reads=[q, Cb[d]])
            P.op("pool", lambda e, ws=col("WS"): e.tensor_scalar(kw[i3][:], kt[:, h * 64:(h + 1) * 64], ws, None, ALU.mult),
                 reads=[kt, G["WS"]], writes=[kw[i3]])
            P.mm(pC, pC[0:64, :129], kw[i3][:], v[:, h, :], reads=[kw[i3], v])

        def stB(n):
            step, d, h, c, cs, G = ctx_of(n)
            hacc = Hacc[d][step % 2]
            i3 = n % 3
            pS, pI, pX, pC = C.ps[n % 2], C.ps[2 + n % 2], C.ps[4 + n % 2], C.ps[6 + n % 2]
            col = lambda nm: G[nm][:, c, h:h + 1]
            P.op("act", lambda e, eu=col("EU"): e.activation(t1[i3][:], pI[:, :129], AF.Identity, scale=eu), reads=[pI, G["EU"]], writes=[t1[i3]])
            P.op("dve", lambda e, wi=col("WI"): e.scalar_tensor_tensor(out=tot[i3][:], in0=pX[:, :129], scalar=wi, in1=t1[i3][:],
                                                                     op0=ALU.mult, op1=ALU.add),
                 reads=[pX, G["WI"], t1[i3]], writes=[tot[i3]])
            P.op("dve", lambda e: e.scalar_tensor_tensor(out=dd[i3][:, 0:1], in0=tot[i3][:, 128:129], scalar=-1.0, in1=tot[i3][:, 128:129],
                                                        op0=ALU.mult, op1=ALU.max), reads=[tot[i3]], writes=[dd[i3]])
            P.op("dve", lambda e, nmc=col("NM"): e.tensor_tensor(out=dd[i3][:, 0:1], in0=dd[i3][:, 0:1], in1=nmc, op=ALU.max),
                 reads=[dd[i3], G["NM"]], writes=[dd[i3]])
            P.op("dve", lambda e: e.reciprocal(dd[i3][:, 1:2], dd[i3][:, 0:1]), reads=[dd[i3]], writes=[dd[i3]])
            P.op("act", lambda e: e.activation(hacc[:, h * 128:(h + 1) * 128], tot[i3][:, 0:128], AF.Identity, scale=dd[i3][:, 1:2]),
                 reads=[tot[i3], dd[i3]], writes=[hacc])
            P.op("dve", lambda e, dec=G["DEC"][0:64, c, h:h + 1]: e.scalar_tensor_tensor(
                out=Cf[d][:, h, :], in0=Cf[d][:, h, :], scalar=dec, in1=pC[0:64, :129], op0=ALU.mult, op1=ALU.add),
                reads=[Cf[d], G["DEC"], pC], writes=[Cf[d]])
            P.op("act", lambda e: e.copy(Cb[d][:, h, :], Cf[d][:, h, :]), reads=[Cf[d]], writes=[Cb[d]])
            if h == 7:
                P.store(hacc, hdst[d][cs, :], hacc[:])

        stA(0)
        for n in range(len(iters)):
            if n + 1 < len(iters):
                stA(n + 1)
            stB(n)
        P.barrier()
        P.release(loc)


def phase_gn_post(C, S, F, dv, g_ap):
    P, T = C.P, C.T
    NH = F // dv
    NFB_ = F // 128
    with ExitStack() as es:
        gt = P.sb("pg", [128, F], F32, es)
        P.load(gt, gt[:], g_ap)
        A = [P.sb("pA%d" % i, [128, F], F32, es) for i in range(2)]
        Bt = [P.sb("pB%d" % i, [128, F], F32, es) for i in range(2)]
        junks = [P.sb("pjunk%d" % i, [128, dv], F32, es) for i in range(2)]
        sts = [P.sb("pst%d" % i, [128, 4, NH], F32, es) for i in range(2)]
        gate = [P.sb("pgate%d" % i, [128, NFB_, 128], BF16, es) for i in range(2)]
        ao = [P.sb("pao%d" % i, [128, NFB_, 128], BF16, es) for i in range(2)]
        eps_g = P.sb("peps", [128, 1], F32, es)
        loc = [gt, eps_g] + junks + sts + A + Bt + gate + ao
        P.op("dve", lambda e: e.memset(eps_g[:], EPS), writes=[eps_g])
        gv = S["og"].rearrange("(c p) t -> p c t", p=128)
        aov = S["ao2"].rearrange("(c p) t -> p c t", p=128)
        k = 0
        for c in range(T // 128):
            cs = slice(c * 128, (c + 1) * 128)
            a, b, gtile, aot = A[c % 2], Bt[c % 2], gate[c % 2], ao[c % 2]
            st, junk = sts[c % 2], junks[c % 2]
            P.load(a, a[:], S["hf"][cs, :])
            P.load(b, b[:], S["hb"][cs, :])
            P.load(gtile, gtile[:], gv[:, :, cs])
            P.op("pool", lambda e, a=a, b=b: e.tensor_tensor(out=a[:], in0=a[:], in1=b[:], op=ALU.add), reads=[a, b], writes=[a])
            for h in range(NH):
                hs = slice(h * dv, (h + 1) * dv)
                P.op("act", lambda e, a=a, hs=hs, h=h, st=st, junk=junk: e.activation(junk[:], a[:, hs], AF.Identity, accum_out=st[:, 0, h:h + 1]), reads=[a], writes=[junk, st])
                P.op("act", lambda e, a=a, hs=hs, h=h, st=st, junk=junk: e.activation(junk[:], a[:, hs], AF.Square, accum_out=st[:, 1, h:h + 1]), reads=[a], writes=[junk, st])
            P.op("dve", lambda e, st=st: e.tensor_single_scalar(st[:, 0, :], st[:, 0, :], 1.0 / dv, ALU.mult), reads=[st], writes=[st])
            P.op("dve", lambda e, st=st: e.tensor_tensor(out=st[:, 2, :], in0=st[:, 0, :], in1=st[:, 0, :], op=ALU.mult), reads=[st], writes=[st])
            P.op("dve", lambda e, st=st: e.scalar_tensor_tensor(out=st[:, 1, :], in0=st[:, 1, :], scalar=1.0 / dv, in1=st[:, 2, :], op0=ALU.mult, op1=ALU.subtract),
                 reads=[st], writes=[st])
            P.op("act", lambda e, st=st: e.activation(st[:, 1, :], st[:, 1, :], AF.Sqrt, bias=eps_g[:, 0:1]), reads=[st, eps_g], writes=[st])
            P.op("dve", lambda e, st=st: e.reciprocal(st[:, 1, :], st[:, 1, :]), reads=[st], writes=[st])
            P.op("dve", lambda e, st=st: e.scalar_tensor_tensor(out=st[:, 3, :], in0=st[:, 0, :], scalar=-1.0, in1=st[:, 1, :], op0=ALU.mult, op1=ALU.mult),
                 reads=[st], writes=[st])
            for h in range(NH):
                hs = slice(h * dv, (h + 1) * dv)
                P.op("act", lambda e, a=a, hs=hs, h=h, st=st, junk=junk: e.activation(a[:, hs], a[:, hs], AF.Identity, scale=st[:, 1, h:h + 1], bias=st[:, 3, h:h + 1]),
                     reads=[a, st], writes=[a])
            P.op("dve", lambda e, a=a: e.tensor_tensor(out=a[:], in0=a[:], in1=gt[:], op=ALU.mult), reads=[a, gt], writes=[a])
            for fb in range(NFB_):
                pb = C.ps[k % 4]; k += 1
                P.tr(pb, pb[:, 0:128], a[:, fb * 128:(fb + 1) * 128], C.ident[:], reads=[a, C.ident])
                P.op("dve", lambda e, fb=fb, pb=pb, aot=aot, gtile=gtile: e.tensor_tensor(out=aot[:, fb, :], in0=pb[:, 0:128], in1=gtile[:, fb, :], op=ALU.mult),
                     reads=[pb, gtile], writes=[aot])
            P.store(aot, aov[:, :, cs], aot[:])
        P.barrier()
        P.release(loc)


def host_mlstm_params(inp):
    return dict(mlstm_w_in=np.ascontiguousarray(inp["mlstm_w_in"][0]), mlstm_w_out=np.ascontiguousarray(inp["mlstm_w_out"][0]),
                mlstm_bg=np.ascontiguousarray(np.broadcast_to(inp["mlstm_b_gates"][0][None, :], (128, 32))),
                mlstm_ng=np.ascontiguousarray(np.broadcast_to(inp["mlstm_norm_g"][0][None, :], (128, 1024))))


def phase_ret_proj(C, li, hin, W, S, NTK=512):
    P, T = C.P, C.T
    hiv = hview(hin)
    sc = float(256 ** -0.5)
    with ExitStack() as es:
        Win = load_w(C, es, "rWin", W["ret_w_in"])
        g0 = load_small(C, es, "g0", W["norm_g"][li * 4 + 0])
        B = NormBufs(C, es, NTK, "r")
        q_st = P.sb("rq_st", [128, 8, NTK], BF16, es)
        k_st = P.sb("rk_st", [128, 8, NTK], BF16, es)
        g_st = P.sb("rg_st", [128, 16, NTK], BF16, es)
        kt_st = P.sb("rkt_st", [128, NTK // 128, 1024], BF16, es)
        v_st = P.sb("rv_st", [128, NTK // 128, 2048], BF16, es)
        loc = [Win, g0, q_st, k_st, g_st, kt_st, v_st] + B.all
        psS = C.ps[6]
        qv = S["rq"].rearrange("(c p) t -> p c t", p=128)
        kv = S["rk"].rearrange("(c p) t -> p c t", p=128)
        gv = S["og"].rearrange("(c p) t -> p c t", p=128)
        ktv = S["rkt"].rearrange("(tb p) f -> p tb f", p=128)
        vv = S["rv"].rearrange("(tb p) f -> p tb f", p=128)
        k = 0
        for ti in range(T // NTK):
            t0 = ti * NTK
            norm_in(C, hiv, PAD + t0, NTK, g0, B, psS)
            for fb in range(8):
                pb = C.ps[k % 4]; k += 1
                for c in range(8):
                    P.mm(pb, pb[:, :NTK], Win[:, c, fb * 128:(fb + 1) * 128], B.xn[:, c, :], reads=[Win, B.xn], start=(c == 0), stop=(c == 7))
                P.op("act", lambda e, fb=fb, pb=pb: e.copy(q_st[:, fb, :], pb[:, :NTK]), reads=[pb], writes=[q_st])
                pb = C.ps[k % 4]; k += 1
                for c in range(8):
                    P.mm(pb, pb[:, :NTK], Win[:, c, 1024 + fb * 128:1024 + (fb + 1) * 128], B.xn[:, c, :], reads=[Win, B.xn], start=(c == 0), stop=(c == 7))
                P.op("dve", lambda e, fb=fb, pb=pb: e.tensor_single_scalar(k_st[:, fb, :], pb[:, :NTK], sc, ALU.mult), reads=[pb], writes=[k_st])
            P.store(q_st, qv[:, :, t0:t0 + NTK], q_st[:])
            P.store(k_st, kv[:, :, t0:t0 + NTK], k_st[:])
            for fb in range(16):
                pb = C.ps[k % 4]; k += 1
                for c in range(8):
                    P.mm(pb, pb[:, :NTK], Win[:, c, 4096 + fb * 128:4096 + (fb + 1) * 128], B.xn[:, c, :], reads=[Win, B.xn], start=(c == 0), stop=(c == 7))
                P.op("act", lambda e, fb=fb, pb=pb: e.activation(g_st[:, fb, :], pb[:, :NTK], AF.Silu), reads=[pb], writes=[g_st])
            P.store(g_st, gv[:, :, t0:t0 + NTK], g_st[:])
            for tb in range(NTK // 128):
                ts_ = slice(tb * 128, (tb + 1) * 128)
                for half in range(2):
                    pb = C.ps[k % 4]; k += 1
                    for c in range(8):
                        P.mm(pb, pb[:, :512], B.xn[:, c, ts_], Win[:, c, 1024 + half * 512:1024 + (half + 1) * 512], reads=[Win, B.xn], start=(c == 0), stop=(c == 7))
                    P.op("dve", lambda e, tb=tb, half=half, pb=pb: e.tensor_single_scalar(kt_st[:, tb, half * 512:(half + 1) * 512], pb[:, :512], sc, ALU.mult),
                         reads=[pb], writes=[kt_st])
                for q4 in range(4):
                    pb = C.ps[k % 4]; k += 1
                    for c in range(8):
                        P.mm(pb, pb[:, :512], B.xn[:, c, ts_], Win[:, c, 2048 + q4 * 512:2048 + (q4 + 1) * 512], reads=[Win, B.xn], start=(c == 0), stop=(c == 7))
                    if q4 % 2 == 0:
                        P.op("act", lambda e, tb=tb, q4=q4, pb=pb: e.copy(v_st[:, tb, q4 * 512:(q4 + 1) * 512], pb[:, :512]), reads=[pb], writes=[v_st])
                    else:
                        P.op("dve", lambda e, tb=tb, q4=q4, pb=pb: e.tensor_copy(v_st[:, tb, q4 * 512:(q4 + 1) * 512], pb[:, :512]), reads=[pb], writes=[v_st])
            P.store(kt_st, ktv[:, t0 // 128:(t0 + NTK) // 128, :], kt_st[:])
            P.store(v_st, vv[:, t0 // 128:(t0 + NTK) // 128, :], v_st[:])
        P.barrier()
        P.release(loc)


def phase_ret_core(C, W, S):
    P, T = C.P, C.T
    NCH = T // 128
    with ExitStack() as es:
        R = make_tri(C, es)
        dl = P.sb("rdl", [1, 8], F32, es)
        ones1 = P.sb("rones1", [1, 128], F32, es)
        one_t = P.sb("rone", [128, 1], F32, es)
        LG = P.sb("rLG", [128, 8], F32, es)
        RAWd = P.sb("rRAWd", [128, 128], F32, es)
        rawc = P.sb("rrawc", [128, 4], F32, es)
        DM = P.sb("rDM", [128, 8, 128], F32, es)
        XI = P.sb("rXI", [128, 8], F32, es)
        ZE = P.sb("rZE", [128, 8], F32, es)
        GL = P.sb("rGL", [128, 8], F32, es)
        Qc = [[P.sb("rQ%d%d" % (d, i), [128, 8, 128], BF16, es) for i in range(2)] for d in range(2)]
        Kc = [[P.sb("rK%d%d" % (d, i), [128, 8, 128], BF16, es) for i in range(2)] for d in range(2)]
        Ktc = [[P.sb("rKt%d%d" % (d, i), [128, 1024], BF16, es) for i in range(2)] for d in range(2)]
        Vc = [[P.sb("rV%d%d" % (d, i), [128, 2048], BF16, es) for i in range(2)] for d in range(2)]
        Rf = [P.sb("rRf%d" % d, [128, 8, 512], F32, es) for d in range(2)]
        Rb = [P.sb("rRb%d" % d, [128, 8, 512], BF16, es) for d in range(2)]
        Hacc = [[P.sb("rH%d%d" % (d, i), [128, 2048], F32, es) for i in range(2)] for d in range(2)]
        sqk = [P.sb("rsqk%d" % i, [128, 128], BF16, es) for i in range(3)]
        t1 = [P.sb("rt1%d" % i, [128, 512], F32, es) for i in range(3)]
        kz = [P.sb("rkz%d" % i, [128, 256], BF16, es) for i in range(3)]
        loc = R.all + [dl, ones1, one_t, LG, RAWd, rawc, DM, XI, ZE, GL] + sum(Qc, []) + sum(Kc, []) + sum(Ktc, []) + sum(Vc, []) + Rf + Rb + sum(Hacc, []) + sqk + t1 + kz
        P.load(dl, dl[:], W["ret_decay"])
        P.op("dve", lambda e: e.memset(ones1[:], 1.0), writes=[ones1])
        P.op("dve", lambda e: e.memset(one_t[:], 1.0), writes=[one_t])
        P.op("act", lambda e: e.activation(dl[:], dl[:], AF.Exp, scale=-1.0), reads=[dl], writes=[dl])
        P.op("act", lambda e: e.activation(dl[:], dl[:], AF.Ln, bias=one_t[0:1, 0:1]), reads=[dl, one_t], writes=[dl])
        pc = C.ps[0]
        P.mm(pc, pc[:, 0:8], ones1[:], dl[:], reads=[ones1, dl])
        P.op("dve", lambda e: e.tensor_single_scalar(LG[:], pc[:, 0:8], -1.0, ALU.mult), reads=[pc], writes=[LG])
        P.op("pool", lambda e: e.iota(RAWd[:], [[1, 128]], base=0, channel_multiplier=-1, allow_small_or_imprecise_dtypes=True), writes=[RAWd])
        P.op("dve", lambda e: e.scalar_tensor_tensor(out=RAWd[:], in0=RAWd[:], scalar=-1.0, in1=RAWd[:], op0=ALU.mult, op1=ALU.max), reads=[RAWd], writes=[RAWd])
        for j, (b0, cm) in enumerate(((1, 1), (128, -1), (127, -1), (0, 1))):
            P.op("pool", lambda e, j=j, b0=b0, cm=cm: e.iota(rawc[:, j:j + 1], [[0, 1]], base=b0, channel_multiplier=cm, allow_small_or_imprecise_dtypes=True), writes=[rawc])
        mask = [R.M01F, R.M01B]
        for d in range(2):
            for h in range(4):
                dh = d * 4 + h
                P.op("act", lambda e, dh=dh: e.activation(DM[:, dh, :], RAWd[:], AF.Exp, scale=LG[:, dh:dh + 1]), reads=[RAWd, LG], writes=[DM])
                P.op("dve", lambda e, dh=dh, d=d: e.tensor_tensor(out=DM[:, dh, :], in0=DM[:, dh, :], in1=mask[d][:], op=ALU.mult), reads=[DM, mask[d]], writes=[DM])
                P.op("act", lambda e, dh=dh, d=d: e.activation(XI[:, dh:dh + 1], rawc[:, d:d + 1], AF.Exp, scale=LG[:, dh:dh + 1]), reads=[rawc, LG], writes=[XI])
                P.op("act", lambda e, dh=dh, d=d: e.activation(ZE[:, dh:dh + 1], rawc[:, 2 + d:3 + d], AF.Exp, scale=LG[:, dh:dh + 1]), reads=[rawc, LG], writes=[ZE])
        P.op("act", lambda e: e.activation(GL[:], LG[:], AF.Exp, scale=128.0), reads=[LG], writes=[GL])
        for d in range(2):
            P.op("dve", lambda e, d=d: e.memset(Rf[d][:], 0.0), writes=[Rf[d]])
            P.op("pool", lambda e, d=d: e.memset(Rb[d][:], 0.0), writes=[Rb[d]])
        qv = S["rq"].rearrange("(c p) t -> p c t", p=128)
        kv = S["rk"].rearrange("(c p) t -> p c t", p=128)
        hdst = [S["hf"], S["hb"]]
        n = 0
        for step in range(NCH):
            for d in range(2):
                c = step if d == 0 else NCH - 1 - step
                cs = slice(c * 128, (c + 1) * 128)
                q, kk_, kt, v = Qc[d][step % 2], Kc[d][step % 2], Ktc[d][step % 2], Vc[d][step % 2]
                hacc = Hacc[d][step % 2]
                P.load(q, q[:], qv[:, :, cs])
                P.load(kk_, kk_[:], kv[:, :, cs])
                P.load(kt, kt[:], S["rkt"][cs, :])
                P.load(v, v[:], S["rv"][cs, :])
                for h in range(4):
                    dh = d * 4 + h
                    i3 = n % 3
                    n += 1
                    pS = C.ps[n % 2]
                    pI = C.ps[2 + n % 2]
                    pX = C.ps[4 + n % 2]
                    vs = v[:, h * 512:(h + 1) * 512]
                    for kc in range(2):
                        P.mm(pS, pS[:, :128], kk_[:, h * 2 + kc, :], q[:, h * 2 + kc, :], reads=[kk_, q], start=(kc == 0), stop=(kc == 1))
                    P.op("dve", lambda e, i3=i3, pS=pS, dh=dh: e.tensor_tensor(out=sqk[i3][:], in0=pS[:, :128], in1=DM[:, dh, :], op=ALU.mult),
                         reads=[pS, DM], writes=[sqk[i3]])
                    P.mm(pI, pI[:, :512], sqk[i3][:], vs, reads=[sqk[i3], v])
                    for kc in range(2):
                        P.mm(pX, pX[:, :512], q[:, h * 2 + kc, :], Rb[d][:, h * 2 + kc, :], reads=[q, Rb[d]], start=(kc == 0), stop=(kc == 1))
                    P.op("act", lambda e, i3=i3, pI=pI: e.copy(t1[i3][:], pI[:, :512]), reads=[pI], writes=[t1[i3]])
                    P.op("dve", lambda e, i3=i3, pX=pX, dh=dh, h=h, hacc=hacc: e.scalar_tensor_tensor(
                        out=hacc[:, h * 512:(h + 1) * 512], in0=pX[:, :512], scalar=XI[:, dh:dh + 1], in1=t1[i3][:], op0=ALU.mult, op1=ALU.add),
                        reads=[pX, XI, t1[i3]], writes=[hacc])
                    P.op("act", lambda e, i3=i3, h=h, kt=kt, dh=dh: e.activation(kz[i3][:], kt[:, h * 256:(h + 1) * 256], AF.Identity, scale=ZE[:, dh:dh + 1]),
                         reads=[kt, ZE], writes=[kz[i3]])
                    for kc in range(2):
                        pC = C.ps[6 + kc]
                        P.mm(pC, pC[:, :512], kz[i3][:, kc * 128:(kc + 1) * 128], vs, reads=[kz[i3], v])
                        P.op("dve", lambda e, d=d, h=h, kc=kc, pC=pC, dh=dh: e.scalar_tensor_tensor(
                            out=Rf[d][:, h * 2 + kc, :], in0=Rf[d][:, h * 2 + kc, :], scalar=GL[:, dh:dh + 1], in1=pC[:, :512], op0=ALU.mult, op1=ALU.add),
                            reads=[Rf[d], GL, pC], writes=[Rf[d]])
                        P.op("act", lambda e, d=d, h=h, kc=kc: e.copy(Rb[d][:, h * 2 + kc, :], Rf[d][:, h * 2 + kc, :]), reads=[Rf[d]], writes=[Rb[d]])
                P.store(hacc, hdst[d][cs, :], hacc[:])
        P.barrier()
        P.release(loc)


def host_ret_params(inp):
    return dict(ret_w_in=np.ascontiguousarray(inp["ret_w_in"][0]), ret_w_o=np.ascontiguousarray(inp["ret_w_o"][0]),
                ret_decay=np.ascontiguousarray(inp["ret_decay_logit"][0].reshape(1, 8)),
                ret_ng=np.ascontiguousarray(np.broadcast_to(inp["ret_norm_g"][0][None, :], (128, 2048))))


SEQ = 8192
NCORES = 4


def build_full(T, hp_shapes, layers=(0, 1, 2, 3)):
    nc = bass.Bass("TRN2", target_bir_lowering=False)
    with ExitStack() as es:
        C = make_ctx(nc, es, T)
        xd = nc.dram_tensor("x", [T, 1024], F32, kind="ExternalInput").ap()
        od = nc.dram_tensor("out", [T, 1024], F32, kind="ExternalOutput").ap()
        W = declare_inputs(nc, hp_shapes)
        ha = nc.dram_tensor("ha", [1024, T + 2 * PAD], F32, kind="Internal").ap()
        hb = nc.dram_tensor("hb", [1024, T + 2 * PAD], F32, kind="Internal").ap()

        def scratch(specs):
            return {nm: nc.dram_tensor("s_" + nm, list(shp), dt, kind="Internal").ap() for nm, shp, dt in specs}

        zero_pads(C, ha)
        zero_pads(C, hb)
        phase_in(C, xd, ha)
        if 0 in layers:
            S = scratch((("qn", (1024, T), BF16), ("kn", (1024, T), BF16), ("qr", (512, T), BF16), ("kr", (64, T), BF16),
                         ("v", (T, 1024), BF16), ("ao", (1024, T), BF16)))
            phase_mla_proj(C, 0, ha, W, S)
            phase_mla_core(C, S)
            phase_tail(C, S["ao"], W["mla_w_o"], W["norm_g"][1], ha, hb)
            phase_ffn(C, 0, hb, ha, W)
        if 1 in layers:
            S = scratch((("dqn", (1024, T), BF16), ("dkn", (1024, T), BF16), ("dv", (T, 1024), BF16), ("dao", (1024, T), BF16)))
            S = dict(qn=S["dqn"], kn=S["dkn"], v=S["dv"], ao=S["dao"])
            phase_diff_proj(C, 1, ha, W, S)
            phase_diff_core(C, 1, W, S)
            phase_tail(C, S["ao"], W["diff_w_o"], W["norm_g"][5], ha, hb)
            phase_ffn(C, 1, hb, ha, W)
        if 2 in layers:
            S = scratch((("mq", (512, T), BF16), ("mk", (512, T), BF16), ("mkt", (T, 512), BF16), ("mv", (T, 8, 129), BF16),
                         ("mog", (1024, T), BF16), ("mhf", (T, 1024), F32), ("mhb", (T, 1024), F32), ("mao2", (1024, T), BF16)))
            S.update(og=S["mog"], hf=S["mhf"], hb=S["mhb"], ao2=S["mao2"])
            phase_mlstm_core(C, 2, ha, W, S)
            phase_gn_post(C, S, 1024, 128, W["mlstm_ng"])
            phase_tail(C, S["ao2"], W["mlstm_w_out"], W["norm_g"][9], ha, hb)
            phase_ffn(C, 2, hb, ha, W)
        if 3 in layers:
            S = scratch((("rq", (1024, T), BF16), ("rk", (1024, T), BF16), ("rkt", (T, 1024), BF16), ("rv", (T, 2048), BF16),
                         ("rog", (2048, T), BF16), ("rhf", (T, 2048), F32), ("rhb", (T, 2048), F32), ("rao2", (2048, T), BF16)))
            S.update(og=S["rog"], hf=S["rhf"], hb=S["rhb"], ao2=S["rao2"])
            phase_ret_proj(C, 3, ha, W, S)
            phase_ret_core(C, W, S)
            phase_gn_post(C, S, 2048, 512, W["ret_ng"])
            phase_tail(C, S["ao2"], W["ret_w_o"], W["norm_g"][13], ha, hb)
            phase_ffn(C, 3, hb, ha, W)
        phase_out(C, ha, od)
        C.P.emit()
    return nc, C


def host_params(inp, T):
    inp = {k: np.asarray(v, dtype=np.float32) for k, v in inp.items() if k != "x"}
    cw, cb, ng = host_ffn_params(inp)
    hp = dict(ffn_w_up=np.ascontiguousarray(inp["ffn_w_up"]), ffn_w_down=np.ascontiguousarray(inp["ffn_w_down"]),
              ffn_cw=cw, ffn_cb=cb, norm_g=ng)
    hp.update(host_mla_params(inp, np.arange(T)))
    hp.update(host_diff_params(inp))
    hp.update(host_mlstm_params(inp))
    hp.update(host_ret_params(inp))
    return hp


REAL_CORES = (0, 1, 4, 5)


def kernel(**inputs):
    x = np.asarray(inputs["x"], dtype=np.float32)
    Bn, T, _ = x.shape
    hp = host_params(inputs, T)
    nc, C = build_full(T, {k: v.shape for k, v in hp.items()})
    ident = np.eye(128, dtype=np.float32)
    zx = np.zeros((T, 1024), np.float32)
    in_maps = []
    real = REAL_CORES[:Bn]
    for c in range(8):
        xb = np.ascontiguousarray(x[real.index(c)]) if c in real else zx
        in_maps.append(dict(x=xb, ident_in=ident, **hp))
    res = run_bass_kernel_spmd(nc, in_maps, core_ids=list(range(8)))
    return np.stack([np.asarray(res.results[c]["out"]) for c in real]).astype(np.float32)
```

```python
import contextlib
import numpy as np
import concourse.bass as bass
import concourse.mybir as mybir

F32 = mybir.dt.float32
BF16 = mybir.dt.bfloat16
AF = mybir.ActivationFunctionType
ALU = mybir.AluOpType
AX = mybir.AxisListType

ENGS = ("pe", "dve", "act", "pool", "sp")


class Res:
    def __init__(self, name, t=None):
        self.name = name
        self.t = t
        self.lw = None
        self.rd = []
        self.sem = None
        self.psum = False

    def __getitem__(self, idx):
        return self.t[idx]


class Sem:
    def __init__(self, h):
        self.h = h
        self.n = 0


class Prog:
    def __init__(self, nc, es):
        self.nc = nc
        self.es = es
        self.ops = []
        self.eng_ops = {e: [] for e in ENGS}
        self.engsem = {e: es.enter_context(nc.semaphore("S_" + e)) for e in ENGS}
        self.res = []
        self.free_sems = []
        self.sems = []

    def sb(self, name, shape, dt, es=None):
        self.uid = getattr(self, "uid", 0) + 1
        name = "%s_u%d" % (name, self.uid)
        t = (es or self.es).enter_context(self.nc.sbuf_tensor(name, list(shape), dt))
        r = Res(name, t)
        self.res.append(r)
        return r

    def ps(self, name, shape, dt=F32, es=None):
        t = (es or self.es).enter_context(self.nc.psum_tensor(name, list(shape), dt))
        r = Res(name, t)
        r.psum = True
        self.res.append(r)
        return r

    def _dsem(self, r):
        if r.sem is None:
            if self.free_sems:
                r.sem = self.free_sems.pop()
            else:
                h = self.es.enter_context(self.nc.semaphore("D%d" % len(self.sems)))
                r.sem = Sem(h)
                self.sems.append(r.sem)
        return r.sem

    def release(self, rs):
        for r in rs:
            if r.sem is not None:
                self.free_sems.append(r.sem)
                r.sem = None
            if r in self.res:
                self.res.remove(r)

    def op(self, eng, fn, reads=(), writes=(), dma=None, waw=True):
        idx = len(self.ops)
        tok = ("op", idx)
        if dma is not None:
            sm = self._dsem(dma)
            sm.n += 1
            tok = ("dma", sm, sm.n)
        deps = set()
        for r in reads:
            if r.lw is not None:
                deps.add(r.lw)
            if r.psum:
                for d in r.rd:
                    if d[0] == "op" and self.ops[d[1]]["eng"] != eng:
                        deps.add(d)
        for w in writes:
            if w.lw is not None:
                d = w.lw
                if d[0] == "op" and self.ops[d[1]]["eng"] == eng:
                    pass
                elif d[0] == "dma" and dma is not None and not waw and d[1] is dma.sem:
                    pass
                else:
                    deps.add(d)
            for d in w.rd:
                if d[0] == "op" and self.ops[d[1]]["eng"] == eng:
                    continue
                deps.add(d)
        if eng == "pe":
            deps = {d for d in deps if not (d[0] == "op" and self.ops[d[1]]["eng"] == "pe")}
        deps.discard(tok)
        o = dict(eng=eng, fn=fn, deps=deps, dma=dma, tok=tok, marked=False,
                 dmasem=(dma.sem if dma is not None else None))
        self.ops.append(o)
        self.eng_ops[eng].append(idx)
        for d in deps:
            if d[0] == "op":
                self.ops[d[1]]["marked"] = True
        for r in reads:
            r.rd.append(tok)
        for w in writes:
            w.lw = tok
            w.rd = []
        return idx

    def barrier(self):
        last = {}
        for e in ENGS:
            for i in reversed(self.eng_ops[e]):
                if self.ops[i]["fn"] is not None and self.ops[i]["dma"] is None:
                    last[e] = i
                    break
        dmas = [("dma", sm, sm.n) for sm in self.sems if sm.n > 0]
        for e in ENGS:
            deps = set(dmas)
            for e2, i in last.items():
                if e2 != e:
                    deps.add(("op", i))
                    self.ops[i]["marked"] = True
            idx = len(self.ops)
            self.ops.append(dict(eng=e, fn=None, deps=deps, dma=None, tok=("op", idx), marked=False))
            self.eng_ops[e].append(idx)
        for r in self.res:
            r.lw = None
            r.rd = []

    def emit(self):
        nc = self.nc
        semval = {}
        cnt = {e: 0 for e in ENGS}
        for i, o in enumerate(self.ops):
            if o["marked"] and o["dma"] is None and o["fn"] is not None:
                cnt[o["eng"]] += 1
                semval[i] = cnt[o["eng"]]
        self.stats = dict(cnt)

        def run(ename, eng):
            waited = {}
            for i in self.eng_ops[ename]:
                o = self.ops[i]
                for d in sorted(o["deps"], key=lambda d: (d[0], d[1] if d[0] == "op" else id(d[1]))):
                    if d[0] == "op":
                        p = self.ops[d[1]]
                        if p["fn"] is None:
                            continue
                        sem, val = self.engsem[p["eng"]], semval[d[1]]
                    else:
                        sem, val = d[1].h, 16 * d[2]
                    k = id(sem)
                    if waited.get(k, 0) < val:
                        eng.wait_ge(sem, val)
                        waited[k] = val
                if o["fn"] is None:
                    continue
                ins = o["fn"](eng)
                if o["dma"] is not None:
                    ins.then_inc(o["dmasem"].h, 16)
                elif o["marked"]:
                    ins.then_inc(self.engsem[ename], 1)

        with nc.Block() as block:
            @block.tensor
            def _(e):
                run("pe", e)

            @block.vector
            def _(e):
                run("dve", e)

            @block.scalar
            def _(e):
                run("act", e)

            @block.gpsimd
            def _(e):
                run("pool", e)

            @block.sync
            def _(e):
                run("sp", e)

    def mm(self, out_r, out_ap, lhsT, rhs, reads, start=True, stop=True):
        return self.op("pe", lambda e: e.matmul(out_ap, lhsT, rhs, start=start, stop=stop),
                       reads=reads, writes=[out_r])

    def tr(self, out_r, out_ap, in_ap, ident_ap, reads):
        return self.op("pe", lambda e: e.transpose(out_ap, in_ap, ident_ap), reads=reads, writes=[out_r])

    def load(self, r, out_ap, in_ap, q="sp", waw=False):
        return self.op(q, lambda e: e.dma_start(out=out_ap, in_=in_ap), writes=[r], dma=r, waw=waw)

    def store(self, r, out_ap, in_ap, q="sp"):
        return self.op(q, lambda e: e.dma_start(out=out_ap, in_=in_ap), reads=[r], dma=r)

from contextlib import ExitStack
from concourse.bass_utils import run_bass_kernel_spmd

D = 1024
FH = 2816
NFB = FH // 128
PAD = 8
EPS = 1e-6


class Ctx:
    pass


def make_ctx(nc, es, T, NT=384):
    C = Ctx()
    C.nc = nc
    C.T = T
    C.NT = NT
    C.P = Prog(nc, es)
    P = C.P
    C.ps = [P.ps("ps%d" % i, [128, 512], F32) for i in range(8)]
    C.ones_bf = P.sb("ones_bf", [128, 128], BF16)
    C.ident = P.sb("ident", [128, 128], F32)
    C.zeros = P.sb("zeros", [128, 8, PAD], F32)
    P.op("dve", lambda e: e.memset(C.ones_bf[:], 1.0), writes=[C.ones_bf])
    P.op("dve", lambda e: e.memset(C.zeros[:], 0.0), writes=[C.zeros])
    ident_d = nc.dram_tensor("ident_in", [128, 128], F32, kind="ExternalInput").ap()
    P.load(C.ident, C.ident[:], ident_d[:, :])
    C.ones_f = P.sb("ones_fc", [128, 128], F32)
    P.op("dve", lambda e: e.memset(C.ones_f[:], 1.0), writes=[C.ones_f])
    C.eps_t = P.sb("eps_t", [128, 1], F32)
    P.op("dve", lambda e: e.memset(C.eps_t[:], EPS), writes=[C.eps_t])
    return C


def hview(h):
    return h.rearrange("(c p) w -> p c w", p=128)


def zero_pads(C, h):
    P = C.P
    hv = hview(h)
    T = C.T
    P.store(C.zeros, hv[:, :, 0:PAD], C.zeros[:])
    P.store(C.zeros, hv[:, :, PAD + T:PAD + T + PAD], C.zeros[:])


def phase_in(C, x, h):
    P, T = C.P, C.T
    hv = hview(h)
    with ExitStack() as es:
        xin = [P.sb("xin%d" % i, [128, D], F32, es) for i in range(8)]
        stage = [P.sb("stg%d" % i, [128, 8, 512], F32, es) for i in range(2)]
        loc = xin + stage
        k = 0
        for g in range(T // 512):
            xs = []
            for j in range(4):
                xt = xin[(g % 2) * 4 + j]
                r0 = g * 512 + j * 128
                P.load(xt, xt[:], x[r0:r0 + 128, :])
                xs.append(xt)
            st = stage[g % 2]
            for c in range(8):
                pb = C.ps[k % 4]
                for j in range(4):
                    P.tr(pb, pb[:, j * 128:(j + 1) * 128], xs[j][:, c * 128:(c + 1) * 128], C.ident[:],
                         reads=[xs[j], C.ident])
                if k % 2 == 0:
                    P.op("dve", lambda e, st=st, c=c, pb=pb: e.tensor_copy(st[:, c, :], pb[:]),
                         reads=[pb], writes=[st])
                else:
                    P.op("act", lambda e, st=st, c=c, pb=pb: e.copy(st[:, c, :], pb[:]),
                         reads=[pb], writes=[st])
                k += 1
            P.store(st, hv[:, :, PAD + g * 512:PAD + (g + 1) * 512], st[:])
        P.barrier()
        P.release(loc)


def phase_out(C, h, out):
    P, T = C.P, C.T
    hv = hview(h)
    with ExitStack() as es:
        hin = [P.sb("hin%d" % i, [128, 8, 512], F32, es) for i in range(2)]
        ot = [P.sb("ot%d" % i, [128, D], F32, es) for i in range(4)]
        loc = hin + ot
        k = 0
        n = 0
        for g in range(T // 512):
            hi = hin[g % 2]
            P.load(hi, hi[:], hv[:, :, PAD + g * 512:PAD + (g + 1) * 512])
            for j in range(4):
                o = ot[n % 4]
                n += 1
                for half in range(2):
                    pb = C.ps[k % 4]
                    for q in range(4):
                        c = half * 4 + q
                        P.tr(pb, pb[:, q * 128:(q + 1) * 128], hi[:, c, j * 128:(j + 1) * 128], C.ident[:],
                             reads=[hi, C.ident])
                    if k % 2 == 0:
                        P.op("dve", lambda e, o=o, half=half, pb=pb: e.tensor_copy(o[:, half * 512:(half + 1) * 512], pb[:]),
                             reads=[pb], writes=[o])
                    else:
                        P.op("act", lambda e, o=o, half=half, pb=pb: e.copy(o[:, half * 512:(half + 1) * 512], pb[:]),
                             reads=[pb], writes=[o])
                    k += 1
                r0 = g * 512 + j * 128
                P.store(o, out[r0:r0 + 128, :], o[:])
        P.barrier()
        P.release(loc)


def rstd_from_sumsq(C, ps_sum, n, width, tmp, rstd):
    P = C.P
    P.op("act", lambda e: e.activation(tmp[:, :width], ps_sum[:, :width], AF.Sqrt, bias=C.eps_t[:, 0:1], scale=1.0 / n),
         reads=[ps_sum, C.eps_t], writes=[tmp])
    P.op("dve", lambda e: e.reciprocal(rstd[:, :width], tmp[:, :width]), reads=[tmp], writes=[rstd])


def phase_ffn(C, li, hin, hout, W):
    P, T, NT = C.P, C.T, C.NT
    NO = NT - 2
    hiv, hov = hview(hin), hview(hout)
    with ExitStack() as es:
        Wup = P.sb("Wup", [128, 8, 2 * FH], BF16, es)
        Wdn = P.sb("Wdn", [128, NFB, D], BF16, es)
        cw = P.sb("cw", [128, 44 * 3], F32, es)
        cb = P.sb("cb", [128, 44], F32, es)
        g2 = P.sb("g2", [128, 8], F32, es)
        g3 = P.sb("g3", [128, 8], F32, es)
        H = P.sb("H", [128, 8, NT], F32, es)
        xn = P.sb("xn", [128, 8, NT], BF16, es)
        hm = P.sb("hm", [128, NFB, NT], BF16, es)
        fT = P.sb("fT", [128, 8, NT], F32, es)
        sq = [P.sb("sq%d" % i, [128, NT], BF16, es) for i in range(2)]
        tmp = P.sb("tmp", [128, NT], F32, es)
        rstd = P.sb("rstd", [128, NT], F32, es)
        rstd2 = P.sb("rstd2", [128, NT], F32, es)
        tg = [[P.sb("tg%d%d" % (i, j), [128, NT], F32, es) for j in range(2)] for i in range(3)]
        tv = [[P.sb("tv%d%d" % (i, j), [128, NT], F32, es) for j in range(2)] for i in range(3)]
        loc = [Wup, Wdn, cw, cb, g2, g3, H, xn, hm, fT, tmp, rstd, rstd2] + sq + sum(tg, []) + sum(tv, [])

        wu = W["ffn_w_up"][li].rearrange("(c p) f -> p c f", p=128)
        for c in range(8):
            P.load(Wup, Wup[:, c, :], wu[:, c, :], q="pool")
        wd = W["ffn_w_down"][li].rearrange("(c p) d -> p c d", p=128)
        for c0 in range(0, NFB, 6):
            c1 = min(NFB, c0 + 6)
            P.load(Wdn, Wdn[:, c0:c1, :], wd[:, c0:c1, :], q="pool")
        P.load(cw, cw[:], W["ffn_cw"][li])
        P.load(cb, cb[:], W["ffn_cb"][li])
        P.load(g2, g2[:], W["norm_g"][li * 4 + 2])
        P.load(g3, g3[:], W["norm_g"][li * 4 + 3])

        import os
        STOP = int(os.environ.get("FFN_STOP", "99"))
        starts = []
        s = 0
        while True:
            if s + NO >= T:
                starts.append(T - NO)
                break
            starts.append(s)
            s += NO
        psS = C.ps[6]
        for ti, s in enumerate(starts):
            c0 = PAD + s - 1
            P.load(H, H[:], hiv[:, :, c0:c0 + NT])
            for c in range(8):
                sqt = sq[c % 2]
                P.op("act", lambda e, sqt=sqt, c=c: e.activation(sqt[:], H[:, c, :], AF.Square),
                     reads=[H], writes=[sqt])
                P.mm(psS, psS[:, :NT], C.ones_bf[:], sqt[:], reads=[C.ones_bf, sqt], start=(c == 0), stop=(c == 7))
            if STOP <= 1:
                continue
            rstd_from_sumsq(C, psS, D, NT, tmp, rstd)
            if STOP <= 2:
                continue
            for c in range(8):
                P.op("dve", lambda e, c=c: e.scalar_tensor_tensor(out=xn[:, c, :], in0=H[:, c, :], scalar=g2[:, c:c + 1],
                                                                  in1=rstd[:], op0=ALU.mult, op1=ALU.mult),
                     reads=[H, g2, rstd], writes=[xn])
            if STOP <= 3:
                continue
            for fb in range(NFB):
                pg = C.ps[(fb % 3) * 2]
                pv = C.ps[(fb % 3) * 2 + 1]
                for c in range(8):
                    P.mm(pg, pg[:, :NT], Wup[:, c, fb * 128:(fb + 1) * 128], xn[:, c, :], reads=[Wup, xn],
                         start=(c == 0), stop=(c == 7))
                for c in range(8):
                    P.mm(pv, pv[:, :NT], Wup[:, c, FH + fb * 128:FH + (fb + 1) * 128], xn[:, c, :], reads=[Wup, xn],
                         start=(c == 0), stop=(c == 7))
                outs = []
                for (pp, tt, fi) in ((pg, tg[fb % 3], fb), (pv, tv[fb % 3], fb + NFB)):
                    t1, t2 = tt
                    w0 = cw[:, fi * 3 + 0:fi * 3 + 1]
                    w1 = cw[:, fi * 3 + 1:fi * 3 + 2]
                    w2 = cw[:, fi * 3 + 2:fi * 3 + 3]
                    bb = cb[:, fi:fi + 1]
                    P.op("act", lambda e, t1=t1, pp=pp, w1=w1, bb=bb: e.activation(t1[:, :NO], pp[:, 1:1 + NO], AF.Identity,
                                                                                  bias=bb, scale=w1),
                         reads=[pp, cw, cb], writes=[t1])
                    P.op("dve", lambda e, t1=t1, t2=t2, pp=pp, w0=w0: e.scalar_tensor_tensor(
                        out=t2[:, :NO], in0=pp[:, 0:NO], scalar=w0, in1=t1[:, :NO], op0=ALU.mult, op1=ALU.add),
                        reads=[pp, cw, t1], writes=[t2])
                    P.op("dve", lambda e, t1=t1, t2=t2, pp=pp, w2=w2: e.scalar_tensor_tensor(
                        out=t1[:, :NO], in0=pp[:, 2:2 + NO], scalar=w2, in1=t2[:, :NO], op0=ALU.mult, op1=ALU.add),
                        reads=[pp, cw, t2], writes=[t1])
                    outs.append((t1, t2))
                (cg, gbuf), (cv, _) = outs
                P.op("act", lambda e, gbuf=gbuf, cg=cg: e.activation(gbuf[:, :NO], cg[:, :NO], AF.Gelu_apprx_tanh),
                     reads=[cg], writes=[gbuf])
                P.op("pool", lambda e, fb=fb, gbuf=gbuf, cv=cv: e.tensor_tensor(out=hm[:, fb, :NO], in0=gbuf[:, :NO], in1=cv[:, :NO],
                                                                                op=ALU.mult),
                     reads=[gbuf, cv], writes=[hm])
            if STOP <= 4:
                continue
            for db in range(8):
                pd = C.ps[db % 6]
                for fc in range(NFB):
                    P.mm(pd, pd[:, :NO], Wdn[:, fc, db * 128:(db + 1) * 128], hm[:, fc, :NO], reads=[Wdn, hm],
                         start=(fc == 0), stop=(fc == NFB - 1))
                sqt = sq[db % 2]
                P.op("dve", lambda e, db=db, pd=pd: e.tensor_copy(fT[:, db, :NO], pd[:, :NO]), reads=[pd], writes=[fT])
                P.op("act", lambda e, sqt=sqt, db=db: e.activation(sqt[:, :NO], fT[:, db, :NO], AF.Square),
                     reads=[fT], writes=[sqt])
                if STOP >= 6:
                    P.mm(psS, psS[:, :NO], C.ones_bf[:], sqt[:, :NO], reads=[C.ones_bf, sqt], start=(db == 0), stop=(db == 7))
            if STOP <= 6:
                continue
            rstd_from_sumsq(C, psS, D, NO, tmp, rstd2)
            if STOP <= 7:
                continue
            for c in range(8):
                P.op("dve", lambda e, c=c: e.scalar_tensor_tensor(out=fT[:, c, :NO], in0=fT[:, c, :NO], scalar=g3[:, c:c + 1],
                                                                  in1=rstd2[:, :NO], op0=ALU.mult, op1=ALU.mult),
                     reads=[fT, g3, rstd2], writes=[fT])
                P.op("pool", lambda e, c=c: e.tensor_tensor(out=fT[:, c, :NO], in0=fT[:, c, :NO], in1=H[:, c, 1:1 + NO], op=ALU.add),
                     reads=[fT, H], writes=[fT])
            if STOP <= 8:
                continue
            P.store(fT, hov[:, :, PAD + s:PAD + s + NO], fT[:, :, :NO])
        P.barrier()
        P.release(loc)


def gelu_tanh(C, out, x, n):
    P = C.P
    P.op("act", lambda e: e.activation(out[:, :n], x[:, :n], AF.Square), reads=[x], writes=[out])
    P.op("pool", lambda e: e.tensor_scalar(out[:, :n], out[:, :n], 0.044715, 1.0, ALU.mult, ALU.add), reads=[out], writes=[out])
    P.op("pool", lambda e: e.tensor_tensor(out=out[:, :n], in0=out[:, :n], in1=x[:, :n], op=ALU.mult), reads=[out, x], writes=[out])
    P.op("act", lambda e: e.activation(out[:, :n], out[:, :n], AF.Sigmoid, scale=1.5957691216057308), reads=[out], writes=[out])
    P.op("pool", lambda e: e.tensor_tensor(out=out[:, :n], in0=out[:, :n], in1=x[:, :n], op=ALU.mult), reads=[out, x], writes=[out])


def declare_inputs(nc, shapes):
    W = {}
    for name, shp in shapes.items():
        W[name] = nc.dram_tensor(name, list(shp), F32, kind="ExternalInput").ap()
    return W


def host_ffn_params(inp):
    L = inp["ffn_conv_w"].shape[0]
    cw = np.ascontiguousarray(inp["ffn_conv_w"].transpose(0, 2, 1).reshape(L, 44, 128, 3).transpose(0, 2, 1, 3).reshape(L, 128, 132))
    cb = np.ascontiguousarray(inp["ffn_conv_b"].reshape(L, 44, 128).transpose(0, 2, 1))
    ng = np.ascontiguousarray(inp["norm_g"].reshape(L * 4, 8, 128).transpose(0, 2, 1))
    return cw, cb, ng


def load_w(C, es, name, ap, q="pool"):
    P = C.P
    K, N = ap.shape
    kc = K // 128
    t = P.sb(name, [128, kc, N], BF16, es)
    v = ap.rearrange("(c p) n -> p c n", p=128)
    step = max(1, 4096 // N)
    for c0 in range(0, kc, step):
        c1 = min(kc, c0 + step)
        P.load(t, t[:, c0:c1, :], v[:, c0:c1, :], q=q)
    return t


def load_small(C, es, name, ap):
    P = C.P
    t = P.sb(name, list(ap.shape), F32, es)
    P.load(t, t[:], ap)
    return t


class NormBufs:
    def __init__(self, C, es, n, pref):
        P = C.P
        self.H = P.sb(pref + "H", [128, 8, n], F32, es)
        self.xns = [P.sb(pref + "xn%d" % i, [128, 8, n], BF16, es) for i in range(2)]
        self.xn = self.xns[0]
        self.ncall = 0
        self.sq = [P.sb(pref + "sq%d" % i, [128, n], BF16, es) for i in range(2)]
        self.tmp = P.sb(pref + "tmp", [128, n], F32, es)
        self.rstd = P.sb(pref + "rstd", [128, n], F32, es)
        self.all = [self.H, self.tmp, self.rstd] + self.xns + self.sq


def norm_in(C, hv, col0, n, g, B, psS):
    P = C.P
    B.xn = B.xns[B.ncall % 2]
    B.ncall += 1
    P.load(B.H, B.H[:, :, :n], hv[:, :, col0:col0 + n])
    for c in range(8):
        sqt = B.sq[c % 2]
        P.op("act", lambda e, sqt=sqt, c=c: e.activation(sqt[:, :n], B.H[:, c, :n], AF.Square), reads=[B.H], writes=[sqt])
        P.mm(psS, psS[:, :n], C.ones_bf[:], sqt[:, :n], reads=[C.ones_bf, sqt], start=(c == 0), stop=(c == 7))
    rstd_from_sumsq(C, psS, D, n, B.tmp, B.rstd)
    for c in range(8):
        P.op("dve", lambda e, c=c, xn=B.xn: e.scalar_tensor_tensor(out=xn[:, c, :n], in0=B.H[:, c, :n], scalar=g[:, c:c + 1],
                                                          in1=B.rstd[:, :n], op0=ALU.mult, op1=ALU.mult),
             reads=[B.H, g, B.rstd], writes=[B.xn])


def sub_norm(C, src, nch, n, nfeat, g, dst, sq, tmp, rstd, psS, extra_scale=None):
    P = C.P
    for c in range(nch):
        sqt = sq[c % 2]
        P.op("act", lambda e, sqt=sqt, c=c: e.activation(sqt[:, :n], src[:, c, :n], AF.Square), reads=[src], writes=[sqt])
        P.mm(psS, psS[:, :n], C.ones_bf[:], sqt[:, :n], reads=[C.ones_bf, sqt], start=(c == 0), stop=(c == nch - 1))
    rstd_from_sumsq(C, psS, nfeat, n, tmp, rstd)
    for c in range(nch):
        P.op("dve", lambda e, c=c: e.scalar_tensor_tensor(out=dst[:, c, :n], in0=src[:, c, :n], scalar=g[:, c:c + 1],
                                                          in1=rstd[:, :n], op0=ALU.mult, op1=ALU.mult),
             reads=[src, g, rstd], writes=[dst])


def phase_tail(C, ao, Wo_ap, g1_ap, hin, hout, NTK=512):
    P, T = C.P, C.T
    KF = ao.shape[0]
    kc = KF // 128
    hiv, hov = hview(hin), hview(hout)
    aov = ao.rearrange("(c p) t -> p c t", p=128)
    with ExitStack() as es:
        Wo = load_w(C, es, "Wo", Wo_ap)
        g1 = load_small(C, es, "g1", g1_ap)
        A = [P.sb("tA%d" % i, [128, kc, NTK], BF16, es) for i in range(2)]
        H = [P.sb("tH%d" % i, [128, 8, NTK], F32, es) for i in range(2)]
        fTs = [P.sb("tfT%d" % i, [128, 8, NTK], F32, es) for i in range(2)]
        sq = [P.sb("tsq%d" % i, [128, NTK], BF16, es) for i in range(4)]
        tmps = [P.sb("ttmp%d" % i, [128, NTK], F32, es) for i in range(2)]
        rstds = [P.sb("trstd%d" % i, [128, NTK], F32, es) for i in range(2)]
        loc = [Wo, g1] + fTs + tmps + rstds + A + H + sq
        psS = C.ps[6]
        for ti in range(T // NTK):
            t0 = ti * NTK
            a, h = A[ti % 2], H[ti % 2]
            fT, tmp, rstd = fTs[ti % 2], tmps[ti % 2], rstds[ti % 2]
            P.load(a, a[:], aov[:, :, t0:t0 + NTK])
            P.load(h, h[:], hiv[:, :, PAD + t0:PAD + t0 + NTK])
            for db in range(8):
                pd = C.ps[db % 6]
                for c in range(kc):
                    P.mm(pd, pd[:, :NTK], Wo[:, c, db * 128:(db + 1) * 128], a[:, c, :], reads=[Wo, a],
                         start=(c == 0), stop=(c == kc - 1))
                sqt = sq[db % 4]
                P.op("dve", lambda e, db=db, pd=pd, fT=fT: e.tensor_copy(fT[:, db, :], pd[:, :NTK]), reads=[pd], writes=[fT])
                P.op("act", lambda e, sqt=sqt, db=db, fT=fT: e.activation(sqt[:], fT[:, db, :], AF.Square), reads=[fT], writes=[sqt])
                P.mm(psS, psS[:, :NTK], C.ones_bf[:], sqt[:], reads=[C.ones_bf, sqt], start=(db == 0), stop=(db == 7))
            rstd_from_sumsq(C, psS, D, NTK, tmp, rstd)
            for c in range(8):
                P.op("dve", lambda e, c=c, fT=fT, rstd=rstd: e.scalar_tensor_tensor(out=fT[:, c, :], in0=fT[:, c, :], scalar=g1[:, c:c + 1],
                                                                  in1=rstd[:], op0=ALU.mult, op1=ALU.mult),
                     reads=[fT, g1, rstd], writes=[fT])
                P.op("pool", lambda e, c=c, h=h, fT=fT: e.tensor_tensor(out=fT[:, c, :], in0=fT[:, c, :], in1=h[:, c, :], op=ALU.add),
                     reads=[fT, h], writes=[fT])
            P.store(fT, hov[:, :, PAD + t0:PAD + t0 + NTK], fT[:])
        P.barrier()
        P.release(loc)


def phase_mla_proj(C, li, hin, W, S, NTK=512):
    P, T = C.P, C.T
    hiv = hview(hin)
    with ExitStack() as es:
        Wdq = load_w(C, es, "Wdq", W["mla_w_dq"])
        Wuq = load_w(C, es, "Wuq", W["mla_w_uq"])
        Wdkv = load_w(C, es, "Wdkv", W["mla_w_dkv"])
        Wukv = load_w(C, es, "Wukv", W["mla_w_ukv"])
        g0 = load_small(C, es, "g0", W["norm_g"][li * 4 + 0])
        qg = load_small(C, es, "qg", W["mla_qg"])
        kvg = load_small(C, es, "kvg", W["mla_kvg"])
        B = NormBufs(C, es, NTK, "m")
        cqf = P.sb("cqf", [128, 3, NTK], F32, es)
        cqn = P.sb("cqn", [128, 3, NTK], BF16, es)
        ckf = P.sb("ckf", [128, 2, NTK], F32, es)
        ckn = P.sb("ckn", [128, 2, NTK], BF16, es)
        cs = P.sb("cs", [64, 2, NTK], F32, es)
        qn_st = P.sb("qn_st", [128, 8, NTK], BF16, es)
        kn_st = P.sb("kn_st", [128, 8, NTK], BF16, es)
        qr_st = P.sb("qr_st", [64, 8, NTK], BF16, es)
        kr_st = P.sb("kr_st", [64, NTK], BF16, es)
        v_st = P.sb("v_st", [128, NTK // 128, 1024], BF16, es)
        r1 = [P.sb("r1_%d" % i, [64, NTK], F32, es) for i in range(2)]
        r2 = [P.sb("r2_%d" % i, [64, NTK], F32, es) for i in range(2)]
        tmp2 = P.sb("mtmp2", [128, NTK], F32, es)
        rstd2 = P.sb("mrstd2", [128, NTK], F32, es)
        loc = [Wdq, Wuq, Wdkv, Wukv, g0, qg, kvg, cqf, cqn, ckf, ckn, cs, qn_st, kn_st, qr_st, kr_st, v_st, tmp2, rstd2] + B.all + r1 + r2
        psS = C.ps[6]
        qnv = S["qn"].rearrange("(h p) t -> p h t", p=128)
        knv = S["kn"].rearrange("(h p) t -> p h t", p=128)
        qrv = S["qr"].rearrange("(h p) t -> p h t", p=64)
        vv = S["v"].rearrange("(tb p) f -> p tb f", p=128)
        k = 0

        def rope(psA, psB, out_ap, out_r, i):
            a, b = r1[i % 2], r2[i % 2]
            P.op("dve", lambda e: e.tensor_tensor(out=a[:], in0=psA[0:64, :NTK], in1=cs[:, 0, :], op=ALU.mult), reads=[psA, cs], writes=[a])
            P.op("dve", lambda e: e.tensor_tensor(out=b[:], in0=psB[0:64, :NTK], in1=cs[:, 1, :], op=ALU.mult), reads=[psB, cs], writes=[b])
            P.op("pool", lambda e: e.tensor_tensor(out=out_ap, in0=a[:], in1=b[:], op=ALU.add), reads=[a, b], writes=[out_r])

        for ti in range(T // NTK):
            t0 = ti * NTK
            norm_in(C, hiv, PAD + t0, NTK, g0, B, psS)
            P.load(cs, cs[:], W["rope_cs"][:, :, t0:t0 + NTK])
            for fo in range(3):
                pb = C.ps[k % 4]; k += 1
                for c in range(8):
                    P.mm(pb, pb[:, :NTK], Wdq[:, c, fo * 128:(fo + 1) * 128], B.xn[:, c, :], reads=[Wdq, B.xn], start=(c == 0), stop=(c == 7))
                P.op("dve", lambda e, fo=fo, pb=pb: e.tensor_copy(cqf[:, fo, :], pb[:, :NTK]), reads=[pb], writes=[cqf])
            sub_norm(C, cqf, 3, NTK, 384, qg, cqn, B.sq, tmp2, rstd2, psS)
            for h in range(8):
                pb = C.ps[k % 4]; k += 1
                for c in range(3):
                    P.mm(pb, pb[:, :NTK], Wuq[:, c, h * 128:(h + 1) * 128], cqn[:, c, :], reads=[Wuq, cqn], start=(c == 0), stop=(c == 2))
                if h % 2 == 0:
                    P.op("act", lambda e, h=h, pb=pb: e.copy(qn_st[:, h, :], pb[:, :NTK]), reads=[pb], writes=[qn_st])
                else:
                    P.op("dve", lambda e, h=h, pb=pb: e.tensor_copy(qn_st[:, h, :], pb[:, :NTK]), reads=[pb], writes=[qn_st])
            P.store(qn_st, qnv[:, :, t0:t0 + NTK], qn_st[:])
            for h in range(8):
                pa = C.ps[k % 4]; k += 1
                pb = C.ps[k % 4]; k += 1
                for c in range(3):
                    P.mm(pa, pa[0:64, :NTK], Wuq[:, c, 1024 + h * 64:1024 + (h + 1) * 64], cqn[:, c, :], reads=[Wuq, cqn], start=(c == 0), stop=(c == 2))
                for c in range(3):
                    P.mm(pb, pb[0:64, :NTK], Wuq[:, c, 1536 + h * 64:1536 + (h + 1) * 64], cqn[:, c, :], reads=[Wuq, cqn], start=(c == 0), stop=(c == 2))
                rope(pa, pb, qr_st[:, h, :], qr_st, h)
            P.store(qr_st, qrv[:, :, t0:t0 + NTK], qr_st[:])
            for fo in range(2):
                pb = C.ps[k % 4]; k += 1
                for c in range(8):
                    P.mm(pb, pb[:, :NTK], Wdkv[:, c, fo * 128:(fo + 1) * 128], B.xn[:, c, :], reads=[Wdkv, B.xn], start=(c == 0), stop=(c == 7))
                P.op("dve", lambda e, fo=fo, pb=pb: e.tensor_copy(ckf[:, fo, :], pb[:, :NTK]), reads=[pb], writes=[ckf])
            pa = C.ps[k % 4]; k += 1
            pb = C.ps[k % 4]; k += 1
            for c in range(8):
                P.mm(pa, pa[0:64, :NTK], Wdkv[:, c, 256:320], B.xn[:, c, :], reads=[Wdkv, B.xn], start=(c == 0), stop=(c == 7))
            for c in range(8):
                P.mm(pb, pb[0:64, :NTK], Wdkv[:, c, 320:384], B.xn[:, c, :], reads=[Wdkv, B.xn], start=(c == 0), stop=(c == 7))
            rope(pa, pb, kr_st[:], kr_st, 0)
            P.store(kr_st, S["kr"][:, t0:t0 + NTK], kr_st[:])
            sub_norm(C, ckf, 2, NTK, 256, kvg, ckn, B.sq, tmp2, rstd2, psS)
            for h in range(8):
                pb = C.ps[k % 4]; k += 1
                for c in range(2):
                    P.mm(pb, pb[:, :NTK], Wukv[:, c, h * 128:(h + 1) * 128], ckn[:, c, :], reads=[Wukv, ckn], start=(c == 0), stop=(c == 1))
                if h % 2 == 0:
                    P.op("act", lambda e, h=h, pb=pb: e.copy(kn_st[:, h, :], pb[:, :NTK]), reads=[pb], writes=[kn_st])
                else:
                    P.op("dve", lambda e, h=h, pb=pb: e.tensor_copy(kn_st[:, h, :], pb[:, :NTK]), reads=[pb], writes=[kn_st])
            P.store(kn_st, knv[:, :, t0:t0 + NTK], kn_st[:])
            for tb in range(NTK // 128):
                for half in range(2):
                    pb = C.ps[k % 4]; k += 1
                    for c in range(2):
                        P.mm(pb, pb[:, :512], ckn[:, c, tb * 128:(tb + 1) * 128], Wukv[:, c, 1024 + half * 512:1024 + (half + 1) * 512],
                             reads=[Wukv, ckn], start=(c == 0), stop=(c == 1))
                    if half == 0:
                        P.op("act", lambda e, tb=tb, half=half, pb=pb: e.copy(v_st[:, tb, half * 512:(half + 1) * 512], pb[:, :512]), reads=[pb], writes=[v_st])
                    else:
                        P.op("dve", lambda e, tb=tb, half=half, pb=pb: e.tensor_copy(v_st[:, tb, half * 512:(half + 1) * 512], pb[:, :512]), reads=[pb], writes=[v_st])
            P.store(v_st, vv[:, t0 // 128:(t0 + NTK) // 128, :], v_st[:])
        P.barrier()
        P.release(loc)


def phase_mla_core(C, S, TQ0=0, TQ=None, NQ=512):
    P, T = C.P, C.T
    TQ = TQ or T
    NKB = T // 128
    scale = float(192 ** -0.5)
    with ExitStack() as es:
        Kn = [P.sb("Kn%d" % i, [128, T], BF16, es) for i in range(2)]
        Vh = [P.sb("Vh%d" % i, [128, NKB, 128], BF16, es) for i in range(2)]
        Kr = P.sb("Kr", [64, T], BF16, es)
        Qn = [P.sb("Qn%d" % i, [128, NQ], BF16, es) for i in range(2)]
        Qr = [P.sb("Qr%d" % i, [64, NQ], BF16, es) for i in range(2)]
        Pt = [P.sb("Pt%d" % i, [128, NQ], BF16, es) for i in range(4)]
        rl = P.sb("rl", [128, NQ], F32, es)
        ob = [P.sb("ob%d" % i, [128, NQ], BF16, es) for i in range(2)]
        loc = Kn + Vh + [Kr, rl] + Qn + Qr + Pt + ob
        P.load(Kr, Kr[:], S["kr"][:, :])
        vv = S["v"].rearrange("(kb p) f -> p kb f", p=128)
        qrv = S["qr"].rearrange("(h p) t -> p h t", p=64)
        it = 0
        for h in range(8):
            kn, vh = Kn[h % 2], Vh[h % 2]
            P.load(kn, kn[:], S["kn"][h * 128:(h + 1) * 128, :])
            P.load(vh, vh[:], vv[:, :, h * 128:(h + 1) * 128])
            for qi in range(TQ // NQ):
                q0 = TQ0 + qi * NQ
                qn, qr = Qn[it % 2], Qr[it % 2]
                o_sb = ob[it % 2]
                pO, pL = C.ps[3 + it % 2], C.ps[5 + it % 2]
                it += 1
                P.load(qn, qn[:], S["qn"][h * 128:(h + 1) * 128, q0:q0 + NQ])
                P.load(qr, qr[:], qrv[:, h, q0:q0 + NQ])

                def stA(kb, kn=kn, qn=qn, qr=qr):
                    pS = C.ps[kb % 3]
                    pt = Pt[kb % 4]
                    ks = slice(kb * 128, (kb + 1) * 128)
                    P.mm(pS, pS[:, :NQ], kn[:, ks], qn[:], reads=[kn, qn], start=True, stop=False)
                    P.mm(pS, pS[:, :NQ], Kr[:, ks], qr[:], reads=[Kr, qr], start=False, stop=True)
                    P.op("act", lambda e, pt=pt, pS=pS: e.activation(pt[:], pS[:, :NQ], AF.Exp, scale=scale), reads=[pS], writes=[pt])

                def stB(kb, vh=vh, pO=pO, pL=pL):
                    pt = Pt[kb % 4]
                    P.mm(pO, pO[:, :NQ], vh[:, kb, :], pt[:], reads=[vh, pt], start=(kb == 0), stop=(kb == NKB - 1))
                    P.mm(pL, pL[:, :NQ], C.ones_bf[:], pt[:], reads=[C.ones_bf, pt], start=(kb == 0), stop=(kb == NKB - 1))

                stA(0)
                if NKB > 1:
                    stA(1)
                for kb in range(NKB):
                    if kb + 2 < NKB:
                        stA(kb + 2)
                    stB(kb)
                P.op("dve", lambda e, pL=pL: e.reciprocal(rl[:], pL[:, :NQ]), reads=[pL], writes=[rl])
                P.op("dve", lambda e, pO=pO, o_sb=o_sb: e.tensor_tensor(out=o_sb[:], in0=pO[:, :NQ], in1=rl[:], op=ALU.mult), reads=[pO, rl], writes=[o_sb])
                P.store(o_sb, S["ao"][h * 128:(h + 1) * 128, q0:q0 + NQ], o_sb[:])
        P.barrier()
        P.release(loc)


def pm(v):
    v = np.asarray(v)
    return np.ascontiguousarray(v.reshape(-1, 128).T)


def host_mla_params(inp, pos):
    wuq = inp["mla_w_uq"][0].reshape(384, 8, 192)
    nope = wuq[:, :, :128].reshape(384, 1024)
    ropew = wuq[:, :, 128:]
    rope_sw = np.concatenate([ropew[:, :, 32:], ropew[:, :, :32]], -1)
    w_uq = np.ascontiguousarray(np.concatenate([nope, ropew.reshape(384, 512), rope_sw.reshape(384, 512)], 1))
    wdkv = inp["mla_w_dkv"][0]
    kr = wdkv[:, 256:]
    w_dkv = np.ascontiguousarray(np.concatenate([wdkv[:, :256], kr, kr[:, 32:], kr[:, :32]], 1))
    wukv = inp["mla_w_ukv"][0].reshape(256, 8, 256)
    w_ukv = np.ascontiguousarray(np.concatenate([wukv[:, :, :128].reshape(256, 1024), wukv[:, :, 128:].reshape(256, 1024)], 1))
    inv = (10000.0 ** (-np.arange(0, 64, 2, dtype=np.float32) / 64)).astype(np.float32)
    ang = pos.astype(np.float32)[None, :] * inv[:, None]
    cos, sin = np.cos(ang).astype(np.float32), np.sin(ang).astype(np.float32)
    cs = np.stack([np.concatenate([cos, cos], 0), np.concatenate([-sin, sin], 0)], 1)
    return dict(mla_w_dq=np.ascontiguousarray(inp["mla_w_dq"][0]), mla_w_uq=w_uq, mla_w_dkv=w_dkv, mla_w_ukv=w_ukv,
                mla_w_o=np.ascontiguousarray(inp["mla_w_o"][0]),
                mla_qg=pm(inp["mla_q_norm_g"][0]), mla_kvg=pm(inp["mla_kv_norm_g"][0]), rope_cs=np.ascontiguousarray(cs.astype(np.float32)))


def phase_diff_proj(C, li, hin, W, S, NTK=512):
    P, T = C.P, C.T
    hiv = hview(hin)
    with ExitStack() as es:
        Wqkv = load_w(C, es, "Wqkv", W["diff_w_qkv"])
        g0 = load_small(C, es, "g0", W["norm_g"][li * 4 + 0])
        B = NormBufs(C, es, NTK, "d")
        q_st = P.sb("dq_st", [128, 8, NTK], BF16, es)
        k_st = P.sb("dk_st", [128, 8, NTK], BF16, es)
        v_st = P.sb("dv_st", [128, NTK // 128, 1024], BF16, es)
        loc = [Wqkv, g0, q_st, k_st, v_st] + B.all
        psS = C.ps[6]
        qv = S["qn"].rearrange("(h p) t -> p h t", p=128)
        kv = S["kn"].rearrange("(h p) t -> p h t", p=128)
        vv = S["v"].rearrange("(tb p) f -> p tb f", p=128)
        k = 0
        for ti in range(T // NTK):
            t0 = ti * NTK
            norm_in(C, hiv, PAD + t0, NTK, g0, B, psS)
            for (st, off, dst) in ((q_st, 0, qv), (k_st, 1024, kv)):
                for h in range(8):
                    pb = C.ps[k % 4]; k += 1
                    for c in range(8):
                        P.mm(pb, pb[:, :NTK], Wqkv[:, c, off + h * 128:off + (h + 1) * 128], B.xn[:, c, :], reads=[Wqkv, B.xn],
                             start=(c == 0), stop=(c == 7))
                    if h % 2 == 0:
                        P.op("act", lambda e, h=h, pb=pb, st=st: e.copy(st[:, h, :], pb[:, :NTK]), reads=[pb], writes=[st])
                    else:
                        P.op("dve", lambda e, h=h, pb=pb, st=st: e.tensor_copy(st[:, h, :], pb[:, :NTK]), reads=[pb], writes=[st])
                P.store(st, dst[:, :, t0:t0 + NTK], st[:])
            for tb in range(NTK // 128):
                for half in range(2):
                    pb = C.ps[k % 4]; k += 1
                    for c in range(8):
                        P.mm(pb, pb[:, :512], B.xn[:, c, tb * 128:(tb + 1) * 128], Wqkv[:, c, 2048 + half * 512:2048 + (half + 1) * 512],
                             reads=[Wqkv, B.xn], start=(c == 0), stop=(c == 7))
                    if half == 0:
                        P.op("act", lambda e, tb=tb, half=half, pb=pb: e.copy(v_st[:, tb, half * 512:(half + 1) * 512], pb[:, :512]), reads=[pb], writes=[v_st])
                    else:
                        P.op("dve", lambda e, tb=tb, half=half, pb=pb: e.tensor_copy(v_st[:, tb, half * 512:(half + 1) * 512], pb[:, :512]), reads=[pb], writes=[v_st])
            P.store(v_st, vv[:, t0 // 128:(t0 + NTK) // 128, :], v_st[:])
        P.barrier()
        P.release(loc)


def phase_diff_core(C, li, W, S, NQ=512):
    P, T = C.P, C.T
    NKB = T // 128
    scale = float(64 ** -0.5)
    lam_init = 0.8 - 0.6 * float(np.exp(-0.3 * li))
    SKIP = 130.0
    with ExitStack() as es:
        Kh = [P.sb("dK%d" % i, [128, T], BF16, es) for i in range(2)]
        Vh = [P.sb("dV%d" % i, [128, NKB, 128], BF16, es) for i in range(2)]
        Q = [P.sb("dQ%d" % i, [128, NQ], BF16, es) for i in range(2)]
        RAW = P.sb("dRAW", [128, 6, NQ], F32, es)
        BRAW = P.sb("dBRAW", [128, 128], F32, es)
        MT = P.sb("dMT", [128, 6, NQ], BF16, es)
        BT = P.sb("dBT", [128, 128], F32, es)
        E = [P.sb("dE%d" % i, [128, NQ], BF16, es) for i in range(4)]
        Pt = [P.sb("dPt%d" % i, [128, NQ], BF16, es) for i in range(6)]
        lp = P.sb("dlp", [1, 256], F32, es)
        lw = P.sb("dlw", [1, 8], F32, es)
        ones1 = P.sb("dones1", [1, 128], F32, es)
        nlam = P.sb("dnlam", [128, 1], F32, es)
        sg = P.sb("dsg", [128, 1], F32, es)
        r1 = P.sb("dr1", [128, NQ], F32, es)
        r2 = P.sb("dr2", [128, NQ], F32, es)
        a1 = P.sb("da1", [128, NQ], F32, es)
        a2 = P.sb("da2", [128, NQ], F32, es)
        sqd = P.sb("dsq", [128, NQ], BF16, es)
        ob = [P.sb("dob%d" % i, [128, NQ], BF16, es) for i in range(2)]
        loc = Kh + Vh + Q + [RAW, BRAW, MT, BT, lp, lw, ones1, nlam, sg, r1, r2, a1, a2, sqd] + E + Pt + ob
        P.op("pool", lambda e: e.iota(RAW[:, 0, :], [[1, NQ]], base=0, channel_multiplier=0, allow_small_or_imprecise_dtypes=True), writes=[RAW])
        P.op("pool", lambda e: e.iota(RAW[:, 1, :], [[-1, NQ]], base=NQ - 1, channel_multiplier=0, allow_small_or_imprecise_dtypes=True), writes=[RAW])
        for kk in range(4):
            P.op("pool", lambda e, kk=kk: e.iota(RAW[:, 2 + kk, :], [[1, NQ]], base=-128 * kk, channel_multiplier=-1,
                                                allow_small_or_imprecise_dtypes=True), writes=[RAW])
        P.op("dve", lambda e: e.scalar_tensor_tensor(out=RAW[:, 2:6, :], in0=RAW[:, 2:6, :], scalar=-1.0, in1=RAW[:, 2:6, :], op0=ALU.mult, op1=ALU.max),
             reads=[RAW], writes=[RAW])
        P.op("pool", lambda e: e.iota(BRAW[:, 0:64], [[-128, 64]], base=0, channel_multiplier=1, allow_small_or_imprecise_dtypes=True), writes=[BRAW])
        P.op("pool", lambda e: e.iota(BRAW[:, 64:128], [[-128, 64]], base=NQ - 1, channel_multiplier=-1, allow_small_or_imprecise_dtypes=True), writes=[BRAW])
        P.load(lp, lp[:], W["diff_lambda"])
        P.load(sg, sg[:], W["diff_subln_g"])
        P.op("dve", lambda e: e.memset(ones1[:], 1.0), writes=[ones1])
        P.op("dve", lambda e: e.tensor_tensor(out=lp[:, 0:64], in0=lp[:, 0:64], in1=lp[:, 64:128], op=ALU.mult), reads=[lp], writes=[lp])
        P.op("dve", lambda e: e.tensor_tensor(out=lp[:, 128:192], in0=lp[:, 128:192], in1=lp[:, 192:256], op=ALU.mult), reads=[lp], writes=[lp])
        P.op("dve", lambda e: e.reduce_sum(lw[:, 0:1], lp[:, 0:64], axis=AX.X), reads=[lp], writes=[lw])
        P.op("dve", lambda e: e.reduce_sum(lw[:, 1:2], lp[:, 128:192], axis=AX.X), reads=[lp], writes=[lw])
        P.op("act", lambda e: e.activation(lw[:, 2:4], lw[:, 0:2], AF.Exp), reads=[lw], writes=[lw])
        P.op("dve", lambda e: e.scalar_tensor_tensor(out=lw[:, 4:5], in0=lw[:, 3:4], scalar=-lam_init, in1=lw[:, 2:3], op0=ALU.add, op1=ALU.subtract),
             reads=[lw], writes=[lw])
        pc = C.ps[7]
        P.mm(pc, pc[:, 0:1], ones1[:], lw[:, 4:5], reads=[ones1, lw])
        P.op("dve", lambda e: e.tensor_copy(nlam[:], pc[:, 0:1]), reads=[pc], writes=[nlam])
        P.op("dve", lambda e: e.tensor_single_scalar(sg[:], sg[:], 1.0 - lam_init, ALU.mult), reads=[sg], writes=[sg])

        it = 0
        for h in range(8):
            slope = float(2.0 ** (-(h + 1)))
            kh, vh = Kh[h % 2], Vh[h % 2]
            P.load(kh, kh[:], S["kn"][h * 128:(h + 1) * 128, :])
            P.load(vh, vh[:], S["v"].rearrange("(kb p) f -> p kb f", p=128)[:, :, h * 128:(h + 1) * 128])
            P.op("act", lambda e, slope=slope: e.activation(MT[:], RAW[:], AF.Exp, scale=-slope), reads=[RAW], writes=[MT])
            P.op("dve", lambda e, slope=slope: e.tensor_single_scalar(BT[:], BRAW[:], slope, ALU.mult), reads=[BRAW], writes=[BT])
            for qi in range(T // NQ):
                q0 = qi * NQ
                q = Q[it % 2]
                o_sb = ob[it % 2]
                it += 1
                P.load(q, q[:], S["qn"][h * 128:(h + 1) * 128, q0:q0 + NQ])
                pO = [C.ps[4], C.ps[5]]
                pL = [C.ps[6], C.ps[7]]
                kbs = []
                for kb in range(NKB):
                    j0 = kb * 128
                    dmin = max(0, q0 - (j0 + 127), j0 - (q0 + NQ - 1))
                    if slope * dmin >= SKIP:
                        continue
                    kbs.append(kb)

                def stA(i, kh=kh, q=q, q0=q0):
                    kb = kbs[i]
                    ks = slice(kb * 128, (kb + 1) * 128)
                    j0 = kb * 128
                    if j0 + 128 <= q0:
                        m = (q0 - j0) // 128
                        bias, mt = BT[:, m:m + 1], MT[:, 0, :]
                    elif j0 >= q0 + NQ:
                        m = (j0 - q0) // 128
                        bias, mt = BT[:, 64 + m:64 + m + 1], MT[:, 1, :]
                    else:
                        kk = (j0 - q0) // 128
                        bias, mt = None, MT[:, 2 + kk, :]
                    for j in range(2):
                        pS = C.ps[(i % 2) * 2 + j]
                        e_t = E[(i % 2) * 2 + j]
                        pt = Pt[(i % 3) * 2 + j]
                        js = slice(j * 64, (j + 1) * 64)
                        P.mm(pS, pS[:, :NQ], kh[js, ks], q[js, :], reads=[kh, q])
                        if bias is None:
                            P.op("act", lambda e, e_t=e_t, pS=pS: e.activation(e_t[:], pS[:, :NQ], AF.Exp, scale=scale), reads=[pS], writes=[e_t])
                        else:
                            P.op("act", lambda e, e_t=e_t, pS=pS, bias=bias: e.activation(e_t[:], pS[:, :NQ], AF.Exp, scale=scale, bias=bias),
                                 reads=[pS, BT], writes=[e_t])
                        eng = "dve" if j == 0 else "pool"
                        P.op(eng, lambda e, pt=pt, e_t=e_t, mt=mt: e.tensor_tensor(out=pt[:], in0=e_t[:], in1=mt, op=ALU.mult),
                             reads=[e_t, MT], writes=[pt])

                def stB(i, vh=vh, pO=pO, pL=pL):
                    kb = kbs[i]
                    for j in range(2):
                        pt = Pt[(i % 3) * 2 + j]
                        P.mm(pO[j], pO[j][:, :NQ], vh[:, kb, :], pt[:], reads=[vh, pt], start=(i == 0), stop=(i == len(kbs) - 1))
                        P.mm(pL[j], pL[j][:, :NQ], C.ones_bf[:], pt[:], reads=[C.ones_bf, pt], start=(i == 0), stop=(i == len(kbs) - 1))

                stA(0)
                if len(kbs) > 1:
                    stA(1)
                for i in range(len(kbs)):
                    if i + 2 < len(kbs):
                        stA(i + 2)
                    stB(i)
                P.op("dve", lambda e, pL=pL: e.reciprocal(r1[:], pL[0][:, :NQ]), reads=[pL[0]], writes=[r1])
                P.op("dve", lambda e, pL=pL: e.reciprocal(r2[:], pL[1][:, :NQ]), reads=[pL[1]], writes=[r2])
                P.op("dve", lambda e, pO=pO: e.tensor_tensor(out=a1[:], in0=pO[0][:, :NQ], in1=r1[:], op=ALU.mult), reads=[pO[0], r1], writes=[a1])
                P.op("dve", lambda e, pO=pO: e.tensor_tensor(out=a2[:], in0=pO[1][:, :NQ], in1=r2[:], op=ALU.mult), reads=[pO[1], r2], writes=[a2])
                P.op("dve", lambda e: e.scalar_tensor_tensor(out=a1[:], in0=a2[:], scalar=nlam[:, 0:1], in1=a1[:], op0=ALU.mult, op1=ALU.add),
                     reads=[a1, a2, nlam], writes=[a1])
                psS = C.ps[0]
                P.op("act", lambda e: e.activation(sqd[:], a1[:], AF.Square), reads=[a1], writes=[sqd])
                P.mm(psS, psS[:, :NQ], C.ones_bf[:], sqd[:], reads=[C.ones_bf, sqd])
                rstd_from_sumsq(C, psS, 128, NQ, r1, r2)
                P.op("dve", lambda e, o_sb=o_sb: e.scalar_tensor_tensor(out=o_sb[:], in0=a1[:], scalar=sg[:, 0:1], in1=r2[:], op0=ALU.mult, op1=ALU.mult),
                     reads=[a1, sg, r2], writes=[o_sb])
                P.store(o_sb, S["ao"][h * 128:(h + 1) * 128, q0:q0 + NQ], o_sb[:])
        P.barrier()
        P.release(loc)


def host_diff_params(inp):
    return dict(diff_w_qkv=np.ascontiguousarray(inp["diff_w_qkv"][0]), diff_w_o=np.ascontiguousarray(inp["diff_w_o"][0]),
                diff_lambda=np.ascontiguousarray(inp["diff_lambda"][0].reshape(1, 256)),
                diff_subln_g=np.ascontiguousarray(inp["diff_subln_g"][0].reshape(128, 1)))


def make_tri(C, es):
    P = C.P
    R = Ctx()
    R.M01F = P.sb("M01F", [128, 128], F32, es)
    R.M01B = P.sb("M01B", [128, 128], F32, es)
    R.ones_f = P.sb("ones_f", [128, 128], F32, es)
    P.op("dve", lambda e: e.memset(R.ones_f[:], 1.0), writes=[R.ones_f])
    P.op("pool", lambda e: e.iota(R.M01F[:], [[1, 128]], base=0, channel_multiplier=-1, allow_small_or_imprecise_dtypes=True), writes=[R.M01F])
    P.op("pool", lambda e: e.iota(R.M01B[:], [[-1, 128]], base=0, channel_multiplier=1, allow_small_or_imprecise_dtypes=True), writes=[R.M01B])
    for m in (R.M01F, R.M01B):
        P.op("dve", lambda e, m=m: e.tensor_scalar(m[:], m[:], 1.0, 0.0, ALU.add, ALU.max), reads=[m], writes=[m])
        P.op("dve", lambda e, m=m: e.tensor_single_scalar(m[:], m[:], 1.0, ALU.min), reads=[m], writes=[m])
    R.all = [R.M01F, R.M01B, R.ones_f]
    return R


def phase_mlstm_proj(C, li, hin, W, S, Gtok, NTK=512):
    P, T = C.P, C.T
    hiv = hview(hin)
    sc = float(64 ** -0.5)
    with ExitStack() as es:
        Win = load_w(C, es, "mWin", W["mlstm_w_in"])
        g0 = load_small(C, es, "g0", W["norm_g"][li * 4 + 0])
        bg = load_small(C, es, "mbg", W["mlstm_bg"])
        B = NormBufs(C, es, NTK, "l")
        q_st = P.sb("lq_st", [64, 8, NTK], BF16, es)
        k_st = P.sb("lk_st", [64, 8, NTK], BF16, es)
        o_st = P.sb("lo_st", [128, 8, NTK], BF16, es)
        kt_st = P.sb("lkt_st", [128, NTK // 128, 512], BF16, es)
        v_st = P.sb("lv_st", [128, NTK // 128, 8, 129], BF16, es)
        loc = [Win, g0, bg, q_st, k_st, o_st, kt_st, v_st] + B.all
        P.op("dve", lambda e: e.memset(v_st[:], 1.0), writes=[v_st])
        psS = C.ps[6]
        qv = S["mq"].rearrange("(h p) t -> p h t", p=64)
        kv = S["mk"].rearrange("(h p) t -> p h t", p=64)
        ov = S["og"].rearrange("(h p) t -> p h t", p=128)
        ktv = S["mkt"].rearrange("(tb p) f -> p tb f", p=128)
        vv = S["mv"].rearrange("(tb p) h e -> p tb h e", p=128)
        k = 0
        for ti in range(T // NTK):
            t0 = ti * NTK
            norm_in(C, hiv, PAD + t0, NTK, g0, B, psS)
            for h in range(8):
                pb = C.ps[k % 4]; k += 1
                for c in range(8):
                    P.mm(pb, pb[0:64, :NTK], Win[:, c, h * 64:(h + 1) * 64], B.xn[:, c, :], reads=[Win, B.xn], start=(c == 0), stop=(c == 7))
                P.op("act", lambda e, h=h, pb=pb: e.copy(q_st[:, h, :], pb[0:64, :NTK]), reads=[pb], writes=[q_st])
                pb = C.ps[k % 4]; k += 1
                for c in range(8):
                    P.mm(pb, pb[0:64, :NTK], Win[:, c, 512 + h * 64:512 + (h + 1) * 64], B.xn[:, c, :], reads=[Win, B.xn], start=(c == 0), stop=(c == 7))
                P.op("dve", lambda e, h=h, pb=pb: e.tensor_single_scalar(k_st[:, h, :], pb[0:64, :NTK], sc, ALU.mult), reads=[pb], writes=[k_st])
            P.store(q_st, qv[:, :, t0:t0 + NTK], q_st[:])
            P.store(k_st, kv[:, :, t0:t0 + NTK], k_st[:])
            for h in range(8):
                pb = C.ps[k % 4]; k += 1
                for c in range(8):
                    P.mm(pb, pb[:, :NTK], Win[:, c, 2048 + h * 128:2048 + (h + 1) * 128], B.xn[:, c, :], reads=[Win, B.xn], start=(c == 0), stop=(c == 7))
                P.op("act", lambda e, h=h, pb=pb: e.activation(o_st[:, h, :], pb[:, :NTK], AF.Sigmoid), reads=[pb], writes=[o_st])
            P.store(o_st, ov[:, :, t0:t0 + NTK], o_st[:])
            for tb in range(NTK // 128):
                ts_ = slice(tb * 128, (tb + 1) * 128)
                ch = t0 // 128 + tb
                pb = C.ps[k % 4]; k += 1
                for c in range(8):
                    P.mm(pb, pb[:, :512], B.xn[:, c, ts_], Win[:, c, 512:1024], reads=[Win, B.xn], start=(c == 0), stop=(c == 7))
                P.op("dve", lambda e, tb=tb, pb=pb: e.tensor_single_scalar(kt_st[:, tb, :], pb[:, :512], sc, ALU.mult), reads=[pb], writes=[kt_st])
                for half in range(2):
                    pb = C.ps[k % 4]; k += 1
                    for c in range(8):
                        P.mm(pb, pb[:, :512], B.xn[:, c, ts_], Win[:, c, 1024 + half * 512:1024 + (half + 1) * 512], reads=[Win, B.xn], start=(c == 0), stop=(c == 7))
                    P.op("act", lambda e, tb=tb, half=half, pb=pb: e.copy(v_st[:, tb, half * 4:(half + 1) * 4, 0:128],
                                                                         pb[:, :512].rearrange("p (h e) -> p h e", e=128)), reads=[pb], writes=[v_st])
                pb = C.ps[k % 4]; k += 1
                for c in range(8):
                    P.mm(pb, pb[:, :32], B.xn[:, c, ts_], Win[:, c, 3072:3104], reads=[Win, B.xn], start=(c == 0), stop=(c == 7))
                P.op("dve", lambda e, ch=ch, pb=pb: e.tensor_tensor(out=Gtok[:, ch, :], in0=pb[:, :32], in1=bg[:], op=ALU.add), reads=[pb, bg], writes=[Gtok])
            P.store(kt_st, ktv[:, t0 // 128:(t0 + NTK) // 128, :], kt_st[:])
            P.store(v_st, vv[:, t0 // 128:(t0 + NTK) // 128, :, :], v_st[:])
        P.barrier()
        P.release(loc)


def phase_mlstm_gates(C, R, Gtok, GS, es):
    P, T = C.P, C.T
    NCH = T // 128
    t8 = [P.sb("g8_%d" % i, [128, 8], F32, es) for i in range(4)]
    bcc = P.sb("gbcc", [128, 16], F32, es)
    rhsD = [P.sb("grhsD%d" % i, [128, 4, 128], F32, es) for i in range(2)]
    tmpD = [P.sb("gtmpD%d" % i, [128, 4, 128], F32, es) for i in range(2)]
    Mneg = [P.sb("gMneg%d" % i, [128, 4, 128], F32, es) for i in range(2)]
    Sel = [P.sb("gSel%d" % i, [128, 128], F32, es) for i in range(2)]
    one_t = P.sb("gone", [128, 1], F32, es)
    mcur = P.sb("gmcur", [128, 8], F32, es)
    loc = t8 + [bcc, one_t, mcur] + rhsD + tmpD + Mneg + Sel
    P.op("dve", lambda e: e.memset(one_t[:], 1.0), writes=[one_t])
    for d, msrc in ((0, R.M01B), (1, R.M01F)):
        for j in range(4):
            P.op("dve", lambda e, d=d, j=j, msrc=msrc: e.tensor_scalar(Mneg[d][:, j, :], msrc[:], -1.0, 1e30, ALU.add, ALU.mult),
                 reads=[msrc], writes=[Mneg[d]])
    P.op("dve", lambda e: e.tensor_scalar(Sel[0][:], R.ones_f[:], R.M01B[:, 127:128], None, ALU.mult), reads=[R.ones_f, R.M01B], writes=[Sel[0]])
    P.op("dve", lambda e: e.tensor_scalar(Sel[1][:], R.ones_f[:], R.M01F[:, 0:1], None, ALU.mult), reads=[R.ones_f, R.M01F], writes=[Sel[1]])
    tri = [R.M01F, R.M01B]
    k = 0
    for c in range(NCH):
        for d in range(2):
            G = GS[d]
            fs = slice(8 + 16 * d, 16 + 16 * d)
            is_ = slice(16 * d, 16 * d + 8)
            nl = t8[0]
            P.op("act", lambda e, c=c, fs=fs: e.activation(nl[:], Gtok[:, c, fs], AF.Exp, scale=-1.0), reads=[Gtok], writes=[nl])
            P.op("act", lambda e: e.activation(nl[:], nl[:], AF.Ln, bias=one_t[:, 0:1]), reads=[nl, one_t], writes=[nl])
            pb = C.ps[k % 4]; k += 1
            P.mm(pb, pb[:, 0:8], tri[d][:], nl[:], reads=[tri[d], nl])
            P.op("dve", lambda e, c=c, pb=pb, G=G: e.tensor_single_scalar(G["BC"][:, c, :], pb[:, 0:8], -1.0, ALU.mult), reads=[pb], writes=[G["BC"]])
            P.op("dve", lambda e, c=c, pb=pb, G=G, is_=is_: e.tensor_tensor(out=G["A"][:, c, :], in0=pb[:, 0:8], in1=Gtok[:, c, is_], op=ALU.add),
                 reads=[pb, Gtok], writes=[G["A"]])
            for g in range(2):
                rd, td = rhsD[g], tmpD[g]
                for j in range(4):
                    if j == 3:
                        P.op("dve", lambda e, c=c, g=g, j=j, rd=rd, G=G: e.tensor_scalar(rd[:, j, :], C.ident[:], G["A"][:, c, 4 * g + j:4 * g + j + 1], None, ALU.mult),
                             reads=[C.ident, G["A"]], writes=[rd])
                    elif j % 2 == 0:
                        P.op("act", lambda e, c=c, g=g, j=j, rd=rd, G=G: e.activation(rd[:, j, :], C.ident[:], AF.Identity, scale=G["A"][:, c, 4 * g + j:4 * g + j + 1]),
                             reads=[C.ident, G["A"]], writes=[rd])
                    else:
                        P.op("pool", lambda e, c=c, g=g, j=j, rd=rd, G=G: e.tensor_scalar(rd[:, j, :], C.ident[:], G["A"][:, c, 4 * g + j:4 * g + j + 1], None, ALU.mult),
                             reads=[C.ident, G["A"]], writes=[rd])
                pa = C.ps[4 + k % 2]; k += 1
                P.mm(pa, pa[:, :512], R.ones_f[:], rd[:].rearrange("p j s -> p (j s)"), reads=[R.ones_f, rd])
                P.op("dve", lambda e, td=td, pa=pa, d=d: e.tensor_tensor(out=td[:].rearrange("p j s -> p (j s)"), in0=pa[:, :512],
                                                                         in1=Mneg[d][:].rearrange("p j s -> p (j s)"), op=ALU.add),
                     reads=[pa, Mneg[d]], writes=[td])
                P.op("dve", lambda e, td=td, c=c, g=g, G=G: e.reduce_max(G["CM"][:, c, 4 * g:4 * g + 4], td[:], axis=AX.X), reads=[td], writes=[G["CM"]])
            P.op("dve", lambda e, c=c, G=G: e.tensor_copy(bcc[:, 0:8], G["BC"][:, c, :]), reads=[G["BC"]], writes=[bcc])
            P.op("dve", lambda e, c=c, G=G: e.tensor_copy(bcc[:, 8:16], G["CM"][:, c, :]), reads=[G["CM"]], writes=[bcc])
            pb = C.ps[k % 4]; k += 1
            P.mm(pb, pb[:, 0:16], Sel[d][:], bcc[:], reads=[Sel[d], bcc])
            P.op("act", lambda e, c=c, pb=pb, G=G: e.copy(G["BL"][:, c, :], pb[:, 0:8]), reads=[pb], writes=[G["BL"]])
            P.op("act", lambda e, c=c, pb=pb, G=G: e.copy(G["CML"][:, c, :], pb[:, 8:16]), reads=[pb], writes=[G["CML"]])
    for d in range(2):
        G = GS[d]
        P.op("dve", lambda e: e.memset(mcur[:], 0.0), writes=[mcur])
        order = range(NCH) if d == 0 else range(NCH - 1, -1, -1)
        for c in order:
            P.op("dve", lambda e, c=c, G=G: e.tensor_copy(G["M"][:, c, :], mcur[:]), reads=[mcur], writes=[G["M"]])
            P.op("dve", lambda e, c=c, G=G: e.tensor_tensor(out=mcur[:], in0=mcur[:], in1=G["CML"][:, c, :], op=ALU.max), reads=[mcur, G["CML"]], writes=[mcur])
            P.op("dve", lambda e, c=c, G=G: e.tensor_tensor(out=mcur[:], in0=mcur[:], in1=G["BL"][:, c, :], op=ALU.add), reads=[mcur, G["BL"]], writes=[mcur])
            P.op("dve", lambda e, c=c, G=G: e.tensor_copy(G["MN"][:, c, :], mcur[:]), reads=[mcur], writes=[G["MN"]])
        def fl(nm):
            return G[nm][:].rearrange("p c h -> p (c h)")
        for nm in ("MX", "EU", "WI", "NM", "WS", "DEC", "EA"):
            pass
        P.op("dve", lambda e, G=G: e.tensor_tensor(out=G["MX"][:], in0=G["CM"][:], in1=G["M"][:], op=ALU.max), reads=[G["CM"], G["M"]], writes=[G["MX"]])
        P.op("act", lambda e, G=G: e.activation(G["EU"][:], G["MX"][:], AF.Exp, scale=-1.0), reads=[G["MX"]], writes=[G["EU"]])
        P.op("dve", lambda e, G=G: e.tensor_tensor(out=G["WI"][:], in0=G["M"][:], in1=G["MX"][:], op=ALU.subtract), reads=[G["M"], G["MX"]], writes=[G["WI"]])
        P.op("act", lambda e, G=G: e.activation(G["WI"][:], G["WI"][:], AF.Exp), reads=[G["WI"]], writes=[G["WI"]])
        P.op("dve", lambda e, G=G: e.tensor_tensor(out=G["NM"][:], in0=G["BC"][:], in1=G["MX"][:], op=ALU.add), reads=[G["BC"], G["MX"]], writes=[G["NM"]])
        P.op("act", lambda e, G=G: e.activation(G["NM"][:], G["NM"][:], AF.Exp, scale=-1.0), reads=[G["NM"]], writes=[G["NM"]])
        P.op("dve", lambda e, G=G: e.tensor_tensor(out=G["WS"][:], in0=G["BL"][:], in1=G["A"][:], op=ALU.add), reads=[G["BL"], G["A"]], writes=[G["WS"]])
        P.op("dve", lambda e, G=G: e.tensor_tensor(out=G["WS"][:], in0=G["WS"][:], in1=G["MN"][:], op=ALU.subtract), reads=[G["WS"], G["MN"]], writes=[G["WS"]])
        P.op("act", lambda e, G=G: e.activation(G["WS"][:], G["WS"][:], AF.Exp), reads=[G["WS"]], writes=[G["WS"]])
        P.op("dve", lambda e, G=G: e.tensor_tensor(out=G["DEC"][:], in0=G["BL"][:], in1=G["M"][:], op=ALU.add), reads=[G["BL"], G["M"]], writes=[G["DEC"]])
        P.op("dve", lambda e, G=G: e.tensor_tensor(out=G["DEC"][:], in0=G["DEC"][:], in1=G["MN"][:], op=ALU.subtract), reads=[G["DEC"], G["MN"]], writes=[G["DEC"]])
        P.op("act", lambda e, G=G: e.activation(G["DEC"][:], G["DEC"][:], AF.Exp), reads=[G["DEC"]], writes=[G["DEC"]])
        P.op("act", lambda e, G=G: e.activation(G["EA"][:], G["A"][:], AF.Exp), reads=[G["A"]], writes=[G["EA"]])
    return loc


def phase_mlstm_core(C, li, hin, W, S):
    P, T = C.P, C.T
    NCH = T // 128
    with ExitStack() as es:
        R = make_tri(C, es)
        Gtok = P.sb("Gtok", [128, NCH, 32], F32, es)
        phase_mlstm_proj(C, li, hin, W, S, Gtok)
        names = ("BC", "A", "CM", "BL", "CML", "M", "MN", "MX", "EU", "WI", "NM", "WS", "DEC", "EA")
        GS = [{nm: P.sb("G%s%d" % (nm, d), [128, NCH, 8], F32, es) for nm in names} for d in range(2)]
        loc = R.all + [Gtok] + [GS[d][nm] for d in range(2) for nm in names]
        with ExitStack() as es2:
            loc2 = phase_mlstm_gates(C, R, Gtok, GS, es2)
            P.barrier()
            P.release(loc2)
        M01b = []
        mask = [R.M01F, R.M01B]
        Qc = [[P.sb("cQ%d%d" % (d, i), [64, 8, 128], BF16, es) for i in range(2)] for d in range(2)]
        Kc = [[P.sb("cK%d%d" % (d, i), [64, 8, 128], BF16, es) for i in range(2)] for d in range(2)]
        Ktc = [[P.sb("cKt%d%d" % (d, i), [128, 512], BF16, es) for i in range(2)] for d in range(2)]
        Vc = [[P.sb("cV%d%d" % (d, i), [128, 8, 129], BF16, es) for i in range(2)] for d in range(2)]
        Cf = [P.sb("cCf%d" % d, [64, 8, 129], F32, es) for d in range(2)]
        Cb = [P.sb("cCb%d" % d, [64, 8, 129], BF16, es) for d in range(2)]
        Hacc = [[P.sb("cH%d%d" % (d, i), [128, 1024], F32, es) for i in range(2)] for d in range(2)]
        sqk = [P.sb("csqk%d" % i, [128, 128], BF16, es) for i in range(3)]
        t1 = [P.sb("ct1%d" % i, [128, 129], F32, es) for i in range(3)]
        tot = [P.sb("ctot%d" % i, [128, 129], F32, es) for i in range(3)]
        kw = [P.sb("ckw%d" % i, [128, 64], BF16, es) for i in range(3)]
        dd = [P.sb("cdd%d" % i, [128, 2], F32, es) for i in range(3)]
        loc += M01b + sum(Qc, []) + sum(Kc, []) + sum(Ktc, []) + sum(Vc, []) + Cf + Cb + sum(Hacc, []) + sqk + t1 + tot + kw + dd
        for d in range(2):
            P.op("dve", lambda e, d=d: e.memset(Cf[d][:], 0.0), writes=[Cf[d]])
            P.op("dve", lambda e, d=d: e.memset(Cb[d][:], 0.0), writes=[Cb[d]])
        qv = S["mq"].rearrange("(h p) t -> p h t", p=64)
        kv = S["mk"].rearrange("(h p) t -> p h t", p=64)
        hdst = [S["hf"], S["hb"]]
        iters = [(step, d, h) for step in range(NCH) for d in range(2) for h in range(8)]

        def ctx_of(n):
            step, d, h = iters[n]
            c = step if d == 0 else NCH - 1 - step
            return step, d, h, c, slice(c * 128, (c + 1) * 128), GS[d]

        def stA(n):
            step, d, h, c, cs, G = ctx_of(n)
            q, kk_, kt, v = Qc[d][step % 2], Kc[d][step % 2], Ktc[d][step % 2], Vc[d][step % 2]
            if h == 0:
                P.load(q, q[:], qv[:, :, cs])
                P.load(kk_, kk_[:], kv[:, :, cs])
                P.load(kt, kt[:], S["mkt"][cs, :])
                P.load(v, v[:], S["mv"][cs, :, :])
            i3 = n % 3
            pS, pI, pX, pC = C.ps[n % 2], C.ps[2 + n % 2], C.ps[4 + n % 2], C.ps[6 + n % 2]
            col = lambda nm: G[nm][:, c, h:h + 1]
            P.mm(pS, pS[:, :128], kk_[:, h, :], q[:, h, :], reads=[kk_, q])
            P.op("dve", lambda e, ea=col("EA"): e.scalar_tensor_tensor(out=sqk[i3][:], in0=pS[:, :128], scalar=ea, in1=mask[d][:],
                                                                     op0=ALU.mult, op1=ALU.mult),
                 reads=[pS, G["EA"], mask[d]], writes=[sqk[i3]])
            P.mm(pI, pI[:, :129], sqk[i3][:], v[:, h, :], reads=[sqk[i3], v])
            P.mm(pX, pX[:, :129], q[:, h, :], Cb[d][:, h, :], reads=[q, Cb[d]])
            P.op("pool", lambda e, ws=col("WS"): e.tensor_scalar(kw[i3][:], kt[:, h * 64:(h + 1) * 64], ws, None, ALU.mult),
                 reads=[kt, G["WS"]], writes=[kw[i3]])
            P.mm(pC, pC[0:64, :129], kw[i3][:], v[:, h, :], reads=[kw[i3], v])

        def stB(n):
            step, d, h, c, cs, G = ctx_of(n)
            hacc = Hacc[d][step % 2]
            i3 = n % 3
            pS, pI, pX, pC = C.ps[n % 2], C.ps[2 + n % 2], C.ps[4 + n % 2], C.ps[6 + n % 2]
            col = lambda nm: G[nm][:, c, h:h + 1]
            P.op("act", lambda e, eu=col("EU"): e.activation(t1[i3][:], pI[:, :129], AF.Identity, scale=eu), reads=[pI, G["EU"]], writes=[t1[i3]])
            P.op("dve", lambda e, wi=col("WI"): e.scalar_tensor_tensor(out=tot[i3][:], in0=pX[:, :129], scalar=wi, in1=t1[i3][:],
                                                                     op0=ALU.mult, op1=ALU.add),
                 reads=[pX, G["WI"], t1[i3]], writes=[tot[i3]])
            P.op("dve", lambda e: e.scalar_tensor_tensor(out=dd[i3][:, 0:1], in0=tot[i3][:, 128:129], scalar=-1.0, in1=tot[i3][:, 128:129],
                                                        op0=ALU.mult, op1=ALU.max), reads=[tot[i3]], writes=[dd[i3]])
            P.op("dve", lambda e, nmc=col("NM"): e.tensor_tensor(out=dd[i3][:, 0:1], in0=dd[i3][:, 0:1], in1=nmc, op=ALU.max),
                 reads=[dd[i3], G["NM"]], writes=[dd[i3]])
            P.op("dve", lambda e: e.reciprocal(dd[i3][:, 1:2], dd[i3][:, 0:1]), reads=[dd[i3]], writes=[dd[i3]])
            P.op("act", lambda e: e.activation(hacc[:, h * 128:(h + 1) * 128], tot[i3][:, 0:128], AF.Identity, scale=dd[i3][:, 1:2]),
                 reads=[tot[i3], dd[i3]], writes=[hacc])
            P.op("dve", lambda e, dec=G["DEC"][0:64, c, h:h + 1]: e.scalar_tensor_tensor(
                out=Cf[d][:, h, :], in0=Cf[d][:, h, :], scalar=dec, in1=pC[0:64, :129], op0=ALU.mult, op1=ALU.add),
                reads=[Cf[d], G["DEC"], pC], writes=[Cf[d]])
            P.op("act", lambda e: e.copy(Cb[d][:, h, :], Cf[d][:, h, :]), reads=[Cf[d]], writes=[Cb[d]])
            if h == 7:
                P.store(hacc, hdst[d][cs, :], hacc[:])

        stA(0)
        for n in range(len(iters)):
            if n + 1 < len(iters):
                stA(n + 1)
            stB(n)
        P.barrier()
        P.release(loc)


def phase_gn_post(C, S, F, dv, g_ap):
    P, T = C.P, C.T
    NH = F // dv
    NFB_ = F // 128
    with ExitStack() as es:
        gt = P.sb("pg", [128, F], F32, es)
        P.load(gt, gt[:], g_ap)
        A = [P.sb("pA%d" % i, [128, F], F32, es) for i in range(2)]
        Bt = [P.sb("pB%d" % i, [128, F], F32, es) for i in range(2)]
        junks = [P.sb("pjunk%d" % i, [128, dv], F32, es) for i in range(2)]
        sts = [P.sb("pst%d" % i, [128, 4, NH], F32, es) for i in range(2)]
        gate = [P.sb("pgate%d" % i, [128, NFB_, 128], BF16, es) for i in range(2)]
        ao = [P.sb("pao%d" % i, [128, NFB_, 128], BF16, es) for i in range(2)]
        eps_g = P.sb("peps", [128, 1], F32, es)
        loc = [gt, eps_g] + junks + sts + A + Bt + gate + ao
        P.op("dve", lambda e: e.memset(eps_g[:], EPS), writes=[eps_g])
        gv = S["og"].rearrange("(c p) t -> p c t", p=128)
        aov = S["ao2"].rearrange("(c p) t -> p c t", p=128)
        k = 0
        for c in range(T // 128):
            cs = slice(c * 128, (c + 1) * 128)
            a, b, gtile, aot = A[c % 2], Bt[c % 2], gate[c % 2], ao[c % 2]
            st, junk = sts[c % 2], junks[c % 2]
            P.load(a, a[:], S["hf"][cs, :])
            P.load(b, b[:], S["hb"][cs, :])
            P.load(gtile, gtile[:], gv[:, :, cs])
            P.op("pool", lambda e, a=a, b=b: e.tensor_tensor(out=a[:], in0=a[:], in1=b[:], op=ALU.add), reads=[a, b], writes=[a])
            for h in range(NH):
                hs = slice(h * dv, (h + 1) * dv)
                P.op("act", lambda e, a=a, hs=hs, h=h, st=st, junk=junk: e.activation(junk[:], a[:, hs], AF.Identity, accum_out=st[:, 0, h:h + 1]), reads=[a], writes=[junk, st])
                P.op("act", lambda e, a=a, hs=hs, h=h, st=st, junk=junk: e.activation(junk[:], a[:, hs], AF.Square, accum_out=st[:, 1, h:h + 1]), reads=[a], writes=[junk, st])
            P.op("dve", lambda e, st=st: e.tensor_single_scalar(st[:, 0, :], st[:, 0, :], 1.0 / dv, ALU.mult), reads=[st], writes=[st])
            P.op("dve", lambda e, st=st: e.tensor_tensor(out=st[:, 2, :], in0=st[:, 0, :], in1=st[:, 0, :], op=ALU.mult), reads=[st], writes=[st])
            P.op("dve", lambda e, st=st: e.scalar_tensor_tensor(out=st[:, 1, :], in0=st[:, 1, :], scalar=1.0 / dv, in1=st[:, 2, :], op0=ALU.mult, op1=ALU.subtract),
                 reads=[st], writes=[st])
            P.op("act", lambda e, st=st: e.activation(st[:, 1, :], st[:, 1, :], AF.Sqrt, bias=eps_g[:, 0:1]), reads=[st, eps_g], writes=[st])
            P.op("dve", lambda e, st=st: e.reciprocal(st[:, 1, :], st[:, 1, :]), reads=[st], writes=[st])
            P.op("dve", lambda e, st=st: e.scalar_tensor_tensor(out=st[:, 3, :], in0=st[:, 0, :], scalar=-1.0, in1=st[:, 1, :], op0=ALU.mult, op1=ALU.mult),
                 reads=[st], writes=[st])
            for h in range(NH):
                hs = slice(h * dv, (h + 1) * dv)
                P.op("act", lambda e, a=a, hs=hs, h=h, st=st, junk=junk: e.activation(a[:, hs], a[:, hs], AF.Identity, scale=st[:, 1, h:h + 1], bias=st[:, 3, h:h + 1]),
                     reads=[a, st], writes=[a])
            P.op("dve", lambda e, a=a: e.tensor_tensor(out=a[:], in0=a[:], in1=gt[:], op=ALU.mult), reads=[a, gt], writes=[a])
            for fb in range(NFB_):
                pb = C.ps[k % 4]; k += 1
                P.tr(pb, pb[:, 0:128], a[:, fb * 128:(fb + 1) * 128], C.ident[:], reads=[a, C.ident])
                P.op("dve", lambda e, fb=fb, pb=pb, aot=aot, gtile=gtile: e.tensor_tensor(out=aot[:, fb, :], in0=pb[:, 0:128], in1=gtile[:, fb, :], op=ALU.mult),
                     reads=[pb, gtile], writes=[aot])
            P.store(aot, aov[:, :, cs], aot[:])
        P.barrier()
        P.release(loc)


def host_mlstm_params(inp):
    return dict(mlstm_w_in=np.ascontiguousarray(inp["mlstm_w_in"][0]), mlstm_w_out=np.ascontiguousarray(inp["mlstm_w_out"][0]),
                mlstm_bg=np.ascontiguousarray(np.broadcast_to(inp["mlstm_b_gates"][0][None, :], (128, 32))),
                mlstm_ng=np.ascontiguousarray(np.broadcast_to(inp["mlstm_norm_g"][0][None, :], (128, 1024))))


def phase_ret_proj(C, li, hin, W, S, NTK=512):
    P, T = C.P, C.T
    hiv = hview(hin)
    sc = float(256 ** -0.5)
    with ExitStack() as es:
        Win = load_w(C, es, "rWin", W["ret_w_in"])
        g0 = load_small(C, es, "g0", W["norm_g"][li * 4 + 0])
        B = NormBufs(C, es, NTK, "r")
        q_st = P.sb("rq_st", [128, 8, NTK], BF16, es)
        k_st = P.sb("rk_st", [128, 8, NTK], BF16, es)
        g_st = P.sb("rg_st", [128, 16, NTK], BF16, es)
        kt_st = P.sb("rkt_st", [128, NTK // 128, 1024], BF16, es)
        v_st = P.sb("rv_st", [128, NTK // 128, 2048], BF16, es)
        loc = [Win, g0, q_st, k_st, g_st, kt_st, v_st] + B.all
        psS = C.ps[6]
        qv = S["rq"].rearrange("(c p) t -> p c t", p=128)
        kv = S["rk"].rearrange("(c p) t -> p c t", p=128)
        gv = S["og"].rearrange("(c p) t -> p c t", p=128)
        ktv = S["rkt"].rearrange("(tb p) f -> p tb f", p=128)
        vv = S["rv"].rearrange("(tb p) f -> p tb f", p=128)
        k = 0
        for ti in range(T // NTK):
            t0 = ti * NTK
            norm_in(C, hiv, PAD + t0, NTK, g0, B, psS)
            for fb in range(8):
                pb = C.ps[k % 4]; k += 1
                for c in range(8):
                    P.mm(pb, pb[:, :NTK], Win[:, c, fb * 128:(fb + 1) * 128], B.xn[:, c, :], reads=[Win, B.xn], start=(c == 0), stop=(c == 7))
                P.op("act", lambda e, fb=fb, pb=pb: e.copy(q_st[:, fb, :], pb[:, :NTK]), reads=[pb], writes=[q_st])
                pb = C.ps[k % 4]; k += 1
                for c in range(8):
                    P.mm(pb, pb[:, :NTK], Win[:, c, 1024 + fb * 128:1024 + (fb + 1) * 128], B.xn[:, c, :], reads=[Win, B.xn], start=(c == 0), stop=(c == 7))
                P.op("dve", lambda e, fb=fb, pb=pb: e.tensor_single_scalar(k_st[:, fb, :], pb[:, :NTK], sc, ALU.mult), reads=[pb], writes=[k_st])
            P.store(q_st, qv[:, :, t0:t0 + NTK], q_st[:])
            P.store(k_st, kv[:, :, t0:t0 + NTK], k_st[:])
            for fb in range(16):
                pb = C.ps[k % 4]; k += 1
                for c in range(8):
                    P.mm(pb, pb[:, :NTK], Win[:, c, 4096 + fb * 128:4096 + (fb + 1) * 128], B.xn[:, c, :], reads=[Win, B.xn], start=(c == 0), stop=(c == 7))
                P.op("act", lambda e, fb=fb, pb=pb: e.activation(g_st[:, fb, :], pb[:, :NTK], AF.Silu), reads=[pb], writes=[g_st])
            P.store(g_st, gv[:, :, t0:t0 + NTK], g_st[:])
            for tb in range(NTK // 128):
                ts_ = slice(tb * 128, (tb + 1) * 128)
                for half in range(2):
                    pb = C.ps[k % 4]; k += 1
                    for c in range(8):
                        P.mm(pb, pb[:, :512], B.xn[:, c, ts_], Win[:, c, 1024 + half * 512:1024 + (half + 1) * 512], reads=[Win, B.xn], start=(c == 0), stop=(c == 7))
                    P.op("dve", lambda e, tb=tb, half=half, pb=pb: e.tensor_single_scalar(kt_st[:, tb, half * 512:(half + 1) * 512], pb[:, :512], sc, ALU.mult),
                         reads=[pb], writes=[kt_st])
                for q4 in range(4):
                    pb = C.ps[k % 4]; k += 1
                    for c in range(8):
                        P.mm(pb, pb[:, :512], B.xn[:, c, ts_], Win[:, c, 2048 + q4 * 512:2048 + (q4 + 1) * 512], reads=[Win, B.xn], start=(c == 0), stop=(c == 7))
                    if q4 % 2 == 0:
                        P.op("act", lambda e, tb=tb, q4=q4, pb=pb: e.copy(v_st[:, tb, q4 * 512:(q4 + 1) * 512], pb[:, :512]), reads=[pb], writes=[v_st])
                    else:
                        P.op("dve", lambda e, tb=tb, q4=q4, pb=pb: e.tensor_copy(v_st[:, tb, q4 * 512:(q4 + 1) * 512], pb[:, :512]), reads=[pb], writes=[v_st])
            P.store(kt_st, ktv[:, t0 // 128:(t0 + NTK) // 128, :], kt_st[:])
            P.store(v_st, vv[:, t0 // 128:(t0 + NTK) // 128, :], v_st[:])
        P.barrier()
        P.release(loc)


def phase_ret_core(C, W, S):
    P, T = C.P, C.T
    NCH = T // 128
    with ExitStack() as es:
        R = make_tri(C, es)
        dl = P.sb("rdl", [1, 8], F32, es)
        ones1 = P.sb("rones1", [1, 128], F32, es)
        one_t = P.sb("rone", [128, 1], F32, es)
        LG = P.sb("rLG", [128, 8], F32, es)
        RAWd = P.sb("rRAWd", [128, 128], F32, es)
        rawc = P.sb("rrawc", [128, 4], F32, es)
        DM = P.sb("rDM", [128, 8, 128], F32, es)
        XI = P.sb("rXI", [128, 8], F32, es)
        ZE = P.sb("rZE", [128, 8], F32, es)
        GL = P.sb("rGL", [128, 8], F32, es)
        Qc = [[P.sb("rQ%d%d" % (d, i), [128, 8, 128], BF16, es) for i in range(2)] for d in range(2)]
        Kc = [[P.sb("rK%d%d" % (d, i), [128, 8, 128], BF16, es) for i in range(2)] for d in range(2)]
        Ktc = [[P.sb("rKt%d%d" % (d, i), [128, 1024], BF16, es) for i in range(2)] for d in range(2)]
        Vc = [[P.sb("rV%d%d" % (d, i), [128, 2048], BF16, es) for i in range(2)] for d in range(2)]
        Rf = [P.sb("rRf%d" % d, [128, 8, 512], F32, es) for d in range(2)]
        Rb = [P.sb("rRb%d" % d, [128, 8, 512], BF16, es) for d in range(2)]
        Hacc = [[P.sb("rH%d%d" % (d, i), [128, 2048], F32, es) for i in range(2)] for d in range(2)]
        sqk = [P.sb("rsqk%d" % i, [128, 128], BF16, es) for i in range(3)]
        t1 = [P.sb("rt1%d" % i, [128, 512], F32, es) for i in range(3)]
        kz = [P.sb("rkz%d" % i, [128, 256], BF16, es) for i in range(3)]
        loc = R.all + [dl, ones1, one_t, LG, RAWd, rawc, DM, XI, ZE, GL] + sum(Qc, []) + sum(Kc, []) + sum(Ktc, []) + sum(Vc, []) + Rf + Rb + sum(Hacc, []) + sqk + t1 + kz
        P.load(dl, dl[:], W["ret_decay"])
        P.op("dve", lambda e: e.memset(ones1[:], 1.0), writes=[ones1])
        P.op("dve", lambda e: e.memset(one_t[:], 1.0), writes=[one_t])
        P.op("act", lambda e: e.activation(dl[:], dl[:], AF.Exp, scale=-1.0), reads=[dl], writes=[dl])
        P.op("act", lambda e: e.activation(dl[:], dl[:], AF.Ln, bias=one_t[0:1, 0:1]), reads=[dl, one_t], writes=[dl])
        pc = C.ps[0]
        P.mm(pc, pc[:, 0:8], ones1[:], dl[:], reads=[ones1, dl])
        P.op("dve", lambda e: e.tensor_single_scalar(LG[:], pc[:, 0:8], -1.0, ALU.mult), reads=[pc], writes=[LG])
        P.op("pool", lambda e: e.iota(RAWd[:], [[1, 128]], base=0, channel_multiplier=-1, allow_small_or_imprecise_dtypes=True), writes=[RAWd])
        P.op("dve", lambda e: e.scalar_tensor_tensor(out=RAWd[:], in0=RAWd[:], scalar=-1.0, in1=RAWd[:], op0=ALU.mult, op1=ALU.max), reads=[RAWd], writes=[RAWd])
        for j, (b0, cm) in enumerate(((1, 1), (128, -1), (127, -1), (0, 1))):
            P.op("pool", lambda e, j=j, b0=b0, cm=cm: e.iota(rawc[:, j:j + 1], [[0, 1]], base=b0, channel_multiplier=cm, allow_small_or_imprecise_dtypes=True), writes=[rawc])
        mask = [R.M01F, R.M01B]
        for d in range(2):
            for h in range(4):
                dh = d * 4 + h
                P.op("act", lambda e, dh=dh: e.activation(DM[:, dh, :], RAWd[:], AF.Exp, scale=LG[:, dh:dh + 1]), reads=[RAWd, LG], writes=[DM])
                P.op("dve", lambda e, dh=dh, d=d: e.tensor_tensor(out=DM[:, dh, :], in0=DM[:, dh, :], in1=mask[d][:], op=ALU.mult), reads=[DM, mask[d]], writes=[DM])
                P.op("act", lambda e, dh=dh, d=d: e.activation(XI[:, dh:dh + 1], rawc[:, d:d + 1], AF.Exp, scale=LG[:, dh:dh + 1]), reads=[rawc, LG], writes=[XI])
                P.op("act", lambda e, dh=dh, d=d: e.activation(ZE[:, dh:dh + 1], rawc[:, 2 + d:3 + d], AF.Exp, scale=LG[:, dh:dh + 1]), reads=[rawc, LG], writes=[ZE])
        P.op("act", lambda e: e.activation(GL[:], LG[:], AF.Exp, scale=128.0), reads=[LG], writes=[GL])
        for d in range(2):
            P.op("dve", lambda e, d=d: e.memset(Rf[d][:], 0.0), writes=[Rf[d]])
            P.op("pool", lambda e, d=d: e.memset(Rb[d][:], 0.0), writes=[Rb[d]])
        qv = S["rq"].rearrange("(c p) t -> p c t", p=128)
        kv = S["rk"].rearrange("(c p) t -> p c t", p=128)
        hdst = [S["hf"], S["hb"]]
        iters = [(step, d, h) for step in range(NCH) for d in range(2) for h in range(4)]

        def ctx_of(n):
            step, d, h = iters[n]
            c = step if d == 0 else NCH - 1 - step
            return step, d, h, c, slice(c * 128, (c + 1) * 128)

        def stA(n):
            step, d, h, c, cs = ctx_of(n)
            q, kk_, kt, v = Qc[d][step % 2], Kc[d][step % 2], Ktc[d][step % 2], Vc[d][step % 2]
            if h == 0:
                P.load(q, q[:], qv[:, :, cs])
                P.load(kk_, kk_[:], kv[:, :, cs])
                P.load(kt, kt[:], S["rkt"][cs, :])
                P.load(v, v[:], S["rv"][cs, :])
            dh = d * 4 + h
            i3 = n % 3
            pS, pI, pX = C.ps[n % 2], C.ps[2 + n % 2], C.ps[4 + n % 2]
            vs = v[:, h * 512:(h + 1) * 512]
            for kc in range(2):
                P.mm(pS, pS[:, :128], kk_[:, h * 2 + kc, :], q[:, h * 2 + kc, :], reads=[kk_, q], start=(kc == 0), stop=(kc == 1))
            P.op("dve", lambda e: e.tensor_tensor(out=sqk[i3][:], in0=pS[:, :128], in1=DM[:, dh, :], op=ALU.mult),
                 reads=[pS, DM], writes=[sqk[i3]])
            P.mm(pI, pI[:, :512], sqk[i3][:], vs, reads=[sqk[i3], v])
            for kc in range(2):
                P.mm(pX, pX[:, :512], q[:, h * 2 + kc, :], Rb[d][:, h * 2 + kc, :], reads=[q, Rb[d]], start=(kc == 0), stop=(kc == 1))
            P.op("act", lambda e: e.activation(kz[i3][:], kt[:, h * 256:(h + 1) * 256], AF.Identity, scale=ZE[:, dh:dh + 1]),
                 reads=[kt, ZE], writes=[kz[i3]])

        def stB(n):
            step, d, h, c, cs = ctx_of(n)
            v = Vc[d][step % 2]
            hacc = Hacc[d][step % 2]
            dh = d * 4 + h
            i3 = n % 3
            pS, pI, pX = C.ps[n % 2], C.ps[2 + n % 2], C.ps[4 + n % 2]
            vs = v[:, h * 512:(h + 1) * 512]
            P.op("act", lambda e: e.copy(t1[i3][:], pI[:, :512]), reads=[pI], writes=[t1[i3]])
            P.op("dve", lambda e: e.scalar_tensor_tensor(
                out=hacc[:, h * 512:(h + 1) * 512], in0=pX[:, :512], scalar=XI[:, dh:dh + 1], in1=t1[i3][:], op0=ALU.mult, op1=ALU.add),
                reads=[pX, XI, t1[i3]], writes=[hacc])
            for kc in range(2):
                pC = C.ps[6 + kc]
                P.mm(pC, pC[:, :512], kz[i3][:, kc * 128:(kc + 1) * 128], vs, reads=[kz[i3], v])
                P.op("dve", lambda e, kc=kc, pC=pC: e.scalar_tensor_tensor(
                    out=Rf[d][:, h * 2 + kc, :], in0=Rf[d][:, h * 2 + kc, :], scalar=GL[:, dh:dh + 1], in1=pC[:, :512], op0=ALU.mult, op1=ALU.add),
                    reads=[Rf[d], GL, pC], writes=[Rf[d]])
                P.op("act", lambda e, kc=kc: e.copy(Rb[d][:, h * 2 + kc, :], Rf[d][:, h * 2 + kc, :]), reads=[Rf[d]], writes=[Rb[d]])
            if h == 3:
                P.store(hacc, hdst[d][cs, :], hacc[:])

        stA(0)
        for n in range(len(iters)):
            if n + 1 < len(iters):
                stA(n + 1)
            stB(n)
        P.barrier()
        P.release(loc)


def host_ret_params(inp):
    return dict(ret_w_in=np.ascontiguousarray(inp["ret_w_in"][0]), ret_w_o=np.ascontiguousarray(inp["ret_w_o"][0]),
                ret_decay=np.ascontiguousarray(inp["ret_decay_logit"][0].reshape(1, 8)),
                ret_ng=np.ascontiguousarray(np.broadcast_to(inp["ret_norm_g"][0][None, :], (128, 2048))))


SEQ = 8192
NCORES = 4


def build_full(T, hp_shapes, layers=(0, 1, 2, 3)):
    nc = bass.Bass("TRN2", target_bir_lowering=False)
    with ExitStack() as es:
        C = make_ctx(nc, es, T)
        xd = nc.dram_tensor("x", [T, 1024], F32, kind="ExternalInput").ap()
        od = nc.dram_tensor("out", [T, 1024], F32, kind="ExternalOutput").ap()
        W = declare_inputs(nc, hp_shapes)
        ha = nc.dram_tensor("ha", [1024, T + 2 * PAD], F32, kind="Internal").ap()
        hb = nc.dram_tensor("hb", [1024, T + 2 * PAD], F32, kind="Internal").ap()

        def scratch(specs):
            return {nm: nc.dram_tensor("s_" + nm, list(shp), dt, kind="Internal").ap() for nm, shp, dt in specs}

        zero_pads(C, ha)
        zero_pads(C, hb)
        phase_in(C, xd, ha)
        if 0 in layers:
            S = scratch((("qn", (1024, T), BF16), ("kn", (1024, T), BF16), ("qr", (512, T), BF16), ("kr", (64, T), BF16),
                         ("v", (T, 1024), BF16), ("ao", (1024, T), BF16)))
            phase_mla_proj(C, 0, ha, W, S)
            phase_mla_core(C, S)
            phase_tail(C, S["ao"], W["mla_w_o"], W["norm_g"][1], ha, hb)
            phase_ffn(C, 0, hb, ha, W)
        if 1 in layers:
            S = scratch((("dqn", (1024, T), BF16), ("dkn", (1024, T), BF16), ("dv", (T, 1024), BF16), ("dao", (1024, T), BF16)))
            S = dict(qn=S["dqn"], kn=S["dkn"], v=S["dv"], ao=S["dao"])
            phase_diff_proj(C, 1, ha, W, S)
            phase_diff_core(C, 1, W, S)
            phase_tail(C, S["ao"], W["diff_w_o"], W["norm_g"][5], ha, hb)
            phase_ffn(C, 1, hb, ha, W)
        if 2 in layers:
            S = scratch((("mq", (512, T), BF16), ("mk", (512, T), BF16), ("mkt", (T, 512), BF16), ("mv", (T, 8, 129), BF16),
                         ("mog", (1024, T), BF16), ("mhf", (T, 1024), F32), ("mhb", (T, 1024), F32), ("mao2", (1024, T), BF16)))
            S.update(og=S["mog"], hf=S["mhf"], hb=S["mhb"], ao2=S["mao2"])
            phase_mlstm_core(C, 2, ha, W, S)
            phase_gn_post(C, S, 1024, 128, W["mlstm_ng"])
            phase_tail(C, S["ao2"], W["mlstm_w_out"], W["norm_g"][9], ha, hb)
            phase_ffn(C, 2, hb, ha, W)
        if 3 in layers:
            S = scratch((("rq", (1024, T), BF16), ("rk", (1024, T), BF16), ("rkt", (T, 1024), BF16), ("rv", (T, 2048), BF16),
                         ("rog", (2048, T), BF16), ("rhf", (T, 2048), F32), ("rhb", (T, 2048), F32), ("rao2", (2048, T), BF16)))
            S.update(og=S["rog"], hf=S["rhf"], hb=S["rhb"], ao2=S["rao2"])
            phase_ret_proj(C, 3, ha, W, S)
            phase_ret_core(C, W, S)
            phase_gn_post(C, S, 2048, 512, W["ret_ng"])
            phase_tail(C, S["ao2"], W["ret_w_o"], W["norm_g"][13], ha, hb)
            phase_ffn(C, 3, hb, ha, W)
        phase_out(C, ha, od)
        C.P.emit()
    return nc, C


def host_params(inp, T):
    inp = {k: np.asarray(v, dtype=np.float32) for k, v in inp.items() if k != "x"}
    cw, cb, ng = host_ffn_params(inp)
    hp = dict(ffn_w_up=np.ascontiguousarray(inp["ffn_w_up"]), ffn_w_down=np.ascontiguousarray(inp["ffn_w_down"]),
              ffn_cw=cw, ffn_cb=cb, norm_g=ng)
    hp.update(host_mla_params(inp, np.arange(T)))
    hp.update(host_diff_params(inp))
    hp.update(host_mlstm_params(inp))
    hp.update(host_ret_params(inp))
    return hp


REAL_CORES = (0, 1, 4, 5)


def kernel(**inputs):
    x = np.asarray(inputs["x"], dtype=np.float32)
    Bn, T, _ = x.shape
    hp = host_params(inputs, T)
    nc, C = build_full(T, {k: v.shape for k, v in hp.items()})
    ident = np.eye(128, dtype=np.float32)
    zx = np.zeros((T, 1024), np.float32)
    in_maps = []
    real = REAL_CORES[:Bn]
    for c in range(8):
        xb = np.ascontiguousarray(x[real.index(c)]) if c in real else zx
        in_maps.append(dict(x=xb, ident_in=ident, **hp))
    res = run_bass_kernel_spmd(nc, in_maps, core_ids=list(range(8)))
    return np.stack([np.asarray(res.results[c]["out"]) for c in real]).astype(np.float32)
```

```python
import contextlib
import numpy as np
import concourse.bass as bass
import concourse.mybir as mybir

F32 = mybir.dt.float32
BF16 = mybir.dt.bfloat16
AF = mybir.ActivationFunctionType
ALU = mybir.AluOpType
AX = mybir.AxisListType

ENGS = ("pe", "dve", "act", "pool", "sp")


class Res:
    def __init__(self, name, t=None):
        self.name = name
        self.t = t
        self.lw = None
        self.rd = []
        self.sem = None
        self.psum = False

    def __getitem__(self, idx):
        return self.t[idx]


class Sem:
    def __init__(self, h):
        self.h = h
        self.n = 0


class Prog:
    def __init__(self, nc, es):
        self.nc = nc
        self.es = es
        self.ops = []
        self.eng_ops = {e: [] for e in ENGS}
        self.engsem = {e: es.enter_context(nc.semaphore("S_" + e)) for e in ENGS}
        self.res = []
        self.free_sems = []
        self.sems = []

    def sb(self, name, shape, dt, es=None):
        self.uid = getattr(self, "uid", 0) + 1
        name = "%s_u%d" % (name, self.uid)
        t = (es or self.es).enter_context(self.nc.sbuf_tensor(name, list(shape), dt))
        r = Res(name, t)
        self.res.append(r)
        return r

    def ps(self, name, shape, dt=F32, es=None):
        t = (es or self.es).enter_context(self.nc.psum_tensor(name, list(shape), dt))
        r = Res(name, t)
        r.psum = True
        self.res.append(r)
        return r

    def _dsem(self, r):
        if r.sem is None:
            if self.free_sems:
                r.sem = self.free_sems.pop()
            else:
                h = self.es.enter_context(self.nc.semaphore("D%d" % len(self.sems)))
                r.sem = Sem(h)
                self.sems.append(r.sem)
        return r.sem

    def release(self, rs):
        for r in rs:
            if r.sem is not None:
                self.free_sems.append(r.sem)
                r.sem = None
            if r in self.res:
                self.res.remove(r)

    def op(self, eng, fn, reads=(), writes=(), dma=None, waw=True):
        idx = len(self.ops)
        tok = ("op", idx)
        if dma is not None:
            sm = self._dsem(dma)
            sm.n += 1
            tok = ("dma", sm, sm.n)
        deps = set()
        for r in reads:
            if r.lw is not None:
                deps.add(r.lw)
            if r.psum:
                for d in r.rd:
                    if d[0] == "op" and self.ops[d[1]]["eng"] != eng:
                        deps.add(d)
        for w in writes:
            if w.lw is not None:
                d = w.lw
                if d[0] == "op" and self.ops[d[1]]["eng"] == eng:
                    pass
                elif d[0] == "dma" and dma is not None and not waw and d[1] is dma.sem:
                    pass
                else:
                    deps.add(d)
            for d in w.rd:
                if d[0] == "op" and self.ops[d[1]]["eng"] == eng:
                    continue
                deps.add(d)
        if eng == "pe":
            deps = {d for d in deps if not (d[0] == "op" and self.ops[d[1]]["eng"] == "pe")}
        deps.discard(tok)
        o = dict(eng=eng, fn=fn, deps=deps, dma=dma, tok=tok, marked=False,
                 dmasem=(dma.sem if dma is not None else None))
        self.ops.append(o)
        self.eng_ops[eng].append(idx)
        for d in deps:
            if d[0] == "op":
                self.ops[d[1]]["marked"] = True
        for r in reads:
            r.rd.append(tok)
        for w in writes:
            w.lw = tok
            w.rd = []
        return idx

    def barrier(self):
        last = {}
        for e in ENGS:
            for i in reversed(self.eng_ops[e]):
                if self.ops[i]["fn"] is not None and self.ops[i]["dma"] is None:
                    last[e] = i
                    break
        dmas = [("dma", sm, sm.n) for sm in self.sems if sm.n > 0]
        for e in ENGS:
            deps = set(dmas)
            for e2, i in last.items():
                if e2 != e:
                    deps.add(("op", i))
                    self.ops[i]["marked"] = True
            idx = len(self.ops)
            self.ops.append(dict(eng=e, fn=None, deps=deps, dma=None, tok=("op", idx), marked=False))
            self.eng_ops[e].append(idx)
        for r in self.res:
            r.lw = None
            r.rd = []

    def emit(self):
        nc = self.nc
        semval = {}
        cnt = {e: 0 for e in ENGS}
        for i, o in enumerate(self.ops):
            if o["marked"] and o["dma"] is None and o["fn"] is not None:
                cnt[o["eng"]] += 1
                semval[i] = cnt[o["eng"]]
        self.stats = dict(cnt)

        def run(ename, eng):
            waited = {}
            for i in self.eng_ops[ename]:
                o = self.ops[i]
                for d in sorted(o["deps"], key=lambda d: (d[0], d[1] if d[0] == "op" else id(d[1]))):
                    if d[0] == "op":
                        p = self.ops[d[1]]
                        if p["fn"] is None:
                            continue
                        sem, val = self.engsem[p["eng"]], semval[d[1]]
                    else:
                        sem, val = d[1].h, 16 * d[2]
                    k = id(sem)
                    if waited.get(k, 0) < val:
                        eng.wait_ge(sem, val)
                        waited[k] = val
                if o["fn"] is None:
                    continue
                ins = o["fn"](eng)
                if o["dma"] is not None:
                    ins.then_inc(o["dmasem"].h, 16)
                elif o["marked"]:
                    ins.then_inc(self.engsem[ename], 1)

        with nc.Block() as block:
            @block.tensor
            def _(e):
                run("pe", e)

            @block.vector
            def _(e):
                run("dve", e)

            @block.scalar
            def _(e):
                run("act", e)

            @block.gpsimd
            def _(e):
                run("pool", e)

            @block.sync
            def _(e):
                run("sp", e)

    def mm(self, out_r, out_ap, lhsT, rhs, reads, start=True, stop=True):
        return self.op("pe", lambda e: e.matmul(out_ap, lhsT, rhs, start=start, stop=stop),
                       reads=reads, writes=[out_r])

    def tr(self, out_r, out_ap, in_ap, ident_ap, reads):
        return self.op("pe", lambda e: e.transpose(out_ap, in_ap, ident_ap), reads=reads, writes=[out_r])

    def load(self, r, out_ap, in_ap, q="sp", waw=False):
        return self.op(q, lambda e: e.dma_start(out=out_ap, in_=in_ap), writes=[r], dma=r, waw=waw)

    def store(self, r, out_ap, in_ap, q="sp"):
        return self.op(q, lambda e: e.dma_start(out=out_ap, in_=in_ap), reads=[r], dma=r)

from contextlib import ExitStack
from concourse.bass_utils import run_bass_kernel_spmd

D = 1024
FH = 2816
NFB = FH // 128
PAD = 8
EPS = 1e-6


class Ctx:
    pass


def make_ctx(nc, es, T, NT=384):
    C = Ctx()
    C.nc = nc
    C.T = T
    C.NT = NT
    C.P = Prog(nc, es)
    P = C.P
    C.ps = [P.ps("ps%d" % i, [128, 512], F32) for i in range(8)]
    C.ones_bf = P.sb("ones_bf", [128, 128], BF16)
    C.ident = P.sb("ident", [128, 128], F32)
    C.zeros = P.sb("zeros", [128, 8, PAD], F32)
    P.op("dve", lambda e: e.memset(C.ones_bf[:], 1.0), writes=[C.ones_bf])
    P.op("dve", lambda e: e.memset(C.zeros[:], 0.0), writes=[C.zeros])
    ident_d = nc.dram_tensor("ident_in", [128, 128], F32, kind="ExternalInput").ap()
    P.load(C.ident, C.ident[:], ident_d[:, :])
    C.ones_f = P.sb("ones_fc", [128, 128], F32)
    P.op("dve", lambda e: e.memset(C.ones_f[:], 1.0), writes=[C.ones_f])
    C.eps_t = P.sb("eps_t", [128, 1], F32)
    P.op("dve", lambda e: e.memset(C.eps_t[:], EPS), writes=[C.eps_t])
    return C


def hview(h):
    return h.rearrange("(c p) w -> p c w", p=128)


def zero_pads(C, h):
    P = C.P
    hv = hview(h)
    T = C.T
    P.store(C.zeros, hv[:, :, 0:PAD], C.zeros[:])
    P.store(C.zeros, hv[:, :, PAD + T:PAD + T + PAD], C.zeros[:])


def phase_in(C, x, h):
    P, T = C.P, C.T
    hv = hview(h)
    with ExitStack() as es:
        xin = [P.sb("xin%d" % i, [128, D], F32, es) for i in range(8)]
        stage = [P.sb("stg%d" % i, [128, 8, 512], F32, es) for i in range(2)]
        loc = xin + stage
        k = 0
        for g in range(T // 512):
            xs = []
            for j in range(4):
                xt = xin[(g % 2) * 4 + j]
                r0 = g * 512 + j * 128
                P.load(xt, xt[:], x[r0:r0 + 128, :])
                xs.append(xt)
            st = stage[g % 2]
            for c in range(8):
                pb = C.ps[k % 4]
                for j in range(4):
                    P.tr(pb, pb[:, j * 128:(j + 1) * 128], xs[j][:, c * 128:(c + 1) * 128], C.ident[:],
                         reads=[xs[j], C.ident])
                if k % 2 == 0:
                    P.op("dve", lambda e, st=st, c=c, pb=pb: e.tensor_copy(st[:, c, :], pb[:]),
                         reads=[pb], writes=[st])
                else:
                    P.op("act", lambda e, st=st, c=c, pb=pb: e.copy(st[:, c, :], pb[:]),
                         reads=[pb], writes=[st])
                k += 1
            P.store(st, hv[:, :, PAD + g * 512:PAD + (g + 1) * 512], st[:])
        P.barrier()
        P.release(loc)


def phase_out(C, h, out):
    P, T = C.P, C.T
    hv = hview(h)
    with ExitStack() as es:
        hin = [P.sb("hin%d" % i, [128, 8, 512], F32, es) for i in range(2)]
        ot = [P.sb("ot%d" % i, [128, D], F32, es) for i in range(4)]
        loc = hin + ot
        k = 0
        n = 0
        for g in range(T // 512):
            hi = hin[g % 2]
            P.load(hi, hi[:], hv[:, :, PAD + g * 512:PAD + (g + 1) * 512])
            for j in range(4):
                o = ot[n % 4]
                n += 1
                for half in range(2):
                    pb = C.ps[k % 4]
                    for q in range(4):
                        c = half * 4 + q
                        P.tr(pb, pb[:, q * 128:(q + 1) * 128], hi[:, c, j * 128:(j + 1) * 128], C.ident[:],
                             reads=[hi, C.ident])
                    if k % 2 == 0:
                        P.op("dve", lambda e, o=o, half=half, pb=pb: e.tensor_copy(o[:, half * 512:(half + 1) * 512], pb[:]),
                             reads=[pb], writes=[o])
                    else:
                        P.op("act", lambda e, o=o, half=half, pb=pb: e.copy(o[:, half * 512:(half + 1) * 512], pb[:]),
                             reads=[pb], writes=[o])
                    k += 1
                r0 = g * 512 + j * 128
                P.store(o, out[r0:r0 + 128, :], o[:])
        P.barrier()
        P.release(loc)


def rstd_from_sumsq(C, ps_sum, n, width, tmp, rstd):
    P = C.P
    P.op("act", lambda e: e.activation(tmp[:, :width], ps_sum[:, :width], AF.Sqrt, bias=C.eps_t[:, 0:1], scale=1.0 / n),
         reads=[ps_sum, C.eps_t], writes=[tmp])
    P.op("dve", lambda e: e.reciprocal(rstd[:, :width], tmp[:, :width]), reads=[tmp], writes=[rstd])


def phase_ffn(C, li, hin, hout, W):
    P, T, NT = C.P, C.T, C.NT
    NO = NT - 2
    hiv, hov = hview(hin), hview(hout)
    with ExitStack() as es:
        Wup = P.sb("Wup", [128, 8, 2 * FH], BF16, es)
        Wdn = P.sb("Wdn", [128, NFB, D], BF16, es)
        cw = P.sb("cw", [128, 44 * 3], F32, es)
        cb = P.sb("cb", [128, 44], F32, es)
        g2 = P.sb("g2", [128, 8], F32, es)
        g3 = P.sb("g3", [128, 8], F32, es)
        H = P.sb("H", [128, 8, NT], F32, es)
        xn = P.sb("xn", [128, 8, NT], BF16, es)
        hm = P.sb("hm", [128, NFB, NT], BF16, es)
        fT = P.sb("fT", [128, 8, NT], F32, es)
        sq = [P.sb("sq%d" % i, [128, NT], BF16, es) for i in range(2)]
        tmp = P.sb("tmp", [128, NT], F32, es)
        rstd = P.sb("rstd", [128, NT], F32, es)
        rstd2 = P.sb("rstd2", [128, NT], F32, es)
        tg = [[P.sb("tg%d%d" % (i, j), [128, NT], F32, es) for j in range(2)] for i in range(3)]
        tv = [[P.sb("tv%d%d" % (i, j), [128, NT], F32, es) for j in range(2)] for i in range(3)]
        loc = [Wup, Wdn, cw, cb, g2, g3, H, xn, hm, fT, tmp, rstd, rstd2] + sq + sum(tg, []) + sum(tv, [])

        wu = W["ffn_w_up"][li].rearrange("(c p) f -> p c f", p=128)
        for c in range(8):
            P.load(Wup, Wup[:, c, :], wu[:, c, :], q="pool")
        wd = W["ffn_w_down"][li].rearrange("(c p) d -> p c d", p=128)
        for c0 in range(0, NFB, 6):
            c1 = min(NFB, c0 + 6)
            P.load(Wdn, Wdn[:, c0:c1, :], wd[:, c0:c1, :], q="pool")
        P.load(cw, cw[:], W["ffn_cw"][li])
        P.load(cb, cb[:], W["ffn_cb"][li])
        P.load(g2, g2[:], W["norm_g"][li * 4 + 2])
        P.load(g3, g3[:], W["norm_g"][li * 4 + 3])

        import os
        STOP = int(os.environ.get("FFN_STOP", "99"))
        starts = []
        s = 0
        while True:
            if s + NO >= T:
                starts.append(T - NO)
                break
            starts.append(s)
            s += NO
        psS = C.ps[6]
        for ti, s in enumerate(starts):
            c0 = PAD + s - 1
            P.load(H, H[:], hiv[:, :, c0:c0 + NT])
            for c in range(8):
                sqt = sq[c % 2]
                P.op("act", lambda e, sqt=sqt, c=c: e.activation(sqt[:], H[:, c, :], AF.Square),
                     reads=[H], writes=[sqt])
                P.mm(psS, psS[:, :NT], C.ones_bf[:], sqt[:], reads=[C.ones_bf, sqt], start=(c == 0), stop=(c == 7))
            if STOP <= 1:
                continue
            rstd_from_sumsq(C, psS, D, NT, tmp, rstd)
            if STOP <= 2:
                continue
            for c in range(8):
                P.op("dve", lambda e, c=c: e.scalar_tensor_tensor(out=xn[:, c, :], in0=H[:, c, :], scalar=g2[:, c:c + 1],
                                                                  in1=rstd[:], op0=ALU.mult, op1=ALU.mult),
                     reads=[H, g2, rstd], writes=[xn])
            if STOP <= 3:
                continue
            for fb in range(NFB):
                pg = C.ps[(fb % 3) * 2]
                pv = C.ps[(fb % 3) * 2 + 1]
                for c in range(8):
                    P.mm(pg, pg[:, :NT], Wup[:, c, fb * 128:(fb + 1) * 128], xn[:, c, :], reads=[Wup, xn],
                         start=(c == 0), stop=(c == 7))
                for c in range(8):
                    P.mm(pv, pv[:, :NT], Wup[:, c, FH + fb * 128:FH + (fb + 1) * 128], xn[:, c, :], reads=[Wup, xn],
                         start=(c == 0), stop=(c == 7))
                outs = []
                for (pp, tt, fi) in ((pg, tg[fb % 3], fb), (pv, tv[fb % 3], fb + NFB)):
                    t1, t2 = tt
                    w0 = cw[:, fi * 3 + 0:fi * 3 + 1]
                    w1 = cw[:, fi * 3 + 1:fi * 3 + 2]
                    w2 = cw[:, fi * 3 + 2:fi * 3 + 3]
                    bb = cb[:, fi:fi + 1]
                    P.op("act", lambda e, t1=t1, pp=pp, w1=w1, bb=bb: e.activation(t1[:, :NO], pp[:, 1:1 + NO], AF.Identity,
                                                                                  bias=bb, scale=w1),
                         reads=[pp, cw, cb], writes=[t1])
                    P.op("dve", lambda e, t1=t1, t2=t2, pp=pp, w0=w0: e.scalar_tensor_tensor(
                        out=t2[:, :NO], in0=pp[:, 0:NO], scalar=w0, in1=t1[:, :NO], op0=ALU.mult, op1=ALU.add),
                        reads=[pp, cw, t1], writes=[t2])
                    P.op("dve", lambda e, t1=t1, t2=t2, pp=pp, w2=w2: e.scalar_tensor_tensor(
                        out=t1[:, :NO], in0=pp[:, 2:2 + NO], scalar=w2, in1=t2[:, :NO], op0=ALU.mult, op1=ALU.add),
                        reads=[pp, cw, t2], writes=[t1])
                    outs.append((t1, t2))
                (cg, gbuf), (cv, _) = outs
                P.op("act", lambda e, gbuf=gbuf, cg=cg: e.activation(gbuf[:, :NO], cg[:, :NO], AF.Gelu_apprx_tanh),
                     reads=[cg], writes=[gbuf])
                P.op("pool", lambda e, fb=fb, gbuf=gbuf, cv=cv: e.tensor_tensor(out=hm[:, fb, :NO], in0=gbuf[:, :NO], in1=cv[:, :NO],
                                                                                op=ALU.mult),
                     reads=[gbuf, cv], writes=[hm])
            if STOP <= 4:
                continue
            for db in range(8):
                pd = C.ps[db % 6]
                for fc in range(NFB):
                    P.mm(pd, pd[:, :NO], Wdn[:, fc, db * 128:(db + 1) * 128], hm[:, fc, :NO], reads=[Wdn, hm],
                         start=(fc == 0), stop=(fc == NFB - 1))
                sqt = sq[db % 2]
                P.op("dve", lambda e, db=db, pd=pd: e.tensor_copy(fT[:, db, :NO], pd[:, :NO]), reads=[pd], writes=[fT])
                P.op("act", lambda e, sqt=sqt, db=db: e.activation(sqt[:, :NO], fT[:, db, :NO], AF.Square),
                     reads=[fT], writes=[sqt])
                if STOP >= 6:
                    P.mm(psS, psS[:, :NO], C.ones_bf[:], sqt[:, :NO], reads=[C.ones_bf, sqt], start=(db == 0), stop=(db == 7))
            if STOP <= 6:
                continue
            rstd_from_sumsq(C, psS, D, NO, tmp, rstd2)
            if STOP <= 7:
                continue
            for c in range(8):
                P.op("dve", lambda e, c=c: e.scalar_tensor_tensor(out=fT[:, c, :NO], in0=fT[:, c, :NO], scalar=g3[:, c:c + 1],
                                                                  in1=rstd2[:, :NO], op0=ALU.mult, op1=ALU.mult),
                     reads=[fT, g3, rstd2], writes=[fT])
                P.op("pool", lambda e, c=c: e.tensor_tensor(out=fT[:, c, :NO], in0=fT[:, c, :NO], in1=H[:, c, 1:1 + NO], op=ALU.add),
                     reads=[fT, H], writes=[fT])
            if STOP <= 8:
                continue
            P.store(fT, hov[:, :, PAD + s:PAD + s + NO], fT[:, :, :NO])
        P.barrier()
        P.release(loc)


def gelu_tanh(C, out, x, n):
    P = C.P
    P.op("act", lambda e: e.activation(out[:, :n], x[:, :n], AF.Square), reads=[x], writes=[out])
    P.op("pool", lambda e: e.tensor_scalar(out[:, :n], out[:, :n], 0.044715, 1.0, ALU.mult, ALU.add), reads=[out], writes=[out])
    P.op("pool", lambda e: e.tensor_tensor(out=out[:, :n], in0=out[:, :n], in1=x[:, :n], op=ALU.mult), reads=[out, x], writes=[out])
    P.op("act", lambda e: e.activation(out[:, :n], out[:, :n], AF.Sigmoid, scale=1.5957691216057308), reads=[out], writes=[out])
    P.op("pool", lambda e: e.tensor_tensor(out=out[:, :n], in0=out[:, :n], in1=x[:, :n], op=ALU.mult), reads=[out, x], writes=[out])


def declare_inputs(nc, shapes):
    W = {}
    for name, shp in shapes.items():
        W[name] = nc.dram_tensor(name, list(shp), F32, kind="ExternalInput").ap()
    return W


def host_ffn_params(inp):
    L = inp["ffn_conv_w"].shape[0]
    cw = np.ascontiguousarray(inp["ffn_conv_w"].transpose(0, 2, 1).reshape(L, 44, 128, 3).transpose(0, 2, 1, 3).reshape(L, 128, 132))
    cb = np.ascontiguousarray(inp["ffn_conv_b"].reshape(L, 44, 128).transpose(0, 2, 1))
    ng = np.ascontiguousarray(inp["norm_g"].reshape(L * 4, 8, 128).transpose(0, 2, 1))
    return cw, cb, ng


def load_w(C, es, name, ap, q="pool"):
    P = C.P
    K, N = ap.shape
    kc = K // 128
    t = P.sb(name, [128, kc, N], BF16, es)
    v = ap.rearrange("(c p) n -> p c n", p=128)
    step = max(1, 4096 // N)
    for c0 in range(0, kc, step):
        c1 = min(kc, c0 + step)
        P.load(t, t[:, c0:c1, :], v[:, c0:c1, :], q=q)
    return t


def load_small(C, es, name, ap):
    P = C.P
    t = P.sb(name, list(ap.shape), F32, es)
    P.load(t, t[:], ap)
    return t


class NormBufs:
    def __init__(self, C, es, n, pref):
        P = C.P
        self.H = P.sb(pref + "H", [128, 8, n], F32, es)
        self.xns = [P.sb(pref + "xn%d" % i, [128, 8, n], BF16, es) for i in range(2)]
        self.xn = self.xns[0]
        self.ncall = 0
        self.sq = [P.sb(pref + "sq%d" % i, [128, n], BF16, es) for i in range(2)]
        self.tmp = P.sb(pref + "tmp", [128, n], F32, es)
        self.rstd = P.sb(pref + "rstd", [128, n], F32, es)
        self.all = [self.H, self.tmp, self.rstd] + self.xns + self.sq


def norm_in(C, hv, col0, n, g, B, psS):
    P = C.P
    B.xn = B.xns[B.ncall % 2]
    B.ncall += 1
    P.load(B.H, B.H[:, :, :n], hv[:, :, col0:col0 + n])
    for c in range(8):
        sqt = B.sq[c % 2]
        P.op("act", lambda e, sqt=sqt, c=c: e.activation(sqt[:, :n], B.H[:, c, :n], AF.Square), reads=[B.H], writes=[sqt])
        P.mm(psS, psS[:, :n], C.ones_bf[:], sqt[:, :n], reads=[C.ones_bf, sqt], start=(c == 0), stop=(c == 7))
    rstd_from_sumsq(C, psS, D, n, B.tmp, B.rstd)
    for c in range(8):
        P.op("dve", lambda e, c=c, xn=B.xn: e.scalar_tensor_tensor(out=xn[:, c, :n], in0=B.H[:, c, :n], scalar=g[:, c:c + 1],
                                                          in1=B.rstd[:, :n], op0=ALU.mult, op1=ALU.mult),
             reads=[B.H, g, B.rstd], writes=[B.xn])


def sub_norm(C, src, nch, n, nfeat, g, dst, sq, tmp, rstd, psS, extra_scale=None):
    P = C.P
    for c in range(nch):
        sqt = sq[c % 2]
        P.op("act", lambda e, sqt=sqt, c=c: e.activation(sqt[:, :n], src[:, c, :n], AF.Square), reads=[src], writes=[sqt])
        P.mm(psS, psS[:, :n], C.ones_bf[:], sqt[:, :n], reads=[C.ones_bf, sqt], start=(c == 0), stop=(c == nch - 1))
    rstd_from_sumsq(C, psS, nfeat, n, tmp, rstd)
    for c in range(nch):
        P.op("dve", lambda e, c=c: e.scalar_tensor_tensor(out=dst[:, c, :n], in0=src[:, c, :n], scalar=g[:, c:c + 1],
                                                          in1=rstd[:, :n], op0=ALU.mult, op1=ALU.mult),
             reads=[src, g, rstd], writes=[dst])


def phase_tail(C, ao, Wo_ap, g1_ap, hin, hout, NTK=512):
    P, T = C.P, C.T
    KF = ao.shape[0]
    kc = KF // 128
    hiv, hov = hview(hin), hview(hout)
    aov = ao.rearrange("(c p) t -> p c t", p=128)
    with ExitStack() as es:
        Wo = load_w(C, es, "Wo", Wo_ap)
        g1 = load_small(C, es, "g1", g1_ap)
        A = [P.sb("tA%d" % i, [128, kc, NTK], BF16, es) for i in range(2)]
        H = [P.sb("tH%d" % i, [128, 8, NTK], F32, es) for i in range(2)]
        fTs = [P.sb("tfT%d" % i, [128, 8, NTK], F32, es) for i in range(2)]
        sq = [P.sb("tsq%d" % i, [128, NTK], BF16, es) for i in range(4)]
        tmps = [P.sb("ttmp%d" % i, [128, NTK], F32, es) for i in range(2)]
        rstds = [P.sb("trstd%d" % i, [128, NTK], F32, es) for i in range(2)]
        loc = [Wo, g1] + fTs + tmps + rstds + A + H + sq
        psS = C.ps[6]
        for ti in range(T // NTK):
            t0 = ti * NTK
            a, h = A[ti % 2], H[ti % 2]
            fT, tmp, rstd = fTs[ti % 2], tmps[ti % 2], rstds[ti % 2]
            P.load(a, a[:], aov[:, :, t0:t0 + NTK])
            P.load(h, h[:], hiv[:, :, PAD + t0:PAD + t0 + NTK])
            for db in range(8):
                pd = C.ps[db % 6]
                for c in range(kc):
                    P.mm(pd, pd[:, :NTK], Wo[:, c, db * 128:(db + 1) * 128], a[:, c, :], reads=[Wo, a],
                         start=(c == 0), stop=(c == kc - 1))
                sqt = sq[db % 4]
                P.op("dve", lambda e, db=db, pd=pd, fT=fT: e.tensor_copy(fT[:, db, :], pd[:, :NTK]), reads=[pd], writes=[fT])
                P.op("act", lambda e, sqt=sqt, db=db, fT=fT: e.activation(sqt[:], fT[:, db, :], AF.Square), reads=[fT], writes=[sqt])
                P.mm(psS, psS[:, :NTK], C.ones_bf[:], sqt[:], reads=[C.ones_bf, sqt], start=(db == 0), stop=(db == 7))
            rstd_from_sumsq(C, psS, D, NTK, tmp, rstd)
            for c in range(8):
                P.op("dve", lambda e, c=c, fT=fT, rstd=rstd: e.scalar_tensor_tensor(out=fT[:, c, :], in0=fT[:, c, :], scalar=g1[:, c:c + 1],
                                                                  in1=rstd[:], op0=ALU.mult, op1=ALU.mult),
                     reads=[fT, g1, rstd], writes=[fT])
                P.op("pool", lambda e, c=c, h=h, fT=fT: e.tensor_tensor(out=fT[:, c, :], in0=fT[:, c, :], in1=h[:, c, :], op=ALU.add),
                     reads=[fT, h], writes=[fT])
            P.store(fT, hov[:, :, PAD + t0:PAD + t0 + NTK], fT[:])
        P.barrier()
        P.release(loc)


def phase_mla_proj(C, li, hin, W, S, NTK=512):
    P, T = C.P, C.T
    hiv = hview(hin)
    with ExitStack() as es:
        Wdq = load_w(C, es, "Wdq", W["mla_w_dq"])
        Wuq = load_w(C, es, "Wuq", W["mla_w_uq"])
        Wdkv = load_w(C, es, "Wdkv", W["mla_w_dkv"])
        Wukv = load_w(C, es, "Wukv", W["mla_w_ukv"])
        g0 = load_small(C, es, "g0", W["norm_g"][li * 4 + 0])
        qg = load_small(C, es, "qg", W["mla_qg"])
        kvg = load_small(C, es, "kvg", W["mla_kvg"])
        B = NormBufs(C, es, NTK, "m")
        cqf = P.sb("cqf", [128, 3, NTK], F32, es)
        cqn = P.sb("cqn", [128, 3, NTK], BF16, es)
        ckf = P.sb("ckf", [128, 2, NTK], F32, es)
        ckn = P.sb("ckn", [128, 2, NTK], BF16, es)
        cs = P.sb("cs", [64, 2, NTK], F32, es)
        qn_st = P.sb("qn_st", [128, 8, NTK], BF16, es)
        kn_st = P.sb("kn_st", [128, 8, NTK], BF16, es)
        qr_st = P.sb("qr_st", [64, 8, NTK], BF16, es)
        kr_st = P.sb("kr_st", [64, NTK], BF16, es)
        v_st = P.sb("v_st", [128, NTK // 128, 1024], BF16, es)
        r1 = [P.sb("r1_%d" % i, [64, NTK], F32, es) for i in range(2)]
        r2 = [P.sb("r2_%d" % i, [64, NTK], F32, es) for i in range(2)]
        tmp2 = P.sb("mtmp2", [128, NTK], F32, es)
        rstd2 = P.sb("mrstd2", [128, NTK], F32, es)
        loc = [Wdq, Wuq, Wdkv, Wukv, g0, qg, kvg, cqf, cqn, ckf, ckn, cs, qn_st, kn_st, qr_st, kr_st, v_st, tmp2, rstd2] + B.all + r1 + r2
        psS = C.ps[6]
        qnv = S["qn"].rearrange("(h p) t -> p h t", p=128)
        knv = S["kn"].rearrange("(h p) t -> p h t", p=128)
        qrv = S["qr"].rearrange("(h p) t -> p h t", p=64)
        vv = S["v"].rearrange("(tb p) f -> p tb f", p=128)
        k = 0

        def rope(psA, psB, out_ap, out_r, i):
            a, b = r1[i % 2], r2[i % 2]
            P.op("dve", lambda e: e.tensor_tensor(out=a[:], in0=psA[0:64, :NTK], in1=cs[:, 0, :], op=ALU.mult), reads=[psA, cs], writes=[a])
            P.op("dve", lambda e: e.tensor_tensor(out=b[:], in0=psB[0:64, :NTK], in1=cs[:, 1, :], op=ALU.mult), reads=[psB, cs], writes=[b])
            P.op("pool", lambda e: e.tensor_tensor(out=out_ap, in0=a[:], in1=b[:], op=ALU.add), reads=[a, b], writes=[out_r])

        for ti in range(T // NTK):
            t0 = ti * NTK
            norm_in(C, hiv, PAD + t0, NTK, g0, B, psS)
            P.load(cs, cs[:], W["rope_cs"][:, :, t0:t0 + NTK])
            for fo in range(3):
                pb = C.ps[k % 6]; k += 1
                for c in range(8):
                    P.mm(pb, pb[:, :NTK], Wdq[:, c, fo * 128:(fo + 1) * 128], B.xn[:, c, :], reads=[Wdq, B.xn], start=(c == 0), stop=(c == 7))
                P.op("dve", lambda e, fo=fo, pb=pb: e.tensor_copy(cqf[:, fo, :], pb[:, :NTK]), reads=[pb], writes=[cqf])
            sub_norm(C, cqf, 3, NTK, 384, qg, cqn, B.sq, tmp2, rstd2, psS)
            for h in range(8):
                pb = C.ps[k % 6]; k += 1
                for c in range(3):
                    P.mm(pb, pb[:, :NTK], Wuq[:, c, h * 128:(h + 1) * 128], cqn[:, c, :], reads=[Wuq, cqn], start=(c == 0), stop=(c == 2))
                if h % 2 == 0:
                    P.op("act", lambda e, h=h, pb=pb: e.copy(qn_st[:, h, :], pb[:, :NTK]), reads=[pb], writes=[qn_st])
                else:
                    P.op("dve", lambda e, h=h, pb=pb: e.tensor_copy(qn_st[:, h, :], pb[:, :NTK]), reads=[pb], writes=[qn_st])
            P.store(qn_st, qnv[:, :, t0:t0 + NTK], qn_st[:])
            for h in range(8):
                pa = C.ps[k % 6]; k += 1
                pb = C.ps[k % 6]; k += 1
                for c in range(3):
                    P.mm(pa, pa[0:64, :NTK], Wuq[:, c, 1024 + h * 64:1024 + (h + 1) * 64], cqn[:, c, :], reads=[Wuq, cqn], start=(c == 0), stop=(c == 2))
                for c in range(3):
                    P.mm(pb, pb[0:64, :NTK], Wuq[:, c, 1536 + h * 64:1536 + (h + 1) * 64], cqn[:, c, :], reads=[Wuq, cqn], start=(c == 0), stop=(c == 2))
                rope(pa, pb, qr_st[:, h, :], qr_st, h)
            P.store(qr_st, qrv[:, :, t0:t0 + NTK], qr_st[:])
            for fo in range(2):
                pb = C.ps[k % 6]; k += 1
                for c in range(8):
                    P.mm(pb, pb[:, :NTK], Wdkv[:, c, fo * 128:(fo + 1) * 128], B.xn[:, c, :], reads=[Wdkv, B.xn], start=(c == 0), stop=(c == 7))
                P.op("dve", lambda e, fo=fo, pb=pb: e.tensor_copy(ckf[:, fo, :], pb[:, :NTK]), reads=[pb], writes=[ckf])
            pa = C.ps[k % 6]; k += 1
            pb = C.ps[k % 6]; k += 1
            for c in range(8):
                P.mm(pa, pa[0:64, :NTK], Wdkv[:, c, 256:320], B.xn[:, c, :], reads=[Wdkv, B.xn], start=(c == 0), stop=(c == 7))
            for c in range(8):
                P.mm(pb, pb[0:64, :NTK], Wdkv[:, c, 320:384], B.xn[:, c, :], reads=[Wdkv, B.xn], start=(c == 0), stop=(c == 7))
            rope(pa, pb, kr_st[:], kr_st, 0)
            P.store(kr_st, S["kr"][:, t0:t0 + NTK], kr_st[:])
            sub_norm(C, ckf, 2, NTK, 256, kvg, ckn, B.sq, tmp2, rstd2, psS)
            for h in range(8):
                pb = C.ps[k % 6]; k += 1
                for c in range(2):
                    P.mm(pb, pb[:, :NTK], Wukv[:, c, h * 128:(h + 1) * 128], ckn[:, c, :], reads=[Wukv, ckn], start=(c == 0), stop=(c == 1))
                if h % 2 == 0:
                    P.op("act", lambda e, h=h, pb=pb: e.copy(kn_st[:, h, :], pb[:, :NTK]), reads=[pb], writes=[kn_st])
                else:
                    P.op("dve", lambda e, h=h, pb=pb: e.tensor_copy(kn_st[:, h, :], pb[:, :NTK]), reads=[pb], writes=[kn_st])
            P.store(kn_st, knv[:, :, t0:t0 + NTK], kn_st[:])
            for tb in range(NTK // 128):
                for half in range(2):
                    pb = C.ps[k % 6]; k += 1
                    for c in range(2):
                        P.mm(pb, pb[:, :512], ckn[:, c, tb * 128:(tb + 1) * 128], Wukv[:, c, 1024 + half * 512:1024 + (half + 1) * 512],
                             reads=[Wukv, ckn], start=(c == 0), stop=(c == 1))
                    if half == 0:
                        P.op("act", lambda e, tb=tb, half=half, pb=pb: e.copy(v_st[:, tb, half * 512:(half + 1) * 512], pb[:, :512]), reads=[pb], writes=[v_st])
                    else:
                        P.op("dve", lambda e, tb=tb, half=half, pb=pb: e.tensor_copy(v_st[:, tb, half * 512:(half + 1) * 512], pb[:, :512]), reads=[pb], writes=[v_st])
            P.store(v_st, vv[:, t0 // 128:(t0 + NTK) // 128, :], v_st[:])
        P.barrier()
        P.release(loc)


def phase_mla_core(C, S, TQ0=0, TQ=None, NQ=512):
    P, T = C.P, C.T
    TQ = TQ or T
    NKB = T // 128
    scale = float(192 ** -0.5)
    with ExitStack() as es:
        Kn = [P.sb("Kn%d" % i, [128, T], BF16, es) for i in range(2)]
        Vh = [P.sb("Vh%d" % i, [128, NKB, 128], BF16, es) for i in range(2)]
        Kr = P.sb("Kr", [64, T], BF16, es)
        Qn = [P.sb("Qn%d" % i, [128, NQ], BF16, es) for i in range(2)]
        Qr = [P.sb("Qr%d" % i, [64, NQ], BF16, es) for i in range(2)]
        Pt = [P.sb("Pt%d" % i, [128, NQ], BF16, es) for i in range(4)]
        rl = P.sb("rl", [128, NQ], F32, es)
        ob = [P.sb("ob%d" % i, [128, NQ], BF16, es) for i in range(2)]
        loc = Kn + Vh + [Kr, rl] + Qn + Qr + Pt + ob
        P.load(Kr, Kr[:], S["kr"][:, :])
        vv = S["v"].rearrange("(kb p) f -> p kb f", p=128)
        qrv = S["qr"].rearrange("(h p) t -> p h t", p=64)
        it = 0
        for h in range(8):
            kn, vh = Kn[h % 2], Vh[h % 2]
            P.load(kn, kn[:], S["kn"][h * 128:(h + 1) * 128, :])
            P.load(vh, vh[:], vv[:, :, h * 128:(h + 1) * 128])
            for qi in range(TQ // NQ):
                q0 = TQ0 + qi * NQ
                qn, qr = Qn[it % 2], Qr[it % 2]
                o_sb = ob[it % 2]
                pO, pL = C.ps[3 + it % 2], C.ps[5 + it % 2]
                it += 1
                P.load(qn, qn[:], S["qn"][h * 128:(h + 1) * 128, q0:q0 + NQ])
                P.load(qr, qr[:], qrv[:, h, q0:q0 + NQ])

                def stA(kb, kn=kn, qn=qn, qr=qr):
                    pS = C.ps[kb % 3]
                    pt = Pt[kb % 4]
                    ks = slice(kb * 128, (kb + 1) * 128)
                    P.mm(pS, pS[:, :NQ], kn[:, ks], qn[:], reads=[kn, qn], start=True, stop=False)
                    P.mm(pS, pS[:, :NQ], Kr[:, ks], qr[:], reads=[Kr, qr], start=False, stop=True)
                    P.op("act", lambda e, pt=pt, pS=pS: e.activation(pt[:], pS[:, :NQ], AF.Exp, scale=scale), reads=[pS], writes=[pt])

                def stB(kb, vh=vh, pO=pO, pL=pL):
                    pt = Pt[kb % 4]
                    P.mm(pO, pO[:, :NQ], vh[:, kb, :], pt[:], reads=[vh, pt], start=(kb == 0), stop=(kb == NKB - 1))
                    P.mm(pL, pL[:, :NQ], C.ones_bf[:], pt[:], reads=[C.ones_bf, pt], start=(kb == 0), stop=(kb == NKB - 1))

                stA(0)
                if NKB > 1:
                    stA(1)
                for kb in range(NKB):
                    if kb + 2 < NKB:
                        stA(kb + 2)
                    stB(kb)
                P.op("dve", lambda e, pL=pL: e.reciprocal(rl[:], pL[:, :NQ]), reads=[pL], writes=[rl])
                P.op("dve", lambda e, pO=pO, o_sb=o_sb: e.tensor_tensor(out=o_sb[:], in0=pO[:, :NQ], in1=rl[:], op=ALU.mult), reads=[pO, rl], writes=[o_sb])
                P.store(o_sb, S["ao"][h * 128:(h + 1) * 128, q0:q0 + NQ], o_sb[:])
        P.barrier()
        P.release(loc)


def pm(v):
    v = np.asarray(v)
    return np.ascontiguousarray(v.reshape(-1, 128).T)


def host_mla_params(inp, pos):
    wuq = inp["mla_w_uq"][0].reshape(384, 8, 192)
    nope = wuq[:, :, :128].reshape(384, 1024)
    ropew = wuq[:, :, 128:]
    rope_sw = np.concatenate([ropew[:, :, 32:], ropew[:, :, :32]], -1)
    w_uq = np.ascontiguousarray(np.concatenate([nope, ropew.reshape(384, 512), rope_sw.reshape(384, 512)], 1))
    wdkv = inp["mla_w_dkv"][0]
    kr = wdkv[:, 256:]
    w_dkv = np.ascontiguousarray(np.concatenate([wdkv[:, :256], kr, kr[:, 32:], kr[:, :32]], 1))
    wukv = inp["mla_w_ukv"][0].reshape(256, 8, 256)
    w_ukv = np.ascontiguousarray(np.concatenate([wukv[:, :, :128].reshape(256, 1024), wukv[:, :, 128:].reshape(256, 1024)], 1))
    inv = (10000.0 ** (-np.arange(0, 64, 2, dtype=np.float32) / 64)).astype(np.float32)
    ang = pos.astype(np.float32)[None, :] * inv[:, None]
    cos, sin = np.cos(ang).astype(np.float32), np.sin(ang).astype(np.float32)
    cs = np.stack([np.concatenate([cos, cos], 0), np.concatenate([-sin, sin], 0)], 1)
    return dict(mla_w_dq=np.ascontiguousarray(inp["mla_w_dq"][0]), mla_w_uq=w_uq, mla_w_dkv=w_dkv, mla_w_ukv=w_ukv,
                mla_w_o=np.ascontiguousarray(inp["mla_w_o"][0]),
                mla_qg=pm(inp["mla_q_norm_g"][0]), mla_kvg=pm(inp["mla_kv_norm_g"][0]), rope_cs=np.ascontiguousarray(cs.astype(np.float32)))


def phase_diff_proj(C, li, hin, W, S, NTK=512):
    P, T = C.P, C.T
    hiv = hview(hin)
    with ExitStack() as es:
        Wqkv = load_w(C, es, "Wqkv", W["diff_w_qkv"])
        g0 = load_small(C, es, "g0", W["norm_g"][li * 4 + 0])
        B = NormBufs(C, es, NTK, "d")
        q_st = P.sb("dq_st", [128, 8, NTK], BF16, es)
        k_st = P.sb("dk_st", [128, 8, NTK], BF16, es)
        v_st = P.sb("dv_st", [128, NTK // 128, 1024], BF16, es)
        loc = [Wqkv, g0, q_st, k_st, v_st] + B.all
        psS = C.ps[6]
        qv = S["qn"].rearrange("(h p) t -> p h t", p=128)
        kv = S["kn"].rearrange("(h p) t -> p h t", p=128)
        vv = S["v"].rearrange("(tb p) f -> p tb f", p=128)
        k = 0
        for ti in range(T // NTK):
            t0 = ti * NTK
            norm_in(C, hiv, PAD + t0, NTK, g0, B, psS)
            for (st, off, dst) in ((q_st, 0, qv), (k_st, 1024, kv)):
                for h in range(8):
                    pb = C.ps[k % 6]; k += 1
                    for c in range(8):
                        P.mm(pb, pb[:, :NTK], Wqkv[:, c, off + h * 128:off + (h + 1) * 128], B.xn[:, c, :], reads=[Wqkv, B.xn],
                             start=(c == 0), stop=(c == 7))
                    if h % 2 == 0:
                        P.op("act", lambda e, h=h, pb=pb, st=st: e.copy(st[:, h, :], pb[:, :NTK]), reads=[pb], writes=[st])
                    else:
                        P.op("dve", lambda e, h=h, pb=pb, st=st: e.tensor_copy(st[:, h, :], pb[:, :NTK]), reads=[pb], writes=[st])
                P.store(st, dst[:, :, t0:t0 + NTK], st[:])
            for tb in range(NTK // 128):
                for half in range(2):
                    pb = C.ps[k % 6]; k += 1
                    for c in range(8):
                        P.mm(pb, pb[:, :512], B.xn[:, c, tb * 128:(tb + 1) * 128], Wqkv[:, c, 2048 + half * 512:2048 + (half + 1) * 512],
                             reads=[Wqkv, B.xn], start=(c == 0), stop=(c == 7))
                    if half == 0:
                        P.op("act", lambda e, tb=tb, half=half, pb=pb: e.copy(v_st[:, tb, half * 512:(half + 1) * 512], pb[:, :512]), reads=[pb], writes=[v_st])
                    else:
                        P.op("dve", lambda e, tb=tb, half=half, pb=pb: e.tensor_copy(v_st[:, tb, half * 512:(half + 1) * 512], pb[:, :512]), reads=[pb], writes=[v_st])
            P.store(v_st, vv[:, t0 // 128:(t0 + NTK) // 128, :], v_st[:])
        P.barrier()
        P.release(loc)


def phase_diff_core(C, li, W, S, NQ=512):
    P, T = C.P, C.T
    NKB = T // 128
    scale = float(64 ** -0.5)
    lam_init = 0.8 - 0.6 * float(np.exp(-0.3 * li))
    SKIP = 130.0
    with ExitStack() as es:
        Kh = [P.sb("dK%d" % i, [128, T], BF16, es) for i in range(2)]
        Vh = [P.sb("dV%d" % i, [128, NKB, 128], BF16, es) for i in range(2)]
        Q = [P.sb("dQ%d" % i, [128, NQ], BF16, es) for i in range(2)]
        RAW = P.sb("dRAW", [128, 6, NQ], F32, es)
        BRAW = P.sb("dBRAW", [128, 128], F32, es)
        MT = P.sb("dMT", [128, 6, NQ], BF16, es)
        BT = P.sb("dBT", [128, 128], F32, es)
        E = [P.sb("dE%d" % i, [128, NQ], BF16, es) for i in range(4)]
        Pt = [P.sb("dPt%d" % i, [128, NQ], BF16, es) for i in range(6)]
        lp = P.sb("dlp", [1, 256], F32, es)
        lw = P.sb("dlw", [1, 8], F32, es)
        ones1 = P.sb("dones1", [1, 128], F32, es)
        nlam = P.sb("dnlam", [128, 1], F32, es)
        sg = P.sb("dsg", [128, 1], F32, es)
        r1 = P.sb("dr1", [128, NQ], F32, es)
        r2 = P.sb("dr2", [128, NQ], F32, es)
        a1 = P.sb("da1", [128, NQ], F32, es)
        a2 = P.sb("da2", [128, NQ], F32, es)
        sqd = P.sb("dsq", [128, NQ], BF16, es)
        ob = [P.sb("dob%d" % i, [128, NQ], BF16, es) for i in range(2)]
        loc = Kh + Vh + Q + [RAW, BRAW, MT, BT, lp, lw, ones1, nlam, sg, r1, r2, a1, a2, sqd] + E + Pt + ob
        P.op("pool", lambda e: e.iota(RAW[:, 0, :], [[1, NQ]], base=0, channel_multiplier=0, allow_small_or_imprecise_dtypes=True), writes=[RAW])
        P.op("pool", lambda e: e.iota(RAW[:, 1, :], [[-1, NQ]], base=NQ - 1, channel_multiplier=0, allow_small_or_imprecise_dtypes=True), writes=[RAW])
        for kk in range(4):
            P.op("pool", lambda e, kk=kk: e.iota(RAW[:, 2 + kk, :], [[1, NQ]], base=-128 * kk, channel_multiplier=-1,
                                                allow_small_or_imprecise_dtypes=True), writes=[RAW])
        P.op("dve", lambda e: e.scalar_tensor_tensor(out=RAW[:, 2:6, :], in0=RAW[:, 2:6, :], scalar=-1.0, in1=RAW[:, 2:6, :], op0=ALU.mult, op1=ALU.max),
             reads=[RAW], writes=[RAW])
        P.op("pool", lambda e: e.iota(BRAW[:, 0:64], [[-128, 64]], base=0, channel_multiplier=1, allow_small_or_imprecise_dtypes=True), writes=[BRAW])
        P.op("pool", lambda e: e.iota(BRAW[:, 64:128], [[-128, 64]], base=NQ - 1, channel_multiplier=-1, allow_small_or_imprecise_dtypes=True), writes=[BRAW])
        P.load(lp, lp[:], W["diff_lambda"])
        P.load(sg, sg[:], W["diff_subln_g"])
        P.op("dve", lambda e: e.memset(ones1[:], 1.0), writes=[ones1])
        P.op("dve", lambda e: e.tensor_tensor(out=lp[:, 0:64], in0=lp[:, 0:64], in1=lp[:, 64:128], op=ALU.mult), reads=[lp], writes=[lp])
        P.op("dve", lambda e: e.tensor_tensor(out=lp[:, 128:192], in0=lp[:, 128:192], in1=lp[:, 192:256], op=ALU.mult), reads=[lp], writes=[lp])
        P.op("dve", lambda e: e.reduce_sum(lw[:, 0:1], lp[:, 0:64], axis=AX.X), reads=[lp], writes=[lw])
        P.op("dve", lambda e: e.reduce_sum(lw[:, 1:2], lp[:, 128:192], axis=AX.X), reads=[lp], writes=[lw])
        P.op("act", lambda e: e.activation(lw[:, 2:4], lw[:, 0:2], AF.Exp), reads=[lw], writes=[lw])
        P.op("dve", lambda e: e.scalar_tensor_tensor(out=lw[:, 4:5], in0=lw[:, 3:4], scalar=-lam_init, in1=lw[:, 2:3], op0=ALU.add, op1=ALU.subtract),
             reads=[lw], writes=[lw])
        pc = C.ps[7]
        P.mm(pc, pc[:, 0:1], ones1[:], lw[:, 4:5], reads=[ones1, lw])
        P.op("dve", lambda e: e.tensor_copy(nlam[:], pc[:, 0:1]), reads=[pc], writes=[nlam])
        P.op("dve", lambda e: e.tensor_single_scalar(sg[:], sg[:], 1.0 - lam_init, ALU.mult), reads=[sg], writes=[sg])

        it = 0
        for h in range(8):
            slope = float(2.0 ** (-(h + 1)))
            kh, vh = Kh[h % 2], Vh[h % 2]
            P.load(kh, kh[:], S["kn"][h * 128:(h + 1) * 128, :])
            P.load(vh, vh[:], S["v"].rearrange("(kb p) f -> p kb f", p=128)[:, :, h * 128:(h + 1) * 128])
            P.op("act", lambda e, slope=slope: e.activation(MT[:], RAW[:], AF.Exp, scale=-slope), reads=[RAW], writes=[MT])
            P.op("dve", lambda e, slope=slope: e.tensor_single_scalar(BT[:], BRAW[:], slope, ALU.mult), reads=[BRAW], writes=[BT])
            for qi in range(T // NQ):
                q0 = qi * NQ
                q = Q[it % 2]
                o_sb = ob[it % 2]
                it += 1
                P.load(q, q[:], S["qn"][h * 128:(h + 1) * 128, q0:q0 + NQ])
                pO = [C.ps[4], C.ps[5]]
                pL = [C.ps[6], C.ps[7]]
                kbs = []
                for kb in range(NKB):
                    j0 = kb * 128
                    dmin = max(0, q0 - (j0 + 127), j0 - (q0 + NQ - 1))
                    if slope * dmin >= SKIP:
                        continue
                    kbs.append(kb)

                def stA(i, kh=kh, q=q, q0=q0):
                    kb = kbs[i]
                    ks = slice(kb * 128, (kb + 1) * 128)
                    j0 = kb * 128
                    if j0 + 128 <= q0:
                        m = (q0 - j0) // 128
                        bias, mt = BT[:, m:m + 1], MT[:, 0, :]
                    elif j0 >= q0 + NQ:
                        m = (j0 - q0) // 128
                        bias, mt = BT[:, 64 + m:64 + m + 1], MT[:, 1, :]
                    else:
                        kk = (j0 - q0) // 128
                        bias, mt = None, MT[:, 2 + kk, :]
                    for j in range(2):
                        pS = C.ps[(i % 2) * 2 + j]
                        e_t = E[(i % 2) * 2 + j]
                        pt = Pt[(i % 3) * 2 + j]
                        js = slice(j * 64, (j + 1) * 64)
                        P.mm(pS, pS[:, :NQ], kh[js, ks], q[js, :], reads=[kh, q])
                        if bias is None:
                            P.op("act", lambda e, e_t=e_t, pS=pS: e.activation(e_t[:], pS[:, :NQ], AF.Exp, scale=scale), reads=[pS], writes=[e_t])
                        else:
                            P.op("act", lambda e, e_t=e_t, pS=pS, bias=bias: e.activation(e_t[:], pS[:, :NQ], AF.Exp, scale=scale, bias=bias),
                                 reads=[pS, BT], writes=[e_t])
                        eng = "dve" if j == 0 else "pool"
                        P.op(eng, lambda e, pt=pt, e_t=e_t, mt=mt: e.tensor_tensor(out=pt[:], in0=e_t[:], in1=mt, op=ALU.mult),
                             reads=[e_t, MT], writes=[pt])

                def stB(i, vh=vh, pO=pO, pL=pL):
                    kb = kbs[i]
                    for j in range(2):
                        pt = Pt[(i % 3) * 2 + j]
                        P.mm(pO[j], pO[j][:, :NQ], vh[:, kb, :], pt[:], reads=[vh, pt], start=(i == 0), stop=(i == len(kbs) - 1))
                        P.mm(pL[j], pL[j][:, :NQ], C.ones_bf[:], pt[:], reads=[C.ones_bf, pt], start=(i == 0), stop=(i == len(kbs) - 1))

                stA(0)
                if len(kbs) > 1:
                    stA(1)
                for i in range(len(kbs)):
                    if i + 2 < len(kbs):
                        stA(i + 2)
                    stB(i)
                P.op("dve", lambda e, pL=pL: e.reciprocal(r1[:], pL[0][:, :NQ]), reads=[pL[0]], writes=[r1])
                P.op("dve", lambda e, pL=pL: e.reciprocal(r2[:], pL[1][:, :NQ]), reads=[pL[1]], writes=[r2])
                P.op("dve", lambda e, pO=pO: e.tensor_tensor(out=a1[:], in0=pO[0][:, :NQ], in1=r1[:], op=ALU.mult), reads=[pO[0], r1], writes=[a1])
                P.op("dve", lambda e, pO=pO: e.tensor_tensor(out=a2[:], in0=pO[1][:, :NQ], in1=r2[:], op=ALU.mult), reads=[pO[1], r2], writes=[a2])
                P.op("dve", lambda e: e.scalar_tensor_tensor(out=a1[:], in0=a2[:], scalar=nlam[:, 0:1], in1=a1[:], op0=ALU.mult, op1=ALU.add),
                     reads=[a1, a2, nlam], writes=[a1])
                psS = C.ps[0]
                P.op("act", lambda e: e.activation(sqd[:], a1[:], AF.Square), reads=[a1], writes=[sqd])
                P.mm(psS, psS[:, :NQ], C.ones_bf[:], sqd[:], reads=[C.ones_bf, sqd])
                rstd_from_sumsq(C, psS, 128, NQ, r1, r2)
                P.op("dve", lambda e, o_sb=o_sb: e.scalar_tensor_tensor(out=o_sb[:], in0=a1[:], scalar=sg[:, 0:1], in1=r2[:], op0=ALU.mult, op1=ALU.mult),
                     reads=[a1, sg, r2], writes=[o_sb])
                P.store(o_sb, S["ao"][h * 128:(h + 1) * 128, q0:q0 + NQ], o_sb[:])
        P.barrier()
        P.release(loc)


def host_diff_params(inp):
    return dict(diff_w_qkv=np.ascontiguousarray(inp["diff_w_qkv"][0]), diff_w_o=np.ascontiguousarray(inp["diff_w_o"][0]),
                diff_lambda=np.ascontiguousarray(inp["diff_lambda"][0].reshape(1, 256)),
                diff_subln_g=np.ascontiguousarray(inp["diff_subln_g"][0].reshape(128, 1)))


def make_tri(C, es):
    P = C.P
    R = Ctx()
    R.M01F = P.sb("M01F", [128, 128], F32, es)
    R.M01B = P.sb("M01B", [128, 128], F32, es)
    R.ones_f = P.sb("ones_f", [128, 128], F32, es)
    P.op("dve", lambda e: e.memset(R.ones_f[:], 1.0), writes=[R.ones_f])
    P.op("pool", lambda e: e.iota(R.M01F[:], [[1, 128]], base=0, channel_multiplier=-1, allow_small_or_imprecise_dtypes=True), writes=[R.M01F])
    P.op("pool", lambda e: e.iota(R.M01B[:], [[-1, 128]], base=0, channel_multiplier=1, allow_small_or_imprecise_dtypes=True), writes=[R.M01B])
    for m in (R.M01F, R.M01B):
        P.op("dve", lambda e, m=m: e.tensor_scalar(m[:], m[:], 1.0, 0.0, ALU.add, ALU.max), reads=[m], writes=[m])
        P.op("dve", lambda e, m=m: e.tensor_single_scalar(m[:], m[:], 1.0, ALU.min), reads=[m], writes=[m])
    R.all = [R.M01F, R.M01B, R.ones_f]
    return R


def phase_mlstm_proj(C, li, hin, W, S, Gtok, NTK=512):
    P, T = C.P, C.T
    hiv = hview(hin)
    sc = float(64 ** -0.5)
    with ExitStack() as es:
        Win = load_w(C, es, "mWin", W["mlstm_w_in"])
        g0 = load_small(C, es, "g0", W["norm_g"][li * 4 + 0])
        bg = load_small(C, es, "mbg", W["mlstm_bg"])
        B = NormBufs(C, es, NTK, "l")
        q_st = P.sb("lq_st", [64, 8, NTK], BF16, es)
        k_st = P.sb("lk_st", [64, 8, NTK], BF16, es)
        o_st = P.sb("lo_st", [128, 8, NTK], BF16, es)
        kt_st = P.sb("lkt_st", [128, NTK // 128, 512], BF16, es)
        v_st = P.sb("lv_st", [128, NTK // 128, 8, 129], BF16, es)
        loc = [Win, g0, bg, q_st, k_st, o_st, kt_st, v_st] + B.all
        P.op("dve", lambda e: e.memset(v_st[:], 1.0), writes=[v_st])
        psS = C.ps[6]
        qv = S["mq"].rearrange("(h p) t -> p h t", p=64)
        kv = S["mk"].rearrange("(h p) t -> p h t", p=64)
        ov = S["og"].rearrange("(h p) t -> p h t", p=128)
        ktv = S["mkt"].rearrange("(tb p) f -> p tb f", p=128)
        vv = S["mv"].rearrange("(tb p) h e -> p tb h e", p=128)
        k = 0
        for ti in range(T // NTK):
            t0 = ti * NTK
            norm_in(C, hiv, PAD + t0, NTK, g0, B, psS)
            for h in range(8):
                pb = C.ps[k % 6]; k += 1
                for c in range(8):
                    P.mm(pb, pb[0:64, :NTK], Win[:, c, h * 64:(h + 1) * 64], B.xn[:, c, :], reads=[Win, B.xn], start=(c == 0), stop=(c == 7))
                P.op("act", lambda e, h=h, pb=pb: e.copy(q_st[:, h, :], pb[0:64, :NTK]), reads=[pb], writes=[q_st])
                pb = C.ps[k % 6]; k += 1
                for c in range(8):
                    P.mm(pb, pb[0:64, :NTK], Win[:, c, 512 + h * 64:512 + (h + 1) * 64], B.xn[:, c, :], reads=[Win, B.xn], start=(c == 0), stop=(c == 7))
                P.op("dve", lambda e, h=h, pb=pb: e.tensor_single_scalar(k_st[:, h, :], pb[0:64, :NTK], sc, ALU.mult), reads=[pb], writes=[k_st])
            P.store(q_st, qv[:, :, t0:t0 + NTK], q_st[:])
            P.store(k_st, kv[:, :, t0:t0 + NTK], k_st[:])
            for h in range(8):
                pb = C.ps[k % 6]; k += 1
                for c in range(8):
                    P.mm(pb, pb[:, :NTK], Win[:, c, 2048 + h * 128:2048 + (h + 1) * 128], B.xn[:, c, :], reads=[Win, B.xn], start=(c == 0), stop=(c == 7))
                P.op("act", lambda e, h=h, pb=pb: e.activation(o_st[:, h, :], pb[:, :NTK], AF.Sigmoid), reads=[pb], writes=[o_st])
            P.store(o_st, ov[:, :, t0:t0 + NTK], o_st[:])
            for tb in range(NTK // 128):
                ts_ = slice(tb * 128, (tb + 1) * 128)
                ch = t0 // 128 + tb
                pb = C.ps[k % 6]; k += 1
                for c in range(8):
                    P.mm(pb, pb[:, :512], B.xn[:, c, ts_], Win[:, c, 512:1024], reads=[Win, B.xn], start=(c == 0), stop=(c == 7))
                P.op("dve", lambda e, tb=tb, pb=pb: e.tensor_single_scalar(kt_st[:, tb, :], pb[:, :512], sc, ALU.mult), reads=[pb], writes=[kt_st])
                for half in range(2):
                    pb = C.ps[k % 6]; k += 1
                    for c in range(8):
                        P.mm(pb, pb[:, :512], B.xn[:, c, ts_], Win[:, c, 1024 + half * 512:1024 + (half + 1) * 512], reads=[Win, B.xn], start=(c == 0), stop=(c == 7))
                    P.op("act", lambda e, tb=tb, half=half, pb=pb: e.copy(v_st[:, tb, half * 4:(half + 1) * 4, 0:128],
                                                                         pb[:, :512].rearrange("p (h e) -> p h e", e=128)), reads=[pb], writes=[v_st])
                pb = C.ps[k % 6]; k += 1
                for c in range(8):
                    P.mm(pb, pb[:, :32], B.xn[:, c, ts_], Win[:, c, 3072:3104], reads=[Win, B.xn], start=(c == 0), stop=(c == 7))
                P.op("dve", lambda e, ch=ch, pb=pb: e.tensor_tensor(out=Gtok[:, ch, :], in0=pb[:, :32], in1=bg[:], op=ALU.add), reads=[pb, bg], writes=[Gtok])
            P.store(kt_st, ktv[:, t0 // 128:(t0 + NTK) // 128, :], kt_st[:])
            P.store(v_st, vv[:, t0 // 128:(t0 + NTK) // 128, :, :], v_st[:])
        P.barrier()
        P.release(loc)


def phase_mlstm_gates(C, R, Gtok, GS, es):
    P, T = C.P, C.T
    NCH = T // 128
    t8 = [P.sb("g8_%d" % i, [128, 8], F32, es) for i in range(4)]
    bcc = P.sb("gbcc", [128, 16], F32, es)
    rhsD = [P.sb("grhsD%d" % i, [128, 4, 128], F32, es) for i in range(2)]
    tmpD = [P.sb("gtmpD%d" % i, [128, 4, 128], F32, es) for i in range(2)]
    Mneg = [P.sb("gMneg%d" % i, [128, 4, 128], F32, es) for i in range(2)]
    Sel = [P.sb("gSel%d" % i, [128, 128], F32, es) for i in range(2)]
    one_t = P.sb("gone", [128, 1], F32, es)
    mcur = P.sb("gmcur", [128, 8], F32, es)
    loc = t8 + [bcc, one_t, mcur] + rhsD + tmpD + Mneg + Sel
    P.op("dve", lambda e: e.memset(one_t[:], 1.0), writes=[one_t])
    for d, msrc in ((0, R.M01B), (1, R.M01F)):
        for j in range(4):
            P.op("dve", lambda e, d=d, j=j, msrc=msrc: e.tensor_scalar(Mneg[d][:, j, :], msrc[:], -1.0, 1e30, ALU.add, ALU.mult),
                 reads=[msrc], writes=[Mneg[d]])
    P.op("dve", lambda e: e.tensor_scalar(Sel[0][:], R.ones_f[:], R.M01B[:, 127:128], None, ALU.mult), reads=[R.ones_f, R.M01B], writes=[Sel[0]])
    P.op("dve", lambda e: e.tensor_scalar(Sel[1][:], R.ones_f[:], R.M01F[:, 0:1], None, ALU.mult), reads=[R.ones_f, R.M01F], writes=[Sel[1]])
    tri = [R.M01F, R.M01B]
    k = 0
    for c in range(NCH):
        for d in range(2):
            G = GS[d]
            fs = slice(8 + 16 * d, 16 + 16 * d)
            is_ = slice(16 * d, 16 * d + 8)
            nl = t8[0]
            P.op("act", lambda e, c=c, fs=fs: e.activation(nl[:], Gtok[:, c, fs], AF.Exp, scale=-1.0), reads=[Gtok], writes=[nl])
            P.op("act", lambda e: e.activation(nl[:], nl[:], AF.Ln, bias=one_t[:, 0:1]), reads=[nl, one_t], writes=[nl])
            pb = C.ps[k % 4]; k += 1
            P.mm(pb, pb[:, 0:8], tri[d][:], nl[:], reads=[tri[d], nl])
            P.op("dve", lambda e, c=c, pb=pb, G=G: e.tensor_single_scalar(G["BC"][:, c, :], pb[:, 0:8], -1.0, ALU.mult), reads=[pb], writes=[G["BC"]])
            P.op("dve", lambda e, c=c, pb=pb, G=G, is_=is_: e.tensor_tensor(out=G["A"][:, c, :], in0=pb[:, 0:8], in1=Gtok[:, c, is_], op=ALU.add),
                 reads=[pb, Gtok], writes=[G["A"]])
            for g in range(2):
                rd, td = rhsD[g], tmpD[g]
                for j in range(4):
                    if j == 3:
                        P.op("dve", lambda e, c=c, g=g, j=j, rd=rd, G=G: e.tensor_scalar(rd[:, j, :], C.ident[:], G["A"][:, c, 4 * g + j:4 * g + j + 1], None, ALU.mult),
                             reads=[C.ident, G["A"]], writes=[rd])
                    elif j % 2 == 0:
                        P.op("act", lambda e, c=c, g=g, j=j, rd=rd, G=G: e.activation(rd[:, j, :], C.ident[:], AF.Identity, scale=G["A"][:, c, 4 * g + j:4 * g + j + 1]),
                             reads=[C.ident, G["A"]], writes=[rd])
                    else:
                        P.op("pool", lambda e, c=c, g=g, j=j, rd=rd, G=G: e.tensor_scalar(rd[:, j, :], C.ident[:], G["A"][:, c, 4 * g + j:4 * g + j + 1], None, ALU.mult),
                             reads=[C.ident, G["A"]], writes=[rd])
                pa = C.ps[4 + k % 2]; k += 1
                P.mm(pa, pa[:, :512], R.ones_f[:], rd[:].rearrange("p j s -> p (j s)"), reads=[R.ones_f, rd])
                P.op("dve", lambda e, td=td, pa=pa, d=d: e.tensor_tensor(out=td[:].rearrange("p j s -> p (j s)"), in0=pa[:, :512],
                                                                         in1=Mneg[d][:].rearrange("p j s -> p (j s)"), op=ALU.add),
                     reads=[pa, Mneg[d]], writes=[td])
                P.op("dve", lambda e, td=td, c=c, g=g, G=G: e.reduce_max(G["CM"][:, c, 4 * g:4 * g + 4], td[:], axis=AX.X), reads=[td], writes=[G["CM"]])
            P.op("dve", lambda e, c=c, G=G: e.tensor_copy(bcc[:, 0:8], G["BC"][:, c, :]), reads=[G["BC"]], writes=[bcc])
            P.op("dve", lambda e, c=c, G=G: e.tensor_copy(bcc[:, 8:16], G["CM"][:, c, :]), reads=[G["CM"]], writes=[bcc])
            pb = C.ps[k % 4]; k += 1
            P.mm(pb, pb[:, 0:16], Sel[d][:], bcc[:], reads=[Sel[d], bcc])
            P.op("act", lambda e, c=c, pb=pb, G=G: e.copy(G["BL"][:, c, :], pb[:, 0:8]), reads=[pb], writes=[G["BL"]])
            P.op("act", lambda e, c=c, pb=pb, G=G: e.copy(G["CML"][:, c, :], pb[:, 8:16]), reads=[pb], writes=[G["CML"]])
    for d in range(2):
        G = GS[d]
        P.op("dve", lambda e: e.memset(mcur[:], 0.0), writes=[mcur])
        order = range(NCH) if d == 0 else range(NCH - 1, -1, -1)
        for c in order:
            P.op("dve", lambda e, c=c, G=G: e.tensor_copy(G["M"][:, c, :], mcur[:]), reads=[mcur], writes=[G["M"]])
            P.op("dve", lambda e, c=c, G=G: e.tensor_tensor(out=mcur[:], in0=mcur[:], in1=G["CML"][:, c, :], op=ALU.max), reads=[mcur, G["CML"]], writes=[mcur])
            P.op("dve", lambda e, c=c, G=G: e.tensor_tensor(out=mcur[:], in0=mcur[:], in1=G["BL"][:, c, :], op=ALU.add), reads=[mcur, G["BL"]], writes=[mcur])
            P.op("dve", lambda e, c=c, G=G: e.tensor_copy(G["MN"][:, c, :], mcur[:]), reads=[mcur], writes=[G["MN"]])
        def fl(nm):
            return G[nm][:].rearrange("p c h -> p (c h)")
        for nm in ("MX", "EU", "WI", "NM", "WS", "DEC", "EA"):
            pass
        P.op("dve", lambda e, G=G: e.tensor_tensor(out=G["MX"][:], in0=G["CM"][:], in1=G["M"][:], op=ALU.max), reads=[G["CM"], G["M"]], writes=[G["MX"]])
        P.op("act", lambda e, G=G: e.activation(G["EU"][:], G["MX"][:], AF.Exp, scale=-1.0), reads=[G["MX"]], writes=[G["EU"]])
        P.op("dve", lambda e, G=G: e.tensor_tensor(out=G["WI"][:], in0=G["M"][:], in1=G["MX"][:], op=ALU.subtract), reads=[G["M"], G["MX"]], writes=[G["WI"]])
        P.op("act", lambda e, G=G: e.activation(G["WI"][:], G["WI"][:], AF.Exp), reads=[G["WI"]], writes=[G["WI"]])
        P.op("dve", lambda e, G=G: e.tensor_tensor(out=G["NM"][:], in0=G["BC"][:], in1=G["MX"][:], op=ALU.add), reads=[G["BC"], G["MX"]], writes=[G["NM"]])
        P.op("act", lambda e, G=G: e.activation(G["NM"][:], G["NM"][:], AF.Exp, scale=-1.0), reads=[G["NM"]], writes=[G["NM"]])
        P.op("dve", lambda e, G=G: e.tensor_tensor(out=G["WS"][:], in0=G["BL"][:], in1=G["A"][:], op=ALU.add), reads=[G["BL"], G["A"]], writes=[G["WS"]])
        P.op("dve", lambda e, G=G: e.tensor_tensor(out=G["WS"][:], in0=G["WS"][:], in1=G["MN"][:], op=ALU.subtract), reads=[G["WS"], G["MN"]], writes=[G["WS"]])
        P.op("act", lambda e, G=G: e.activation(G["WS"][:], G["WS"][:], AF.Exp), reads=[G["WS"]], writes=[G["WS"]])
        P.op("dve", lambda e, G=G: e.tensor_tensor(out=G["DEC"][:], in0=G["BL"][:], in1=G["M"][:], op=ALU.add), reads=[G["BL"], G["M"]], writes=[G["DEC"]])
        P.op("dve", lambda e, G=G: e.tensor_tensor(out=G["DEC"][:], in0=G["DEC"][:], in1=G["MN"][:], op=ALU.subtract), reads=[G["DEC"], G["MN"]], writes=[G["DEC"]])
        P.op("act", lambda e, G=G: e.activation(G["DEC"][:], G["DEC"][:], AF.Exp), reads=[G["DEC"]], writes=[G["DEC"]])
        P.op("act", lambda e, G=G: e.activation(G["EA"][:], G["A"][:], AF.Exp), reads=[G["A"]], writes=[G["EA"]])
    return loc


def phase_mlstm_core(C, li, hin, W, S):
    P, T = C.P, C.T
    NCH = T // 128
    with ExitStack() as es:
        R = make_tri(C, es)
        Gtok = P.sb("Gtok", [128, NCH, 32], F32, es)
        phase_mlstm_proj(C, li, hin, W, S, Gtok)
        names = ("BC", "A", "CM", "BL", "CML", "M", "MN", "MX", "EU", "WI", "NM", "WS", "DEC", "EA")
        GS = [{nm: P.sb("G%s%d" % (nm, d), [128, NCH, 8], F32, es) for nm in names} for d in range(2)]
        loc = R.all + [Gtok] + [GS[d][nm] for d in range(2) for nm in names]
        with ExitStack() as es2:
            loc2 = phase_mlstm_gates(C, R, Gtok, GS, es2)
            P.barrier()
            P.release(loc2)
        M01b = []
        mask = [R.M01F, R.M01B]
        Qc = [[P.sb("cQ%d%d" % (d, i), [64, 8, 128], BF16, es) for i in range(2)] for d in range(2)]
        Kc = [[P.sb("cK%d%d" % (d, i), [64, 8, 128], BF16, es) for i in range(2)] for d in range(2)]
        Ktc = [[P.sb("cKt%d%d" % (d, i), [128, 512], BF16, es) for i in range(2)] for d in range(2)]
        Vc = [[P.sb("cV%d%d" % (d, i), [128, 8, 129], BF16, es) for i in range(2)] for d in range(2)]
        Cf = [P.sb("cCf%d" % d, [64, 8, 129], F32, es) for d in range(2)]
        Cb = [P.sb("cCb%d" % d, [64, 8, 129], BF16, es) for d in range(2)]
        Hacc = [[P.sb("cH%d%d" % (d, i), [128, 1024], F32, es) for i in range(2)] for d in range(2)]
        sqk = [P.sb("csqk%d" % i, [128, 128], BF16, es) for i in range(3)]
        t1 = [P.sb("ct1%d" % i, [128, 129], F32, es) for i in range(3)]
        tot = [P.sb("ctot%d" % i, [128, 129], F32, es) for i in range(3)]
        kw = [P.sb("ckw%d" % i, [128, 64], BF16, es) for i in range(3)]
        dd = [P.sb("cdd%d" % i, [128, 2], F32, es) for i in range(3)]
        loc += M01b + sum(Qc, []) + sum(Kc, []) + sum(Ktc, []) + sum(Vc, []) + Cf + Cb + sum(Hacc, []) + sqk + t1 + tot + kw + dd
        for d in range(2):
            P.op("dve", lambda e, d=d: e.memset(Cf[d][:], 0.0), writes=[Cf[d]])
            P.op("dve", lambda e, d=d: e.memset(Cb[d][:], 0.0), writes=[Cb[d]])
        qv = S["mq"].rearrange("(h p) t -> p h t", p=64)
        kv = S["mk"].rearrange("(h p) t -> p h t", p=64)
        hdst = [S["hf"], S["hb"]]
        iters = [(step, d, h) for step in range(NCH) for d in range(2) for h in range(8)]

        def ctx_of(n):
            step, d, h = iters[n]
            c = step if d == 0 else NCH - 1 - step
            return step, d, h, c, slice(c * 128, (c + 1) * 128), GS[d]

        def stA(n):
            step, d, h, c, cs, G = ctx_of(n)
            q, kk_, kt, v = Qc[d][step % 2], Kc[d][step % 2], Ktc[d][step % 2], Vc[d][step % 2]
            if h == 0:
                P.load(q, q[:], qv[:, :, cs])
                P.load(kk_, kk_[:], kv[:, :, cs])
                P.load(kt, kt[:], S["mkt"][cs, :])
                P.load(v, v[:], S["mv"][cs, :, :])
            i3 = n % 3
            pS, pI, pX, pC = C.ps[n % 2], C.ps[2 + n % 2], C.ps[4 + n % 2], C.ps[6 + n % 2]
            col = lambda nm: G[nm][:, c, h:h + 1]
            P.mm(pS, pS[:, :128], kk_[:, h, :], q[:, h, :], reads=[kk_, q])
            P.op("dve", lambda e, ea=col("EA"): e.scalar_tensor_tensor(out=sqk[i3][:], in0=pS[:, :128], scalar=ea, in1=mask[d][:],
                                                                     op0=ALU.mult, op1=ALU.mult),
                 reads=[pS, G["EA"], mask[d]], writes=[sqk[i3]])
            P.mm(pI, pI[:, :129], sqk[i3][:], v[:, h, :], reads=[sqk[i3], v])
            P.mm(pX, pX[:, :129], q[:, h, :], Cb[d][:, h, :], reads=[q, Cb[d]])
            P.op("pool", lambda e, ws=col("WS"): e.tensor_scalar(kw[i3][:], kt[:, h * 64:(h + 1) * 64], ws, None, ALU.mult),
                 reads=[kt, G["WS"]], writes=[kw[i3]])
            P.mm(pC, pC[0:64, :129], kw[i3][:], v[:, h, :], reads=[kw[i3], v])

        def stB(n):
            step, d, h, c, cs, G = ctx_of(n)
            hacc = Hacc[d][step % 2]
            i3 = n % 3
            pS, pI, pX, pC = C.ps[n % 2], C.ps[2 + n % 2], C.ps[4 + n % 2], C.ps[6 + n % 2]
            col = lambda nm: G[nm][:, c, h:h + 1]
            P.op("act", lambda e, eu=col("EU"): e.activation(t1[i3][:], pI[:, :129], AF.Identity, scale=eu), reads=[pI, G["EU"]], writes=[t1[i3]])
            P.op("dve", lambda e, wi=col("WI"): e.scalar_tensor_tensor(out=tot[i3][:], in0=pX[:, :129], scalar=wi, in1=t1[i3][:],
                                                                     op0=ALU.mult, op1=ALU.add),
                 reads=[pX, G["WI"], t1[i3]], writes=[tot[i3]])
            P.op("dve", lambda e: e.scalar_tensor_tensor(out=dd[i3][:, 0:1], in0=tot[i3][:, 128:129], scalar=-1.0, in1=tot[i3][:, 128:129],
                                                        op0=ALU.mult, op1=ALU.max), reads=[tot[i3]], writes=[dd[i3]])
            P.op("dve", lambda e, nmc=col("NM"): e.tensor_tensor(out=dd[i3][:, 0:1], in0=dd[i3][:, 0:1], in1=nmc, op=ALU.max),
                 reads=[dd[i3], G["NM"]], writes=[dd[i3]])
            P.op("dve", lambda e: e.reciprocal(dd[i3][:, 1:2], dd[i3][:, 0:1]), reads=[dd[i3]], writes=[dd[i3]])
            P.op("act", lambda e: e.activation(hacc[:, h * 128:(h + 1) * 128], tot[i3][:, 0:128], AF.Identity, scale=dd[i3][:, 1:2]),
                 reads=[tot[i3], dd[i3]], writes=[hacc])
            P.op("dve", lambda e, dec=G["DEC"][0:64, c, h:h + 1]: e.scalar_tensor_tensor(
                out=Cf[d][:, h, :], in0=Cf[d][:, h, :], scalar=dec, in1=pC[0:64, :129], op0=ALU.mult, op1=ALU.add),
                reads=[Cf[d], G["DEC"], pC], writes=[Cf[d]])
            P.op("act", lambda e: e.copy(Cb[d][:, h, :], Cf[d][:, h, :]), reads=[Cf[d]], writes=[Cb[d]])
            if h == 7:
                P.store(hacc, hdst[d][cs, :], hacc[:])

        stA(0)
        for n in range(len(iters)):
            if n + 1 < len(iters):
                stA(n + 1)
            stB(n)
        P.barrier()
        P.release(loc)


def phase_gn_post(C, S, F, dv, g_ap):
    P, T = C.P, C.T
    NH = F // dv
    NFB_ = F // 128
    with ExitStack() as es:
        gt = P.sb("pg", [128, F], F32, es)
        P.load(gt, gt[:], g_ap)
        A = [P.sb("pA%d" % i, [128, F], F32, es) for i in range(2)]
        Bt = [P.sb("pB%d" % i, [128, F], F32, es) for i in range(2)]
        junks = [P.sb("pjunk%d" % i, [128, dv], F32, es) for i in range(2)]
        sts = [P.sb("pst%d" % i, [128, 4, NH], F32, es) for i in range(2)]
        gate = [P.sb("pgate%d" % i, [128, NFB_, 128], BF16, es) for i in range(2)]
        ao = [P.sb("pao%d" % i, [128, NFB_, 128], BF16, es) for i in range(2)]
        eps_g = P.sb("peps", [128, 1], F32, es)
        loc = [gt, eps_g] + junks + sts + A + Bt + gate + ao
        P.op("dve", lambda e: e.memset(eps_g[:], EPS), writes=[eps_g])
        gv = S["og"].rearrange("(c p) t -> p c t", p=128)
        aov = S["ao2"].rearrange("(c p) t -> p c t", p=128)
        k = 0
        for c in range(T // 128):
            cs = slice(c * 128, (c + 1) * 128)
            a, b, gtile, aot = A[c % 2], Bt[c % 2], gate[c % 2], ao[c % 2]
            st, junk = sts[c % 2], junks[c % 2]
            P.load(a, a[:], S["hf"][cs, :])
            P.load(b, b[:], S["hb"][cs, :])
            P.load(gtile, gtile[:], gv[:, :, cs])
            P.op("pool", lambda e, a=a, b=b: e.tensor_tensor(out=a[:], in0=a[:], in1=b[:], op=ALU.add), reads=[a, b], writes=[a])
            for h in range(NH):
                hs = slice(h * dv, (h + 1) * dv)
                P.op("act", lambda e, a=a, hs=hs, h=h, st=st, junk=junk: e.activation(junk[:], a[:, hs], AF.Identity, accum_out=st[:, 0, h:h + 1]), reads=[a], writes=[junk, st])
                P.op("act", lambda e, a=a, hs=hs, h=h, st=st, junk=junk: e.activation(junk[:], a[:, hs], AF.Square, accum_out=st[:, 1, h:h + 1]), reads=[a], writes=[junk, st])
            P.op("dve", lambda e, st=st: e.tensor_single_scalar(st[:, 0, :], st[:, 0, :], 1.0 / dv, ALU.mult), reads=[st], writes=[st])
            P.op("dve", lambda e, st=st: e.tensor_tensor(out=st[:, 2, :], in0=st[:, 0, :], in1=st[:, 0, :], op=ALU.mult), reads=[st], writes=[st])
            P.op("dve", lambda e, st=st: e.scalar_tensor_tensor(out=st[:, 1, :], in0=st[:, 1, :], scalar=1.0 / dv, in1=st[:, 2, :], op0=ALU.mult, op1=ALU.subtract),
                 reads=[st], writes=[st])
            P.op("act", lambda e, st=st: e.activation(st[:, 1, :], st[:, 1, :], AF.Sqrt, bias=eps_g[:, 0:1]), reads=[st, eps_g], writes=[st])
            P.op("dve", lambda e, st=st: e.reciprocal(st[:, 1, :], st[:, 1, :]), reads=[st], writes=[st])
            P.op("dve", lambda e, st=st: e.scalar_tensor_tensor(out=st[:, 3, :], in0=st[:, 0, :], scalar=-1.0, in1=st[:, 1, :], op0=ALU.mult, op1=ALU.mult),
                 reads=[st], writes=[st])
            for h in range(NH):
                hs = slice(h * dv, (h + 1) * dv)
                P.op("act", lambda e, a=a, hs=hs, h=h, st=st, junk=junk: e.activation(a[:, hs], a[:, hs], AF.Identity, scale=st[:, 1, h:h + 1], bias=st[:, 3, h:h + 1]),
                     reads=[a, st], writes=[a])
            P.op("dve", lambda e, a=a: e.tensor_tensor(out=a[:], in0=a[:], in1=gt[:], op=ALU.mult), reads=[a, gt], writes=[a])
            for fb in range(NFB_):
                pb = C.ps[k % 4]; k += 1
                P.tr(pb, pb[:, 0:128], a[:, fb * 128:(fb + 1) * 128], C.ident[:], reads=[a, C.ident])
                P.op("dve", lambda e, fb=fb, pb=pb, aot=aot, gtile=gtile: e.tensor_tensor(out=aot[:, fb, :], in0=pb[:, 0:128], in1=gtile[:, fb, :], op=ALU.mult),
                     reads=[pb, gtile], writes=[aot])
            P.store(aot, aov[:, :, cs], aot[:])
        P.barrier()
        P.release(loc)


def host_mlstm_params(inp):
    return dict(mlstm_w_in=np.ascontiguousarray(inp["mlstm_w_in"][0]), mlstm_w_out=np.ascontiguousarray(inp["mlstm_w_out"][0]),
                mlstm_bg=np.ascontiguousarray(np.broadcast_to(inp["mlstm_b_gates"][0][None, :], (128, 32))),
                mlstm_ng=np.ascontiguousarray(np.broadcast_to(inp["mlstm_norm_g"][0][None, :], (128, 1024))))


def phase_ret_proj(C, li, hin, W, S, NTK=512):
    P, T = C.P, C.T
    hiv = hview(hin)
    sc = float(256 ** -0.5)
    with ExitStack() as es:
        Win = load_w(C, es, "rWin", W["ret_w_in"])
        g0 = load_small(C, es, "g0", W["norm_g"][li * 4 + 0])
        B = NormBufs(C, es, NTK, "r")
        q_st = P.sb("rq_st", [128, 8, NTK], BF16, es)
        k_st = P.sb("rk_st", [128, 8, NTK], BF16, es)
        g_st = P.sb("rg_st", [128, 16, NTK], BF16, es)
        kt_st = P.sb("rkt_st", [128, NTK // 128, 1024], BF16, es)
        v_st = P.sb("rv_st", [128, NTK // 128, 2048], BF16, es)
        loc = [Win, g0, q_st, k_st, g_st, kt_st, v_st] + B.all
        psS = C.ps[6]
        qv = S["rq"].rearrange("(c p) t -> p c t", p=128)
        kv = S["rk"].rearrange("(c p) t -> p c t", p=128)
        gv = S["og"].rearrange("(c p) t -> p c t", p=128)
        ktv = S["rkt"].rearrange("(tb p) f -> p tb f", p=128)
        vv = S["rv"].rearrange("(tb p) f -> p tb f", p=128)
        k = 0
        for ti in range(T // NTK):
            t0 = ti * NTK
            norm_in(C, hiv, PAD + t0, NTK, g0, B, psS)
            for fb in range(8):
                pb = C.ps[k % 6]; k += 1
                for c in range(8):
                    P.mm(pb, pb[:, :NTK], Win[:, c, fb * 128:(fb + 1) * 128], B.xn[:, c, :], reads=[Win, B.xn], start=(c == 0), stop=(c == 7))
                P.op("act", lambda e, fb=fb, pb=pb: e.copy(q_st[:, fb, :], pb[:, :NTK]), reads=[pb], writes=[q_st])
                pb = C.ps[k % 6]; k += 1
                for c in range(8):
                    P.mm(pb, pb[:, :NTK], Win[:, c, 1024 + fb * 128:1024 + (fb + 1) * 128], B.xn[:, c, :], reads=[Win, B.xn], start=(c == 0), stop=(c == 7))
                P.op("dve", lambda e, fb=fb, pb=pb: e.tensor_single_scalar(k_st[:, fb, :], pb[:, :NTK], sc, ALU.mult), reads=[pb], writes=[k_st])
            P.store(q_st, qv[:, :, t0:t0 + NTK], q_st[:])
            P.store(k_st, kv[:, :, t0:t0 + NTK], k_st[:])
            for fb in range(16):
                pb = C.ps[k % 6]; k += 1
                for c in range(8):
                    P.mm(pb, pb[:, :NTK], Win[:, c, 4096 + fb * 128:4096 + (fb + 1) * 128], B.xn[:, c, :], reads=[Win, B.xn], start=(c == 0), stop=(c == 7))
                P.op("act", lambda e, fb=fb, pb=pb: e.activation(g_st[:, fb, :], pb[:, :NTK], AF.Silu), reads=[pb], writes=[g_st])
            P.store(g_st, gv[:, :, t0:t0 + NTK], g_st[:])
            for tb in range(NTK // 128):
                ts_ = slice(tb * 128, (tb + 1) * 128)
                for half in range(2):
                    pb = C.ps[k % 6]; k += 1
                    for c in range(8):
                        P.mm(pb, pb[:, :512], B.xn[:, c, ts_], Win[:, c, 1024 + half * 512:1024 + (half + 1) * 512], reads=[Win, B.xn], start=(c == 0), stop=(c == 7))
                    P.op("dve", lambda e, tb=tb, half=half, pb=pb: e.tensor_single_scalar(kt_st[:, tb, half * 512:(half + 1) * 512], pb[:, :512], sc, ALU.mult),
                         reads=[pb], writes=[kt_st])
                for q4 in range(4):
                    pb = C.ps[k % 6]; k += 1
                    for c in range(8):
                        P.mm(pb, pb[:, :512], B.xn[:, c, ts_], Win[:, c, 2048 + q4 * 512:2048 + (q4 + 1) * 512], reads=[Win, B.xn], start=(c == 0), stop=(c == 7))
                    if q4 % 2 == 0:
                        P.op("act", lambda e, tb=tb, q4=q4, pb=pb: e.copy(v_st[:, tb, q4 * 512:(q4 + 1) * 512], pb[:, :512]), reads=[pb], writes=[v_st])
                    else:
                        P.op("dve", lambda e, tb=tb, q4=q4, pb=pb: e.tensor_copy(v_st[:, tb, q4 * 512:(q4 + 1) * 512], pb[:, :512]), reads=[pb], writes=[v_st])
            P.store(kt_st, ktv[:, t0 // 128:(t0 + NTK) // 128, :], kt_st[:])
            P.store(v_st, vv[:, t0 // 128:(t0 + NTK) // 128, :], v_st[:])
        P.barrier()
        P.release(loc)


def phase_ret_core(C, W, S):
    P, T = C.P, C.T
    NCH = T // 128
    with ExitStack() as es:
        R = make_tri(C, es)
        dl = P.sb("rdl", [1, 8], F32, es)
        ones1 = P.sb("rones1", [1, 128], F32, es)
        one_t = P.sb("rone", [128, 1], F32, es)
        LG = P.sb("rLG", [128, 8], F32, es)
        RAWd = P.sb("rRAWd", [128, 128], F32, es)
        rawc = P.sb("rrawc", [128, 4], F32, es)
        DM = P.sb("rDM", [128, 8, 128], F32, es)
        XI = P.sb("rXI", [128, 8], F32, es)
        ZE = P.sb("rZE", [128, 8], F32, es)
        GL = P.sb("rGL", [128, 8], F32, es)
        Qc = [[P.sb("rQ%d%d" % (d, i), [128, 8, 128], BF16, es) for i in range(2)] for d in range(2)]
        Kc = [[P.sb("rK%d%d" % (d, i), [128, 8, 128], BF16, es) for i in range(2)] for d in range(2)]
        Ktc = [[P.sb("rKt%d%d" % (d, i), [128, 1024], BF16, es) for i in range(2)] for d in range(2)]
        Vc = [[P.sb("rV%d%d" % (d, i), [128, 2048], BF16, es) for i in range(2)] for d in range(2)]
        Rf = [P.sb("rRf%d" % d, [128, 8, 512], F32, es) for d in range(2)]
        Rb = [P.sb("rRb%d" % d, [128, 8, 512], BF16, es) for d in range(2)]
        Hacc = [[P.sb("rH%d%d" % (d, i), [128, 2048], F32, es) for i in range(2)] for d in range(2)]
        sqk = [P.sb("rsqk%d" % i, [128, 128], BF16, es) for i in range(3)]
        t1 = [P.sb("rt1%d" % i, [128, 512], F32, es) for i in range(3)]
        kz = [P.sb("rkz%d" % i, [128, 256], BF16, es) for i in range(3)]
        loc = R.all + [dl, ones1, one_t, LG, RAWd, rawc, DM, XI, ZE, GL] + sum(Qc, []) + sum(Kc, []) + sum(Ktc, []) + sum(Vc, []) + Rf + Rb + sum(Hacc, []) + sqk + t1 + kz
        P.load(dl, dl[:], W["ret_decay"])
        P.op("dve", lambda e: e.memset(ones1[:], 1.0), writes=[ones1])
        P.op("dve", lambda e: e.memset(one_t[:], 1.0), writes=[one_t])
        P.op("act", lambda e: e.activation(dl[:], dl[:], AF.Exp, scale=-1.0), reads=[dl], writes=[dl])
        P.op("act", lambda e: e.activation(dl[:], dl[:], AF.Ln, bias=one_t[0:1, 0:1]), reads=[dl, one_t], writes=[dl])
        pc = C.ps[0]
        P.mm(pc, pc[:, 0:8], ones1[:], dl[:], reads=[ones1, dl])
        P.op("dve", lambda e: e.tensor_single_scalar(LG[:], pc[:, 0:8], -1.0, ALU.mult), reads=[pc], writes=[LG])
        P.op("pool", lambda e: e.iota(RAWd[:], [[1, 128]], base=0, channel_multiplier=-1, allow_small_or_imprecise_dtypes=True), writes=[RAWd])
        P.op("dve", lambda e: e.scalar_tensor_tensor(out=RAWd[:], in0=RAWd[:], scalar=-1.0, in1=RAWd[:], op0=ALU.mult, op1=ALU.max), reads=[RAWd], writes=[RAWd])
        for j, (b0, cm) in enumerate(((1, 1), (128, -1), (127, -1), (0, 1))):
            P.op("pool", lambda e, j=j, b0=b0, cm=cm: e.iota(rawc[:, j:j + 1], [[0, 1]], base=b0, channel_multiplier=cm, allow_small_or_imprecise_dtypes=True), writes=[rawc])
        mask = [R.M01F, R.M01B]
        for d in range(2):
            for h in range(4):
                dh = d * 4 + h
                P.op("act", lambda e, dh=dh: e.activation(DM[:, dh, :], RAWd[:], AF.Exp, scale=LG[:, dh:dh + 1]), reads=[RAWd, LG], writes=[DM])
                P.op("dve", lambda e, dh=dh, d=d: e.tensor_tensor(out=DM[:, dh, :], in0=DM[:, dh, :], in1=mask[d][:], op=ALU.mult), reads=[DM, mask[d]], writes=[DM])
                P.op("act", lambda e, dh=dh, d=d: e.activation(XI[:, dh:dh + 1], rawc[:, d:d + 1], AF.Exp, scale=LG[:, dh:dh + 1]), reads=[rawc, LG], writes=[XI])
                P.op("act", lambda e, dh=dh, d=d: e.activation(ZE[:, dh:dh + 1], rawc[:, 2 + d:3 + d], AF.Exp, scale=LG[:, dh:dh + 1]), reads=[rawc, LG], writes=[ZE])
        P.op("act", lambda e: e.activation(GL[:], LG[:], AF.Exp, scale=128.0), reads=[LG], writes=[GL])
        for d in range(2):
            P.op("dve", lambda e, d=d: e.memset(Rf[d][:], 0.0), writes=[Rf[d]])
            P.op("pool", lambda e, d=d: e.memset(Rb[d][:], 0.0), writes=[Rb[d]])
        qv = S["rq"].rearrange("(c p) t -> p c t", p=128)
        kv = S["rk"].rearrange("(c p) t -> p c t", p=128)
        hdst = [S["hf"], S["hb"]]
        iters = [(step, d, h) for step in range(NCH) for d in range(2) for h in range(4)]

        def ctx_of(n):
            step, d, h = iters[n]
            c = step if d == 0 else NCH - 1 - step
            return step, d, h, c, slice(c * 128, (c + 1) * 128)

        def stA(n):
            step, d, h, c, cs = ctx_of(n)
            q, kk_, kt, v = Qc[d][step % 2], Kc[d][step % 2], Ktc[d][step % 2], Vc[d][step % 2]
            if h == 0:
                P.load(q, q[:], qv[:, :, cs])
                P.load(kk_, kk_[:], kv[:, :, cs])
                P.load(kt, kt[:], S["rkt"][cs, :])
                P.load(v, v[:], S["rv"][cs, :])
            dh = d * 4 + h
            i3 = n % 3
            pS, pI, pX = C.ps[n % 2], C.ps[2 + n % 2], C.ps[4 + n % 2]
            vs = v[:, h * 512:(h + 1) * 512]
            for kc in range(2):
                P.mm(pS, pS[:, :128], kk_[:, h * 2 + kc, :], q[:, h * 2 + kc, :], reads=[kk_, q], start=(kc == 0), stop=(kc == 1))
            P.op("dve", lambda e: e.tensor_tensor(out=sqk[i3][:], in0=pS[:, :128], in1=DM[:, dh, :], op=ALU.mult),
                 reads=[pS, DM], writes=[sqk[i3]])
            P.mm(pI, pI[:, :512], sqk[i3][:], vs, reads=[sqk[i3], v])
            for kc in range(2):
                P.mm(pX, pX[:, :512], q[:, h * 2 + kc, :], Rb[d][:, h * 2 + kc, :], reads=[q, Rb[d]], start=(kc == 0), stop=(kc == 1))
            P.op("act", lambda e: e.activation(kz[i3][:], kt[:, h * 256:(h + 1) * 256], AF.Identity, scale=ZE[:, dh:dh + 1]),
                 reads=[kt, ZE], writes=[kz[i3]])

        def stB(n):
            step, d, h, c, cs = ctx_of(n)
            v = Vc[d][step % 2]
            hacc = Hacc[d][step % 2]
            dh = d * 4 + h
            i3 = n % 3
            pS, pI, pX = C.ps[n % 2], C.ps[2 + n % 2], C.ps[4 + n % 2]
            vs = v[:, h * 512:(h + 1) * 512]
            P.op("act", lambda e: e.copy(t1[i3][:], pI[:, :512]), reads=[pI], writes=[t1[i3]])
            P.op("dve", lambda e: e.scalar_tensor_tensor(
                out=hacc[:, h * 512:(h + 1) * 512], in0=pX[:, :512], scalar=XI[:, dh:dh + 1], in1=t1[i3][:], op0=ALU.mult, op1=ALU.add),
                reads=[pX, XI, t1[i3]], writes=[hacc])
            for kc in range(2):
                pC = C.ps[6 + kc]
                P.mm(pC, pC[:, :512], kz[i3][:, kc * 128:(kc + 1) * 128], vs, reads=[kz[i3], v])
                P.op("dve", lambda e, kc=kc, pC=pC: e.scalar_tensor_tensor(
                    out=Rf[d][:, h * 2 + kc, :], in0=Rf[d][:, h * 2 + kc, :], scalar=GL[:, dh:dh + 1], in1=pC[:, :512], op0=ALU.mult, op1=ALU.add),
                    reads=[Rf[d], GL, pC], writes=[Rf[d]])
                P.op("act", lambda e, kc=kc: e.copy(Rb[d][:, h * 2 + kc, :], Rf[d][:, h * 2 + kc, :]), reads=[Rf[d]], writes=[Rb[d]])
            if h == 3:
                P.store(hacc, hdst[d][cs, :], hacc[:])

        stA(0)
        for n in range(len(iters)):
            if n + 1 < len(iters):
                stA(n + 1)
            stB(n)
        P.barrier()
        P.release(loc)


def host_ret_params(inp):
    return dict(ret_w_in=np.ascontiguousarray(inp["ret_w_in"][0]), ret_w_o=np.ascontiguousarray(inp["ret_w_o"][0]),
                ret_decay=np.ascontiguousarray(inp["ret_decay_logit"][0].reshape(1, 8)),
                ret_ng=np.ascontiguousarray(np.broadcast_to(inp["ret_norm_g"][0][None, :], (128, 2048))))


SEQ = 8192
NCORES = 4


def build_full(T, hp_shapes, layers=(0, 1, 2, 3)):
    nc = bass.Bass("TRN2", target_bir_lowering=False)
    with ExitStack() as es:
        C = make_ctx(nc, es, T)
        xd = nc.dram_tensor("x", [T, 1024], F32, kind="ExternalInput").ap()
        od = nc.dram_tensor("out", [T, 1024], F32, kind="ExternalOutput").ap()
        W = declare_inputs(nc, hp_shapes)
        ha = nc.dram_tensor("ha", [1024, T + 2 * PAD], F32, kind="Internal").ap()
        hb = nc.dram_tensor("hb", [1024, T + 2 * PAD], F32, kind="Internal").ap()

        def scratch(specs):
            return {nm: nc.dram_tensor("s_" + nm, list(shp), dt, kind="Internal").ap() for nm, shp, dt in specs}

        zero_pads(C, ha)
        zero_pads(C, hb)
        phase_in(C, xd, ha)
        if 0 in layers:
            S = scratch((("qn", (1024, T), BF16), ("kn", (1024, T), BF16), ("qr", (512, T), BF16), ("kr", (64, T), BF16),
                         ("v", (T, 1024), BF16), ("ao", (1024, T), BF16)))
            phase_mla_proj(C, 0, ha, W, S)
            phase_mla_core(C, S)
            phase_tail(C, S["ao"], W["mla_w_o"], W["norm_g"][1], ha, hb)
            phase_ffn(C, 0, hb, ha, W)
        if 1 in layers:
            S = scratch((("dqn", (1024, T), BF16), ("dkn", (1024, T), BF16), ("dv", (T, 1024), BF16), ("dao", (1024, T), BF16)))
            S = dict(qn=S["dqn"], kn=S["dkn"], v=S["dv"], ao=S["dao"])
            phase_diff_proj(C, 1, ha, W, S)
            phase_diff_core(C, 1, W, S)
            phase_tail(C, S["ao"], W["diff_w_o"], W["norm_g"][5], ha, hb)
            phase_ffn(C, 1, hb, ha, W)
        if 2 in layers:
            S = scratch((("mq", (512, T), BF16), ("mk", (512, T), BF16), ("mkt", (T, 512), BF16), ("mv", (T, 8, 129), BF16),
                         ("mog", (1024, T), BF16), ("mhf", (T, 1024), F32), ("mhb", (T, 1024), F32), ("mao2", (1024, T), BF16)))
            S.update(og=S["mog"], hf=S["mhf"], hb=S["mhb"], ao2=S["mao2"])
            phase_mlstm_core(C, 2, ha, W, S)
            phase_gn_post(C, S, 1024, 128, W["mlstm_ng"])
            phase_tail(C, S["ao2"], W["mlstm_w_out"], W["norm_g"][9], ha, hb)
            phase_ffn(C, 2, hb, ha, W)
        if 3 in layers:
            S = scratch((("rq", (1024, T), BF16), ("rk", (1024, T), BF16), ("rkt", (T, 1024), BF16), ("rv", (T, 2048), BF16),
                         ("rog", (2048, T), BF16), ("rhf", (T, 2048), F32), ("rhb", (T, 2048), F32), ("rao2", (2048, T), BF16)))
            S.update(og=S["rog"], hf=S["rhf"], hb=S["rhb"], ao2=S["rao2"])
            phase_ret_proj(C, 3, ha, W, S)
            phase_ret_core(C, W, S)
            phase_gn_post(C, S, 2048, 512, W["ret_ng"])
            phase_tail(C, S["ao2"], W["ret_w_o"], W["norm_g"][13], ha, hb)
            phase_ffn(C, 3, hb, ha, W)
        phase_out(C, ha, od)
        C.P.emit()
    return nc, C


def host_params(inp, T):
    inp = {k: np.asarray(v, dtype=np.float32) for k, v in inp.items() if k != "x"}
    cw, cb, ng = host_ffn_params(inp)
    hp = dict(ffn_w_up=np.ascontiguousarray(inp["ffn_w_up"]), ffn_w_down=np.ascontiguousarray(inp["ffn_w_down"]),
              ffn_cw=cw, ffn_cb=cb, norm_g=ng)
    hp.update(host_mla_params(inp, np.arange(T)))
    hp.update(host_diff_params(inp))
    hp.update(host_mlstm_params(inp))
    hp.update(host_ret_params(inp))
    return hp


REAL_CORES = (0, 1, 4, 5)


def kernel(**inputs):
    x = np.asarray(inputs["x"], dtype=np.float32)
    Bn, T, _ = x.shape
    hp = host_params(inputs, T)
    nc, C = build_full(T, {k: v.shape for k, v in hp.items()})
    ident = np.eye(128, dtype=np.float32)
    zx = np.zeros((T, 1024), np.float32)
    in_maps = []
    real = REAL_CORES[:Bn]
    for c in range(8):
        xb = np.ascontiguousarray(x[real.index(c)]) if c in real else zx
        in_maps.append(dict(x=xb, ident_in=ident, **hp))
    res = run_bass_kernel_spmd(nc, in_maps, core_ids=list(range(8)))
    return np.stack([np.asarray(res.results[c]["out"]) for c in real]).astype(np.float32)
```
